# Optimizing a Trainium2 kernel written in Bass

```python
import math
import jax
import jax.numpy as jnp
from jax import lax
import numpy as np

D_MODEL = 1024
BATCH = 8
SEQ = 4096
DEPTH = 1

PLE_DIM = 256
EPS = 1e-6

D_RNN = 1024
RNN_BLOCKS = 8
RNN_BLOCK = D_RNN // RNN_BLOCKS
CONV_W = 4
LRU_C = 8.0

N_HEADS = 8
N_KV = 2
HPG = N_HEADS // N_KV
HEAD_DIM = 128
D_ATTN = N_HEADS * HEAD_DIM
CMP_LEN = 32
CMP_STRIDE = 16
CMP_HID = 256
SEL_BLOCK = 64
SEL_TOPK = 16
WINDOW = 512
Q_BLOCK = 32
ROPE_THETA = 10000.0

PEER_HEADS = 8
N_KEYS = 128
N_EXPERTS = N_KEYS * N_KEYS
PEER_QDIM = 128
PEER_HALF = PEER_QDIM // 2
PEER_TOPK = 16
PEER_CHUNK = 128

SPLIT_SIZES = (D_RNN, D_RNN, D_ATTN, 6 * N_KV * HEAD_DIM, 3 * N_HEADS, 2 * D_MODEL)
D_IN = sum(SPLIT_SIZES)
SPLIT_POINTS = tuple(int(v) for v in np.cumsum(SPLIT_SIZES)[:-1])

NEG_INF = -1e30
FORCE_SCORE = 1e3

kernel_name = 'hybrid_rglru_nsa_peer'


def rmsnorm(x, g):
    xf = x.astype(jnp.float32)
    y = xf * lax.rsqrt(jnp.mean(xf * xf, axis=-1, keepdims=True) + EPS)
    return (y * g.astype(jnp.float32)).astype(x.dtype)


def rope(x, pos):
    half = HEAD_DIM // 2
    inv = ROPE_THETA ** (-jnp.arange(half, dtype=jnp.float32) / half)
    ang = pos.astype(jnp.float32)[..., None] * inv
    cos = jnp.cos(ang)[:, :, None, :]
    sin = jnp.sin(ang)[:, :, None, :]
    xf = x.astype(jnp.float32)
    x1, x2 = xf[..., :half], xf[..., half:]
    return jnp.concatenate([x1 * cos - x2 * sin, x2 * cos + x1 * sin], axis=-1).astype(x.dtype)


def causal_dwconv(x, w, b):
    y = lax.conv_general_dilated(x, w[:, None, :].astype(x.dtype), window_strides=(1,),
                                 padding=[(CONV_W - 1, 0)], dimension_numbers=('NWC', 'WIO', 'NWC'),
                                 feature_group_count=x.shape[-1])
    return y + b


def rg_lru(x, w_r, b_r, w_i, b_i, lam):
    B, S, C = x.shape
    xf = x.astype(jnp.float32)
    xb = xf.reshape(B, S, RNN_BLOCKS, RNN_BLOCK)
    r = jax.nn.sigmoid(jnp.einsum('bsnc,ncd->bsnd', xb, w_r.astype(jnp.float32)).reshape(B, S, C) + b_r)
    ig = jax.nn.sigmoid(jnp.einsum('bsnc,ncd->bsnd', xb, w_i.astype(jnp.float32)).reshape(B, S, C) + b_i)
    log_a = -LRU_C * r * jax.nn.softplus(-lam.astype(jnp.float32))
    a = jnp.exp(log_a)
    u = jnp.sqrt(-jnp.expm1(2.0 * log_a)) * (ig * xf)

    def combine(lhs, rhs):
        a1, b1 = lhs
        a2, b2 = rhs
        return a1 * a2, a2 * b1 + b2

    _, hs = lax.associative_scan(combine, (a, u), axis=1)
    return hs.astype(x.dtype)


def compress_blocks(t, idx, pe, w1, b1, w2, b2):
    blk = t[:, idx] + pe[:, None, :]
    B, n = blk.shape[0], blk.shape[1]
    flat = blk.transpose(0, 3, 1, 2, 4).reshape(B, N_KV, n, CMP_LEN * HEAD_DIM)
    hid = jax.nn.gelu(jnp.einsum('bgnf,fh->bgnh', flat, w1) + b1)
    return jnp.einsum('bgnh,hd->bgnd', hid, w2) + b2


def masked_softmax(s, mask):
    return jax.nn.softmax(jnp.where(mask, s, NEG_INF), axis=-1)


def nsa_attention(q, kv, gate_logits, pos, cmp_k, cmp_v):
    B, S = q.shape[0], q.shape[1]
    f32 = jnp.float32
    kv = kv.reshape(B, S, 6, N_KV, HEAD_DIM)
    k_c, v_c, k_s, v_s, k_w, v_w = (kv[:, :, j] for j in range(6))
    q = q.reshape(B, S, N_HEADS, HEAD_DIM)
    q_rot = rope(q, pos)
    k_s = rope(k_s, pos)
    k_w = rope(k_w, pos)

    def heads_first(t):
        return t.reshape(B, S, N_KV, HPG, HEAD_DIM).transpose(0, 2, 3, 1, 4)

    q_n = heads_first(q)
    q_r = heads_first(q_rot)
    gates = jax.nn.sigmoid(gate_logits.astype(f32)).reshape(B, S, N_KV, HPG, 3).transpose(0, 2, 3, 1, 4)

    n_cmp = (S - CMP_LEN) // CMP_STRIDE + 1
    cmp_start = jnp.arange(n_cmp) * CMP_STRIDE
    cmp_idx = cmp_start[:, None] + jnp.arange(CMP_LEN)[None, :]
    cmp_end = cmp_start + CMP_LEN - 1
    kc = compress_blocks(k_c, cmp_idx, *cmp_k)
    vc = compress_blocks(v_c, cmp_idx, *cmp_v)

    n_sel = S // SEL_BLOCK
    sel_start = jnp.arange(n_sel) * SEL_BLOCK
    overlap = ((cmp_start[:, None] < sel_start[None, :] + SEL_BLOCK) &
               (cmp_end[:, None] >= sel_start[None, :])).astype(f32)
    top_n = min(SEL_TOPK, n_sel)
    ks_blk = k_s.reshape(B, n_sel, SEL_BLOCK, N_KV, HEAD_DIM).transpose(0, 3, 1, 2, 4)
    vs_blk = v_s.reshape(B, n_sel, SEL_BLOCK, N_KV, HEAD_DIM).transpose(0, 3, 1, 2, 4)

    kw_pad = jnp.pad(k_w.transpose(0, 2, 1, 3), ((0, 0), (0, 0), (WINDOW, 0), (0, 0)))
    vw_pad = jnp.pad(v_w.transpose(0, 2, 1, 3), ((0, 0), (0, 0), (WINDOW, 0), (0, 0)))

    scale = HEAD_DIM ** -0.5
    b_ix = jnp.arange(B)[:, None, None, None]
    g_ix = jnp.arange(N_KV)[None, :, None, None]
    j_sel = jnp.arange(n_sel)

    def block(c):
        t0 = c * Q_BLOCK
        tq = t0 + jnp.arange(Q_BLOCK)
        qn = lax.dynamic_slice_in_dim(q_n, t0, Q_BLOCK, axis=3)
        qr = lax.dynamic_slice_in_dim(q_r, t0, Q_BLOCK, axis=3)
        gt = lax.dynamic_slice_in_dim(gates, t0, Q_BLOCK, axis=3)

        s_c = jnp.einsum('bghtd,bgnd->bghtn', qn, kc, preferred_element_type=f32) * scale
        m_c = cmp_end[None, :] <= tq[:, None]
        p_c = masked_softmax(s_c, m_c) * (tq >= CMP_LEN - 1).astype(f32)[:, None]
        o_c = jnp.einsum('bghtn,bgnd->bghtd', p_c.astype(vc.dtype), vc)

        imp = jnp.einsum('bghtn,nj->bgtj', p_c, overlap)
        cur = tq // SEL_BLOCK
        forced = (j_sel[None, :] == 0) | (j_sel[None, :] == cur[:, None]) | (j_sel[None, :] == cur[:, None] - 1)
        imp = jnp.where(forced, FORCE_SCORE, imp)
        imp = jnp.where(sel_start[None, :] <= tq[:, None], imp, NEG_INF)
        top_v, top_i = lax.top_k(imp, top_n)

        ks = ks_blk[b_ix, g_ix, top_i].reshape(B, N_KV, Q_BLOCK, top_n * SEL_BLOCK, HEAD_DIM)
        vs = vs_blk[b_ix, g_ix, top_i].reshape(B, N_KV, Q_BLOCK, top_n * SEL_BLOCK, HEAD_DIM)
        kpos = (top_i[..., None] * SEL_BLOCK + jnp.arange(SEL_BLOCK)).reshape(B, N_KV, Q_BLOCK, top_n * SEL_BLOCK)
        m_s = (kpos <= tq[None, None, :, None]) & jnp.repeat(top_v > 0.5 * NEG_INF, SEL_BLOCK, axis=-1)
        s_s = jnp.einsum('bghtd,bgtkd->bghtk', qr, ks, preferred_element_type=f32) * scale
        p_s = masked_softmax(s_s, m_s[:, :, None])
        o_s = jnp.einsum('bghtk,bgtkd->bghtd', p_s.astype(vs.dtype), vs)

        kw = lax.dynamic_slice_in_dim(kw_pad, t0, Q_BLOCK + WINDOW, axis=2)
        vw = lax.dynamic_slice_in_dim(vw_pad, t0, Q_BLOCK + WINDOW, axis=2)
        wpos = t0 - WINDOW + jnp.arange(Q_BLOCK + WINDOW)
        dist = tq[:, None] - wpos[None, :]
        m_w = (wpos[None, :] >= 0) & (dist >= 0) & (dist < WINDOW)
        s_w = jnp.einsum('bghtd,bgkd->bghtk', qr, kw, preferred_element_type=f32) * scale
        p_w = masked_softmax(s_w, m_w)
        o_w = jnp.einsum('bghtk,bgkd->bghtd', p_w.astype(vw.dtype), vw)

        return gt[..., 0:1] * o_c + gt[..., 1:2] * o_s + gt[..., 2:3] * o_w

    o = lax.map(block, jnp.arange(S // Q_BLOCK))
    return o.transpose(1, 0, 4, 2, 3, 5).reshape(B, S, D_ATTN)


def peer_ffn(x, w_q, sub_keys, u_tab, v_tab):
    B, S, D = x.shape
    T = B * S
    xt = x.reshape(T, D)
    q = jnp.einsum('td,de->te', xt, w_q).reshape(T, PEER_HEADS, 2, PEER_HALF)
    s = jnp.einsum('thcq,cnq->thcn', q, sub_keys, preferred_element_type=jnp.float32)
    v1, i1 = lax.top_k(s[:, :, 0], PEER_TOPK)
    v2, i2 = lax.top_k(s[:, :, 1], PEER_TOPK)
    cand = (v1[..., :, None] + v2[..., None, :]).reshape(T, PEER_HEADS, PEER_TOPK * PEER_TOPK)
    best, ci = lax.top_k(cand, PEER_TOPK)
    e_idx = (jnp.take_along_axis(i1, ci // PEER_TOPK, axis=-1) * N_KEYS +
             jnp.take_along_axis(i2, ci % PEER_TOPK, axis=-1))
    g = jax.nn.softmax(best, axis=-1).astype(x.dtype)
    n_chunks = T // PEER_CHUNK

    def chunk(args):
        xc, ec, gc = args
        u = u_tab[ec]
        act = jax.nn.gelu(jnp.einsum('cd,chkd->chk', xc, u)) * gc
        v = v_tab[ec]
        return jnp.einsum('chk,chkd->cd', act, v)

    out = lax.map(chunk, (xt.reshape(n_chunks, PEER_CHUNK, D),
                          e_idx.reshape(n_chunks, PEER_CHUNK, PEER_HEADS, PEER_TOPK),
                          g.reshape(n_chunks, PEER_CHUNK, PEER_HEADS, PEER_TOPK)))
    return out.reshape(B, S, D)


def setup_inputs(seed: int = 0) -> dict:
    key = jax.random.key(seed)
    keys = jax.random.split(key, 48)
    counter = [0]
    f32 = jnp.float32

    def nxt():
        k = keys[counter[0]]
        counter[0] += 1
        return k

    def nrm(shape, scale):
        return jax.random.normal(nxt(), shape, f32) * scale

    def gain(shape):
        return 1.0 + nrm(shape, 0.01)

    L = DEPTH
    x = nrm((BATCH, SEQ, D_MODEL), 1.0)
    p = nrm((DEPTH, BATCH, SEQ, PLE_DIM), 1.0)
    offsets = jax.random.randint(nxt(), (BATCH, 1), 0, 1024, dtype=jnp.int32)
    positions = offsets + jnp.arange(SEQ, dtype=jnp.int32)[None, :]
    u = jax.random.uniform(nxt(), (L, D_RNN), f32, 0.9, 0.999)
    s = u ** (1.0 / LRU_C)
    lru_lam = jnp.log(s) - jnp.log1p(-s)
    fl = CMP_LEN * HEAD_DIM
    return {
        'x': x,
        'p': p,
        'positions': positions,
        'norm_mix': gain((L, D_MODEL)),
        'w_in': nrm((L, D_MODEL, D_IN), D_MODEL ** -0.5),
        'conv_w': nrm((L, CONV_W, D_RNN), CONV_W ** -0.5),
        'conv_b': nrm((L, D_RNN), 0.01),
        'lru_wr': nrm((L, RNN_BLOCKS, RNN_BLOCK, RNN_BLOCK), RNN_BLOCK ** -0.5),
        'lru_br': nrm((L, D_RNN), 0.01),
        'lru_wi': nrm((L, RNN_BLOCKS, RNN_BLOCK, RNN_BLOCK), RNN_BLOCK ** -0.5),
        'lru_bi': nrm((L, D_RNN), 0.01),
        'lru_lam': lru_lam,
        'cmp_pe_k': nrm((L, CMP_LEN, HEAD_DIM), 0.02),
        'cmp_w1_k': nrm((L, fl, CMP_HID), fl ** -0.5),
        'cmp_b1_k': nrm((L, CMP_HID), 0.01),
        'cmp_w2_k': nrm((L, CMP_HID, HEAD_DIM), CMP_HID ** -0.5),
        'cmp_b2_k': nrm((L, HEAD_DIM), 0.01),
        'cmp_pe_v': nrm((L, CMP_LEN, HEAD_DIM), 0.02),
        'cmp_w1_v': nrm((L, fl, CMP_HID), fl ** -0.5),
        'cmp_b1_v': nrm((L, CMP_HID), 0.01),
        'cmp_w2_v': nrm((L, CMP_HID, HEAD_DIM), CMP_HID ** -0.5),
        'cmp_b2_v': nrm((L, HEAD_DIM), 0.01),
        'w_a': nrm((L, D_RNN, D_MODEL), D_RNN ** -0.5),
        'w_b': nrm((L, D_ATTN, D_MODEL), D_ATTN ** -0.5),
        'w_out': nrm((L, D_MODEL, D_MODEL), D_MODEL ** -0.5),
        'norm_ffn': gain((L, D_MODEL)),
        'peer_wq': nrm((L, D_MODEL, PEER_HEADS * PEER_QDIM), D_MODEL ** -0.5),
        'peer_keys': nrm((L, 2, N_KEYS, PEER_HALF), PEER_HALF ** -0.5),
        'peer_u': nrm((L, N_EXPERTS, D_MODEL), D_MODEL ** -0.5),
        'peer_v': nrm((L, N_EXPERTS, D_MODEL), PEER_HEADS ** -0.5),
        'norm_ple': gain((L, D_MODEL)),
        'ple_wg': nrm((L, D_MODEL, D_MODEL), D_MODEL ** -0.5),
        'ple_wp': nrm((L, PLE_DIM, D_MODEL), PLE_DIM ** -0.5),
        'norm_final': gain((D_MODEL,)),
    }


def reference(x, p, positions, norm_mix, w_in, conv_w, conv_b, lru_wr, lru_br, lru_wi, lru_bi, lru_lam,
              cmp_pe_k, cmp_w1_k, cmp_b1_k, cmp_w2_k, cmp_b2_k, cmp_pe_v, cmp_w1_v, cmp_b1_v, cmp_w2_v, cmp_b2_v,
              w_a, w_b, w_out, norm_ffn, peer_wq, peer_keys, peer_u, peer_v, norm_ple, ple_wg, ple_wp,
              norm_final):
    B, S, D = x.shape
    h = x
    for i in range(DEPTH):
        n = rmsnorm(h, norm_mix[i])
        z = jnp.einsum('bsd,de->bse', n, w_in[i])
        z_rx, z_rg, z_q, z_kv, z_ng, z_m = jnp.split(z, SPLIT_POINTS, axis=-1)

        xa = causal_dwconv(z_rx, conv_w[i], conv_b[i])
        ha = rg_lru(xa, lru_wr[i], lru_br[i], lru_wi[i], lru_bi[i], lru_lam[i])
        y_a = jnp.einsum('bsc,cd->bsd', ha * jax.nn.gelu(z_rg), w_a[i])

        o_b = nsa_attention(z_q, z_kv, z_ng, positions,
                            (cmp_pe_k[i], cmp_w1_k[i], cmp_b1_k[i], cmp_w2_k[i], cmp_b2_k[i]),
                            (cmp_pe_v[i], cmp_w1_v[i], cmp_b1_v[i], cmp_w2_v[i], cmp_b2_v[i]))
        y_b = jnp.einsum('bsc,cd->bsd', o_b.astype(h.dtype), w_b[i])

        g_m = jax.nn.sigmoid(z_m.astype(jnp.float32)).reshape(B, S, 2, D)
        merged = (g_m[:, :, 0] * y_a + g_m[:, :, 1] * y_b).astype(h.dtype)
        h = h + jnp.einsum('bsd,de->bse', merged, w_out[i])

        h = h + peer_ffn(rmsnorm(h, norm_ffn[i]), peer_wq[i], peer_keys[i], peer_u[i], peer_v[i])

        gate = jax.nn.sigmoid(jnp.einsum('bsd,de->bse', rmsnorm(h, norm_ple[i]), ple_wg[i]).astype(jnp.float32))
        h = h + (gate * jnp.einsum('bsk,kd->bsd', p[i], ple_wp[i])).astype(h.dtype)
    return rmsnorm(h, norm_final)
```

```python
from contextlib import ExitStack
import numpy as np
import ml_dtypes
import concourse.bass as bass
import concourse.mybir as mybir
from concourse.bass_utils import run_bass_kernel_spmd

F32 = mybir.dt.float32
BF16 = mybir.dt.bfloat16
I32 = mybir.dt.int32
AF = mybir.ActivationFunctionType
ALU = mybir.AluOpType
AX = mybir.AxisListType

S = 4096
D = 1024
NQC = 8
NTT = 32
NEG = -30000.0
SCALE = 128 ** -0.5
COMPUTE = ("pe", "dve", "act", "pool")
import os as _os
NROT = int(_os.environ.get("K_NROT", "8"))
NCH = 65
WCOLS = NCH * 128

C_RX, C_RG, C_Q, C_QS, C_KV, C_KSW, C_M, C_NG = 0, 8, 16, 24, 32, 44, 48, 64


class Res:
    __slots__ = ("w", "r", "name")

    def __init__(self, name=""):
        self.w = None
        self.r = {}
        self.name = name


class TT:
    def __init__(self, t, nres, name):
        self.t = t
        self.r = [Res(f"{name}.{i}") for i in range(nres)]

    @property
    def r0(self):
        return self.r[0]


class Prog:
    def __init__(self, nc):
        self.nc = nc
        self.scopes = [ExitStack()]
        self.streams = {e: [] for e in ("pe", "dve", "act", "pool", "sp")}
        self.cnt = {e: 0 for e in COMPUTE}
        self.seen = {e: {} for e in self.streams}
        self.sems = {}
        for i in range(int(_os.environ.get("K_SEMPAD", "0"))):
            self.scopes[0].enter_context(nc.semaphore(f"s_pad{i}"))
        for e in COMPUTE:
            self.sems[e] = self.scopes[0].enter_context(nc.semaphore(f"s_{e}"))
        self.dq = {}
        self.dma_total = {}
        for q in ("sp", "act", "pool"):
            self.dq[q] = {"n": 0, "sems": []}
            nrot_q = int(_os.environ.get("K_NROT_" + q.upper(), "1" if q == "sp" else str(NROT)))
            self.dq[q]["nrot"] = nrot_q
            for i in range(nrot_q):
                k = ("dma", q, i)
                self.sems[k] = self.scopes[0].enter_context(nc.semaphore(f"s_dma_{q}_{i}"))
                self.dq[q]["sems"].append(k)
                self.dma_total[k] = 0
        self.n_instr = 0

    def push(self):
        self.scopes.append(ExitStack())

    def pop(self):
        self.scopes.pop().close()

    def sbuf(self, name, shape, dtype, nres=1):
        self.n_instr += 0
        self.uid = getattr(self, "uid", 0) + 1
        t = self.scopes[-1].enter_context(self.nc.sbuf_tensor(f"{name}_{self.uid}", list(shape), dtype))
        return TT(t, nres, name)

    def psum(self, name, shape, dtype, nres=1):
        self.uid = getattr(self, "uid", 0) + 1
        t = self.scopes[-1].enter_context(self.nc.psum_tensor(f"{name}_{self.uid}", list(shape), dtype))
        return TT(t, nres, name)

    def _deps(self, eng, reads, writes):
        deps = {}

        def add(tok):
            if tok is None:
                return
            k, v = tok
            if deps.get(k, 0) < v:
                deps[k] = v

        for r in reads:
            add(r.w)
        for w in writes:
            add(w.w)
            for k, v in w.r.items():
                add((k, v))
        out = []
        for k, v in deps.items():
            if k == eng and eng == "pe":
                continue
            if self.seen[eng].get(k, 0) >= v:
                continue
            self.seen[eng][k] = v
            out.append((k, v))
        return out

    def _record(self, tok, reads, writes):
        k, v = tok
        for r in reads:
            if r.r.get(k, 0) < v:
                r.r[k] = v
        for w in writes:
            w.w = tok
            w.r = {}

    def op(self, eng, meth, kw, reads=(), writes=()):
        waits = self._deps(eng, reads, writes)
        self.cnt[eng] += 1
        tok = (eng, self.cnt[eng])
        self._record(tok, reads, writes)
        self.streams[eng].append((meth, kw, waits, (eng, 1)))
        self.n_instr += 1

    def dma(self, q, out, in_, reads=(), writes=(), indirect=None, **kw):
        d = self.dq[q]
        i = d["n"]
        d["n"] += 1
        sk = d["sems"][i % d["nrot"]]
        prev = self.dma_total[sk]
        waits = self._deps(q, reads, writes)
        if prev > 0 and self.seen[q].get(sk, 0) < prev:
            self.seen[q][sk] = prev
            waits.append((sk, prev))
        self.dma_total[sk] = prev + 16
        tok = (sk, prev + 16)
        self._record(tok, reads, writes)
        kk = dict(out=out, in_=in_)
        if indirect is not None:
            kk.update(indirect)
        kk.update(kw)
        self.streams[q].append(("indirect_dma_start" if indirect is not None else "dma_start", kk, waits, (sk, 16)))
        self.n_instr += 1
        return tok

    def emit_stage(self):
        targets = [(e, self.cnt[e]) for e in COMPUTE if self.cnt[e] > 0]
        targets += [(k, v) for k, v in self.dma_total.items() if v > 0]
        for e in self.streams:
            ws = []
            for k, v in targets:
                if self.seen[e].get(k, 0) < v:
                    self.seen[e][k] = v
                    ws.append((k, v))
            if ws:
                self.streams[e].append((None, None, ws, None))
        sems = self.sems
        streams = self.streams

        def run(eng_obj, items):
            for meth, kw, waits, inc in items:
                for k, v in waits:
                    eng_obj.wait_ge(sems[k], v)
                if meth is None:
                    continue
                ins = getattr(eng_obj, meth)(**kw)
                ins.then_inc(sems[inc[0]], inc[1])

        with self.nc.Block() as block:
            @block.tensor
            def _(e):
                run(e, streams["pe"])

            @block.vector
            def _(e):
                run(e, streams["dve"])

            @block.scalar
            def _(e):
                run(e, streams["act"])

            @block.gpsimd
            def _(e):
                run(e, streams["pool"])

            @block.sync
            def _(e):
                run(e, streams["sp"])
        self.streams = {e: [] for e in streams}

    def close(self):
        while self.scopes:
            self.scopes.pop().close()


def _bf(a):
    return np.ascontiguousarray(a).astype(ml_dtypes.bfloat16)


def make_consts():
    c = {}
    c["c_identb"] = _bf(np.eye(128, dtype=np.float32))
    c["c_identf"] = np.eye(128, dtype=np.float32)
    half = 64
    inv = (10000.0 ** (-np.arange(half, dtype=np.float32) / np.float32(half))).astype(np.float32)
    c["c_invf"] = np.concatenate([inv, inv]).reshape(128, 1).astype(np.float32)
    c["c_sgn"] = np.concatenate([-np.ones(64), np.ones(64)]).reshape(128, 1).astype(np.float32)
    kp = np.arange(128)[:, None]
    q = np.arange(512)[None, :]
    cb = np.zeros((128, 4, 512), np.float32)
    wb = np.zeros((128, 4, 512), np.float32)
    for r in range(4):
        k = r * 128 + kp
        cb[:, r, :] = np.where(k <= q, 0.0, NEG)
        k2 = (r - 4) * 128 + kp
        wb[:, r, :] = np.where(q - k2 < 512, 0.0, NEG)
    c["c_cb"] = _bf(cb)
    c["c_wb"] = _bf(wb)
    n = (np.arange(2)[None, :, None] * 128 + np.arange(128)[:, None, None])
    t = np.arange(S)[None, None, :]
    cmb = np.where((16 * n + 31 <= t) & (n < 255), 0.0, NEG).astype(np.float32)
    c["c_cmb"] = _bf(cmb)
    j = np.arange(64)[:, None]
    kk = np.arange(S)[None, :]
    c["c_E"] = _bf((kk // 64 == j).astype(np.float32))
    nn = np.arange(256)
    jj = np.arange(64)
    ov = ((16 * nn[:, None] < 64 * jj[None, :] + 64) & (16 * nn[:, None] + 31 >= 64 * jj[None, :]) & (nn[:, None] < 255))
    c["c_ovl"] = _bf(ov.astype(np.float32).reshape(2, 128, 64).transpose(1, 0, 2))
    tt = np.arange(S).reshape(NTT, 128)[:, :, None]
    cur = tt // 64
    j3 = np.arange(64)[None, None, :]
    forced = (j3 == 0) | (j3 == cur) | (j3 == cur - 1)
    valid = (64 * j3 <= tt)
    c["c_fa"] = np.ascontiguousarray(np.where(forced, 1000.0, 0.0).astype(np.float32).transpose(1, 0, 2))
    c["c_na"] = np.ascontiguousarray(np.where(valid, 0.0, -1e30).astype(np.float32).transpose(1, 0, 2))
    c["c_vm"] = np.ascontiguousarray(valid.astype(np.float32).transpose(1, 0, 2))
    c["c_iota128"] = np.ascontiguousarray(np.broadcast_to(np.arange(128, dtype=np.int32)[None, None, :], (128, 16, 128)))
    c["c_iota256"] = np.ascontiguousarray(np.broadcast_to(np.arange(256, dtype=np.int32)[None, None, :], (128, 8, 256)))
    c["c_iota16"] = np.ascontiguousarray(np.broadcast_to(np.arange(16, dtype=np.float32)[None, None, None, :], (128, 8, 16, 16)))
    return c


def pc_layout(v):
    return np.ascontiguousarray(np.asarray(v).reshape(8, 128).T)


def prep_shared(inp):
    m = {}
    def pk(w):
        K_, N_ = w.shape
        return np.ascontiguousarray(w.reshape(K_ // 128, 128, N_).transpose(1, 0, 2))
    m["w_in"] = pk(inp["w_in"][0])
    for k in ("w_a", "w_b", "w_out", "peer_wq", "ple_wg", "ple_wp"):
        m[k] = pk(inp[k][0])
    for k in ("peer_u", "peer_v"):
        m[k] = np.ascontiguousarray(inp[k][0])
    m["lru_wr"] = np.ascontiguousarray(inp["lru_wr"][0].transpose(1, 0, 2))
    m["lru_wi"] = np.ascontiguousarray(inp["lru_wi"][0].transpose(1, 0, 2))
    for k in ("cmp_w1_k", "cmp_w1_v"):
        m[k] = np.ascontiguousarray(inp[k][0].reshape(32, 128, 256).transpose(1, 0, 2))
    for k in ("cmp_w2_k", "cmp_w2_v"):
        m[k] = pk(inp[k][0])
    for k in ("norm_mix", "norm_ffn", "norm_ple"):
        m[k] = np.ascontiguousarray(inp[k][0].reshape(1, D))
    m["norm_final"] = np.ascontiguousarray(inp["norm_final"].reshape(1, D))
    m["conv_w"] = np.ascontiguousarray(inp["conv_w"][0].reshape(4, 8, 128).transpose(2, 1, 0))
    for k in ("conv_b", "lru_br", "lru_bi", "lru_lam"):
        m[k] = pc_layout(inp[k][0])
    for k in ("cmp_b1_k", "cmp_b1_v"):
        m[k] = np.ascontiguousarray(inp[k][0].reshape(2, 128).T)
    m["cmp_b2_k"] = np.ascontiguousarray(inp["cmp_b2_k"][0].reshape(128, 1))
    m["cmp_b2_v"] = np.ascontiguousarray(inp["cmp_b2_v"][0].reshape(1, 128))
    m["cmp_pe_k"] = np.ascontiguousarray(inp["cmp_pe_k"][0].T)
    m["cmp_pe_v"] = np.ascontiguousarray(inp["cmp_pe_v"][0].T)
    keys = inp["peer_keys"][0]
    kbd = np.zeros((128, 256), np.float32)
    kbd[0:64, 0:128] = keys[0].T
    kbd[64:128, 128:256] = keys[1].T
    m["peer_kbd"] = kbd
    return m


def win_segments():
    segs = [(0, 0, 2048), (2048, 2048, 1024)]
    d = 3072
    for h in range(8):
        segs.append((d, 2048 + h * 128 + 64, 64))
        segs.append((d + 64, 2048 + h * 128, 64))
        d += 128
    segs.append((4096, 3072, 1536))
    d = 5632
    for jkv in (2, 4):
        for g in range(2):
            s0 = 3072 + jkv * 256 + g * 128
            segs.append((d, s0 + 64, 64))
            segs.append((d + 64, s0, 64))
            d += 128
    segs.append((6144, 4632, 2048))
    segs.append((8192, 4608, 24))
    return segs


def build_nc(upto="D", debug=False):
    nc = bass.Bass("TRN2", target_bir_lowering=False)
    P = Prog(nc)
    ein = {}

    def IN(name, shape, dt=F32):
        ein[name] = nc.dram_tensor(name, list(shape), dt, kind="ExternalInput").ap()
        return ein[name]

    x_d = IN("x", [S, D])
    p_d = IN("p", [S, 256])
    pos_d = IN("positions", [1, S], I32)
    win_d = IN("w_in", [128, 8, 6680])
    w5_d = [IN(k, [128, 8, D]) for k in ("w_a", "w_b", "w_out", "peer_wq", "ple_wg")]
    wp_d = IN("ple_wp", [128, 2, D])
    u_d = IN("peer_u", [16384, D])
    v_d = IN("peer_v", [16384, D])
    wr_d = IN("lru_wr", [128, 8, 128])
    wi_d = IN("lru_wi", [128, 8, 128])
    w1_d = [IN("cmp_w1_k", [128, 32, 256]), IN("cmp_w1_v", [128, 32, 256])]
    w2_d = [IN("cmp_w2_k", [128, 2, 128]), IN("cmp_w2_v", [128, 2, 128])]
    gm_d = IN("norm_mix", [1, D])
    gf_d = IN("norm_ffn", [1, D])
    gp_d = IN("norm_ple", [1, D])
    gl_d = IN("norm_final", [1, D])
    cw_d = IN("conv_w", [128, 8, 4])
    cbias_d = IN("conv_b", [128, 8])
    br_d = IN("lru_br", [128, 8])
    bi_d = IN("lru_bi", [128, 8])
    lam_d = IN("lru_lam", [128, 8])
    b1_d = [IN("cmp_b1_k", [128, 2]), IN("cmp_b1_v", [128, 2])]
    b2k_d = IN("cmp_b2_k", [128, 1])
    b2v_d = IN("cmp_b2_v", [1, 128])
    pe_d = [IN("cmp_pe_k", [128, 32]), IN("cmp_pe_v", [128, 32])]
    kbd_d = IN("peer_kbd", [128, 256])
    identb_d = IN("c_identb", [128, 128], BF16)
    identf_d = IN("c_identf", [128, 128])
    invf_d = IN("c_invf", [128, 1])
    sgn_d = IN("c_sgn", [128, 1])
    cb_d = IN("c_cb", [128, 4, 512], BF16)
    wb_d = IN("c_wb", [128, 4, 512], BF16)
    cmb_d = IN("c_cmb", [128, 2, S], BF16)
    E_d = IN("c_E", [64, S], BF16)
    ovl_d = IN("c_ovl", [128, 2, 64], BF16)
    fa_d = IN("c_fa", [128, NTT, 64])
    na_d = IN("c_na", [128, NTT, 64])
    vm_d = IN("c_vm", [128, NTT, 64])
    io128_d = IN("c_iota128", [128, 16, 128], I32)
    io256_d = IN("c_iota256", [128, 8, 256], I32)
    io16_d = IN("c_iota16", [128, 8, 16, 16])

    out_d = nc.dram_tensor("out", [S, D], F32, kind="ExternalOutput").ap()

    dbg_n = [0]

    def DBG(tag, tt_, shape, dt=F32):
        if not debug:
            return
        dbg_n[0] += 1
        d = nc.dram_tensor(f"dbg_{tag}", list(shape), dt, kind="ExternalOutput").ap()
        P.dma("sp", d, tt_.t[:], reads=tt_.r)

    def SCR(name, shape, dt):
        kind = "ExternalOutput" if debug else "Internal"
        return nc.dram_tensor(name, list(shape), dt, kind=kind).ap()

    winbf = SCR("s_winbf", [128, 8, WCOLS], BF16)
    w5bf = [SCR(f"s_w5bf{i}", [128, 8, D], BF16) for i in range(5)]
    wpbf = SCR("s_wpbf", [128, 2, D], BF16)
    w1bf = [SCR(f"s_w1bf{i}", [128, 32, 256], BF16) for i in range(2)]
    zfm = SCR("s_zfm", [NCH, 128, S], F32)
    ma_s = SCR("s_ma", [8, 128, S], BF16)
    obt_s = SCR("s_obt", [8, 128, S], BF16)
    h1_s = SCR("s_h1", [S, D], F32)

    identb = P.sbuf("identb", [128, 128], BF16)
    identf = P.sbuf("identf", [128, 128], F32)
    P.dma("sp", identb.t[:, :], identb_d, writes=[identb.r0])
    P.dma("sp", identf.t[:, :], identf_d, writes=[identf.r0])

    cast_rr = [0]

    def cast(out, in_, reads, writes, engs=tuple(_os.environ.get("K_CAST", "dve,act").split(","))):
        e = engs[cast_rr[0] % len(engs)]
        cast_rr[0] += 1
        if e == "act":
            P.op("act", "copy", dict(out=out, in_=in_), reads, writes)
        else:
            P.op(e, "tensor_copy", dict(out=out, in_=in_), reads, writes)

    P.push()
    stg = P.sbuf("stg", [128, 2, 4096], F32, nres=2)
    stgb = P.sbuf("stgb", [128, 2, 4096], BF16, nres=2)
    pi = [0]

    def piece(loads, n, dst, dshape):
        b = pi[0] % 2
        pi[0] += 1
        for mk, src in loads:
            P.dma("sp", mk(stg.t[:, b, 0:n]), src, writes=[stg.r[b]])
        cast(stgb.t[:, b, 0:n], stg.t[:, b, 0:n], [stg.r[b]], [stgb.r[b]])
        P.dma("pool", dst, dshape(stgb.t[:, b, 0:n]), reads=[stgb.r[b]])

    win_v = win_d
    segs = win_segments()
    for c0 in range(0, WCOLS, 512):
        c1 = min(c0 + 512, WCOLS)
        w = c1 - c0
        loads = []
        for (dcol, scol, ncol) in segs:
            lo, hi = max(dcol, c0), min(dcol + ncol, c1)
            if lo >= hi:
                continue
            so = scol + (lo - dcol)
            loads.append((lambda a, lo=lo, hi=hi, w=w, c0=c0: a.rearrange("p (kc n) -> p kc n", kc=8)[:, :, lo - c0:hi - c0],
                          win_v[:, :, so:so + (hi - lo)]))
        if c1 == WCOLS and 8192 + 24 < WCOLS:
            pass
        piece(loads, 8 * w, winbf[:, :, c0:c1], lambda a, w=w: a.rearrange("p (kc n) -> p kc n", kc=8))
    for i in range(5):
        wv = w5_d[i]
        for hh in range(2):
            piece([(lambda a: a.rearrange("p (kc n) -> p kc n", kc=8), wv[:, :, hh * 512:(hh + 1) * 512])], 4096,
                  w5bf[i][:, :, hh * 512:(hh + 1) * 512], lambda a: a.rearrange("p (kc n) -> p kc n", kc=8))
    piece([(lambda a: a.rearrange("p (kc n) -> p kc n", kc=2), wp_d)], 2048,
          wpbf[:, :, :], lambda a: a.rearrange("p (kc n) -> p kc n", kc=2))
    for i in range(2):
        wv = w1_d[i]
        for hh in range(2):
            piece([(lambda a: a.rearrange("p (l h) -> p l h", l=16), wv[:, hh * 16:(hh + 1) * 16, :])], 4096,
                  w1bf[i][:, hh * 16:(hh + 1) * 16, :], lambda a: a.rearrange("p (l h) -> p l h", l=16))
    P.emit_stage()
    P.pop()
    if upto == "P":
        P.close()
        return nc

    P.push()
    nT = P.sbuf("nT", [128, 8, S], BF16, nres=NTT)
    gbc = P.sbuf("gbc", [128, D], F32)
    xt = P.sbuf("xt", [128, 2, D], F32, nres=2)
    nb = P.sbuf("nb", [128, 2, D], BF16, nres=2)
    junk = P.sbuf("junk", [128, D], F32)
    ss = P.sbuf("ss", [128, NTT], F32, nres=NTT)
    rs = P.sbuf("rs", [128, NTT], F32, nres=NTT)
    tp = P.psum("tp", [128, 2, 1024], BF16, nres=2)
    P.dma("sp", gbc.t[:, :], gm_d.partition_broadcast(128), writes=[gbc.r0])
    for tt in range(NTT):
        b = tt % 2
        P.dma("sp", xt.t[:, b, :], x_d[tt * 128:(tt + 1) * 128, :], writes=[xt.r[b]])
        P.op("act", "activation", dict(out=junk.t[:, :], in_=xt.t[:, b, :], func=AF.Square, accum_out=ss.t[:, tt:tt + 1]),
             reads=[xt.r[b]], writes=[junk.r0, ss.r[tt]])
        P.op("act", "activation", dict(out=rs.t[:, tt:tt + 1], in_=ss.t[:, tt:tt + 1], func=AF.Sqrt, scale=1.0 / D, bias=1e-6),
             reads=[ss.r[tt]], writes=[rs.r[tt]])
        P.op("dve", "reciprocal", dict(out=rs.t[:, tt:tt + 1], in_=rs.t[:, tt:tt + 1]), reads=[rs.r[tt]], writes=[rs.r[tt]])
        P.op("dve", "scalar_tensor_tensor", dict(out=nb.t[:, b, :], in0=xt.t[:, b, :], scalar=rs.t[:, tt:tt + 1], in1=gbc.t[:, :],
                                                 op0=ALU.mult, op1=ALU.mult), reads=[xt.r[b], rs.r[tt], gbc.r0], writes=[nb.r[b]])
        for c in range(8):
            P.op("pe", "transpose", dict(out=tp.t[:, b, c * 128:(c + 1) * 128], in_=nb.t[:, b, c * 128:(c + 1) * 128], identity=identb.t[:, :]),
                 reads=[nb.r[b], identb.r0], writes=[tp.r[b]])
        cast(nT.t[:, :, tt * 128:(tt + 1) * 128], tp.t[:, b, :].rearrange("p (c t) -> p c t", c=8), [tp.r[b]], [nT.r[tt]])

    wbuf = P.sbuf("wbuf", [128, 2, 8, 512], BF16, nres=2)
    zst = P.sbuf("zst", [128, 2, S], F32, nres=2)
    pz = P.psum("pz", [128, 4, 512], F32, nres=4)
    nTall = list(nT.r)
    zi = 0
    pzi = 0
    for wpi, c0 in enumerate(range(0, WCOLS, 512)):
        c1 = min(c0 + 512, WCOLS)
        wb_ = wpi % 2
        P.dma("sp", wbuf.t[:, wb_, :, 0:c1 - c0], winbf[:, :, c0:c1], writes=[wbuf.r[wb_]])
        for cc in range((c1 - c0) // 128):
            ch = c0 // 128 + cc
            M = 24 if ch == C_NG else 128
            zb = zi % 2
            zi += 1
            for qc in range(NQC):
                pb = pzi % 4
                pzi += 1
                for kc in range(8):
                    P.op("pe", "matmul", dict(out=pz.t[0:M, pb, :], lhsT=wbuf.t[:, wb_, kc, cc * 128:cc * 128 + M],
                                              rhs=nT.t[:, kc, qc * 512:(qc + 1) * 512], start=(kc == 0), stop=(kc == 7),
                                              skip_group_check=True),
                         reads=[wbuf.r[wb_]] + nTall[qc * 4:(qc + 1) * 4], writes=[pz.r[pb]])
                dst = zst.t[0:M, zb, qc * 512:(qc + 1) * 512]
                src = pz.t[0:M, pb, :]
                if C_RG <= ch < C_RG + 8:
                    P.op("act", "activation", dict(out=dst, in_=src, func=AF.Gelu_apprx_tanh), [pz.r[pb]], [zst.r[zb]])
                elif ch >= C_M:
                    P.op("act", "activation", dict(out=dst, in_=src, func=AF.Sigmoid), [pz.r[pb]], [zst.r[zb]])
                else:
                    P.op("dve", "tensor_copy", dict(out=dst, in_=src), [pz.r[pb]], [zst.r[zb]])
            P.dma("pool", zfm[ch, 0:M, :], zst.t[0:M, zb, :], reads=[zst.r[zb]])
    P.emit_stage()
    P.pop()
    if upto == "A":
        P.close()
        return nc

    P.push()
    cw = P.sbuf("cw", [128, 8, 4], F32)
    cbias = P.sbuf("cbias", [128, 8], F32)
    brs = P.sbuf("brs", [128, 8], F32)
    bis = P.sbuf("bis", [128, 8], F32)
    lam = P.sbuf("lam", [128, 8], F32)
    m8 = P.sbuf("m8", [128, 8], F32)
    m16 = P.sbuf("m16", [128, 8], F32)
    wst = P.sbuf("wst", [128, 2, 8, 128], F32, nres=2)
    wrb = P.sbuf("wrb", [128, 8, 128], BF16)
    wib = P.sbuf("wib", [128, 8, 128], BF16)
    for (t_, d_) in ((cw, cw_d), (cbias, cbias_d), (brs, br_d), (bis, bi_d), (lam, lam_d)):
        P.dma("sp", t_.t[:], d_, writes=[t_.r0])
    P.dma("sp", wst.t[:, 0], wr_d, writes=[wst.r[0]])
    P.dma("sp", wst.t[:, 1], wi_d, writes=[wst.r[1]])
    P.op("dve", "tensor_copy", dict(out=wrb.t[:, :, :], in_=wst.t[:, 0]), [wst.r[0]], [wrb.r0])
    P.op("dve", "tensor_copy", dict(out=wib.t[:, :, :], in_=wst.t[:, 1]), [wst.r[1]], [wib.r0])
    P.op("act", "activation", dict(out=m8.t[:, :], in_=lam.t[:, :], func=AF.Exp, scale=-1.0), [lam.r0], [m8.r0])
    P.op("act", "activation", dict(out=m8.t[:, :], in_=m8.t[:, :], func=AF.Ln, bias=1.0), [m8.r0], [m8.r0])
    P.op("dve", "tensor_scalar", dict(out=m16.t[:, :], in0=m8.t[:, :], scalar1=-16.0, scalar2=None, op0=ALU.mult), [m8.r0], [m16.r0])
    P.op("dve", "tensor_scalar", dict(out=m8.t[:, :], in0=m8.t[:, :], scalar1=-8.0, scalar2=None, op0=ALU.mult), [m8.r0, m16.r0], [m8.r0])
    zx = P.sbuf("zx", [128, S + 3], F32)
    gz = P.sbuf("gz", [128, S], F32)
    xa = P.sbuf("xa", [128, S], F32)
    xab = P.sbuf("xab", [128, S], BF16)
    rr = P.sbuf("rr", [128, S], F32)
    ig = P.sbuf("ig", [128, S], F32)
    t1 = P.sbuf("t1", [128, S], F32)
    t2 = P.sbuf("t2", [128, S], F32)
    mab = P.sbuf("mab", [128, S], BF16)
    pg = P.psum("pg", [128, 4, 512], F32, nres=4)
    P.op("dve", "memset", dict(ap=zx.t[:, 0:3], constant=0.0), [], [zx.r0])
    pgi = 0
    for ch in range(8):
        P.dma("sp", zx.t[:, 3:S + 3], zfm[C_RX + ch], writes=[zx.r0])
        P.dma("sp", gz.t[:, :], zfm[C_RG + ch], writes=[gz.r0])
        P.op("dve", "tensor_scalar", dict(out=xa.t[:, :], in0=zx.t[:, 0:S], scalar1=cw.t[:, ch, 0:1], scalar2=cbias.t[:, ch:ch + 1],
                                          op0=ALU.mult, op1=ALU.add), [zx.r0, cw.r0, cbias.r0], [xa.r0])
        for k in range(1, 4):
            P.op("dve", "scalar_tensor_tensor", dict(out=xa.t[:, :], in0=zx.t[:, k:k + S], scalar=cw.t[:, ch, k:k + 1], in1=xa.t[:, :],
                                                     op0=ALU.mult, op1=ALU.add), [zx.r0, cw.r0, xa.r0], [xa.r0])
        P.op("act", "copy", dict(out=xab.t[:, :], in_=xa.t[:, :]), [xa.r0], [xab.r0])
        for (wgt, bia, dst) in ((wrb, brs, rr), (wib, bis, ig)):
            for qc in range(NQC):
                pb = pgi % 4
                pgi += 1
                P.op("pe", "matmul", dict(out=pg.t[:, pb, :], lhsT=wgt.t[:, ch, :], rhs=xab.t[:, qc * 512:(qc + 1) * 512], start=True, stop=True,
                                          skip_group_check=True), [wgt.r0, xab.r0], [pg.r[pb]])
                P.op("act", "activation", dict(out=dst.t[:, qc * 512:(qc + 1) * 512], in_=pg.t[:, pb, :], func=AF.Sigmoid, bias=bia.t[:, ch:ch + 1]),
                     [pg.r[pb], bia.r0], [dst.r0])
        P.op("act", "activation", dict(out=t1.t[:, :], in_=rr.t[:, :], func=AF.Exp, scale=m8.t[:, ch:ch + 1]), [rr.r0, m8.r0], [t1.r0])
        P.op("act", "activation", dict(out=t2.t[:, :], in_=rr.t[:, :], func=AF.Exp, scale=m16.t[:, ch:ch + 1]), [rr.r0, m16.r0], [t2.r0])
        P.op("act", "activation", dict(out=t2.t[:, :], in_=t2.t[:, :], func=AF.Sqrt, scale=-1.0, bias=1.0), [t2.r0], [t2.r0])
        P.op("pool", "tensor_tensor", dict(out=ig.t[:, :], in0=ig.t[:, :], in1=xa.t[:, :], op=ALU.mult), [ig.r0, xa.r0], [ig.r0])
        P.op("pool", "tensor_tensor", dict(out=ig.t[:, :], in0=ig.t[:, :], in1=t2.t[:, :], op=ALU.mult), [ig.r0, t2.r0], [ig.r0])
        P.op("dve", "tensor_tensor_scan", dict(out=rr.t[:, :], data0=t1.t[:, :], data1=ig.t[:, :], initial=0.0, op0=ALU.mult, op1=ALU.add),
             [t1.r0, ig.r0, rr.r0], [rr.r0])
        P.op("dve", "tensor_tensor", dict(out=mab.t[:, :], in0=rr.t[:, :], in1=gz.t[:, :], op=ALU.mult), [rr.r0, gz.r0], [mab.r0])
        P.dma("pool", ma_s[ch], mab.t[:, :], reads=[mab.r0])
    P.emit_stage()
    P.pop()
    if upto == "B":
        P.close()
        return nc

    P.push()
    cosT = P.sbuf("cosT", [128, S], F32)
    sinT = P.sbuf("sinT", [128, S], F32)
    P.push()
    posi = P.sbuf("posi", [128, S], I32)
    tq_ = P.sbuf("tq", [128, S], F32)
    tk_ = P.sbuf("tk", [128, S], F32)
    ki_ = P.sbuf("ki", [128, S], I32)
    invf = P.sbuf("invf", [128, 1], F32)
    sgn = P.sbuf("sgn", [128, 1], F32)
    P.dma("sp", posi.t[:, :], pos_d.partition_broadcast(128), writes=[posi.r0])
    P.dma("sp", invf.t[:, :], invf_d, writes=[invf.r0])
    P.dma("sp", sgn.t[:, :], sgn_d, writes=[sgn.r0])
    P.op("dve", "tensor_copy", dict(out=tq_.t[:, :], in_=posi.t[:, :]), [posi.r0], [tq_.r0])
    P.op("dve", "tensor_scalar", dict(out=tq_.t[:, :], in0=tq_.t[:, :], scalar1=invf.t[:, 0:1], scalar2=float(1.0 / (2 * np.pi)),
                                      op0=ALU.mult, op1=ALU.mult), [tq_.r0, invf.r0], [tq_.r0])
    TWO_PI = float(2 * np.pi * (1 - 1e-6))
    for which, dstT in (("sin", sinT), ("cos", cosT)):
        if which == "cos":
            P.op("dve", "tensor_scalar", dict(out=tq_.t[:, :], in0=tq_.t[:, :], scalar1=0.25, scalar2=None, op0=ALU.add), [tq_.r0], [tq_.r0])
        P.op("dve", "tensor_copy", dict(out=ki_.t[:, :], in_=tq_.t[:, :]), [tq_.r0], [ki_.r0])
        P.op("dve", "tensor_copy", dict(out=tk_.t[:, :], in_=ki_.t[:, :]), [ki_.r0], [tk_.r0])
        P.op("dve", "tensor_tensor", dict(out=tk_.t[:, :], in0=tq_.t[:, :], in1=tk_.t[:, :], op=ALU.subtract), [tq_.r0, tk_.r0], [tk_.r0])
        P.op("act", "activation", dict(out=dstT.t[:, :], in_=tk_.t[:, :], func=AF.Sin, scale=TWO_PI), [tk_.r0], [dstT.r0])
    P.op("dve", "tensor_scalar", dict(out=sinT.t[:, :], in0=sinT.t[:, :], scalar1=sgn.t[:, 0:1], scalar2=None, op0=ALU.mult), [sinT.r0, sgn.r0], [sinT.r0])
    P.emit_stage()
    P.pop()

    ksT = P.sbuf("ksT", [128, S], BF16)
    kwT = P.sbuf("kwT", [128, S], BF16)
    vsa = P.sbuf("vsa", [128, NTT, 129], BF16)
    vwa = P.sbuf("vwa", [128, NTT, 129], BF16)
    kcT = P.sbuf("kcT", [128, 256], BF16)
    vca = P.sbuf("vca", [128, 2, 193], BF16)
    E_s = P.sbuf("E_s", [64, S], BF16)
    cb_s = P.sbuf("cb_s", [128, 4, 512], BF16)
    wb_s = P.sbuf("wb_s", [128, 4, 512], BF16)
    b1s = [P.sbuf(f"b1s{i}", [128, 2], F32) for i in range(2)]
    pes = [P.sbuf(f"pes{i}", [128, 32], F32) for i in range(2)]
    b2k = P.sbuf("b2k", [128, 1], F32)
    b2vb = P.sbuf("b2vb", [128, 128], F32)
    ps_s = P.psum("ps_s", [128, 2, 512], F32, nres=2)
    ps_acc = P.psum("ps_acc", [128, 4, 512], F32, nres=4)
    ps_trb = P.psum("ps_trb", [128, 1024], BF16)
    ps_trf = P.psum("ps_trf", [128, 512], F32)
    P.dma("sp", E_s.t[:, :], E_d, writes=[E_s.r0])
    P.dma("sp", cb_s.t[:], cb_d, writes=[cb_s.r0])
    P.dma("sp", wb_s.t[:], wb_d, writes=[wb_s.r0])
    for i in range(2):
        P.dma("sp", b1s[i].t[:, :], b1_d[i], writes=[b1s[i].r0])
        P.dma("sp", pes[i].t[:, :], pe_d[i], writes=[pes[i].r0])
    P.dma("sp", b2k.t[:, :], b2k_d, writes=[b2k.r0])
    P.dma("sp", b2vb.t[:, :], b2v_d.partition_broadcast(128), writes=[b2vb.r0])
    P.op("dve", "memset", dict(ap=vsa.t[:, :, 128:129], constant=1.0), [], [vsa.r0])
    P.op("dve", "memset", dict(ap=vwa.t[:, :, 128:129], constant=1.0), [], [vwa.r0])
    srot = [0]

    def sbank():
        srot[0] += 1
        return srot[0] % 2

    def mm(out, lhsT, rhs, start, stop, reads, writes):
        P.op("pe", "matmul", dict(out=out, lhsT=lhsT, rhs=rhs, start=start, stop=stop, skip_group_check=True), reads, writes)

    for g in range(2):
        P.push()
        ld = P.sbuf("ld", [128, 2, S], F32, nres=2)
        lohi = P.sbuf("lohi", [128, 2, S], BF16, nres=2)
        w1s = P.sbuf("w1s", [128, 32, 256], BF16)
        w2f = P.sbuf("w2f", [128, 2, 128], F32)
        w2s = P.sbuf("w2s", [128, 2, 128], BF16)
        hidT = P.sbuf("hidT", [128, 2, 256], BF16)
        vb = P.sbuf("vb", [128, S], BF16)
        P.op("dve", "memset", dict(ap=kcT.t[:, :], constant=0.0), [], [kcT.r0])
        P.op("dve", "memset", dict(ap=vca.t[:], constant=0.0), [], [vca.r0])
        P.op("dve", "memset", dict(ap=vca.t[:, :, 128:129], constant=1.0), [vca.r0], [vca.r0])
        P.dma("sp", vca.t[:, :, 129:193], ovl_d, writes=[vca.r0])
        for kvi in range(2):
            ch = C_KV + kvi * 2 + g
            P.dma("sp", ld.t[:, 0, :], zfm[ch], writes=[ld.r[0]])
            P.dma("sp", w1s.t[:], w1bf[kvi], writes=[w1s.r0])
            P.dma("sp", w2f.t[:], w2_d[kvi], writes=[w2f.r0])
            P.op("dve", "tensor_copy", dict(out=w2s.t[:], in_=w2f.t[:]), [w2f.r0], [w2s.r0])
            for v_ in range(2):
                P.op("dve", "tensor_tensor", dict(out=lohi.t[:, v_, :].rearrange("p (n l) -> p n l", l=16),
                                                  in0=ld.t[:, 0, :].rearrange("p (n l) -> p n l", l=16),
                                                  in1=pes[kvi].t[:, v_ * 16:(v_ + 1) * 16].unsqueeze(1).broadcast_to([128, 256, 16]),
                                                  op=ALU.add), [ld.r[0], pes[kvi].r0], [lohi.r[v_]])
            for hc in range(2):
                for l in range(32):
                    v_ = 0 if l < 16 else 1
                    mm(ps_s.t[:, hc, 0:255], w1s.t[:, l, hc * 128:(hc + 1) * 128], lohi.t[:, v_, l:l + 16 * 254 + 1:16],
                       l == 0, l == 31, [w1s.r0, lohi.r[v_]], [ps_s.r[hc]])
                P.op("act", "activation", dict(out=hidT.t[:, hc, 0:255], in_=ps_s.t[:, hc, 0:255], func=AF.Gelu_apprx_tanh,
                                               bias=b1s[kvi].t[:, hc:hc + 1]), [ps_s.r[hc], b1s[kvi].r0], [hidT.r0])
            if kvi == 0:
                for hc in range(2):
                    mm(ps_s.t[:, 0, 0:255], w2s.t[:, hc, :], hidT.t[:, hc, 0:255], hc == 0, hc == 1, [w2s.r0, hidT.r0], [ps_s.r[0]])
                P.op("dve", "tensor_scalar", dict(out=kcT.t[:, 0:255], in0=ps_s.t[:, 0, 0:255], scalar1=b2k.t[:, 0:1], scalar2=None, op0=ALU.add),
                     [ps_s.r[0], b2k.r0], [kcT.r0])
            else:
                for nch in range(2):
                    rows = 128 if nch == 0 else 127
                    for hc in range(2):
                        mm(ps_acc.t[0:rows, nch, 0:128], hidT.t[:, hc, nch * 128:nch * 128 + rows], w2s.t[:, hc, :], hc == 0, hc == 1,
                           [w2s.r0, hidT.r0], [ps_acc.r[nch]])
                    P.op("dve", "tensor_tensor", dict(out=vca.t[0:rows, nch, 0:128], in0=ps_acc.t[0:rows, nch, 0:128], in1=b2vb.t[0:rows, :], op=ALU.add),
                         [ps_acc.r[nch], b2vb.r0], [vca.r0])
        for (kch, swch, dstK) in ((C_KV + 4 + g, C_KSW + g, ksT), (C_KV + 8 + g, C_KSW + 2 + g, kwT)):
            P.dma("sp", ld.t[:, 0, :], zfm[kch], writes=[ld.r[0]])
            P.dma("sp", ld.t[:, 1, :], zfm[swch], writes=[ld.r[1]])
            if debug and g == 1 and dstK is ksT:
                DBG("g1_ld_raw", ld, [128, 2, S])
                DBG("g1_cosT", cosT, [128, S])
                DBG("g1_sinT", sinT, [128, S])
            P.op("dve", "tensor_tensor", dict(out=ld.t[:, 0, :], in0=ld.t[:, 0, :], in1=cosT.t[:, :], op=ALU.mult), [ld.r[0], cosT.r0], [ld.r[0]])
            P.op("pool", "tensor_tensor", dict(out=ld.t[:, 1, :], in0=ld.t[:, 1, :], in1=sinT.t[:, :], op=ALU.mult), [ld.r[1], sinT.r0], [ld.r[1]])
            if debug and g == 1 and dstK is ksT:
                DBG("g1_ld_mul", ld, [128, 2, S])
            P.op("dve", "tensor_tensor", dict(out=dstK.t[:, :], in0=ld.t[:, 0, :], in1=ld.t[:, 1, :], op=ALU.add), [ld.r[0], ld.r[1]], [dstK.r0])
        for (vch, dstV) in ((C_KV + 6 + g, vsa), (C_KV + 10 + g, vwa)):
            P.dma("sp", ld.t[:, 0, :], zfm[vch], writes=[ld.r[0]])
            P.op("act", "copy", dict(out=vb.t[:, :], in_=ld.t[:, 0, :]), [ld.r[0]], [vb.r0])
            for t4 in range(8):
                for a in range(4):
                    tt = t4 * 4 + a
                    P.op("pe", "transpose", dict(out=ps_trb.t[:, a * 128:(a + 1) * 128], in_=vb.t[:, tt * 128:(tt + 1) * 128], identity=identb.t[:, :]),
                         [vb.r0, identb.r0], [ps_trb.r0])
                cast(dstV.t[:, t4 * 4:(t4 + 1) * 4, 0:128], ps_trb.t[:, 0:512].rearrange("p (a d) -> p a d", a=4), [ps_trb.r0], [dstV.r0])
        P.emit_stage()
        P.pop()
        if upto == "C0":
            P.close()
            return nc

        P.push()
        zq = P.sbuf("zq", [128, 4, 512], F32, nres=4)
        zqs = P.sbuf("zqs", [128, 4, 512], F32, nres=4)
        qn = P.sbuf("qn", [128, 4, 512], BF16)
        qr = P.sbuf("qr", [128, 4, 512], BF16)
        cmbq = P.sbuf("cmbq", [128, 2, 512], BF16)
        fa = P.sbuf("fa", [128, 4, 64], F32)
        na = P.sbuf("na", [128, 4, 64], F32)
        vm = P.sbuf("vm", [128, 4, 64], F32)
        gTs = P.sbuf("gTs", [32, 512], F32)
        gts = P.sbuf("gts", [128, 4, 24], F32)
        pc = P.sbuf("pc", [128, 2, 2, 512], BF16, nres=2)
        pt = P.sbuf("pt", [128, 3, 512], BF16, nres=3)
        oacc = P.sbuf("oacc", [128, 4, 4, 128], F32)
        imp = P.sbuf("imp", [128, 4, 64], F32)
        impp = P.sbuf("impp", [128, 64], F32)
        imp2 = P.sbuf("imp2", [128, 64], F32)
        m8a = P.sbuf("m8a", [128, 8], F32)
        m8b = P.sbuf("m8b", [128, 8], F32)
        msel = P.sbuf("msel", [128, 64], F32)
        bmb = P.sbuf("bmb", [128, 64], BF16)
        biasT = P.sbuf("biasT", [64, 512], BF16)
        sm = P.sbuf("sm", [128, 8], F32)
        obb = P.sbuf("obb", [128, 4, 4, 128], BF16)
        obT = P.sbuf("obT", [128, 4, 512], BF16)
        ptr = [0]
        aset = [0]
        for qc in range(NQC):
            q0 = qc * 512
            for hq in range(4):
                P.dma("sp", zq.t[:, hq, :], zfm[C_Q + 4 * g + hq, :, q0:q0 + 512], writes=[zq.r[hq]])
                P.dma("sp", zqs.t[:, hq, :], zfm[C_QS + 4 * g + hq, :, q0:q0 + 512], writes=[zqs.r[hq]])
            P.dma("sp", cmbq.t[:], cmb_d[:, :, q0:q0 + 512], writes=[cmbq.r0])
            P.dma("sp", fa.t[:], fa_d[:, qc * 4:qc * 4 + 4, :], writes=[fa.r0])
            P.dma("sp", na.t[:], na_d[:, qc * 4:qc * 4 + 4, :], writes=[na.r0])
            P.dma("sp", vm.t[:], vm_d[:, qc * 4:qc * 4 + 4, :], writes=[vm.r0])
            P.dma("sp", gTs.t[:, :], zfm[C_NG, 0:32, q0:q0 + 512], writes=[gTs.r0])
            P.op("act", "copy", dict(out=qn.t[:], in_=zq.t[:]), zq.r, [qn.r0])
            cs = cosT.t[:, q0:q0 + 512].unsqueeze(1).broadcast_to([128, 4, 512])
            sn = sinT.t[:, q0:q0 + 512].unsqueeze(1).broadcast_to([128, 4, 512])
            P.op("dve", "tensor_tensor", dict(out=zq.t[:], in0=zq.t[:], in1=cs, op=ALU.mult), zq.r + [cosT.r0], zq.r)
            P.op("pool", "tensor_tensor", dict(out=zqs.t[:], in0=zqs.t[:], in1=sn, op=ALU.mult), zqs.r + [sinT.r0], zqs.r)
            P.op("dve", "tensor_tensor", dict(out=qr.t[:], in0=zq.t[:], in1=zqs.t[:], op=ALU.add), zq.r + zqs.r, [qr.r0])
            for qt in range(4):
                P.op("pe", "transpose", dict(out=ps_trf.t[:, qt * 24:(qt + 1) * 24], in_=gTs.t[0:24, qt * 128:(qt + 1) * 128], identity=identf.t[0:24, 0:24]),
                     [gTs.r0, identf.r0], [ps_trf.r0])
            P.op("dve", "tensor_copy", dict(out=gts.t[:].rearrange("p a c -> p (a c)"), in_=ps_trf.t[:, 0:96]), [ps_trf.r0], [gts.r0])

            def post(hh, qt, bank, off, br, first, aux=None):
                h = 4 * g + hh
                accv = ps_acc.t[:, bank, :]
                P.op("dve", "tensor_scalar", dict(out=sm.t[:, 0:1], in0=accv[:, off + 128:off + 129], scalar1=1e-30, scalar2=None, op0=ALU.max),
                     [ps_acc.r[bank]], [sm.r0])
                P.op("dve", "reciprocal", dict(out=sm.t[:, 1:2], in_=sm.t[:, 0:1]), [sm.r0], [sm.r0])
                P.op("dve", "tensor_tensor", dict(out=sm.t[:, 2:3], in0=sm.t[:, 1:2], in1=gts.t[:, qt, h * 3 + br:h * 3 + br + 1], op=ALU.mult),
                     [sm.r0, gts.r0], [sm.r0])
                if first:
                    P.op("dve", "tensor_scalar", dict(out=oacc.t[:, qt, hh, :], in0=accv[:, off:off + 128], scalar1=sm.t[:, 2:3], scalar2=None, op0=ALU.mult),
                         [ps_acc.r[bank], sm.r0], [oacc.r0])
                else:
                    P.op("dve", "scalar_tensor_tensor", dict(out=oacc.t[:, qt, hh, :], in0=accv[:, off:off + 128], scalar=sm.t[:, 2:3], in1=oacc.t[:, qt, hh, :],
                                                             op0=ALU.mult, op1=ALU.add), [ps_acc.r[bank], sm.r0, oacc.r0], [oacc.r0])
                if aux is not None:
                    if hh == 0:
                        P.op("dve", "tensor_scalar", dict(out=imp.t[:, qt, :], in0=accv[:, off + 129:off + 193], scalar1=sm.t[:, 1:2], scalar2=None, op0=ALU.mult),
                             [ps_acc.r[bank], sm.r0], [imp.r0])
                    else:
                        P.op("dve", "scalar_tensor_tensor", dict(out=imp.t[:, qt, :], in0=accv[:, off + 129:off + 193], scalar=sm.t[:, 1:2], in1=imp.t[:, qt, :],
                                                                 op0=ALU.mult, op1=ALU.add), [ps_acc.r[bank], sm.r0, imp.r0], [imp.r0])

            if (g, qc) in ((0, 0), (1, 1)):
                tg = f"g{g}q{qc}"
                DBG(tg + "_gts", gts, [128, 4, 24]); DBG(tg + "_qn", qn, [128, 4, 512], BF16); DBG(tg + "_qr", qr, [128, 4, 512], BF16)
                DBG(tg + "_kcT", kcT, [128, 256], BF16); DBG(tg + "_vca", vca, [128, 2, 193], BF16)
                DBG(tg + "_ksT", ksT, [128, S], BF16); DBG(tg + "_kwT", kwT, [128, S], BF16)
                DBG(tg + "_vsa", vsa, [128, NTT, 129], BF16); DBG(tg + "_vwa", vwa, [128, NTT, 129], BF16)
            n_nch = 2 if (q0 + 511 >= 16 * 128 + 31) else 1
            for hh in range(4):
                pcb = hh % 2
                for nch in range(n_nch):
                    sb = sbank()
                    mm(ps_s.t[:, sb, :], kcT.t[:, nch * 128:(nch + 1) * 128], qn.t[:, hh, :], True, False, [kcT.r0, qn.r0], [ps_s.r[sb]])
                    mm(ps_s.t[:, sb, :], identb.t[:, :], cmbq.t[:, nch, :], False, True, [identb.r0, cmbq.r0], [ps_s.r[sb]])
                    P.op("act", "activation", dict(out=pc.t[:, pcb, nch, :], in_=ps_s.t[:, sb, :], func=AF.Exp, scale=SCALE), [ps_s.r[sb]], [pc.r[pcb]])
                aset[0] ^= 1
                started = {}
                for qt in range(4):
                    bank = aset[0] * 2 + qt // 2
                    off = (qt % 2) * 193
                    for nch in range(n_nch):
                        mm(ps_acc.t[:, bank, off:off + 193], pc.t[:, pcb, nch, qt * 128:(qt + 1) * 128], vca.t[:, nch, :],
                           bank not in started, False, [pc.r[pcb], vca.r0], [ps_acc.r[bank]])
                        started[bank] = 1
                for qt in range(4):
                    post(hh, qt, aset[0] * 2 + qt // 2, (qt % 2) * 193, 0, True, aux=True)
            if (g, qc) in ((0, 0), (1, 1)):
                DBG(tg + "_oacc_c", oacc, [128, 4, 4, 128]); DBG(tg + "_imp", imp, [128, 4, 64])
            for qt in range(4):
                P.op("dve", "tensor_tensor", dict(out=impp.t[:, :], in0=imp.t[:, qt, :], in1=fa.t[:, qt, :], op=ALU.max), [imp.r0, fa.r0], [impp.r0])
                P.op("dve", "tensor_tensor", dict(out=impp.t[:, :], in0=impp.t[:, :], in1=na.t[:, qt, :], op=ALU.add), [impp.r0, na.r0], [impp.r0])
                P.op("dve", "max", dict(out=m8a.t[:, :], in_=impp.t[:, :]), [impp.r0], [m8a.r0])
                P.op("dve", "match_replace", dict(out=imp2.t[:, :], in_to_replace=m8a.t[:, :], in_values=impp.t[:, :], imm_value=-3.0e38),
                     [impp.r0, m8a.r0], [imp2.r0])
                P.op("dve", "max", dict(out=m8b.t[:, :], in_=imp2.t[:, :]), [imp2.r0], [m8b.r0])
                P.op("dve", "scalar_tensor_tensor", dict(out=msel.t[:, :], in0=impp.t[:, :], scalar=m8b.t[:, 7:8], in1=vm.t[:, qt, :],
                                                         op0=ALU.is_ge, op1=ALU.mult), [impp.r0, m8b.r0, vm.r0], [msel.r0])
                P.op("dve", "tensor_scalar", dict(out=bmb.t[:, :], in0=msel.t[:, :], scalar1=-NEG, scalar2=NEG, op0=ALU.mult, op1=ALU.add),
                     [msel.r0], [bmb.r0])
                P.op("pe", "transpose", dict(out=ps_trb.t[0:64, qt * 128:(qt + 1) * 128], in_=bmb.t[:, :], identity=identb.t[:, :]),
                     [bmb.r0, identb.r0], [ps_trb.r0])
            P.op("dve", "tensor_copy", dict(out=biasT.t[:, :], in_=ps_trb.t[0:64, 0:512]), [ps_trb.r0], [biasT.r0])
            if (g, qc) in ((0, 0), (1, 1)):
                DBG(tg + "_biasT", biasT, [64, 512], BF16)
            for br in (1, 2):
                if br == 2 and (g, qc) in ((0, 0), (1, 1)):
                    DBG(tg + "_oacc_s", oacc, [128, 4, 4, 128])
                for hh in range(4):
                    aset[0] ^= 1
                    started = {}
                    if br == 1:
                        kts = list(range(0, qc * 4 + 4))
                    else:
                        kts = list(range(max(0, qc * 4 - 4), qc * 4 + 4))
                    for kt in kts:
                        r = kt - qc * 4
                        sb = sbank()
                        if br == 1:
                            mm(ps_s.t[:, sb, :], ksT.t[:, kt * 128:(kt + 1) * 128], qr.t[:, hh, :], True, False, [ksT.r0, qr.r0], [ps_s.r[sb]])
                            mm(ps_s.t[:, sb, :], E_s.t[0:64, kt * 128:(kt + 1) * 128], biasT.t[0:64, :], False, r < 0, [E_s.r0, biasT.r0], [ps_s.r[sb]])
                            if r >= 0:
                                mm(ps_s.t[:, sb, :], identb.t[:, :], cb_s.t[:, r, :], False, True, [identb.r0, cb_s.r0], [ps_s.r[sb]])
                        else:
                            mm(ps_s.t[:, sb, :], kwT.t[:, kt * 128:(kt + 1) * 128], qr.t[:, hh, :], True, False, [kwT.r0, qr.r0], [ps_s.r[sb]])
                            btab = wb_s.t[:, r + 4, :] if r < 0 else cb_s.t[:, r, :]
                            mm(ps_s.t[:, sb, :], identb.t[:, :], btab, False, True, [identb.r0, cb_s.r0, wb_s.r0], [ps_s.r[sb]])
                        pb = ptr[0] % 3
                        ptr[0] += 1
                        P.op("act", "activation", dict(out=pt.t[:, pb, :], in_=ps_s.t[:, sb, :], func=AF.Exp, scale=SCALE), [ps_s.r[sb]], [pt.r[pb]])
                        vsrc = vsa if br == 1 else vwa
                        for qt in range(4):
                            if r > qt:
                                continue
                            if br == 2 and r < qt - 4:
                                continue
                            bank = aset[0] * 2 + qt // 2
                            off = (qt % 2) * 129
                            mm(ps_acc.t[:, bank, off:off + 129], pt.t[:, pb, qt * 128:(qt + 1) * 128], vsrc.t[:, kt, :],
                               bank not in started, False, [pt.r[pb], vsrc.r0], [ps_acc.r[bank]])
                            started[bank] = 1
                    for qt in range(4):
                        post(hh, qt, aset[0] * 2 + qt // 2, (qt % 2) * 129, br, False)
            if (g, qc) in ((0, 0), (1, 1)):
                DBG(tg + "_oacc_w", oacc, [128, 4, 4, 128])
            P.op("act", "copy", dict(out=obb.t[:], in_=oacc.t[:]), [oacc.r0], [obb.r0])
            for hh in range(4):
                for qt in range(4):
                    P.op("pe", "transpose", dict(out=ps_trb.t[:, qt * 128:(qt + 1) * 128], in_=obb.t[:, qt, hh, :], identity=identb.t[:, :]),
                         [obb.r0, identb.r0], [ps_trb.r0])
                cast(obT.t[:, hh, :], ps_trb.t[:, 0:512], [ps_trb.r0], [obT.r0])
            for hq in range(4):
                P.dma("pool", obt_s[4 * g + hq, :, q0:q0 + 512], obT.t[:, hq, :], reads=[obT.r0])
        P.emit_stage()
        P.pop()
    P.pop()
    if upto == "C":
        P.close()
        return nc

    def rmsnorm_tile(src, gain, dst_f32, dst_bf, sq, junk_):
        P.op("act", "activation", dict(out=junk_.t[:, :], in_=src.t[:, :], func=AF.Square, accum_out=sq.t[:, 0:1]), [src.r0], [junk_.r0, sq.r0])
        P.op("act", "activation", dict(out=sq.t[:, 1:2], in_=sq.t[:, 0:1], func=AF.Sqrt, scale=1.0 / D, bias=1e-6), [sq.r0], [sq.r0])
        P.op("dve", "reciprocal", dict(out=sq.t[:, 2:3], in_=sq.t[:, 1:2]), [sq.r0], [sq.r0])
        P.op("dve", "scalar_tensor_tensor", dict(out=dst_f32.t[:, :], in0=src.t[:, :], scalar=sq.t[:, 2:3], in1=gain.t[:, :], op0=ALU.mult, op1=ALU.mult),
             [src.r0, sq.r0, gain.r0], [dst_f32.r0])
        if dst_bf is not None:
            P.op("act", "copy", dict(out=dst_bf.t[:, :], in_=dst_f32.t[:, :]), [dst_f32.r0], [dst_bf.r0])

    P.push()
    w3 = [P.sbuf(f"w3s{i}", [128, 8, D], BF16) for i in range(3)]
    for i in range(3):
        P.dma("sp", w3[i].t[:], w5bf[i], writes=[w3[i].r0])
    wA, wB, wO = w3
    obTq = P.sbuf("obTq", [128, 8, 512], BF16, nres=8)
    maTq = P.sbuf("maTq", [128, 8, 512], BF16, nres=8)
    mgT = P.sbuf("mgT", [128, 8, 512], BF16)
    g01 = P.sbuf("g01", [128, 2, 2, 512], F32, nres=2)
    tA = P.sbuf("tA", [128, 2, 512], F32, nres=2)
    tB = P.sbuf("tB", [128, 2, 512], F32, nres=2)
    xtl = P.sbuf("xtl", [128, 2, D], F32, nres=2)
    h1t = P.sbuf("h1t", [128, 2, D], F32, nres=2)
    ps_y = P.psum("ps_y", [128, 4, 512], F32, nres=4)
    ps_h = P.psum("ps_h", [128, 2, 512], F32, nres=2)
    ti = 0
    for qc in range(NQC):
        q0 = qc * 512
        for c8 in range(8):
            P.dma("sp", obTq.t[:, c8, :], obt_s[c8, :, q0:q0 + 512], writes=[obTq.r[c8]])
            P.dma("sp", maTq.t[:, c8, :], ma_s[c8, :, q0:q0 + 512], writes=[maTq.r[c8]])
        for c in range(8):
            b = c % 2
            P.dma("sp", g01.t[:, b, 0, :], zfm[C_M + c, :, q0:q0 + 512], writes=[g01.r[b]])
            P.dma("sp", g01.t[:, b, 1, :], zfm[C_M + 8 + c, :, q0:q0 + 512], writes=[g01.r[b]])
            for k in range(8):
                mm(ps_y.t[:, 2 * b, :], wA.t[:, k, c * 128:(c + 1) * 128], maTq.t[:, k, :], k == 0, k == 7, [wA.r0, maTq.r[k]], [ps_y.r[2 * b]])
            for k in range(8):
                mm(ps_y.t[:, 2 * b + 1, :], wB.t[:, k, c * 128:(c + 1) * 128], obTq.t[:, k, :], k == 0, k == 7, [wB.r0, obTq.r[k]], [ps_y.r[2 * b + 1]])
            P.op("dve", "tensor_tensor", dict(out=tA.t[:, b, :], in0=ps_y.t[:, 2 * b, :], in1=g01.t[:, b, 0, :], op=ALU.mult), [ps_y.r[2 * b], g01.r[b]], [tA.r[b]])
            P.op("dve", "tensor_tensor", dict(out=tB.t[:, b, :], in0=ps_y.t[:, 2 * b + 1, :], in1=g01.t[:, b, 1, :], op=ALU.mult), [ps_y.r[2 * b + 1], g01.r[b]], [tB.r[b]])
            P.op("pool", "tensor_tensor", dict(out=mgT.t[:, c, :], in0=tA.t[:, b, :], in1=tB.t[:, b, :], op=ALU.add), [tA.r[b], tB.r[b]], [mgT.r0])
        for qt in range(4):
            tt = qc * 4 + qt
            b = ti % 2
            ti += 1
            P.dma("sp", xtl.t[:, b, :], x_d[tt * 128:(tt + 1) * 128, :], writes=[xtl.r[b]])
            for half in range(2):
                for c in range(8):
                    mm(ps_h.t[:, half, :], mgT.t[:, c, qt * 128:(qt + 1) * 128], wO.t[:, c, half * 512:(half + 1) * 512], c == 0, c == 7, [mgT.r0, wO.r0], [ps_h.r[half]])
                P.op("dve", "tensor_tensor", dict(out=h1t.t[:, b, half * 512:(half + 1) * 512], in0=ps_h.t[:, half, :], in1=xtl.t[:, b, half * 512:(half + 1) * 512], op=ALU.add),
                     [ps_h.r[half], xtl.r[b]], [h1t.r[b]])
            P.dma("pool", h1_s[tt * 128:(tt + 1) * 128, :], h1t.t[:, b, :], reads=[h1t.r[b]])
    P.emit_stage()
    P.pop()
    if upto == "D1":
        P.close()
        return nc

    P.push()
    wQ = P.sbuf("wQ", [128, 8, D], BF16)
    wG = P.sbuf("wG", [128, 8, D], BF16)
    wPs = P.sbuf("wPs", [128, 2, D], BF16)
    P.dma("sp", wQ.t[:], w5bf[3], writes=[wQ.r0])
    P.dma("sp", wG.t[:], w5bf[4], writes=[wG.r0])
    P.dma("sp", wPs.t[:], wpbf, writes=[wPs.r0])
    kbdf = P.sbuf("kbdf", [128, 256], F32)
    kbdb = P.sbuf("kbdb", [128, 256], BF16)
    P.dma("sp", kbdf.t[:, :], kbd_d, writes=[kbdf.r0])
    P.op("dve", "tensor_copy", dict(out=kbdb.t[:, :], in_=kbdf.t[:, :]), [kbdf.r0], [kbdb.r0])
    gains = []
    for nm, gd in (("gfb", gf_d), ("gpb", gp_d), ("glb", gl_d)):
        t_ = P.sbuf(nm, [128, D], F32)
        P.dma("sp", t_.t[:, :], gd.partition_broadcast(128), writes=[t_.r0])
        gains.append(t_)
    gfb, gpb, glb = gains
    io128 = P.sbuf("io128", [128, 16, 128], I32)
    io256 = P.sbuf("io256", [128, 8, 256], I32)
    io16 = P.sbuf("io16", [128, 8, 16, 16], F32)
    P.dma("sp", io128.t[:], io128_d, writes=[io128.r0])
    P.dma("sp", io256.t[:], io256_d, writes=[io256.r0])
    P.dma("sp", io16.t[:], io16_d, writes=[io16.r0])
    NG = 6
    hh_ = P.sbuf("hh", [128, D], F32)
    ptl = P.sbuf("ptl", [128, 256], F32)
    ptb = P.sbuf("ptb", [128, 256], BF16)
    pT = P.sbuf("pT", [128, 2, 128], BF16)
    xn = P.sbuf("xn", [128, D], F32)
    xnb = P.sbuf("xnb", [128, D], BF16)
    xnT = P.sbuf("xnT", [128, 8, 128], BF16)
    qpT = P.sbuf("qpT", [128, 8, 128], BF16)
    junk2 = P.sbuf("junk2", [128, D], F32)
    sq = P.sbuf("sq", [128, 4], F32)
    s_all = P.sbuf("s_all", [128, 16, 128], F32)
    stmp = P.sbuf("stmp", [128, 256], F32)
    v12 = P.sbuf("v12", [128, 16, 16], F32)
    i12i = P.sbuf("i12i", [128, 16, 16], I32)
    i12f = P.sbuf("i12f", [128, 16, 16], F32)
    cand = P.sbuf("cand", [128, 8, 256], F32)
    best = P.sbuf("best", [128, 8, 16], F32)
    posi2 = P.sbuf("posi2", [128, 8, 16], I32)
    abi = P.sbuf("abi", [128, 2, 8, 16], I32)
    abf = P.sbuf("abf", [128, 2, 8, 16], F32)
    oh = P.sbuf("oh", [128, 8, 16, 16], F32)
    isel = P.sbuf("isel", [128, 2, 8, 16], F32)
    ef = P.sbuf("ef", [128, 128], F32)
    eidx = P.sbuf("eidx", [128, 128], I32)
    bex = P.sbuf("bex", [128, 8, 16], F32)
    bsm = P.sbuf("bsm", [128, 2, 8], F32)
    gte = P.sbuf("gte", [128, 128], F32)
    dots = P.sbuf("dots", [128, 128], F32)
    actv = P.sbuf("actv", [128, 128], F32)
    ug = P.sbuf("ug", [128, NG, D], F32, nres=NG)
    gate = P.sbuf("gate", [128, D], F32)
    tmpg = P.sbuf("tmpg", [128, 512], F32)
    ot = P.sbuf("ot", [128, D], F32)
    ps_t = P.psum("ps_t2", [128, 1024], BF16)
    ps_q = P.psum("ps_q", [128, 2, 512], F32, nres=2)
    ps_c = P.psum("ps_c", [128, 4, 512], F32, nres=4)
    gi = 0

    def transpose8(src_bf, dst, n):
        for c in range(n):
            P.op("pe", "transpose", dict(out=ps_t.t[:, c * 128:(c + 1) * 128], in_=src_bf.t[:, c * 128:(c + 1) * 128], identity=identb.t[:, :]),
                 [src_bf.r0, identb.r0], [ps_t.r0])
        cast(dst.t[:, 0:n, :], ps_t.t[:, 0:n * 128].rearrange("p (c t) -> p c t", c=n), [ps_t.r0], [dst.r0])

    v12v = v12.t[:].rearrange("p (h c) k -> p h c k", c=2)
    i12v = i12f.t[:].rearrange("p (h c) k -> p h c k", c=2)
    for tt in range(NTT):
        P.dma("sp", hh_.t[:, :], h1_s[tt * 128:(tt + 1) * 128, :], writes=[hh_.r0])
        P.dma("sp", ptl.t[:, :], p_d[tt * 128:(tt + 1) * 128, :], writes=[ptl.r0])
        rmsnorm_tile(hh_, gfb, xn, xnb, sq, junk2)
        transpose8(xnb, xnT, 8)
        for hd in range(8):
            bk = hd // 4
            for k in range(8):
                mm(ps_q.t[:, bk, (hd % 4) * 128:(hd % 4 + 1) * 128], wQ.t[:, k, hd * 128:(hd + 1) * 128], xnT.t[:, k, :],
                   (k == 0 and hd % 4 == 0), False, [wQ.r0, xnT.r0], [ps_q.r[bk]])
        for bk in range(2):
            cast(qpT.t[:, bk * 4:(bk + 1) * 4, :], ps_q.t[:, bk, :].rearrange("p (a t) -> p a t", a=4), [ps_q.r[bk]], [qpT.r0])
        for hd in range(8):
            bk = hd // 2
            mm(ps_c.t[:, bk, (hd % 2) * 256:(hd % 2 + 1) * 256], qpT.t[:, hd, :], kbdb.t[:, :], hd % 2 == 0, False, [qpT.r0, kbdb.r0], [ps_c.r[bk]])
        for bk in range(4):
            cast(s_all.t[:, bk * 4:(bk + 1) * 4, :], ps_c.t[:, bk, :].rearrange("p (a n) -> p a n", a=4), [ps_c.r[bk]], [s_all.r0])
        sI = s_all.t[:].bitcast(I32)
        P.op("dve", "tensor_scalar", dict(out=sI, in0=sI, scalar1=-128, scalar2=None, op0=ALU.bitwise_and), [s_all.r0], [s_all.r0])
        P.op("dve", "tensor_tensor", dict(out=sI, in0=sI, in1=io128.t[:], op=ALU.bitwise_or), [s_all.r0, io128.r0], [s_all.r0])
        for j in range(16):
            P.op("dve", "max", dict(out=v12.t[:, j, 0:8], in_=s_all.t[:, j, :]), [s_all.r0], [v12.r0])
            P.op("dve", "match_replace", dict(out=stmp.t[:, 0:128], in_to_replace=v12.t[:, j, 0:8], in_values=s_all.t[:, j, :], imm_value=-3.0e38),
                 [s_all.r0, v12.r0], [stmp.r0])
            P.op("dve", "max", dict(out=v12.t[:, j, 8:16], in_=stmp.t[:, 0:128]), [stmp.r0], [v12.r0])
        P.op("dve", "tensor_scalar", dict(out=i12i.t[:], in0=v12.t[:].bitcast(I32), scalar1=127, scalar2=None, op0=ALU.bitwise_and), [v12.r0], [i12i.r0])
        P.op("dve", "tensor_copy", dict(out=i12f.t[:], in_=i12i.t[:]), [i12i.r0], [i12f.r0])
        candv = cand.t[:].rearrange("p h (a b) -> p h a b", a=16)
        P.op("dve", "tensor_tensor", dict(out=candv, in0=v12v[:, :, 0, :].unsqueeze(3).broadcast_to([128, 8, 16, 16]),
                                          in1=v12v[:, :, 1, :].unsqueeze(2).broadcast_to([128, 8, 16, 16]), op=ALU.add), [v12.r0], [cand.r0])
        cI = cand.t[:].bitcast(I32)
        P.op("dve", "tensor_scalar", dict(out=cI, in0=cI, scalar1=-256, scalar2=None, op0=ALU.bitwise_and), [cand.r0], [cand.r0])
        P.op("dve", "tensor_tensor", dict(out=cI, in0=cI, in1=io256.t[:], op=ALU.bitwise_or), [cand.r0, io256.r0], [cand.r0])
        for hd in range(8):
            P.op("dve", "max", dict(out=best.t[:, hd, 0:8], in_=cand.t[:, hd, :]), [cand.r0], [best.r0])
            P.op("dve", "match_replace", dict(out=stmp.t[:, :], in_to_replace=best.t[:, hd, 0:8], in_values=cand.t[:, hd, :], imm_value=-3.0e38),
                 [cand.r0, best.r0], [stmp.r0])
            P.op("dve", "max", dict(out=best.t[:, hd, 8:16], in_=stmp.t[:, :]), [stmp.r0], [best.r0])
        P.op("dve", "tensor_scalar", dict(out=posi2.t[:], in0=best.t[:].bitcast(I32), scalar1=255, scalar2=None, op0=ALU.bitwise_and), [best.r0], [posi2.r0])
        P.op("dve", "tensor_scalar", dict(out=abi.t[:, 0], in0=posi2.t[:], scalar1=4, scalar2=None, op0=ALU.arith_shift_right), [posi2.r0], [abi.r0])
        P.op("dve", "tensor_scalar", dict(out=abi.t[:, 1], in0=posi2.t[:], scalar1=15, scalar2=None, op0=ALU.bitwise_and), [posi2.r0, abi.r0], [abi.r0])
        P.op("dve", "tensor_copy", dict(out=abf.t[:], in_=abi.t[:]), [abi.r0], [abf.r0])
        for c in range(2):
            P.op("dve", "tensor_tensor", dict(out=oh.t[:], in0=abf.t[:, c].unsqueeze(3).broadcast_to([128, 8, 16, 16]), in1=io16.t[:], op=ALU.is_equal),
                 [abf.r0, io16.r0], [oh.r0])
            P.op("dve", "tensor_tensor", dict(out=oh.t[:], in0=oh.t[:], in1=i12v[:, :, c, :].unsqueeze(2).broadcast_to([128, 8, 16, 16]), op=ALU.mult),
                 [oh.r0, i12f.r0], [oh.r0])
            P.op("dve", "tensor_reduce", dict(out=isel.t[:, c], in_=oh.t[:], axis=AX.X, op=ALU.add), [oh.r0], [isel.r0])
        P.op("dve", "scalar_tensor_tensor", dict(out=ef.t[:, :], in0=isel.t[:, 0].rearrange("p h k -> p (h k)"), scalar=128.0,
                                                 in1=isel.t[:, 1].rearrange("p h k -> p (h k)"), op0=ALU.mult, op1=ALU.add), [isel.r0], [ef.r0])
        P.op("dve", "tensor_copy", dict(out=eidx.t[:, :], in_=ef.t[:, :]), [ef.r0], [eidx.r0])
        P.op("dve", "tensor_tensor", dict(out=bex.t[:], in0=best.t[:], in1=best.t[:, :, 0:1].broadcast_to([128, 8, 16]), op=ALU.subtract), [best.r0], [bex.r0])
        P.op("act", "activation", dict(out=bex.t[:], in_=bex.t[:], func=AF.Exp), [bex.r0], [bex.r0])
        P.op("dve", "tensor_reduce", dict(out=bsm.t[:, 0], in_=bex.t[:], axis=AX.X, op=ALU.add), [bex.r0], [bsm.r0])
        P.op("dve", "reciprocal", dict(out=bsm.t[:, 1], in_=bsm.t[:, 0]), [bsm.r0], [bsm.r0])
        P.op("dve", "tensor_tensor", dict(out=gte.t[:, :].rearrange("p (h k) -> p h k", h=8), in0=bex.t[:],
                                          in1=bsm.t[:, 1].unsqueeze(2).broadcast_to([128, 8, 16]), op=ALU.mult), [bex.r0, bsm.r0], [gte.r0])
        for slot in range(128):
            b = gi % NG
            gi += 1
            P.dma("pool", ug.t[:, b, :], u_d[:, :], reads=[eidx.r0], writes=[ug.r[b]],
                  indirect=dict(out_offset=None, in_offset=bass.IndirectOffsetOnAxis(ap=eidx.t[:, slot:slot + 1], axis=0)))
            P.op("dve", "scalar_tensor_tensor", dict(out=junk2.t[:, :], in0=ug.t[:, b, :], scalar=1.0, in1=xn.t[:, :],
                                                     op0=ALU.mult, op1=ALU.mult, accum_out=dots.t[:, slot:slot + 1]),
                 [ug.r[b], xn.r0], [junk2.r0, dots.r0])
        P.op("act", "activation", dict(out=actv.t[:, :], in_=dots.t[:, :], func=AF.Gelu_apprx_tanh), [dots.r0], [actv.r0])
        P.op("dve", "tensor_tensor", dict(out=actv.t[:, :], in0=actv.t[:, :], in1=gte.t[:, :], op=ALU.mult), [actv.r0, gte.r0], [actv.r0])
        for slot in range(128):
            b = gi % NG
            gi += 1
            P.dma("pool", ug.t[:, b, :], v_d[:, :], reads=[eidx.r0], writes=[ug.r[b]],
                  indirect=dict(out_offset=None, in_offset=bass.IndirectOffsetOnAxis(ap=eidx.t[:, slot:slot + 1], axis=0)))
            P.op("dve", "scalar_tensor_tensor", dict(out=hh_.t[:, :], in0=ug.t[:, b, :], scalar=actv.t[:, slot:slot + 1], in1=hh_.t[:, :],
                                                     op0=ALU.mult, op1=ALU.add), [ug.r[b], actv.r0, hh_.r0], [hh_.r0])
        rmsnorm_tile(hh_, gpb, xn, xnb, sq, junk2)
        transpose8(xnb, xnT, 8)
        P.op("act", "copy", dict(out=ptb.t[:, :], in_=ptl.t[:, :]), [ptl.r0], [ptb.r0])
        transpose8(ptb, pT, 2)
        for half in range(2):
            for k in range(8):
                mm(ps_q.t[:, half, :], xnT.t[:, k, :], wG.t[:, k, half * 512:(half + 1) * 512], k == 0, k == 7, [xnT.r0, wG.r0], [ps_q.r[half]])
            P.op("act", "activation", dict(out=gate.t[:, half * 512:(half + 1) * 512], in_=ps_q.t[:, half, :], func=AF.Sigmoid), [ps_q.r[half]], [gate.r0])
            for k in range(2):
                mm(ps_c.t[:, half, :], pT.t[:, k, :], wPs.t[:, k, half * 512:(half + 1) * 512], k == 0, k == 1, [pT.r0, wPs.r0], [ps_c.r[half]])
            P.op("dve", "tensor_tensor", dict(out=tmpg.t[:, :], in0=ps_c.t[:, half, :], in1=gate.t[:, half * 512:(half + 1) * 512], op=ALU.mult),
                 [ps_c.r[half], gate.r0], [tmpg.r0])
            P.op("dve", "tensor_tensor", dict(out=hh_.t[:, half * 512:(half + 1) * 512], in0=hh_.t[:, half * 512:(half + 1) * 512], in1=tmpg.t[:, :], op=ALU.add),
                 [hh_.r0, tmpg.r0], [hh_.r0])
        rmsnorm_tile(hh_, glb, ot, None, sq, junk2)
        P.dma("sp", out_d[tt * 128:(tt + 1) * 128, :], ot.t[:, :], reads=[ot.r0])
    P.emit_stage()
    P.pop()

    P.close()
    return nc


def kernel(**inputs):
    consts = make_consts()
    shared = prep_shared(inputs)
    shared.update(consts)
    in_maps = []
    for b in range(8):
        m = dict(shared)
        m["x"] = np.ascontiguousarray(inputs["x"][b])
        m["p"] = np.ascontiguousarray(inputs["p"][0, b])
        m["positions"] = np.ascontiguousarray(inputs["positions"][b].reshape(1, S).astype(np.int32))
        in_maps.append(m)
    nc = build_nc()
    res = run_bass_kernel_spmd(nc, in_maps, core_ids=list(range(8)))
    return np.stack([np.asarray(r["out"]) for r in res.results], axis=0).astype(np.float32)
```

```python
from contextlib import ExitStack
import numpy as np
import ml_dtypes
import concourse.bass as bass
import concourse.mybir as mybir
from concourse.bass_utils import run_bass_kernel_spmd

F32 = mybir.dt.float32
BF16 = mybir.dt.bfloat16
I32 = mybir.dt.int32
AF = mybir.ActivationFunctionType
ALU = mybir.AluOpType
AX = mybir.AxisListType

S = 4096
D = 1024
NQC = 8
NTT = 32
NEG = -30000.0
SCALE = 128 ** -0.5
COMPUTE = ("pe", "dve", "act", "pool")
import os as _os
NROT = int(_os.environ.get("K_NROT", "8"))
NCH = 65
WCOLS = NCH * 128

C_RX, C_RG, C_Q, C_QS, C_KV, C_KSW, C_M, C_NG = 0, 8, 16, 24, 32, 44, 48, 64


class Res:
    __slots__ = ("w", "r", "name")

    def __init__(self, name=""):
        self.w = None
        self.r = {}
        self.name = name


class TT:
    def __init__(self, t, nres, name):
        self.t = t
        self.r = [Res(f"{name}.{i}") for i in range(nres)]

    @property
    def r0(self):
        return self.r[0]


class Prog:
    def __init__(self, nc):
        self.nc = nc
        self.scopes = [ExitStack()]
        self.streams = {e: [] for e in ("pe", "dve", "act", "pool", "sp")}
        self.cnt = {e: 0 for e in COMPUTE}
        self.seen = {e: {} for e in self.streams}
        self.sems = {}
        for i in range(int(_os.environ.get("K_SEMPAD", "0"))):
            self.scopes[0].enter_context(nc.semaphore(f"s_pad{i}"))
        for e in COMPUTE:
            self.sems[e] = self.scopes[0].enter_context(nc.semaphore(f"s_{e}"))
        self.dq = {}
        self.dma_total = {}
        for q in ("sp", "act", "pool"):
            self.dq[q] = {"n": 0, "sems": []}
            nrot_q = int(_os.environ.get("K_NROT_" + q.upper(), "1" if q == "sp" else str(NROT)))
            self.dq[q]["nrot"] = nrot_q
            for i in range(nrot_q):
                k = ("dma", q, i)
                self.sems[k] = self.scopes[0].enter_context(nc.semaphore(f"s_dma_{q}_{i}"))
                self.dq[q]["sems"].append(k)
                self.dma_total[k] = 0
        self.n_instr = 0

    def push(self):
        self.scopes.append(ExitStack())

    def pop(self):
        self.scopes.pop().close()

    def sbuf(self, name, shape, dtype, nres=1):
        self.n_instr += 0
        self.uid = getattr(self, "uid", 0) + 1
        t = self.scopes[-1].enter_context(self.nc.sbuf_tensor(f"{name}_{self.uid}", list(shape), dtype))
        return TT(t, nres, name)

    def psum(self, name, shape, dtype, nres=1):
        self.uid = getattr(self, "uid", 0) + 1
        t = self.scopes[-1].enter_context(self.nc.psum_tensor(f"{name}_{self.uid}", list(shape), dtype))
        return TT(t, nres, name)

    def _deps(self, eng, reads, writes):
        deps = {}

        def add(tok):
            if tok is None:
                return
            k, v = tok
            if deps.get(k, 0) < v:
                deps[k] = v

        for r in reads:
            add(r.w)
        for w in writes:
            add(w.w)
            for k, v in w.r.items():
                add((k, v))
        out = []
        for k, v in deps.items():
            if k == eng and eng == "pe":
                continue
            if self.seen[eng].get(k, 0) >= v:
                continue
            self.seen[eng][k] = v
            out.append((k, v))
        return out

    def _record(self, tok, reads, writes):
        k, v = tok
        for r in reads:
            if r.r.get(k, 0) < v:
                r.r[k] = v
        for w in writes:
            w.w = tok
            w.r = {}

    def op(self, eng, meth, kw, reads=(), writes=()):
        waits = self._deps(eng, reads, writes)
        self.cnt[eng] += 1
        tok = (eng, self.cnt[eng])
        self._record(tok, reads, writes)
        self.streams[eng].append((meth, kw, waits, (eng, 1)))
        self.n_instr += 1

    def dma(self, q, out, in_, reads=(), writes=(), indirect=None, **kw):
        d = self.dq[q]
        i = d["n"]
        d["n"] += 1
        sk = d["sems"][i % d["nrot"]]
        prev = self.dma_total[sk]
        waits = self._deps(q, reads, writes)
        if prev > 0 and self.seen[q].get(sk, 0) < prev:
            self.seen[q][sk] = prev
            waits.append((sk, prev))
        self.dma_total[sk] = prev + 16
        tok = (sk, prev + 16)
        self._record(tok, reads, writes)
        kk = dict(out=out, in_=in_)
        if indirect is not None:
            kk.update(indirect)
        kk.update(kw)
        self.streams[q].append(("indirect_dma_start" if indirect is not None else "dma_start", kk, waits, (sk, 16)))
        self.n_instr += 1
        return tok

    def emit_stage(self):
        targets = [(e, self.cnt[e]) for e in COMPUTE if self.cnt[e] > 0]
        targets += [(k, v) for k, v in self.dma_total.items() if v > 0]
        for e in self.streams:
            ws = []
            for k, v in targets:
                if self.seen[e].get(k, 0) < v:
                    self.seen[e][k] = v
                    ws.append((k, v))
            if ws:
                self.streams[e].append((None, None, ws, None))
        sems = self.sems
        streams = self.streams

        def run(eng_obj, items):
            for meth, kw, waits, inc in items:
                for k, v in waits:
                    eng_obj.wait_ge(sems[k], v)
                if meth is None:
                    continue
                ins = getattr(eng_obj, meth)(**kw)
                ins.then_inc(sems[inc[0]], inc[1])

        with self.nc.Block() as block:
            @block.tensor
            def _(e):
                run(e, streams["pe"])

            @block.vector
            def _(e):
                run(e, streams["dve"])

            @block.scalar
            def _(e):
                run(e, streams["act"])

            @block.gpsimd
            def _(e):
                run(e, streams["pool"])

            @block.sync
            def _(e):
                run(e, streams["sp"])
        self.streams = {e: [] for e in streams}

    def close(self):
        while self.scopes:
            self.scopes.pop().close()


def _bf(a):
    return np.ascontiguousarray(a).astype(ml_dtypes.bfloat16)


def make_consts():
    c = {}
    c["c_identb"] = _bf(np.eye(128, dtype=np.float32))
    c["c_identf"] = np.eye(128, dtype=np.float32)
    half = 64
    inv = (10000.0 ** (-np.arange(half, dtype=np.float32) / np.float32(half))).astype(np.float32)
    c["c_invf"] = np.concatenate([inv, inv]).reshape(128, 1).astype(np.float32)
    c["c_sgn"] = np.concatenate([-np.ones(64), np.ones(64)]).reshape(128, 1).astype(np.float32)
    kp = np.arange(128)[:, None]
    q = np.arange(512)[None, :]
    cb = np.zeros((128, 4, 512), np.float32)
    wb = np.zeros((128, 4, 512), np.float32)
    for r in range(4):
        k = r * 128 + kp
        cb[:, r, :] = np.where(k <= q, 0.0, NEG)
        k2 = (r - 4) * 128 + kp
        wb[:, r, :] = np.where(q - k2 < 512, 0.0, NEG)
    c["c_cb"] = _bf(cb)
    c["c_wb"] = _bf(wb)
    n = (np.arange(2)[None, :, None] * 128 + np.arange(128)[:, None, None])
    t = np.arange(S)[None, None, :]
    cmb = np.where((16 * n + 31 <= t) & (n < 255), 0.0, NEG).astype(np.float32)
    c["c_cmb"] = _bf(cmb)
    j = np.arange(64)[:, None]
    kk = np.arange(S)[None, :]
    c["c_E"] = _bf((kk // 64 == j).astype(np.float32))
    nn = np.arange(256)
    jj = np.arange(64)
    ov = ((16 * nn[:, None] < 64 * jj[None, :] + 64) & (16 * nn[:, None] + 31 >= 64 * jj[None, :]) & (nn[:, None] < 255))
    c["c_ovl"] = _bf(ov.astype(np.float32).reshape(2, 128, 64).transpose(1, 0, 2))
    tt = np.arange(S).reshape(NTT, 128)[:, :, None]
    cur = tt // 64
    j3 = np.arange(64)[None, None, :]
    forced = (j3 == 0) | (j3 == cur) | (j3 == cur - 1)
    valid = (64 * j3 <= tt)
    c["c_fa"] = np.ascontiguousarray(np.where(forced, 1000.0, 0.0).astype(np.float32).transpose(1, 0, 2))
    c["c_na"] = np.ascontiguousarray(np.where(valid, 0.0, -1e30).astype(np.float32).transpose(1, 0, 2))
    c["c_vm"] = np.ascontiguousarray(valid.astype(np.float32).transpose(1, 0, 2))
    c["c_iota128"] = np.ascontiguousarray(np.broadcast_to(np.arange(128, dtype=np.int32)[None, None, :], (128, 16, 128)))
    c["c_iota256"] = np.ascontiguousarray(np.broadcast_to(np.arange(256, dtype=np.int32)[None, None, :], (128, 8, 256)))
    c["c_iota16"] = np.ascontiguousarray(np.broadcast_to(np.arange(16, dtype=np.float32)[None, None, None, :], (128, 8, 16, 16)))
    return c


def pc_layout(v):
    return np.ascontiguousarray(np.asarray(v).reshape(8, 128).T)


def prep_shared(inp):
    m = {}
    def pk(w):
        K_, N_ = w.shape
        return np.ascontiguousarray(w.reshape(K_ // 128, 128, N_).transpose(1, 0, 2))
    m["w_in"] = pk(inp["w_in"][0])
    for k in ("w_a", "w_b", "w_out", "peer_wq", "ple_wg", "ple_wp"):
        m[k] = pk(inp[k][0])
    for k in ("peer_u", "peer_v"):
        m[k] = np.ascontiguousarray(inp[k][0])
    m["lru_wr"] = np.ascontiguousarray(inp["lru_wr"][0].transpose(1, 0, 2))
    m["lru_wi"] = np.ascontiguousarray(inp["lru_wi"][0].transpose(1, 0, 2))
    for k in ("cmp_w1_k", "cmp_w1_v"):
        m[k] = np.ascontiguousarray(inp[k][0].reshape(32, 128, 256).transpose(1, 0, 2))
    for k in ("cmp_w2_k", "cmp_w2_v"):
        m[k] = pk(inp[k][0])
    for k in ("norm_mix", "norm_ffn", "norm_ple"):
        m[k] = np.ascontiguousarray(inp[k][0].reshape(1, D))
    m["norm_final"] = np.ascontiguousarray(inp["norm_final"].reshape(1, D))
    m["conv_w"] = np.ascontiguousarray(inp["conv_w"][0].reshape(4, 8, 128).transpose(2, 1, 0))
    for k in ("conv_b", "lru_br", "lru_bi", "lru_lam"):
        m[k] = pc_layout(inp[k][0])
    for k in ("cmp_b1_k", "cmp_b1_v"):
        m[k] = np.ascontiguousarray(inp[k][0].reshape(2, 128).T)
    m["cmp_b2_k"] = np.ascontiguousarray(inp["cmp_b2_k"][0].reshape(128, 1))
    m["cmp_b2_v"] = np.ascontiguousarray(inp["cmp_b2_v"][0].reshape(1, 128))
    m["cmp_pe_k"] = np.ascontiguousarray(inp["cmp_pe_k"][0].T)
    m["cmp_pe_v"] = np.ascontiguousarray(inp["cmp_pe_v"][0].T)
    keys = inp["peer_keys"][0]
    kbd = np.zeros((128, 256), np.float32)
    kbd[0:64, 0:128] = keys[0].T
    kbd[64:128, 128:256] = keys[1].T
    m["peer_kbd"] = kbd
    return m


def win_segments():
    segs = [(0, 0, 2048), (2048, 2048, 1024)]
    d = 3072
    for h in range(8):
        segs.append((d, 2048 + h * 128 + 64, 64))
        segs.append((d + 64, 2048 + h * 128, 64))
        d += 128
    segs.append((4096, 3072, 1536))
    d = 5632
    for jkv in (2, 4):
        for g in range(2):
            s0 = 3072 + jkv * 256 + g * 128
            segs.append((d, s0 + 64, 64))
            segs.append((d + 64, s0, 64))
            d += 128
    segs.append((6144, 4632, 2048))
    segs.append((8192, 4608, 24))
    return segs


def build_nc(upto="D", debug=False):
    nc = bass.Bass("TRN2", target_bir_lowering=False)
    P = Prog(nc)
    ein = {}

    def IN(name, shape, dt=F32):
        ein[name] = nc.dram_tensor(name, list(shape), dt, kind="ExternalInput").ap()
        return ein[name]

    x_d = IN("x", [S, D])
    p_d = IN("p", [S, 256])
    pos_d = IN("positions", [1, S], I32)
    win_d = IN("w_in", [128, 8, 6680])
    w5_d = [IN(k, [128, 8, D]) for k in ("w_a", "w_b", "w_out", "peer_wq", "ple_wg")]
    wp_d = IN("ple_wp", [128, 2, D])
    u_d = IN("peer_u", [16384, D])
    v_d = IN("peer_v", [16384, D])
    wr_d = IN("lru_wr", [128, 8, 128])
    wi_d = IN("lru_wi", [128, 8, 128])
    w1_d = [IN("cmp_w1_k", [128, 32, 256]), IN("cmp_w1_v", [128, 32, 256])]
    w2_d = [IN("cmp_w2_k", [128, 2, 128]), IN("cmp_w2_v", [128, 2, 128])]
    gm_d = IN("norm_mix", [1, D])
    gf_d = IN("norm_ffn", [1, D])
    gp_d = IN("norm_ple", [1, D])
    gl_d = IN("norm_final", [1, D])
    cw_d = IN("conv_w", [128, 8, 4])
    cbias_d = IN("conv_b", [128, 8])
    br_d = IN("lru_br", [128, 8])
    bi_d = IN("lru_bi", [128, 8])
    lam_d = IN("lru_lam", [128, 8])
    b1_d = [IN("cmp_b1_k", [128, 2]), IN("cmp_b1_v", [128, 2])]
    b2k_d = IN("cmp_b2_k", [128, 1])
    b2v_d = IN("cmp_b2_v", [1, 128])
    pe_d = [IN("cmp_pe_k", [128, 32]), IN("cmp_pe_v", [128, 32])]
    kbd_d = IN("peer_kbd", [128, 256])
    identb_d = IN("c_identb", [128, 128], BF16)
    identf_d = IN("c_identf", [128, 128])
    invf_d = IN("c_invf", [128, 1])
    sgn_d = IN("c_sgn", [128, 1])
    cb_d = IN("c_cb", [128, 4, 512], BF16)
    wb_d = IN("c_wb", [128, 4, 512], BF16)
    cmb_d = IN("c_cmb", [128, 2, S], BF16)
    E_d = IN("c_E", [64, S], BF16)
    ovl_d = IN("c_ovl", [128, 2, 64], BF16)
    fa_d = IN("c_fa", [128, NTT, 64])
    na_d = IN("c_na", [128, NTT, 64])
    vm_d = IN("c_vm", [128, NTT, 64])
    io128_d = IN("c_iota128", [128, 16, 128], I32)
    io256_d = IN("c_iota256", [128, 8, 256], I32)
    io16_d = IN("c_iota16", [128, 8, 16, 16])

    out_d = nc.dram_tensor("out", [S, D], F32, kind="ExternalOutput").ap()

    dbg_n = [0]

    def DBG(tag, tt_, shape, dt=F32):
        if not debug:
            return
        dbg_n[0] += 1
        d = nc.dram_tensor(f"dbg_{tag}", list(shape), dt, kind="ExternalOutput").ap()
        P.dma("sp", d, tt_.t[:], reads=tt_.r)

    def SCR(name, shape, dt):
        kind = "ExternalOutput" if debug else "Internal"
        return nc.dram_tensor(name, list(shape), dt, kind=kind).ap()

    winbf = SCR("s_winbf", [128, 8, WCOLS], BF16)
    w5bf = [SCR(f"s_w5bf{i}", [128, 8, D], BF16) for i in range(5)]
    wpbf = SCR("s_wpbf", [128, 2, D], BF16)
    w1bf = [SCR(f"s_w1bf{i}", [128, 32, 256], BF16) for i in range(2)]
    zfm = SCR("s_zfm", [NCH, 128, S], F32)
    ma_s = SCR("s_ma", [8, 128, S], BF16)
    obt_s = SCR("s_obt", [8, 128, S], BF16)
    h1_s = SCR("s_h1", [S, D], F32)
    uv_s = nc.dram_tensor("s_uv", [16384, 2048], BF16, kind="Internal").ap()

    identb = P.sbuf("identb", [128, 128], BF16)
    identf = P.sbuf("identf", [128, 128], F32)
    P.dma("sp", identb.t[:, :], identb_d, writes=[identb.r0])
    P.dma("sp", identf.t[:, :], identf_d, writes=[identf.r0])

    cast_rr = [0]

    def cast(out, in_, reads, writes, engs=tuple(_os.environ.get("K_CAST", "dve,act").split(","))):
        e = engs[cast_rr[0] % len(engs)]
        cast_rr[0] += 1
        if e == "act":
            P.op("act", "copy", dict(out=out, in_=in_), reads, writes)
        else:
            P.op(e, "tensor_copy", dict(out=out, in_=in_), reads, writes)

    P.push()
    stg = P.sbuf("stg", [128, 2, 4096], F32, nres=2)
    stgb = P.sbuf("stgb", [128, 2, 4096], BF16, nres=2)
    pi = [0]

    def piece(loads, n, dst, dshape):
        b = pi[0] % 2
        pi[0] += 1
        for mk, src in loads:
            P.dma("sp", mk(stg.t[:, b, 0:n]), src, writes=[stg.r[b]])
        cast(stgb.t[:, b, 0:n], stg.t[:, b, 0:n], [stg.r[b]], [stgb.r[b]])
        P.dma("pool", dst, dshape(stgb.t[:, b, 0:n]), reads=[stgb.r[b]])

    win_v = win_d
    segs = win_segments()
    for c0 in range(0, WCOLS, 512):
        c1 = min(c0 + 512, WCOLS)
        w = c1 - c0
        loads = []
        for (dcol, scol, ncol) in segs:
            lo, hi = max(dcol, c0), min(dcol + ncol, c1)
            if lo >= hi:
                continue
            so = scol + (lo - dcol)
            loads.append((lambda a, lo=lo, hi=hi, w=w, c0=c0: a.rearrange("p (kc n) -> p kc n", kc=8)[:, :, lo - c0:hi - c0],
                          win_v[:, :, so:so + (hi - lo)]))
        if c1 == WCOLS and 8192 + 24 < WCOLS:
            pass
        piece(loads, 8 * w, winbf[:, :, c0:c1], lambda a, w=w: a.rearrange("p (kc n) -> p kc n", kc=8))
    for i in range(5):
        wv = w5_d[i]
        for hh in range(2):
            piece([(lambda a: a.rearrange("p (kc n) -> p kc n", kc=8), wv[:, :, hh * 512:(hh + 1) * 512])], 4096,
                  w5bf[i][:, :, hh * 512:(hh + 1) * 512], lambda a: a.rearrange("p (kc n) -> p kc n", kc=8))
    piece([(lambda a: a.rearrange("p (kc n) -> p kc n", kc=2), wp_d)], 2048,
          wpbf[:, :, :], lambda a: a.rearrange("p (kc n) -> p kc n", kc=2))
    for i in range(2):
        wv = w1_d[i]
        for hh in range(2):
            piece([(lambda a: a.rearrange("p (l h) -> p l h", l=16), wv[:, hh * 16:(hh + 1) * 16, :])], 4096,
                  w1bf[i][:, hh * 16:(hh + 1) * 16, :], lambda a: a.rearrange("p (l h) -> p l h", l=16))
    ust = P.sbuf("ust", [128, 2, 4096], F32, nres=2)
    uvb = P.sbuf("uvb", [128, 2, 4, 2048], BF16, nres=2)
    u_v = u_d.rearrange("(r p j) d -> r p (j d)", p=128, j=4)
    v_v = v_d.rearrange("(r p j) d -> r p (j d)", p=128, j=4)
    uv_v = uv_s.rearrange("(r p j) c -> r p (j c)", p=128, j=4)
    for rb in range(32):
        ob_ = rb % 2
        for ti_, (src_v, eng_) in enumerate(((u_v, "act"), (v_v, "dve"))):
            P.dma("sp", ust.t[:, ti_, :], src_v[rb], writes=[ust.r[ti_]])
            dst_ = uvb.t[:, ob_, :, ti_ * 1024:(ti_ + 1) * 1024]
            src_ = ust.t[:, ti_, :].rearrange("p (j d) -> p j d", j=4)
            if eng_ == "act":
                P.op("act", "copy", dict(out=dst_, in_=src_), [ust.r[ti_]], [uvb.r[ob_]])
            else:
                P.op("dve", "tensor_copy", dict(out=dst_, in_=src_), [ust.r[ti_]], [uvb.r[ob_]])
        P.dma("pool", uv_v[rb], uvb.t[:, ob_].rearrange("p j c -> p (j c)"), reads=[uvb.r[ob_]])
    P.emit_stage()
    P.pop()
    if upto == "P":
        P.close()
        return nc

    P.push()
    nT = P.sbuf("nT", [128, 8, S], BF16, nres=NTT)
    gbc = P.sbuf("gbc", [128, D], F32)
    xt = P.sbuf("xt", [128, 2, D], F32, nres=2)
    nb = P.sbuf("nb", [128, 2, D], BF16, nres=2)
    junk = P.sbuf("junk", [128, D], F32)
    ss = P.sbuf("ss", [128, NTT], F32, nres=NTT)
    rs = P.sbuf("rs", [128, NTT], F32, nres=NTT)
    tp = P.psum("tp", [128, 2, 1024], BF16, nres=2)
    P.dma("sp", gbc.t[:, :], gm_d.partition_broadcast(128), writes=[gbc.r0])
    for tt in range(NTT):
        b = tt % 2
        P.dma("sp", xt.t[:, b, :], x_d[tt * 128:(tt + 1) * 128, :], writes=[xt.r[b]])
        P.op("act", "activation", dict(out=junk.t[:, :], in_=xt.t[:, b, :], func=AF.Square, accum_out=ss.t[:, tt:tt + 1]),
             reads=[xt.r[b]], writes=[junk.r0, ss.r[tt]])
        P.op("act", "activation", dict(out=rs.t[:, tt:tt + 1], in_=ss.t[:, tt:tt + 1], func=AF.Sqrt, scale=1.0 / D, bias=1e-6),
             reads=[ss.r[tt]], writes=[rs.r[tt]])
        P.op("dve", "reciprocal", dict(out=rs.t[:, tt:tt + 1], in_=rs.t[:, tt:tt + 1]), reads=[rs.r[tt]], writes=[rs.r[tt]])
        P.op("dve", "scalar_tensor_tensor", dict(out=nb.t[:, b, :], in0=xt.t[:, b, :], scalar=rs.t[:, tt:tt + 1], in1=gbc.t[:, :],
                                                 op0=ALU.mult, op1=ALU.mult), reads=[xt.r[b], rs.r[tt], gbc.r0], writes=[nb.r[b]])
        for c in range(8):
            P.op("pe", "transpose", dict(out=tp.t[:, b, c * 128:(c + 1) * 128], in_=nb.t[:, b, c * 128:(c + 1) * 128], identity=identb.t[:, :]),
                 reads=[nb.r[b], identb.r0], writes=[tp.r[b]])
        cast(nT.t[:, :, tt * 128:(tt + 1) * 128], tp.t[:, b, :].rearrange("p (c t) -> p c t", c=8), [tp.r[b]], [nT.r[tt]])

    wbuf = P.sbuf("wbuf", [128, 2, 8, 512], BF16, nres=2)
    zst = P.sbuf("zst", [128, 2, S], F32, nres=2)
    pz = P.psum("pz", [128, 4, 512], F32, nres=4)
    nTall = list(nT.r)
    zi = 0
    pzi = 0
    for wpi, c0 in enumerate(range(0, WCOLS, 512)):
        c1 = min(c0 + 512, WCOLS)
        wb_ = wpi % 2
        P.dma("sp", wbuf.t[:, wb_, :, 0:c1 - c0], winbf[:, :, c0:c1], writes=[wbuf.r[wb_]])
        for cc in range((c1 - c0) // 128):
            ch = c0 // 128 + cc
            M = 24 if ch == C_NG else 128
            zb = zi % 2
            zi += 1
            for qc in range(NQC):
                pb = pzi % 4
                pzi += 1
                for kc in range(8):
                    P.op("pe", "matmul", dict(out=pz.t[0:M, pb, :], lhsT=wbuf.t[:, wb_, kc, cc * 128:cc * 128 + M],
                                              rhs=nT.t[:, kc, qc * 512:(qc + 1) * 512], start=(kc == 0), stop=(kc == 7),
                                              skip_group_check=True),
                         reads=[wbuf.r[wb_]] + nTall[qc * 4:(qc + 1) * 4], writes=[pz.r[pb]])
                dst = zst.t[0:M, zb, qc * 512:(qc + 1) * 512]
                src = pz.t[0:M, pb, :]
                if C_RG <= ch < C_RG + 8:
                    P.op("act", "activation", dict(out=dst, in_=src, func=AF.Gelu_apprx_tanh), [pz.r[pb]], [zst.r[zb]])
                elif ch >= C_M:
                    P.op("act", "activation", dict(out=dst, in_=src, func=AF.Sigmoid), [pz.r[pb]], [zst.r[zb]])
                else:
                    P.op("dve", "tensor_copy", dict(out=dst, in_=src), [pz.r[pb]], [zst.r[zb]])
            P.dma("pool", zfm[ch, 0:M, :], zst.t[0:M, zb, :], reads=[zst.r[zb]])
    P.emit_stage()
    P.pop()
    if upto == "A":
        P.close()
        return nc

    P.push()
    cw = P.sbuf("cw", [128, 8, 4], F32)
    cbias = P.sbuf("cbias", [128, 8], F32)
    brs = P.sbuf("brs", [128, 8], F32)
    bis = P.sbuf("bis", [128, 8], F32)
    lam = P.sbuf("lam", [128, 8], F32)
    m8 = P.sbuf("m8", [128, 8], F32)
    m16 = P.sbuf("m16", [128, 8], F32)
    wst = P.sbuf("wst", [128, 2, 8, 128], F32, nres=2)
    wrb = P.sbuf("wrb", [128, 8, 128], BF16)
    wib = P.sbuf("wib", [128, 8, 128], BF16)
    for (t_, d_) in ((cw, cw_d), (cbias, cbias_d), (brs, br_d), (bis, bi_d), (lam, lam_d)):
        P.dma("sp", t_.t[:], d_, writes=[t_.r0])
    P.dma("sp", wst.t[:, 0], wr_d, writes=[wst.r[0]])
    P.dma("sp", wst.t[:, 1], wi_d, writes=[wst.r[1]])
    P.op("dve", "tensor_copy", dict(out=wrb.t[:, :, :], in_=wst.t[:, 0]), [wst.r[0]], [wrb.r0])
    P.op("dve", "tensor_copy", dict(out=wib.t[:, :, :], in_=wst.t[:, 1]), [wst.r[1]], [wib.r0])
    P.op("act", "activation", dict(out=m8.t[:, :], in_=lam.t[:, :], func=AF.Exp, scale=-1.0), [lam.r0], [m8.r0])
    P.op("act", "activation", dict(out=m8.t[:, :], in_=m8.t[:, :], func=AF.Ln, bias=1.0), [m8.r0], [m8.r0])
    P.op("dve", "tensor_scalar", dict(out=m16.t[:, :], in0=m8.t[:, :], scalar1=-16.0, scalar2=None, op0=ALU.mult), [m8.r0], [m16.r0])
    P.op("dve", "tensor_scalar", dict(out=m8.t[:, :], in0=m8.t[:, :], scalar1=-8.0, scalar2=None, op0=ALU.mult), [m8.r0, m16.r0], [m8.r0])
    zx = P.sbuf("zx", [128, S + 3], F32)
    gz = P.sbuf("gz", [128, S], F32)
    xa = P.sbuf("xa", [128, S], F32)
    xab = P.sbuf("xab", [128, S], BF16)
    rr = P.sbuf("rr", [128, S], F32)
    ig = P.sbuf("ig", [128, S], F32)
    t1 = P.sbuf("t1", [128, S], F32)
    t2 = P.sbuf("t2", [128, S], F32)
    mab = P.sbuf("mab", [128, S], BF16)
    pg = P.psum("pg", [128, 4, 512], F32, nres=4)
    P.op("dve", "memset", dict(ap=zx.t[:, 0:3], constant=0.0), [], [zx.r0])
    pgi = 0
    for ch in range(8):
        P.dma("sp", zx.t[:, 3:S + 3], zfm[C_RX + ch], writes=[zx.r0])
        P.dma("sp", gz.t[:, :], zfm[C_RG + ch], writes=[gz.r0])
        P.op("dve", "tensor_scalar", dict(out=xa.t[:, :], in0=zx.t[:, 0:S], scalar1=cw.t[:, ch, 0:1], scalar2=cbias.t[:, ch:ch + 1],
                                          op0=ALU.mult, op1=ALU.add), [zx.r0, cw.r0, cbias.r0], [xa.r0])
        for k in range(1, 4):
            P.op("dve", "scalar_tensor_tensor", dict(out=xa.t[:, :], in0=zx.t[:, k:k + S], scalar=cw.t[:, ch, k:k + 1], in1=xa.t[:, :],
                                                     op0=ALU.mult, op1=ALU.add), [zx.r0, cw.r0, xa.r0], [xa.r0])
        P.op("act", "copy", dict(out=xab.t[:, :], in_=xa.t[:, :]), [xa.r0], [xab.r0])
        for (wgt, bia, dst) in ((wrb, brs, rr), (wib, bis, ig)):
            for qc in range(NQC):
                pb = pgi % 4
                pgi += 1
                P.op("pe", "matmul", dict(out=pg.t[:, pb, :], lhsT=wgt.t[:, ch, :], rhs=xab.t[:, qc * 512:(qc + 1) * 512], start=True, stop=True,
                                          skip_group_check=True), [wgt.r0, xab.r0], [pg.r[pb]])
                P.op("act", "activation", dict(out=dst.t[:, qc * 512:(qc + 1) * 512], in_=pg.t[:, pb, :], func=AF.Sigmoid, bias=bia.t[:, ch:ch + 1]),
                     [pg.r[pb], bia.r0], [dst.r0])
        P.op("act", "activation", dict(out=t1.t[:, :], in_=rr.t[:, :], func=AF.Exp, scale=m8.t[:, ch:ch + 1]), [rr.r0, m8.r0], [t1.r0])
        P.op("act", "activation", dict(out=t2.t[:, :], in_=rr.t[:, :], func=AF.Exp, scale=m16.t[:, ch:ch + 1]), [rr.r0, m16.r0], [t2.r0])
        P.op("act", "activation", dict(out=t2.t[:, :], in_=t2.t[:, :], func=AF.Sqrt, scale=-1.0, bias=1.0), [t2.r0], [t2.r0])
        P.op("pool", "tensor_tensor", dict(out=ig.t[:, :], in0=ig.t[:, :], in1=xa.t[:, :], op=ALU.mult), [ig.r0, xa.r0], [ig.r0])
        P.op("pool", "tensor_tensor", dict(out=ig.t[:, :], in0=ig.t[:, :], in1=t2.t[:, :], op=ALU.mult), [ig.r0, t2.r0], [ig.r0])
        P.op("dve", "tensor_tensor_scan", dict(out=rr.t[:, :], data0=t1.t[:, :], data1=ig.t[:, :], initial=0.0, op0=ALU.mult, op1=ALU.add),
             [t1.r0, ig.r0, rr.r0], [rr.r0])
        P.op("dve", "tensor_tensor", dict(out=mab.t[:, :], in0=rr.t[:, :], in1=gz.t[:, :], op=ALU.mult), [rr.r0, gz.r0], [mab.r0])
        P.dma("pool", ma_s[ch], mab.t[:, :], reads=[mab.r0])
    P.emit_stage()
    P.pop()
    if upto == "B":
        P.close()
        return nc

    P.push()
    cosT = P.sbuf("cosT", [128, S], F32)
    sinT = P.sbuf("sinT", [128, S], F32)
    P.push()
    posi = P.sbuf("posi", [128, S], I32)
    tq_ = P.sbuf("tq", [128, S], F32)
    tk_ = P.sbuf("tk", [128, S], F32)
    ki_ = P.sbuf("ki", [128, S], I32)
    invf = P.sbuf("invf", [128, 1], F32)
    sgn = P.sbuf("sgn", [128, 1], F32)
    P.dma("sp", posi.t[:, :], pos_d.partition_broadcast(128), writes=[posi.r0])
    P.dma("sp", invf.t[:, :], invf_d, writes=[invf.r0])
    P.dma("sp", sgn.t[:, :], sgn_d, writes=[sgn.r0])
    P.op("dve", "tensor_copy", dict(out=tq_.t[:, :], in_=posi.t[:, :]), [posi.r0], [tq_.r0])
    P.op("dve", "tensor_scalar", dict(out=tq_.t[:, :], in0=tq_.t[:, :], scalar1=invf.t[:, 0:1], scalar2=float(1.0 / (2 * np.pi)),
                                      op0=ALU.mult, op1=ALU.mult), [tq_.r0, invf.r0], [tq_.r0])
    TWO_PI = float(2 * np.pi * (1 - 1e-6))
    for which, dstT in (("sin", sinT), ("cos", cosT)):
        if which == "cos":
            P.op("dve", "tensor_scalar", dict(out=tq_.t[:, :], in0=tq_.t[:, :], scalar1=0.25, scalar2=None, op0=ALU.add), [tq_.r0], [tq_.r0])
        P.op("dve", "tensor_copy", dict(out=ki_.t[:, :], in_=tq_.t[:, :]), [tq_.r0], [ki_.r0])
        P.op("dve", "tensor_copy", dict(out=tk_.t[:, :], in_=ki_.t[:, :]), [ki_.r0], [tk_.r0])
        P.op("dve", "tensor_tensor", dict(out=tk_.t[:, :], in0=tq_.t[:, :], in1=tk_.t[:, :], op=ALU.subtract), [tq_.r0, tk_.r0], [tk_.r0])
        P.op("act", "activation", dict(out=dstT.t[:, :], in_=tk_.t[:, :], func=AF.Sin, scale=TWO_PI), [tk_.r0], [dstT.r0])
    P.op("dve", "tensor_scalar", dict(out=sinT.t[:, :], in0=sinT.t[:, :], scalar1=sgn.t[:, 0:1], scalar2=None, op0=ALU.mult), [sinT.r0, sgn.r0], [sinT.r0])
    P.emit_stage()
    P.pop()

    ksT = P.sbuf("ksT", [128, S], BF16)
    kwT = P.sbuf("kwT", [128, S], BF16)
    vsa = P.sbuf("vsa", [128, NTT, 129], BF16)
    vwa = P.sbuf("vwa", [128, NTT, 129], BF16)
    kcT = P.sbuf("kcT", [128, 256], BF16)
    vca = P.sbuf("vca", [128, 2, 193], BF16)
    E_s = P.sbuf("E_s", [64, S], BF16)
    cb_s = P.sbuf("cb_s", [128, 4, 512], BF16)
    wb_s = P.sbuf("wb_s", [128, 4, 512], BF16)
    b1s = [P.sbuf(f"b1s{i}", [128, 2], F32) for i in range(2)]
    pes = [P.sbuf(f"pes{i}", [128, 32], F32) for i in range(2)]
    b2k = P.sbuf("b2k", [128, 1], F32)
    b2vb = P.sbuf("b2vb", [128, 128], F32)
    ps_s = P.psum("ps_s", [128, 2, 512], F32, nres=2)
    ps_acc = P.psum("ps_acc", [128, 4, 512], F32, nres=4)
    ps_trb = P.psum("ps_trb", [128, 1024], BF16)
    ps_trf = P.psum("ps_trf", [128, 512], F32)
    P.dma("sp", E_s.t[:, :], E_d, writes=[E_s.r0])
    P.dma("sp", cb_s.t[:], cb_d, writes=[cb_s.r0])
    P.dma("sp", wb_s.t[:], wb_d, writes=[wb_s.r0])
    for i in range(2):
        P.dma("sp", b1s[i].t[:, :], b1_d[i], writes=[b1s[i].r0])
        P.dma("sp", pes[i].t[:, :], pe_d[i], writes=[pes[i].r0])
    P.dma("sp", b2k.t[:, :], b2k_d, writes=[b2k.r0])
    P.dma("sp", b2vb.t[:, :], b2v_d.partition_broadcast(128), writes=[b2vb.r0])
    P.op("dve", "memset", dict(ap=vsa.t[:, :, 128:129], constant=1.0), [], [vsa.r0])
    P.op("dve", "memset", dict(ap=vwa.t[:, :, 128:129], constant=1.0), [], [vwa.r0])
    srot = [0]

    def sbank():
        srot[0] += 1
        return srot[0] % 2

    def mm(out, lhsT, rhs, start, stop, reads, writes):
        P.op("pe", "matmul", dict(out=out, lhsT=lhsT, rhs=rhs, start=start, stop=stop, skip_group_check=True), reads, writes)

    for g in range(2):
        P.push()
        ld = P.sbuf("ld", [128, 2, S], F32, nres=2)
        lohi = P.sbuf("lohi", [128, 2, S], BF16, nres=2)
        w1s = P.sbuf("w1s", [128, 32, 256], BF16)
        w2f = P.sbuf("w2f", [128, 2, 128], F32)
        w2s = P.sbuf("w2s", [128, 2, 128], BF16)
        hidT = P.sbuf("hidT", [128, 2, 256], BF16)
        vb = P.sbuf("vb", [128, S], BF16)
        P.op("dve", "memset", dict(ap=kcT.t[:, :], constant=0.0), [], [kcT.r0])
        P.op("dve", "memset", dict(ap=vca.t[:], constant=0.0), [], [vca.r0])
        P.op("dve", "memset", dict(ap=vca.t[:, :, 128:129], constant=1.0), [vca.r0], [vca.r0])
        P.dma("sp", vca.t[:, :, 129:193], ovl_d, writes=[vca.r0])
        for kvi in range(2):
            ch = C_KV + kvi * 2 + g
            P.dma("sp", ld.t[:, 0, :], zfm[ch], writes=[ld.r[0]])
            P.dma("sp", w1s.t[:], w1bf[kvi], writes=[w1s.r0])
            P.dma("sp", w2f.t[:], w2_d[kvi], writes=[w2f.r0])
            P.op("dve", "tensor_copy", dict(out=w2s.t[:], in_=w2f.t[:]), [w2f.r0], [w2s.r0])
            for v_ in range(2):
                P.op("dve", "tensor_tensor", dict(out=lohi.t[:, v_, :].rearrange("p (n l) -> p n l", l=16),
                                                  in0=ld.t[:, 0, :].rearrange("p (n l) -> p n l", l=16),
                                                  in1=pes[kvi].t[:, v_ * 16:(v_ + 1) * 16].unsqueeze(1).broadcast_to([128, 256, 16]),
                                                  op=ALU.add), [ld.r[0], pes[kvi].r0], [lohi.r[v_]])
            for hc in range(2):
                for l in range(32):
                    v_ = 0 if l < 16 else 1
                    mm(ps_s.t[:, hc, 0:255], w1s.t[:, l, hc * 128:(hc + 1) * 128], lohi.t[:, v_, l:l + 16 * 254 + 1:16],
                       l == 0, l == 31, [w1s.r0, lohi.r[v_]], [ps_s.r[hc]])
                P.op("act", "activation", dict(out=hidT.t[:, hc, 0:255], in_=ps_s.t[:, hc, 0:255], func=AF.Gelu_apprx_tanh,
                                               bias=b1s[kvi].t[:, hc:hc + 1]), [ps_s.r[hc], b1s[kvi].r0], [hidT.r0])
            if kvi == 0:
                for hc in range(2):
                    mm(ps_s.t[:, 0, 0:255], w2s.t[:, hc, :], hidT.t[:, hc, 0:255], hc == 0, hc == 1, [w2s.r0, hidT.r0], [ps_s.r[0]])
                P.op("dve", "tensor_scalar", dict(out=kcT.t[:, 0:255], in0=ps_s.t[:, 0, 0:255], scalar1=b2k.t[:, 0:1], scalar2=None, op0=ALU.add),
                     [ps_s.r[0], b2k.r0], [kcT.r0])
            else:
                for nch in range(2):
                    rows = 128 if nch == 0 else 127
                    for hc in range(2):
                        mm(ps_acc.t[0:rows, nch, 0:128], hidT.t[:, hc, nch * 128:nch * 128 + rows], w2s.t[:, hc, :], hc == 0, hc == 1,
                           [w2s.r0, hidT.r0], [ps_acc.r[nch]])
                    P.op("dve", "tensor_tensor", dict(out=vca.t[0:rows, nch, 0:128], in0=ps_acc.t[0:rows, nch, 0:128], in1=b2vb.t[0:rows, :], op=ALU.add),
                         [ps_acc.r[nch], b2vb.r0], [vca.r0])
        for (kch, swch, dstK) in ((C_KV + 4 + g, C_KSW + g, ksT), (C_KV + 8 + g, C_KSW + 2 + g, kwT)):
            P.dma("sp", ld.t[:, 0, :], zfm[kch], writes=[ld.r[0]])
            P.dma("sp", ld.t[:, 1, :], zfm[swch], writes=[ld.r[1]])
            if debug and g == 1 and dstK is ksT:
                DBG("g1_ld_raw", ld, [128, 2, S])
                DBG("g1_cosT", cosT, [128, S])
                DBG("g1_sinT", sinT, [128, S])
            P.op("dve", "tensor_tensor", dict(out=ld.t[:, 0, :], in0=ld.t[:, 0, :], in1=cosT.t[:, :], op=ALU.mult), [ld.r[0], cosT.r0], [ld.r[0]])
            P.op("pool", "tensor_tensor", dict(out=ld.t[:, 1, :], in0=ld.t[:, 1, :], in1=sinT.t[:, :], op=ALU.mult), [ld.r[1], sinT.r0], [ld.r[1]])
            if debug and g == 1 and dstK is ksT:
                DBG("g1_ld_mul", ld, [128, 2, S])
            P.op("dve", "tensor_tensor", dict(out=dstK.t[:, :], in0=ld.t[:, 0, :], in1=ld.t[:, 1, :], op=ALU.add), [ld.r[0], ld.r[1]], [dstK.r0])
        for (vch, dstV) in ((C_KV + 6 + g, vsa), (C_KV + 10 + g, vwa)):
            P.dma("sp", ld.t[:, 0, :], zfm[vch], writes=[ld.r[0]])
            P.op("act", "copy", dict(out=vb.t[:, :], in_=ld.t[:, 0, :]), [ld.r[0]], [vb.r0])
            for t4 in range(8):
                for a in range(4):
                    tt = t4 * 4 + a
                    P.op("pe", "transpose", dict(out=ps_trb.t[:, a * 128:(a + 1) * 128], in_=vb.t[:, tt * 128:(tt + 1) * 128], identity=identb.t[:, :]),
                         [vb.r0, identb.r0], [ps_trb.r0])
                cast(dstV.t[:, t4 * 4:(t4 + 1) * 4, 0:128], ps_trb.t[:, 0:512].rearrange("p (a d) -> p a d", a=4), [ps_trb.r0], [dstV.r0])
        P.emit_stage()
        P.pop()
        if upto == "C0":
            P.close()
            return nc

        P.push()
        zq = P.sbuf("zq", [128, 4, 512], F32, nres=4)
        zqs = P.sbuf("zqs", [128, 4, 512], F32, nres=4)
        qn = P.sbuf("qn", [128, 4, 512], BF16)
        qr = P.sbuf("qr", [128, 4, 512], BF16)
        cmbq = P.sbuf("cmbq", [128, 2, 512], BF16)
        fa = P.sbuf("fa", [128, 4, 64], F32)
        na = P.sbuf("na", [128, 4, 64], F32)
        vm = P.sbuf("vm", [128, 4, 64], F32)
        gTs = P.sbuf("gTs", [32, 512], F32)
        gts = P.sbuf("gts", [128, 4, 24], F32)
        pc = P.sbuf("pc", [128, 2, 2, 512], BF16, nres=2)
        pt = P.sbuf("pt", [128, 3, 512], BF16, nres=3)
        oacc = P.sbuf("oacc", [128, 4, 4, 128], F32)
        imp = P.sbuf("imp", [128, 4, 64], F32)
        impp = P.sbuf("impp", [128, 64], F32)
        imp2 = P.sbuf("imp2", [128, 64], F32)
        m8a = P.sbuf("m8a", [128, 8], F32)
        m8b = P.sbuf("m8b", [128, 8], F32)
        msel = P.sbuf("msel", [128, 64], F32)
        bmb = P.sbuf("bmb", [128, 64], BF16)
        biasT = P.sbuf("biasT", [64, 512], BF16)
        sm = P.sbuf("sm", [128, 8], F32)
        obb = P.sbuf("obb", [128, 4, 4, 128], BF16)
        obT = P.sbuf("obT", [128, 4, 512], BF16)
        ptr = [0]
        aset = [0]
        for qc in range(NQC):
            q0 = qc * 512
            for hq in range(4):
                P.dma("sp", zq.t[:, hq, :], zfm[C_Q + 4 * g + hq, :, q0:q0 + 512], writes=[zq.r[hq]])
                P.dma("sp", zqs.t[:, hq, :], zfm[C_QS + 4 * g + hq, :, q0:q0 + 512], writes=[zqs.r[hq]])
            P.dma("sp", cmbq.t[:], cmb_d[:, :, q0:q0 + 512], writes=[cmbq.r0])
            P.dma("sp", fa.t[:], fa_d[:, qc * 4:qc * 4 + 4, :], writes=[fa.r0])
            P.dma("sp", na.t[:], na_d[:, qc * 4:qc * 4 + 4, :], writes=[na.r0])
            P.dma("sp", vm.t[:], vm_d[:, qc * 4:qc * 4 + 4, :], writes=[vm.r0])
            P.dma("sp", gTs.t[:, :], zfm[C_NG, 0:32, q0:q0 + 512], writes=[gTs.r0])
            P.op("act", "copy", dict(out=qn.t[:], in_=zq.t[:]), zq.r, [qn.r0])
            cs = cosT.t[:, q0:q0 + 512].unsqueeze(1).broadcast_to([128, 4, 512])
            sn = sinT.t[:, q0:q0 + 512].unsqueeze(1).broadcast_to([128, 4, 512])
            P.op("dve", "tensor_tensor", dict(out=zq.t[:], in0=zq.t[:], in1=cs, op=ALU.mult), zq.r + [cosT.r0], zq.r)
            P.op("pool", "tensor_tensor", dict(out=zqs.t[:], in0=zqs.t[:], in1=sn, op=ALU.mult), zqs.r + [sinT.r0], zqs.r)
            P.op("dve", "tensor_tensor", dict(out=qr.t[:], in0=zq.t[:], in1=zqs.t[:], op=ALU.add), zq.r + zqs.r, [qr.r0])
            for qt in range(4):
                P.op("pe", "transpose", dict(out=ps_trf.t[:, qt * 24:(qt + 1) * 24], in_=gTs.t[0:24, qt * 128:(qt + 1) * 128], identity=identf.t[0:24, 0:24]),
                     [gTs.r0, identf.r0], [ps_trf.r0])
            P.op("dve", "tensor_copy", dict(out=gts.t[:].rearrange("p a c -> p (a c)"), in_=ps_trf.t[:, 0:96]), [ps_trf.r0], [gts.r0])

            def post(hh, qt, bank, off, br, first, aux=None):
                h = 4 * g + hh
                accv = ps_acc.t[:, bank, :]
                P.op("dve", "tensor_scalar", dict(out=sm.t[:, 0:1], in0=accv[:, off + 128:off + 129], scalar1=1e-30, scalar2=None, op0=ALU.max),
                     [ps_acc.r[bank]], [sm.r0])
                P.op("dve", "reciprocal", dict(out=sm.t[:, 1:2], in_=sm.t[:, 0:1]), [sm.r0], [sm.r0])
                P.op("dve", "tensor_tensor", dict(out=sm.t[:, 2:3], in0=sm.t[:, 1:2], in1=gts.t[:, qt, h * 3 + br:h * 3 + br + 1], op=ALU.mult),
                     [sm.r0, gts.r0], [sm.r0])
                if first:
                    P.op("dve", "tensor_scalar", dict(out=oacc.t[:, qt, hh, :], in0=accv[:, off:off + 128], scalar1=sm.t[:, 2:3], scalar2=None, op0=ALU.mult),
                         [ps_acc.r[bank], sm.r0], [oacc.r0])
                else:
                    P.op("dve", "scalar_tensor_tensor", dict(out=oacc.t[:, qt, hh, :], in0=accv[:, off:off + 128], scalar=sm.t[:, 2:3], in1=oacc.t[:, qt, hh, :],
                                                             op0=ALU.mult, op1=ALU.add), [ps_acc.r[bank], sm.r0, oacc.r0], [oacc.r0])
                if aux is not None:
                    if hh == 0:
                        P.op("dve", "tensor_scalar", dict(out=imp.t[:, qt, :], in0=accv[:, off + 129:off + 193], scalar1=sm.t[:, 1:2], scalar2=None, op0=ALU.mult),
                             [ps_acc.r[bank], sm.r0], [imp.r0])
                    else:
                        P.op("dve", "scalar_tensor_tensor", dict(out=imp.t[:, qt, :], in0=accv[:, off + 129:off + 193], scalar=sm.t[:, 1:2], in1=imp.t[:, qt, :],
                                                                 op0=ALU.mult, op1=ALU.add), [ps_acc.r[bank], sm.r0, imp.r0], [imp.r0])

            if (g, qc) in ((0, 0), (1, 1)):
                tg = f"g{g}q{qc}"
                DBG(tg + "_gts", gts, [128, 4, 24]); DBG(tg + "_qn", qn, [128, 4, 512], BF16); DBG(tg + "_qr", qr, [128, 4, 512], BF16)
                DBG(tg + "_kcT", kcT, [128, 256], BF16); DBG(tg + "_vca", vca, [128, 2, 193], BF16)
                DBG(tg + "_ksT", ksT, [128, S], BF16); DBG(tg + "_kwT", kwT, [128, S], BF16)
                DBG(tg + "_vsa", vsa, [128, NTT, 129], BF16); DBG(tg + "_vwa", vwa, [128, NTT, 129], BF16)
            n_nch = 2 if (q0 + 511 >= 16 * 128 + 31) else 1
            for hh in range(4):
                pcb = hh % 2
                for nch in range(n_nch):
                    sb = sbank()
                    mm(ps_s.t[:, sb, :], kcT.t[:, nch * 128:(nch + 1) * 128], qn.t[:, hh, :], True, False, [kcT.r0, qn.r0], [ps_s.r[sb]])
                    mm(ps_s.t[:, sb, :], identb.t[:, :], cmbq.t[:, nch, :], False, True, [identb.r0, cmbq.r0], [ps_s.r[sb]])
                    P.op("act", "activation", dict(out=pc.t[:, pcb, nch, :], in_=ps_s.t[:, sb, :], func=AF.Exp, scale=SCALE), [ps_s.r[sb]], [pc.r[pcb]])
                aset[0] ^= 1
                started = {}
                for qt in range(4):
                    bank = aset[0] * 2 + qt // 2
                    off = (qt % 2) * 193
                    for nch in range(n_nch):
                        mm(ps_acc.t[:, bank, off:off + 193], pc.t[:, pcb, nch, qt * 128:(qt + 1) * 128], vca.t[:, nch, :],
                           bank not in started, False, [pc.r[pcb], vca.r0], [ps_acc.r[bank]])
                        started[bank] = 1
                for qt in range(4):
                    post(hh, qt, aset[0] * 2 + qt // 2, (qt % 2) * 193, 0, True, aux=True)
            if (g, qc) in ((0, 0), (1, 1)):
                DBG(tg + "_oacc_c", oacc, [128, 4, 4, 128]); DBG(tg + "_imp", imp, [128, 4, 64])
            for qt in range(4):
                P.op("dve", "tensor_tensor", dict(out=impp.t[:, :], in0=imp.t[:, qt, :], in1=fa.t[:, qt, :], op=ALU.max), [imp.r0, fa.r0], [impp.r0])
                P.op("dve", "tensor_tensor", dict(out=impp.t[:, :], in0=impp.t[:, :], in1=na.t[:, qt, :], op=ALU.add), [impp.r0, na.r0], [impp.r0])
                P.op("dve", "max", dict(out=m8a.t[:, :], in_=impp.t[:, :]), [impp.r0], [m8a.r0])
                P.op("dve", "match_replace", dict(out=imp2.t[:, :], in_to_replace=m8a.t[:, :], in_values=impp.t[:, :], imm_value=-3.0e38),
                     [impp.r0, m8a.r0], [imp2.r0])
                P.op("dve", "max", dict(out=m8b.t[:, :], in_=imp2.t[:, :]), [imp2.r0], [m8b.r0])
                P.op("dve", "scalar_tensor_tensor", dict(out=msel.t[:, :], in0=impp.t[:, :], scalar=m8b.t[:, 7:8], in1=vm.t[:, qt, :],
                                                         op0=ALU.is_ge, op1=ALU.mult), [impp.r0, m8b.r0, vm.r0], [msel.r0])
                P.op("dve", "tensor_scalar", dict(out=bmb.t[:, :], in0=msel.t[:, :], scalar1=-NEG, scalar2=NEG, op0=ALU.mult, op1=ALU.add),
                     [msel.r0], [bmb.r0])
                P.op("pe", "transpose", dict(out=ps_trb.t[0:64, qt * 128:(qt + 1) * 128], in_=bmb.t[:, :], identity=identb.t[:, :]),
                     [bmb.r0, identb.r0], [ps_trb.r0])
            P.op("dve", "tensor_copy", dict(out=biasT.t[:, :], in_=ps_trb.t[0:64, 0:512]), [ps_trb.r0], [biasT.r0])
            if (g, qc) in ((0, 0), (1, 1)):
                DBG(tg + "_biasT", biasT, [64, 512], BF16)
            for br in (1, 2):
                if br == 2 and (g, qc) in ((0, 0), (1, 1)):
                    DBG(tg + "_oacc_s", oacc, [128, 4, 4, 128])
                for hh in range(4):
                    aset[0] ^= 1
                    started = {}
                    if br == 1:
                        kts = list(range(0, qc * 4 + 4))
                    else:
                        kts = list(range(max(0, qc * 4 - 4), qc * 4 + 4))
                    for kt in kts:
                        r = kt - qc * 4
                        sb = sbank()
                        if br == 1:
                            mm(ps_s.t[:, sb, :], ksT.t[:, kt * 128:(kt + 1) * 128], qr.t[:, hh, :], True, False, [ksT.r0, qr.r0], [ps_s.r[sb]])
                            mm(ps_s.t[:, sb, :], E_s.t[0:64, kt * 128:(kt + 1) * 128], biasT.t[0:64, :], False, r < 0, [E_s.r0, biasT.r0], [ps_s.r[sb]])
                            if r >= 0:
                                mm(ps_s.t[:, sb, :], identb.t[:, :], cb_s.t[:, r, :], False, True, [identb.r0, cb_s.r0], [ps_s.r[sb]])
                        else:
                            mm(ps_s.t[:, sb, :], kwT.t[:, kt * 128:(kt + 1) * 128], qr.t[:, hh, :], True, False, [kwT.r0, qr.r0], [ps_s.r[sb]])
                            btab = wb_s.t[:, r + 4, :] if r < 0 else cb_s.t[:, r, :]
                            mm(ps_s.t[:, sb, :], identb.t[:, :], btab, False, True, [identb.r0, cb_s.r0, wb_s.r0], [ps_s.r[sb]])
                        pb = ptr[0] % 3
                        ptr[0] += 1
                        P.op("act", "activation", dict(out=pt.t[:, pb, :], in_=ps_s.t[:, sb, :], func=AF.Exp, scale=SCALE), [ps_s.r[sb]], [pt.r[pb]])
                        vsrc = vsa if br == 1 else vwa
                        for qt in range(4):
                            if r > qt:
                                continue
                            if br == 2 and r < qt - 4:
                                continue
                            bank = aset[0] * 2 + qt // 2
                            off = (qt % 2) * 129
                            mm(ps_acc.t[:, bank, off:off + 129], pt.t[:, pb, qt * 128:(qt + 1) * 128], vsrc.t[:, kt, :],
                               bank not in started, False, [pt.r[pb], vsrc.r0], [ps_acc.r[bank]])
                            started[bank] = 1
                    for qt in range(4):
                        post(hh, qt, aset[0] * 2 + qt // 2, (qt % 2) * 129, br, False)
            if (g, qc) in ((0, 0), (1, 1)):
                DBG(tg + "_oacc_w", oacc, [128, 4, 4, 128])
            P.op("act", "copy", dict(out=obb.t[:], in_=oacc.t[:]), [oacc.r0], [obb.r0])
            for hh in range(4):
                for qt in range(4):
                    P.op("pe", "transpose", dict(out=ps_trb.t[:, qt * 128:(qt + 1) * 128], in_=obb.t[:, qt, hh, :], identity=identb.t[:, :]),
                         [obb.r0, identb.r0], [ps_trb.r0])
                cast(obT.t[:, hh, :], ps_trb.t[:, 0:512], [ps_trb.r0], [obT.r0])
            for hq in range(4):
                P.dma("pool", obt_s[4 * g + hq, :, q0:q0 + 512], obT.t[:, hq, :], reads=[obT.r0])
        P.emit_stage()
        P.pop()
    P.pop()
    if upto == "C":
        P.close()
        return nc

    def rmsnorm_tile(src, gain, dst_f32, dst_bf, sq, junk_):
        P.op("act", "activation", dict(out=junk_.t[:, :], in_=src.t[:, :], func=AF.Square, accum_out=sq.t[:, 0:1]), [src.r0], [junk_.r0, sq.r0])
        P.op("act", "activation", dict(out=sq.t[:, 1:2], in_=sq.t[:, 0:1], func=AF.Sqrt, scale=1.0 / D, bias=1e-6), [sq.r0], [sq.r0])
        P.op("dve", "reciprocal", dict(out=sq.t[:, 2:3], in_=sq.t[:, 1:2]), [sq.r0], [sq.r0])
        P.op("dve", "scalar_tensor_tensor", dict(out=dst_f32.t[:, :], in0=src.t[:, :], scalar=sq.t[:, 2:3], in1=gain.t[:, :], op0=ALU.mult, op1=ALU.mult),
             [src.r0, sq.r0, gain.r0], [dst_f32.r0])
        if dst_bf is not None:
            P.op("act", "copy", dict(out=dst_bf.t[:, :], in_=dst_f32.t[:, :]), [dst_f32.r0], [dst_bf.r0])

    P.push()
    w3 = [P.sbuf(f"w3s{i}", [128, 8, D], BF16) for i in range(3)]
    for i in range(3):
        P.dma("sp", w3[i].t[:], w5bf[i], writes=[w3[i].r0])
    wA, wB, wO = w3
    obTq = P.sbuf("obTq", [128, 8, 512], BF16, nres=8)
    maTq = P.sbuf("maTq", [128, 8, 512], BF16, nres=8)
    mgT = P.sbuf("mgT", [128, 8, 512], BF16)
    g01 = P.sbuf("g01", [128, 2, 2, 512], F32, nres=2)
    tA = P.sbuf("tA", [128, 2, 512], F32, nres=2)
    tB = P.sbuf("tB", [128, 2, 512], F32, nres=2)
    xtl = P.sbuf("xtl", [128, 2, D], F32, nres=2)
    h1t = P.sbuf("h1t", [128, 2, D], F32, nres=2)
    ps_y = P.psum("ps_y", [128, 4, 512], F32, nres=4)
    ps_h = P.psum("ps_h", [128, 2, 512], F32, nres=2)
    ti = 0
    for qc in range(NQC):
        q0 = qc * 512
        for c8 in range(8):
            P.dma("sp", obTq.t[:, c8, :], obt_s[c8, :, q0:q0 + 512], writes=[obTq.r[c8]])
            P.dma("sp", maTq.t[:, c8, :], ma_s[c8, :, q0:q0 + 512], writes=[maTq.r[c8]])
        for c in range(8):
            b = c % 2
            P.dma("sp", g01.t[:, b, 0, :], zfm[C_M + c, :, q0:q0 + 512], writes=[g01.r[b]])
            P.dma("sp", g01.t[:, b, 1, :], zfm[C_M + 8 + c, :, q0:q0 + 512], writes=[g01.r[b]])
            for k in range(8):
                mm(ps_y.t[:, 2 * b, :], wA.t[:, k, c * 128:(c + 1) * 128], maTq.t[:, k, :], k == 0, k == 7, [wA.r0, maTq.r[k]], [ps_y.r[2 * b]])
            for k in range(8):
                mm(ps_y.t[:, 2 * b + 1, :], wB.t[:, k, c * 128:(c + 1) * 128], obTq.t[:, k, :], k == 0, k == 7, [wB.r0, obTq.r[k]], [ps_y.r[2 * b + 1]])
            P.op("dve", "tensor_tensor", dict(out=tA.t[:, b, :], in0=ps_y.t[:, 2 * b, :], in1=g01.t[:, b, 0, :], op=ALU.mult), [ps_y.r[2 * b], g01.r[b]], [tA.r[b]])
            P.op("dve", "tensor_tensor", dict(out=tB.t[:, b, :], in0=ps_y.t[:, 2 * b + 1, :], in1=g01.t[:, b, 1, :], op=ALU.mult), [ps_y.r[2 * b + 1], g01.r[b]], [tB.r[b]])
            P.op("pool", "tensor_tensor", dict(out=mgT.t[:, c, :], in0=tA.t[:, b, :], in1=tB.t[:, b, :], op=ALU.add), [tA.r[b], tB.r[b]], [mgT.r0])
        for qt in range(4):
            tt = qc * 4 + qt
            b = ti % 2
            ti += 1
            P.dma("sp", xtl.t[:, b, :], x_d[tt * 128:(tt + 1) * 128, :], writes=[xtl.r[b]])
            for half in range(2):
                for c in range(8):
                    mm(ps_h.t[:, half, :], mgT.t[:, c, qt * 128:(qt + 1) * 128], wO.t[:, c, half * 512:(half + 1) * 512], c == 0, c == 7, [mgT.r0, wO.r0], [ps_h.r[half]])
                P.op("dve", "tensor_tensor", dict(out=h1t.t[:, b, half * 512:(half + 1) * 512], in0=ps_h.t[:, half, :], in1=xtl.t[:, b, half * 512:(half + 1) * 512], op=ALU.add),
                     [ps_h.r[half], xtl.r[b]], [h1t.r[b]])
            P.dma("pool", h1_s[tt * 128:(tt + 1) * 128, :], h1t.t[:, b, :], reads=[h1t.r[b]])
    P.emit_stage()
    P.pop()
    if upto == "D1":
        P.close()
        return nc

    P.push()
    wQ = P.sbuf("wQ", [128, 8, D], BF16)
    wG = P.sbuf("wG", [128, 8, D], BF16)
    wPs = P.sbuf("wPs", [128, 2, D], BF16)
    P.dma("sp", wQ.t[:], w5bf[3], writes=[wQ.r0])
    P.dma("sp", wG.t[:], w5bf[4], writes=[wG.r0])
    P.dma("sp", wPs.t[:], wpbf, writes=[wPs.r0])
    kbdf = P.sbuf("kbdf", [128, 256], F32)
    kbdb = P.sbuf("kbdb", [128, 256], BF16)
    P.dma("sp", kbdf.t[:, :], kbd_d, writes=[kbdf.r0])
    P.op("dve", "tensor_copy", dict(out=kbdb.t[:, :], in_=kbdf.t[:, :]), [kbdf.r0], [kbdb.r0])
    gains = []
    for nm, gd in (("gfb", gf_d), ("gpb", gp_d), ("glb", gl_d)):
        t_ = P.sbuf(nm, [128, D], F32)
        P.dma("sp", t_.t[:, :], gd.partition_broadcast(128), writes=[t_.r0])
        gains.append(t_)
    gfb, gpb, glb = gains
    io128 = P.sbuf("io128", [128, 16, 128], I32)
    io256 = P.sbuf("io256", [128, 8, 256], I32)
    io16 = P.sbuf("io16", [128, 8, 16, 16], F32)
    P.dma("sp", io128.t[:], io128_d, writes=[io128.r0])
    P.dma("sp", io256.t[:], io256_d, writes=[io256.r0])
    P.dma("sp", io16.t[:], io16_d, writes=[io16.r0])
    NG = 12
    GW = int(_os.environ.get("K_GW", str(D)))
    hh_ = P.sbuf("hh", [128, D], F32)
    ptl = P.sbuf("ptl", [128, 256], F32)
    ptb = P.sbuf("ptb", [128, 256], BF16)
    pT = P.sbuf("pT", [128, 2, 128], BF16)
    xn = P.sbuf("xn", [128, D], F32)
    xnb = P.sbuf("xnb", [128, D], BF16)
    xnT = P.sbuf("xnT", [128, 8, 128], BF16)
    qpT = P.sbuf("qpT", [128, 8, 128], BF16)
    junk2 = P.sbuf("junk2", [128, D], F32)
    sq = P.sbuf("sq", [128, 4], F32)
    s_all = P.sbuf("s_all", [128, 16, 128], F32)
    stmp = P.sbuf("stmp", [128, 256], F32)
    v12 = P.sbuf("v12", [128, 16, 16], F32)
    i12i = P.sbuf("i12i", [128, 16, 16], I32)
    i12f = P.sbuf("i12f", [128, 16, 16], F32)
    cand = P.sbuf("cand", [128, 8, 256], F32)
    best = P.sbuf("best", [128, 8, 16], F32)
    posi2 = P.sbuf("posi2", [128, 8, 16], I32)
    abi = P.sbuf("abi", [128, 2, 8, 16], I32)
    abf = P.sbuf("abf", [128, 2, 8, 16], F32)
    oh = P.sbuf("oh", [128, 8, 16, 16], F32)
    isel = P.sbuf("isel", [128, 2, 8, 16], F32)
    ef = P.sbuf("ef", [128, 128], F32)
    eidx = P.sbuf("eidx", [128, 128], I32)
    bex = P.sbuf("bex", [128, 8, 16], F32)
    bsm = P.sbuf("bsm", [128, 2, 8], F32)
    gte = P.sbuf("gte", [128, 128], F32)
    dots = P.sbuf("dots", [128, 128], F32)
    actv = P.sbuf("actv", [128, 128], F32)
    ug = P.sbuf("ug", [128, NG, 2 * D], BF16, nres=NG)
    tmpv = P.sbuf("tmpv", [128, 4, D], BF16, nres=4)
    a1 = P.sbuf("a1", [128, 128], F32)
    gate = P.sbuf("gate", [128, D], F32)
    tmpg = P.sbuf("tmpg", [128, 512], F32)
    ot = P.sbuf("ot", [128, D], F32)
    ps_t = P.psum("ps_t2", [128, 1024], BF16)
    ps_q = P.psum("ps_q", [128, 2, 512], F32, nres=2)
    ps_c = P.psum("ps_c", [128, 2, 512], F32, nres=2)
    ps_o = P.psum("ps_o", [128, 2, 512], F32, nres=2)
    gi = 0
    tvi = 0

    def transpose8(src_bf, dst, n):
        for c in range(n):
            P.op("pe", "transpose", dict(out=ps_t.t[:, c * 128:(c + 1) * 128], in_=src_bf.t[:, c * 128:(c + 1) * 128], identity=identb.t[:, :]),
                 [src_bf.r0, identb.r0], [ps_t.r0])
        cast(dst.t[:, 0:n, :], ps_t.t[:, 0:n * 128].rearrange("p (c t) -> p c t", c=n), [ps_t.r0], [dst.r0])

    v12v = v12.t[:].rearrange("p (h c) k -> p h c k", c=2)
    i12v = i12f.t[:].rearrange("p (h c) k -> p h c k", c=2)
    for tt in range(NTT):
        P.dma("sp", hh_.t[:, :], h1_s[tt * 128:(tt + 1) * 128, :], writes=[hh_.r0])
        P.dma("sp", ptl.t[:, :], p_d[tt * 128:(tt + 1) * 128, :], writes=[ptl.r0])
        rmsnorm_tile(hh_, gfb, xn, xnb, sq, junk2)
        transpose8(xnb, xnT, 8)
        for hd in range(8):
            bk = hd // 4
            for k in range(8):
                mm(ps_q.t[:, bk, (hd % 4) * 128:(hd % 4 + 1) * 128], wQ.t[:, k, hd * 128:(hd + 1) * 128], xnT.t[:, k, :],
                   (k == 0 and hd % 4 == 0), False, [wQ.r0, xnT.r0], [ps_q.r[bk]])
        for bk in range(2):
            cast(qpT.t[:, bk * 4:(bk + 1) * 4, :], ps_q.t[:, bk, :].rearrange("p (a t) -> p a t", a=4), [ps_q.r[bk]], [qpT.r0])
        for rnd in range(2):
            for h4 in range(4):
                hd = rnd * 4 + h4
                bk = h4 // 2
                mm(ps_c.t[:, bk, (hd % 2) * 256:(hd % 2 + 1) * 256], qpT.t[:, hd, :], kbdb.t[:, :], hd % 2 == 0, False, [qpT.r0, kbdb.r0], [ps_c.r[bk]])
            for bk in range(2):
                cast(s_all.t[:, rnd * 8 + bk * 4:rnd * 8 + (bk + 1) * 4, :], ps_c.t[:, bk, :].rearrange("p (a n) -> p a n", a=4), [ps_c.r[bk]], [s_all.r0])
        sI = s_all.t[:].bitcast(I32)
        P.op("dve", "tensor_scalar", dict(out=sI, in0=sI, scalar1=-128, scalar2=None, op0=ALU.bitwise_and), [s_all.r0], [s_all.r0])
        P.op("dve", "tensor_tensor", dict(out=sI, in0=sI, in1=io128.t[:], op=ALU.bitwise_or), [s_all.r0, io128.r0], [s_all.r0])
        for j in range(16):
            P.op("dve", "max", dict(out=v12.t[:, j, 0:8], in_=s_all.t[:, j, :]), [s_all.r0], [v12.r0])
            P.op("dve", "match_replace", dict(out=stmp.t[:, 0:128], in_to_replace=v12.t[:, j, 0:8], in_values=s_all.t[:, j, :], imm_value=-3.0e38),
                 [s_all.r0, v12.r0], [stmp.r0])
            P.op("dve", "max", dict(out=v12.t[:, j, 8:16], in_=stmp.t[:, 0:128]), [stmp.r0], [v12.r0])
        P.op("dve", "tensor_scalar", dict(out=i12i.t[:], in0=v12.t[:].bitcast(I32), scalar1=127, scalar2=None, op0=ALU.bitwise_and), [v12.r0], [i12i.r0])
        P.op("dve", "tensor_copy", dict(out=i12f.t[:], in_=i12i.t[:]), [i12i.r0], [i12f.r0])
        candv = cand.t[:].rearrange("p h (a b) -> p h a b", a=16)
        P.op("dve", "tensor_tensor", dict(out=candv, in0=v12v[:, :, 0, :].unsqueeze(3).broadcast_to([128, 8, 16, 16]),
                                          in1=v12v[:, :, 1, :].unsqueeze(2).broadcast_to([128, 8, 16, 16]), op=ALU.add), [v12.r0], [cand.r0])
        cI = cand.t[:].bitcast(I32)
        P.op("dve", "tensor_scalar", dict(out=cI, in0=cI, scalar1=-256, scalar2=None, op0=ALU.bitwise_and), [cand.r0], [cand.r0])
        P.op("dve", "tensor_tensor", dict(out=cI, in0=cI, in1=io256.t[:], op=ALU.bitwise_or), [cand.r0, io256.r0], [cand.r0])
        for hd in range(8):
            P.op("dve", "max", dict(out=best.t[:, hd, 0:8], in_=cand.t[:, hd, :]), [cand.r0], [best.r0])
            P.op("dve", "match_replace", dict(out=stmp.t[:, :], in_to_replace=best.t[:, hd, 0:8], in_values=cand.t[:, hd, :], imm_value=-3.0e38),
                 [cand.r0, best.r0], [stmp.r0])
            P.op("dve", "max", dict(out=best.t[:, hd, 8:16], in_=stmp.t[:, :]), [stmp.r0], [best.r0])
        P.op("dve", "tensor_scalar", dict(out=posi2.t[:], in0=best.t[:].bitcast(I32), scalar1=255, scalar2=None, op0=ALU.bitwise_and), [best.r0], [posi2.r0])
        P.op("dve", "tensor_scalar", dict(out=abi.t[:, 0], in0=posi2.t[:], scalar1=4, scalar2=None, op0=ALU.arith_shift_right), [posi2.r0], [abi.r0])
        P.op("dve", "tensor_scalar", dict(out=abi.t[:, 1], in0=posi2.t[:], scalar1=15, scalar2=None, op0=ALU.bitwise_and), [posi2.r0, abi.r0], [abi.r0])
        P.op("dve", "tensor_copy", dict(out=abf.t[:], in_=abi.t[:]), [abi.r0], [abf.r0])
        for c in range(2):
            P.op("dve", "tensor_tensor", dict(out=oh.t[:], in0=abf.t[:, c].unsqueeze(3).broadcast_to([128, 8, 16, 16]), in1=io16.t[:], op=ALU.is_equal),
                 [abf.r0, io16.r0], [oh.r0])
            P.op("dve", "tensor_tensor", dict(out=oh.t[:], in0=oh.t[:], in1=i12v[:, :, c, :].unsqueeze(2).broadcast_to([128, 8, 16, 16]), op=ALU.mult),
                 [oh.r0, i12f.r0], [oh.r0])
            P.op("dve", "tensor_reduce", dict(out=isel.t[:, c], in_=oh.t[:], axis=AX.X, op=ALU.add), [oh.r0], [isel.r0])
        P.op("dve", "scalar_tensor_tensor", dict(out=ef.t[:, :], in0=isel.t[:, 0].rearrange("p h k -> p (h k)"), scalar=128.0,
                                                 in1=isel.t[:, 1].rearrange("p h k -> p (h k)"), op0=ALU.mult, op1=ALU.add), [isel.r0], [ef.r0])
        P.op("dve", "tensor_copy", dict(out=eidx.t[:, :], in_=ef.t[:, :]), [ef.r0], [eidx.r0])
        P.op("dve", "tensor_tensor", dict(out=bex.t[:], in0=best.t[:], in1=best.t[:, :, 0:1].broadcast_to([128, 8, 16]), op=ALU.subtract), [best.r0], [bex.r0])
        P.op("act", "activation", dict(out=bex.t[:], in_=bex.t[:], func=AF.Exp), [bex.r0], [bex.r0])
        P.op("dve", "tensor_reduce", dict(out=bsm.t[:, 0], in_=bex.t[:], axis=AX.X, op=ALU.add), [bex.r0], [bsm.r0])
        P.op("dve", "reciprocal", dict(out=bsm.t[:, 1], in_=bsm.t[:, 0]), [bsm.r0], [bsm.r0])
        P.op("dve", "tensor_tensor", dict(out=gte.t[:, :].rearrange("p (h k) -> p h k", h=8), in0=bex.t[:],
                                          in1=bsm.t[:, 1].unsqueeze(2).broadcast_to([128, 8, 16]), op=ALU.mult), [bex.r0, bsm.r0], [gte.r0])
        for grp in range(32):
            bufs = []
            for j in range(4):
                slot = grp * 4 + j
                b = gi % NG
                gi += 1
                bufs.append(b)
                P.dma("pool", ug.t[:, b, :], uv_s[:, :], reads=[eidx.r0], writes=[ug.r[b]],
                      indirect=dict(out_offset=None, in_offset=bass.IndirectOffsetOnAxis(ap=eidx.t[:, slot:slot + 1], axis=0)))
                P.op("dve", "scalar_tensor_tensor", dict(out=junk2.t[:, :], in0=ug.t[:, b, 0:D], scalar=1.0, in1=xn.t[:, :],
                                                         op0=ALU.mult, op1=ALU.mult, accum_out=dots.t[:, slot:slot + 1]),
                     [ug.r[b], xn.r0], [junk2.r0, dots.r0])
            gs = slice(grp * 4, grp * 4 + 4)
            P.op("act", "activation", dict(out=a1.t[:, gs], in_=dots.t[:, gs], func=AF.Gelu_apprx_tanh), [dots.r0], [a1.r0])
            P.op("dve", "tensor_tensor", dict(out=actv.t[:, gs], in0=a1.t[:, gs], in1=gte.t[:, gs], op=ALU.mult), [a1.r0, gte.r0], [actv.r0])
            for j in range(4):
                slot = grp * 4 + j
                b = bufs[j]
                tb = tvi % 4
                tvi += 1
                P.op("act", "activation", dict(out=tmpv.t[:, tb, :], in_=ug.t[:, b, D:2 * D], func=AF.Copy, scale=actv.t[:, slot:slot + 1]),
                     [ug.r[b], actv.r0], [tmpv.r[tb]])
                for half in range(2):
                    mm(ps_o.t[:, half, :], identb.t[:, :], tmpv.t[:, tb, half * 512:(half + 1) * 512], slot == 0, slot == 127,
                       [identb.r0, tmpv.r[tb]], [ps_o.r[half]])
        for half in range(2):
            P.op("dve", "tensor_tensor", dict(out=hh_.t[:, half * 512:(half + 1) * 512], in0=ps_o.t[:, half, :], in1=hh_.t[:, half * 512:(half + 1) * 512], op=ALU.add),
                 [ps_o.r[half], hh_.r0], [hh_.r0])
        rmsnorm_tile(hh_, gpb, xn, xnb, sq, junk2)
        transpose8(xnb, xnT, 8)
        P.op("act", "copy", dict(out=ptb.t[:, :], in_=ptl.t[:, :]), [ptl.r0], [ptb.r0])
        transpose8(ptb, pT, 2)
        for half in range(2):
            for k in range(8):
                mm(ps_q.t[:, half, :], xnT.t[:, k, :], wG.t[:, k, half * 512:(half + 1) * 512], k == 0, k == 7, [xnT.r0, wG.r0], [ps_q.r[half]])
            P.op("act", "activation", dict(out=gate.t[:, half * 512:(half + 1) * 512], in_=ps_q.t[:, half, :], func=AF.Sigmoid), [ps_q.r[half]], [gate.r0])
            for k in range(2):
                mm(ps_c.t[:, half, :], pT.t[:, k, :], wPs.t[:, k, half * 512:(half + 1) * 512], k == 0, k == 1, [pT.r0, wPs.r0], [ps_c.r[half]])
            P.op("dve", "tensor_tensor", dict(out=tmpg.t[:, :], in0=ps_c.t[:, half, :], in1=gate.t[:, half * 512:(half + 1) * 512], op=ALU.mult),
                 [ps_c.r[half], gate.r0], [tmpg.r0])
            P.op("dve", "tensor_tensor", dict(out=hh_.t[:, half * 512:(half + 1) * 512], in0=hh_.t[:, half * 512:(half + 1) * 512], in1=tmpg.t[:, :], op=ALU.add),
                 [hh_.r0, tmpg.r0], [hh_.r0])
        rmsnorm_tile(hh_, glb, ot, None, sq, junk2)
        P.dma("sp", out_d[tt * 128:(tt + 1) * 128, :], ot.t[:, :], reads=[ot.r0])
    P.emit_stage()
    P.pop()

    P.close()
    return nc


def kernel(**inputs):
    consts = make_consts()
    shared = prep_shared(inputs)
    shared.update(consts)
    in_maps = []
    for b in range(8):
        m = dict(shared)
        m["x"] = np.ascontiguousarray(inputs["x"][b])
        m["p"] = np.ascontiguousarray(inputs["p"][0, b])
        m["positions"] = np.ascontiguousarray(inputs["positions"][b].reshape(1, S).astype(np.int32))
        in_maps.append(m)
    nc = build_nc()
    res = run_bass_kernel_spmd(nc, in_maps, core_ids=list(range(8)))
    return np.stack([np.asarray(r["out"]) for r in res.results], axis=0).astype(np.float32)
```

```python
from contextlib import ExitStack
import numpy as np
import ml_dtypes
import concourse.bass as bass
import concourse.mybir as mybir
from concourse.bass_utils import run_bass_kernel_spmd

F32 = mybir.dt.float32
BF16 = mybir.dt.bfloat16
I32 = mybir.dt.int32
AF = mybir.ActivationFunctionType
ALU = mybir.AluOpType
AX = mybir.AxisListType

S = 4096
D = 1024
NQC = 8
NTT = 32
NEG = -30000.0
SCALE = 128 ** -0.5
COMPUTE = ("pe", "dve", "act", "pool")
import os as _os
NROT = int(_os.environ.get("K_NROT", "8"))
NCH = 65
WCOLS = NCH * 128

C_RX, C_RG, C_Q, C_QS, C_KV, C_KSW, C_M, C_NG = 0, 8, 16, 24, 32, 44, 48, 64


class Res:
    __slots__ = ("w", "r", "name")

    def __init__(self, name=""):
        self.w = None
        self.r = {}
        self.name = name


class TT:
    def __init__(self, t, nres, name):
        self.t = t
        self.r = [Res(f"{name}.{i}") for i in range(nres)]

    @property
    def r0(self):
        return self.r[0]


class Prog:
    def __init__(self, nc):
        self.nc = nc
        self.scopes = [ExitStack()]
        self.streams = {e: [] for e in ("pe", "dve", "act", "pool", "sp")}
        self.cnt = {e: 0 for e in COMPUTE}
        self.seen = {e: {} for e in self.streams}
        self.sems = {}
        for i in range(int(_os.environ.get("K_SEMPAD", "0"))):
            self.scopes[0].enter_context(nc.semaphore(f"s_pad{i}"))
        for e in COMPUTE:
            self.sems[e] = self.scopes[0].enter_context(nc.semaphore(f"s_{e}"))
        self.dq = {}
        self.dma_total = {}
        for q in ("sp", "act", "pool"):
            self.dq[q] = {"n": 0, "sems": []}
            nrot_q = int(_os.environ.get("K_NROT_" + q.upper(), "1" if q == "sp" else str(NROT)))
            self.dq[q]["nrot"] = nrot_q
            for i in range(nrot_q):
                k = ("dma", q, i)
                self.sems[k] = self.scopes[0].enter_context(nc.semaphore(f"s_dma_{q}_{i}"))
                self.dq[q]["sems"].append(k)
                self.dma_total[k] = 0
        self.n_instr = 0

    def push(self):
        self.scopes.append(ExitStack())

    def pop(self):
        self.scopes.pop().close()

    def sbuf(self, name, shape, dtype, nres=1):
        self.n_instr += 0
        self.uid = getattr(self, "uid", 0) + 1
        t = self.scopes[-1].enter_context(self.nc.sbuf_tensor(f"{name}_{self.uid}", list(shape), dtype))
        return TT(t, nres, name)

    def psum(self, name, shape, dtype, nres=1):
        self.uid = getattr(self, "uid", 0) + 1
        t = self.scopes[-1].enter_context(self.nc.psum_tensor(f"{name}_{self.uid}", list(shape), dtype))
        return TT(t, nres, name)

    def _deps(self, eng, reads, writes):
        deps = {}

        def add(tok):
            if tok is None:
                return
            k, v = tok
            if deps.get(k, 0) < v:
                deps[k] = v

        for r in reads:
            add(r.w)
        for w in writes:
            add(w.w)
            for k, v in w.r.items():
                add((k, v))
        out = []
        for k, v in deps.items():
            if k == eng and eng == "pe":
                continue
            if self.seen[eng].get(k, 0) >= v:
                continue
            self.seen[eng][k] = v
            out.append((k, v))
        return out

    def _record(self, tok, reads, writes):
        k, v = tok
        for r in reads:
            if r.r.get(k, 0) < v:
                r.r[k] = v
        for w in writes:
            w.w = tok
            w.r = {}

    def op(self, eng, meth, kw, reads=(), writes=()):
        waits = self._deps(eng, reads, writes)
        self.cnt[eng] += 1
        tok = (eng, self.cnt[eng])
        self._record(tok, reads, writes)
        self.streams[eng].append((meth, kw, waits, (eng, 1)))
        self.n_instr += 1

    def dma(self, q, out, in_, reads=(), writes=(), indirect=None, **kw):
        d = self.dq[q]
        i = d["n"]
        d["n"] += 1
        sk = d["sems"][i % d["nrot"]]
        prev = self.dma_total[sk]
        waits = self._deps(q, reads, writes)
        if prev > 0 and self.seen[q].get(sk, 0) < prev:
            self.seen[q][sk] = prev
            waits.append((sk, prev))
        self.dma_total[sk] = prev + 16
        tok = (sk, prev + 16)
        self._record(tok, reads, writes)
        kk = dict(out=out, in_=in_)
        if indirect is not None:
            kk.update(indirect)
        kk.update(kw)
        self.streams[q].append(("indirect_dma_start" if indirect is not None else "dma_start", kk, waits, (sk, 16)))
        self.n_instr += 1
        return tok

    def emit_stage(self):
        targets = [(e, self.cnt[e]) for e in COMPUTE if self.cnt[e] > 0]
        targets += [(k, v) for k, v in self.dma_total.items() if v > 0]
        for e in self.streams:
            ws = []
            for k, v in targets:
                if self.seen[e].get(k, 0) < v:
                    self.seen[e][k] = v
                    ws.append((k, v))
            if ws:
                self.streams[e].append((None, None, ws, None))
        sems = self.sems
        streams = self.streams

        def run(eng_obj, items):
            for meth, kw, waits, inc in items:
                for k, v in waits:
                    eng_obj.wait_ge(sems[k], v)
                if meth is None:
                    continue
                ins = getattr(eng_obj, meth)(**kw)
                ins.then_inc(sems[inc[0]], inc[1])

        with self.nc.Block() as block:
            @block.tensor
            def _(e):
                run(e, streams["pe"])

            @block.vector
            def _(e):
                run(e, streams["dve"])

            @block.scalar
            def _(e):
                run(e, streams["act"])

            @block.gpsimd
            def _(e):
                run(e, streams["pool"])

            @block.sync
            def _(e):
                run(e, streams["sp"])
        self.streams = {e: [] for e in streams}

    def close(self):
        while self.scopes:
            self.scopes.pop().close()


def _bf(a):
    return np.ascontiguousarray(a).astype(ml_dtypes.bfloat16)


def make_consts():
    c = {}
    c["c_identb"] = _bf(np.eye(128, dtype=np.float32))
    c["c_identf"] = np.eye(128, dtype=np.float32)
    half = 64
    inv = (10000.0 ** (-np.arange(half, dtype=np.float32) / np.float32(half))).astype(np.float32)
    c["c_invf"] = np.concatenate([inv, inv]).reshape(128, 1).astype(np.float32)
    c["c_sgn"] = np.concatenate([-np.ones(64), np.ones(64)]).reshape(128, 1).astype(np.float32)
    kp = np.arange(128)[:, None]
    q = np.arange(512)[None, :]
    cb = np.zeros((128, 4, 512), np.float32)
    wb = np.zeros((128, 4, 512), np.float32)
    for r in range(4):
        k = r * 128 + kp
        cb[:, r, :] = np.where(k <= q, 0.0, NEG)
        k2 = (r - 4) * 128 + kp
        wb[:, r, :] = np.where(q - k2 < 512, 0.0, NEG)
    c["c_cb"] = _bf(cb)
    c["c_wb"] = _bf(wb)
    n = (np.arange(2)[None, :, None] * 128 + np.arange(128)[:, None, None])
    t = np.arange(S)[None, None, :]
    cmb = np.where((16 * n + 31 <= t) & (n < 255), 0.0, NEG).astype(np.float32)
    c["c_cmb"] = _bf(cmb)
    j = np.arange(64)[:, None]
    kk = np.arange(S)[None, :]
    c["c_E"] = _bf((kk // 64 == j).astype(np.float32))
    nn = np.arange(256)
    jj = np.arange(64)
    ov = ((16 * nn[:, None] < 64 * jj[None, :] + 64) & (16 * nn[:, None] + 31 >= 64 * jj[None, :]) & (nn[:, None] < 255))
    c["c_ovl"] = _bf(ov.astype(np.float32).reshape(2, 128, 64).transpose(1, 0, 2))
    tt = np.arange(S).reshape(NTT, 128)[:, :, None]
    cur = tt // 64
    j3 = np.arange(64)[None, None, :]
    forced = (j3 == 0) | (j3 == cur) | (j3 == cur - 1)
    valid = (64 * j3 <= tt)
    c["c_fa"] = np.ascontiguousarray(np.where(forced, 1000.0, 0.0).astype(np.float32).transpose(1, 0, 2))
    c["c_na"] = np.ascontiguousarray(np.where(valid, 0.0, -1e30).astype(np.float32).transpose(1, 0, 2))
    c["c_vm"] = np.ascontiguousarray(valid.astype(np.float32).transpose(1, 0, 2))
    c["c_iota128"] = np.ascontiguousarray(np.broadcast_to(np.arange(128, dtype=np.int32)[None, None, :], (128, 16, 128)))
    c["c_iota256"] = np.ascontiguousarray(np.broadcast_to(np.arange(256, dtype=np.int32)[None, None, :], (128, 8, 256)))
    c["c_iota16"] = np.ascontiguousarray(np.broadcast_to(np.arange(16, dtype=np.float32)[None, None, None, :], (128, 8, 16, 16)))
    return c


def pc_layout(v):
    return np.ascontiguousarray(np.asarray(v).reshape(8, 128).T)


def prep_shared(inp):
    m = {}
    def pk(w):
        K_, N_ = w.shape
        return np.ascontiguousarray(w.reshape(K_ // 128, 128, N_).transpose(1, 0, 2))
    m["w_in"] = pk(inp["w_in"][0])
    for k in ("w_a", "w_b", "w_out", "peer_wq", "ple_wg", "ple_wp"):
        m[k] = pk(inp[k][0])
    for k in ("peer_u", "peer_v"):
        m[k] = np.ascontiguousarray(inp[k][0])
    m["lru_wr"] = np.ascontiguousarray(inp["lru_wr"][0].transpose(1, 0, 2))
    m["lru_wi"] = np.ascontiguousarray(inp["lru_wi"][0].transpose(1, 0, 2))
    for k in ("cmp_w1_k", "cmp_w1_v"):
        m[k] = np.ascontiguousarray(inp[k][0].reshape(32, 128, 256).transpose(1, 0, 2))
    for k in ("cmp_w2_k", "cmp_w2_v"):
        m[k] = pk(inp[k][0])
    for k in ("norm_mix", "norm_ffn", "norm_ple"):
        m[k] = np.ascontiguousarray(inp[k][0].reshape(1, D))
    m["norm_final"] = np.ascontiguousarray(inp["norm_final"].reshape(1, D))
    m["conv_w"] = np.ascontiguousarray(inp["conv_w"][0].reshape(4, 8, 128).transpose(2, 1, 0))
    for k in ("conv_b", "lru_br", "lru_bi", "lru_lam"):
        m[k] = pc_layout(inp[k][0])
    for k in ("cmp_b1_k", "cmp_b1_v"):
        m[k] = np.ascontiguousarray(inp[k][0].reshape(2, 128).T)
    m["cmp_b2_k"] = np.ascontiguousarray(inp["cmp_b2_k"][0].reshape(128, 1))
    m["cmp_b2_v"] = np.ascontiguousarray(inp["cmp_b2_v"][0].reshape(1, 128))
    m["cmp_pe_k"] = np.ascontiguousarray(inp["cmp_pe_k"][0].T)
    m["cmp_pe_v"] = np.ascontiguousarray(inp["cmp_pe_v"][0].T)
    keys = inp["peer_keys"][0]
    kbd = np.zeros((128, 256), np.float32)
    kbd[0:64, 0:128] = keys[0].T
    kbd[64:128, 128:256] = keys[1].T
    m["peer_kbd"] = kbd
    return m


def win_segments():
    segs = [(0, 0, 2048), (2048, 2048, 1024)]
    d = 3072
    for h in range(8):
        segs.append((d, 2048 + h * 128 + 64, 64))
        segs.append((d + 64, 2048 + h * 128, 64))
        d += 128
    segs.append((4096, 3072, 1536))
    d = 5632
    for jkv in (2, 4):
        for g in range(2):
            s0 = 3072 + jkv * 256 + g * 128
            segs.append((d, s0 + 64, 64))
            segs.append((d + 64, s0, 64))
            d += 128
    segs.append((6144, 4632, 2048))
    segs.append((8192, 4608, 24))
    return segs


def build_nc(upto="D", debug=False):
    nc = bass.Bass("TRN2", target_bir_lowering=False)
    P = Prog(nc)
    ein = {}

    def IN(name, shape, dt=F32):
        ein[name] = nc.dram_tensor(name, list(shape), dt, kind="ExternalInput").ap()
        return ein[name]

    x_d = IN("x", [S, D])
    p_d = IN("p", [S, 256])
    pos_d = IN("positions", [1, S], I32)
    win_d = IN("w_in", [128, 8, 6680])
    w5_d = [IN(k, [128, 8, D]) for k in ("w_a", "w_b", "w_out", "peer_wq", "ple_wg")]
    wp_d = IN("ple_wp", [128, 2, D])
    u_d = IN("peer_u", [16384, D])
    v_d = IN("peer_v", [16384, D])
    wr_d = IN("lru_wr", [128, 8, 128])
    wi_d = IN("lru_wi", [128, 8, 128])
    w1_d = [IN("cmp_w1_k", [128, 32, 256]), IN("cmp_w1_v", [128, 32, 256])]
    w2_d = [IN("cmp_w2_k", [128, 2, 128]), IN("cmp_w2_v", [128, 2, 128])]
    gm_d = IN("norm_mix", [1, D])
    gf_d = IN("norm_ffn", [1, D])
    gp_d = IN("norm_ple", [1, D])
    gl_d = IN("norm_final", [1, D])
    cw_d = IN("conv_w", [128, 8, 4])
    cbias_d = IN("conv_b", [128, 8])
    br_d = IN("lru_br", [128, 8])
    bi_d = IN("lru_bi", [128, 8])
    lam_d = IN("lru_lam", [128, 8])
    b1_d = [IN("cmp_b1_k", [128, 2]), IN("cmp_b1_v", [128, 2])]
    b2k_d = IN("cmp_b2_k", [128, 1])
    b2v_d = IN("cmp_b2_v", [1, 128])
    pe_d = [IN("cmp_pe_k", [128, 32]), IN("cmp_pe_v", [128, 32])]
    kbd_d = IN("peer_kbd", [128, 256])
    identb_d = IN("c_identb", [128, 128], BF16)
    identf_d = IN("c_identf", [128, 128])
    invf_d = IN("c_invf", [128, 1])
    sgn_d = IN("c_sgn", [128, 1])
    cb_d = IN("c_cb", [128, 4, 512], BF16)
    wb_d = IN("c_wb", [128, 4, 512], BF16)
    cmb_d = IN("c_cmb", [128, 2, S], BF16)
    E_d = IN("c_E", [64, S], BF16)
    ovl_d = IN("c_ovl", [128, 2, 64], BF16)
    fa_d = IN("c_fa", [128, NTT, 64])
    na_d = IN("c_na", [128, NTT, 64])
    vm_d = IN("c_vm", [128, NTT, 64])
    io128_d = IN("c_iota128", [128, 16, 128], I32)
    io256_d = IN("c_iota256", [128, 8, 256], I32)
    io16_d = IN("c_iota16", [128, 8, 16, 16])

    out_d = nc.dram_tensor("out", [S, D], F32, kind="ExternalOutput").ap()

    dbg_n = [0]

    def DBG(tag, tt_, shape, dt=F32):
        if not debug:
            return
        dbg_n[0] += 1
        d = nc.dram_tensor(f"dbg_{tag}", list(shape), dt, kind="ExternalOutput").ap()
        P.dma("sp", d, tt_.t[:], reads=tt_.r)

    def SCR(name, shape, dt):
        kind = "ExternalOutput" if debug else "Internal"
        return nc.dram_tensor(name, list(shape), dt, kind=kind).ap()

    winbf = SCR("s_winbf", [128, 8, WCOLS], BF16)
    w5bf = [SCR(f"s_w5bf{i}", [128, 8, D], BF16) for i in range(5)]
    wpbf = SCR("s_wpbf", [128, 2, D], BF16)
    w1bf = [SCR(f"s_w1bf{i}", [128, 32, 256], BF16) for i in range(2)]
    zfm = SCR("s_zfm", [NCH, 128, S], F32)
    ma_s = SCR("s_ma", [8, 128, S], BF16)
    obt_s = SCR("s_obt", [8, 128, S], BF16)
    h1_s = SCR("s_h1", [S, D], F32)
    uv_s = nc.dram_tensor("s_uv", [16384, 2048], BF16, kind="Internal").ap()

    identb = P.sbuf("identb", [128, 128], BF16)
    identf = P.sbuf("identf", [128, 128], F32)
    P.dma("sp", identb.t[:, :], identb_d, writes=[identb.r0])
    P.dma("sp", identf.t[:, :], identf_d, writes=[identf.r0])

    cast_rr = [0]

    def cast(out, in_, reads, writes, engs=tuple(_os.environ.get("K_CAST", "dve,act").split(","))):
        e = engs[cast_rr[0] % len(engs)]
        cast_rr[0] += 1
        if e == "act":
            P.op("act", "copy", dict(out=out, in_=in_), reads, writes)
        else:
            P.op(e, "tensor_copy", dict(out=out, in_=in_), reads, writes)

    P.push()
    stg = P.sbuf("stg", [128, 2, 4096], F32, nres=2)
    stgb = P.sbuf("stgb", [128, 2, 4096], BF16, nres=2)
    P.op("dve", "memset", dict(ap=stg.t[:, 0, :], constant=0.0), [], [stg.r[0]])
    P.op("dve", "memset", dict(ap=stg.t[:, 1, :], constant=0.0), [], [stg.r[1]])
    pi = [0]

    def piece(loads, n, dst, dshape):
        b = pi[0] % 2
        pi[0] += 1
        for mk, src in loads:
            P.dma("sp", mk(stg.t[:, b, 0:n]), src, writes=[stg.r[b]])
        cast(stgb.t[:, b, 0:n], stg.t[:, b, 0:n], [stg.r[b]], [stgb.r[b]])
        P.dma("pool", dst, dshape(stgb.t[:, b, 0:n]), reads=[stgb.r[b]])

    win_v = win_d
    segs = win_segments()
    for c0 in range(0, WCOLS, 512):
        c1 = min(c0 + 512, WCOLS)
        w = c1 - c0
        loads = []
        for (dcol, scol, ncol) in segs:
            lo, hi = max(dcol, c0), min(dcol + ncol, c1)
            if lo >= hi:
                continue
            so = scol + (lo - dcol)
            loads.append((lambda a, lo=lo, hi=hi, w=w, c0=c0: a.rearrange("p (kc n) -> p kc n", kc=8)[:, :, lo - c0:hi - c0],
                          win_v[:, :, so:so + (hi - lo)]))
        if c1 == WCOLS and 8192 + 24 < WCOLS:
            pass
        piece(loads, 8 * w, winbf[:, :, c0:c1], lambda a, w=w: a.rearrange("p (kc n) -> p kc n", kc=8))
    for i in range(5):
        wv = w5_d[i]
        for hh in range(2):
            piece([(lambda a: a.rearrange("p (kc n) -> p kc n", kc=8), wv[:, :, hh * 512:(hh + 1) * 512])], 4096,
                  w5bf[i][:, :, hh * 512:(hh + 1) * 512], lambda a: a.rearrange("p (kc n) -> p kc n", kc=8))
    piece([(lambda a: a.rearrange("p (kc n) -> p kc n", kc=2), wp_d)], 2048,
          wpbf[:, :, :], lambda a: a.rearrange("p (kc n) -> p kc n", kc=2))
    for i in range(2):
        wv = w1_d[i]
        for hh in range(2):
            piece([(lambda a: a.rearrange("p (l h) -> p l h", l=16), wv[:, hh * 16:(hh + 1) * 16, :])], 4096,
                  w1bf[i][:, hh * 16:(hh + 1) * 16, :], lambda a: a.rearrange("p (l h) -> p l h", l=16))
    ust = P.sbuf("ust", [128, 2, 4096], F32, nres=2)
    uvb = P.sbuf("uvb", [128, 2, 4, 2048], BF16, nres=2)
    u_v = u_d.rearrange("(r p j) d -> r p (j d)", p=128, j=4)
    v_v = v_d.rearrange("(r p j) d -> r p (j d)", p=128, j=4)
    uv_v = uv_s.rearrange("(r p j) c -> r p (j c)", p=128, j=4)
    for rb in range(32):
        ob_ = rb % 2
        for ti_, (src_v, eng_) in enumerate(((u_v, "act"), (v_v, "dve"))):
            P.dma("sp", ust.t[:, ti_, :], src_v[rb], writes=[ust.r[ti_]])
            dst_ = uvb.t[:, ob_, :, ti_ * 1024:(ti_ + 1) * 1024]
            src_ = ust.t[:, ti_, :].rearrange("p (j d) -> p j d", j=4)
            if eng_ == "act":
                P.op("act", "copy", dict(out=dst_, in_=src_), [ust.r[ti_]], [uvb.r[ob_]])
            else:
                P.op("dve", "tensor_copy", dict(out=dst_, in_=src_), [ust.r[ti_]], [uvb.r[ob_]])
        P.dma("pool", uv_v[rb], uvb.t[:, ob_].rearrange("p j c -> p (j c)"), reads=[uvb.r[ob_]])
    P.emit_stage()
    P.pop()
    if upto == "P":
        P.close()
        return nc

    P.push()
    nT = P.sbuf("nT", [128, 8, S], BF16, nres=NTT)
    gbc = P.sbuf("gbc", [128, D], F32)
    xt = P.sbuf("xt", [128, 2, D], F32, nres=2)
    nb = P.sbuf("nb", [128, 2, D], BF16, nres=2)
    junk = P.sbuf("junk", [128, D], F32)
    ss = P.sbuf("ss", [128, NTT], F32, nres=NTT)
    rs = P.sbuf("rs", [128, NTT], F32, nres=NTT)
    tp = P.psum("tp", [128, 2, 1024], BF16, nres=2)
    P.dma("sp", gbc.t[:, :], gm_d.partition_broadcast(128), writes=[gbc.r0])
    for tt in range(NTT):
        b = tt % 2
        P.dma("sp", xt.t[:, b, :], x_d[tt * 128:(tt + 1) * 128, :], writes=[xt.r[b]])
        P.op("act", "activation", dict(out=junk.t[:, :], in_=xt.t[:, b, :], func=AF.Square, accum_out=ss.t[:, tt:tt + 1]),
             reads=[xt.r[b]], writes=[junk.r0, ss.r[tt]])
        P.op("act", "activation", dict(out=rs.t[:, tt:tt + 1], in_=ss.t[:, tt:tt + 1], func=AF.Sqrt, scale=1.0 / D, bias=1e-6),
             reads=[ss.r[tt]], writes=[rs.r[tt]])
        P.op("dve", "reciprocal", dict(out=rs.t[:, tt:tt + 1], in_=rs.t[:, tt:tt + 1]), reads=[rs.r[tt]], writes=[rs.r[tt]])
        P.op("dve", "scalar_tensor_tensor", dict(out=nb.t[:, b, :], in0=xt.t[:, b, :], scalar=rs.t[:, tt:tt + 1], in1=gbc.t[:, :],
                                                 op0=ALU.mult, op1=ALU.mult), reads=[xt.r[b], rs.r[tt], gbc.r0], writes=[nb.r[b]])
        for c in range(8):
            P.op("pe", "transpose", dict(out=tp.t[:, b, c * 128:(c + 1) * 128], in_=nb.t[:, b, c * 128:(c + 1) * 128], identity=identb.t[:, :]),
                 reads=[nb.r[b], identb.r0], writes=[tp.r[b]])
        cast(nT.t[:, :, tt * 128:(tt + 1) * 128], tp.t[:, b, :].rearrange("p (c t) -> p c t", c=8), [tp.r[b]], [nT.r[tt]])

    wbuf = P.sbuf("wbuf", [128, 2, 8, 512], BF16, nres=2)
    zst = P.sbuf("zst", [128, 2, S], F32, nres=2)
    pz = P.psum("pz", [128, 4, 512], F32, nres=4)
    nTall = list(nT.r)
    zi = 0
    pzi = 0
    for wpi, c0 in enumerate(range(0, WCOLS, 512)):
        c1 = min(c0 + 512, WCOLS)
        wb_ = wpi % 2
        P.dma("sp", wbuf.t[:, wb_, :, 0:c1 - c0], winbf[:, :, c0:c1], writes=[wbuf.r[wb_]])
        for cc in range((c1 - c0) // 128):
            ch = c0 // 128 + cc
            M = 24 if ch == C_NG else 128
            zb = zi % 2
            zi += 1
            for qc in range(NQC):
                pb = pzi % 4
                pzi += 1
                for kc in range(8):
                    P.op("pe", "matmul", dict(out=pz.t[0:M, pb, :], lhsT=wbuf.t[:, wb_, kc, cc * 128:cc * 128 + M],
                                              rhs=nT.t[:, kc, qc * 512:(qc + 1) * 512], start=(kc == 0), stop=(kc == 7),
                                              skip_group_check=True),
                         reads=[wbuf.r[wb_]] + nTall[qc * 4:(qc + 1) * 4], writes=[pz.r[pb]])
                dst = zst.t[0:M, zb, qc * 512:(qc + 1) * 512]
                src = pz.t[0:M, pb, :]
                if C_RG <= ch < C_RG + 8:
                    P.op("act", "activation", dict(out=dst, in_=src, func=AF.Gelu_apprx_tanh), [pz.r[pb]], [zst.r[zb]])
                elif ch >= C_M:
                    P.op("act", "activation", dict(out=dst, in_=src, func=AF.Sigmoid), [pz.r[pb]], [zst.r[zb]])
                else:
                    P.op("dve", "tensor_copy", dict(out=dst, in_=src), [pz.r[pb]], [zst.r[zb]])
            P.dma("pool", zfm[ch, 0:M, :], zst.t[0:M, zb, :], reads=[zst.r[zb]])
    P.emit_stage()
    P.pop()
    if upto == "A":
        P.close()
        return nc

    P.push()
    cw = P.sbuf("cw", [128, 8, 4], F32)
    cbias = P.sbuf("cbias", [128, 8], F32)
    brs = P.sbuf("brs", [128, 8], F32)
    bis = P.sbuf("bis", [128, 8], F32)
    lam = P.sbuf("lam", [128, 8], F32)
    m8 = P.sbuf("m8", [128, 8], F32)
    m16 = P.sbuf("m16", [128, 8], F32)
    wst = P.sbuf("wst", [128, 2, 8, 128], F32, nres=2)
    wrb = P.sbuf("wrb", [128, 8, 128], BF16)
    wib = P.sbuf("wib", [128, 8, 128], BF16)
    for (t_, d_) in ((cw, cw_d), (cbias, cbias_d), (brs, br_d), (bis, bi_d), (lam, lam_d)):
        P.dma("sp", t_.t[:], d_, writes=[t_.r0])
    P.dma("sp", wst.t[:, 0], wr_d, writes=[wst.r[0]])
    P.dma("sp", wst.t[:, 1], wi_d, writes=[wst.r[1]])
    P.op("dve", "tensor_copy", dict(out=wrb.t[:, :, :], in_=wst.t[:, 0]), [wst.r[0]], [wrb.r0])
    P.op("dve", "tensor_copy", dict(out=wib.t[:, :, :], in_=wst.t[:, 1]), [wst.r[1]], [wib.r0])
    P.op("act", "activation", dict(out=m8.t[:, :], in_=lam.t[:, :], func=AF.Exp, scale=-1.0), [lam.r0], [m8.r0])
    P.op("act", "activation", dict(out=m8.t[:, :], in_=m8.t[:, :], func=AF.Ln, bias=1.0), [m8.r0], [m8.r0])
    P.op("dve", "tensor_scalar", dict(out=m16.t[:, :], in0=m8.t[:, :], scalar1=-16.0, scalar2=None, op0=ALU.mult), [m8.r0], [m16.r0])
    P.op("dve", "tensor_scalar", dict(out=m8.t[:, :], in0=m8.t[:, :], scalar1=-8.0, scalar2=None, op0=ALU.mult), [m8.r0, m16.r0], [m8.r0])
    zx = P.sbuf("zx", [128, S + 3], F32)
    gz = P.sbuf("gz", [128, S], F32)
    xa = P.sbuf("xa", [128, S], F32)
    xab = P.sbuf("xab", [128, S], BF16)
    rr = P.sbuf("rr", [128, S], F32)
    ig = P.sbuf("ig", [128, S], F32)
    t1 = P.sbuf("t1", [128, S], F32)
    t2 = P.sbuf("t2", [128, S], F32)
    mab = P.sbuf("mab", [128, S], BF16)
    pg = P.psum("pg", [128, 4, 512], F32, nres=4)
    P.op("dve", "memset", dict(ap=zx.t[:, 0:3], constant=0.0), [], [zx.r0])
    pgi = 0
    for ch in range(8):
        P.dma("sp", zx.t[:, 3:S + 3], zfm[C_RX + ch], writes=[zx.r0])
        P.dma("sp", gz.t[:, :], zfm[C_RG + ch], writes=[gz.r0])
        P.op("dve", "tensor_scalar", dict(out=xa.t[:, :], in0=zx.t[:, 0:S], scalar1=cw.t[:, ch, 0:1], scalar2=cbias.t[:, ch:ch + 1],
                                          op0=ALU.mult, op1=ALU.add), [zx.r0, cw.r0, cbias.r0], [xa.r0])
        for k in range(1, 4):
            P.op("dve", "scalar_tensor_tensor", dict(out=xa.t[:, :], in0=zx.t[:, k:k + S], scalar=cw.t[:, ch, k:k + 1], in1=xa.t[:, :],
                                                     op0=ALU.mult, op1=ALU.add), [zx.r0, cw.r0, xa.r0], [xa.r0])
        P.op("act", "copy", dict(out=xab.t[:, :], in_=xa.t[:, :]), [xa.r0], [xab.r0])
        for (wgt, bia, dst) in ((wrb, brs, rr), (wib, bis, ig)):
            for qc in range(NQC):
                pb = pgi % 4
                pgi += 1
                P.op("pe", "matmul", dict(out=pg.t[:, pb, :], lhsT=wgt.t[:, ch, :], rhs=xab.t[:, qc * 512:(qc + 1) * 512], start=True, stop=True,
                                          skip_group_check=True), [wgt.r0, xab.r0], [pg.r[pb]])
                P.op("act", "activation", dict(out=dst.t[:, qc * 512:(qc + 1) * 512], in_=pg.t[:, pb, :], func=AF.Sigmoid, bias=bia.t[:, ch:ch + 1]),
                     [pg.r[pb], bia.r0], [dst.r0])
        P.op("act", "activation", dict(out=t1.t[:, :], in_=rr.t[:, :], func=AF.Exp, scale=m8.t[:, ch:ch + 1]), [rr.r0, m8.r0], [t1.r0])
        P.op("act", "activation", dict(out=t2.t[:, :], in_=rr.t[:, :], func=AF.Exp, scale=m16.t[:, ch:ch + 1]), [rr.r0, m16.r0], [t2.r0])
        P.op("act", "activation", dict(out=t2.t[:, :], in_=t2.t[:, :], func=AF.Sqrt, scale=-1.0, bias=1.0), [t2.r0], [t2.r0])
        P.op("pool", "tensor_tensor", dict(out=ig.t[:, :], in0=ig.t[:, :], in1=xa.t[:, :], op=ALU.mult), [ig.r0, xa.r0], [ig.r0])
        P.op("pool", "tensor_tensor", dict(out=ig.t[:, :], in0=ig.t[:, :], in1=t2.t[:, :], op=ALU.mult), [ig.r0, t2.r0], [ig.r0])
        P.op("dve", "tensor_tensor_scan", dict(out=rr.t[:, :], data0=t1.t[:, :], data1=ig.t[:, :], initial=0.0, op0=ALU.mult, op1=ALU.add),
             [t1.r0, ig.r0, rr.r0], [rr.r0])
        P.op("dve", "tensor_tensor", dict(out=mab.t[:, :], in0=rr.t[:, :], in1=gz.t[:, :], op=ALU.mult), [rr.r0, gz.r0], [mab.r0])
        P.dma("pool", ma_s[ch], mab.t[:, :], reads=[mab.r0])
    P.emit_stage()
    P.pop()
    if upto == "B":
        P.close()
        return nc

    P.push()
    cosT = P.sbuf("cosT", [128, S], F32)
    sinT = P.sbuf("sinT", [128, S], F32)
    P.push()
    posi = P.sbuf("posi", [128, S], I32)
    tq_ = P.sbuf("tq", [128, S], F32)
    tk_ = P.sbuf("tk", [128, S], F32)
    ki_ = P.sbuf("ki", [128, S], I32)
    invf = P.sbuf("invf", [128, 1], F32)
    sgn = P.sbuf("sgn", [128, 1], F32)
    P.dma("sp", posi.t[:, :], pos_d.partition_broadcast(128), writes=[posi.r0])
    P.dma("sp", invf.t[:, :], invf_d, writes=[invf.r0])
    P.dma("sp", sgn.t[:, :], sgn_d, writes=[sgn.r0])
    P.op("dve", "tensor_copy", dict(out=tq_.t[:, :], in_=posi.t[:, :]), [posi.r0], [tq_.r0])
    P.op("dve", "tensor_scalar", dict(out=tq_.t[:, :], in0=tq_.t[:, :], scalar1=invf.t[:, 0:1], scalar2=float(1.0 / (2 * np.pi)),
                                      op0=ALU.mult, op1=ALU.mult), [tq_.r0, invf.r0], [tq_.r0])
    TWO_PI = float(2 * np.pi * (1 - 1e-6))
    for which, dstT in (("sin", sinT), ("cos", cosT)):
        if which == "cos":
            P.op("dve", "tensor_scalar", dict(out=tq_.t[:, :], in0=tq_.t[:, :], scalar1=0.25, scalar2=None, op0=ALU.add), [tq_.r0], [tq_.r0])
        P.op("dve", "tensor_copy", dict(out=ki_.t[:, :], in_=tq_.t[:, :]), [tq_.r0], [ki_.r0])
        P.op("dve", "tensor_copy", dict(out=tk_.t[:, :], in_=ki_.t[:, :]), [ki_.r0], [tk_.r0])
        P.op("dve", "tensor_tensor", dict(out=tk_.t[:, :], in0=tq_.t[:, :], in1=tk_.t[:, :], op=ALU.subtract), [tq_.r0, tk_.r0], [tk_.r0])
        P.op("act", "activation", dict(out=dstT.t[:, :], in_=tk_.t[:, :], func=AF.Sin, scale=TWO_PI), [tk_.r0], [dstT.r0])
    P.op("dve", "tensor_scalar", dict(out=sinT.t[:, :], in0=sinT.t[:, :], scalar1=sgn.t[:, 0:1], scalar2=None, op0=ALU.mult), [sinT.r0, sgn.r0], [sinT.r0])
    P.emit_stage()
    P.pop()

    ksT = P.sbuf("ksT", [128, S], BF16)
    kwT = P.sbuf("kwT", [128, S], BF16)
    vsa = P.sbuf("vsa", [128, NTT, 129], BF16)
    vwa = P.sbuf("vwa", [128, NTT, 129], BF16)
    kcT = P.sbuf("kcT", [128, 256], BF16)
    vca = P.sbuf("vca", [128, 2, 193], BF16)
    E_s = P.sbuf("E_s", [64, S], BF16)
    cb_s = P.sbuf("cb_s", [128, 4, 512], BF16)
    wb_s = P.sbuf("wb_s", [128, 4, 512], BF16)
    b1s = [P.sbuf(f"b1s{i}", [128, 2], F32) for i in range(2)]
    pes = [P.sbuf(f"pes{i}", [128, 32], F32) for i in range(2)]
    b2k = P.sbuf("b2k", [128, 1], F32)
    b2vb = P.sbuf("b2vb", [128, 128], F32)
    ps_s = P.psum("ps_s", [128, 2, 512], F32, nres=2)
    ps_acc = P.psum("ps_acc", [128, 4, 512], F32, nres=4)
    ps_trb = P.psum("ps_trb", [128, 1024], BF16)
    ps_trf = P.psum("ps_trf", [128, 512], F32)
    P.dma("sp", E_s.t[:, :], E_d, writes=[E_s.r0])
    P.dma("sp", cb_s.t[:], cb_d, writes=[cb_s.r0])
    P.dma("sp", wb_s.t[:], wb_d, writes=[wb_s.r0])
    for i in range(2):
        P.dma("sp", b1s[i].t[:, :], b1_d[i], writes=[b1s[i].r0])
        P.dma("sp", pes[i].t[:, :], pe_d[i], writes=[pes[i].r0])
    P.dma("sp", b2k.t[:, :], b2k_d, writes=[b2k.r0])
    P.dma("sp", b2vb.t[:, :], b2v_d.partition_broadcast(128), writes=[b2vb.r0])
    P.op("dve", "memset", dict(ap=vsa.t[:, :, 128:129], constant=1.0), [], [vsa.r0])
    P.op("dve", "memset", dict(ap=vwa.t[:, :, 128:129], constant=1.0), [], [vwa.r0])
    srot = [0]

    def sbank():
        srot[0] += 1
        return srot[0] % 2

    def mm(out, lhsT, rhs, start, stop, reads, writes):
        P.op("pe", "matmul", dict(out=out, lhsT=lhsT, rhs=rhs, start=start, stop=stop, skip_group_check=True), reads, writes)

    for g in range(2):
        P.push()
        ld = P.sbuf("ld", [128, 2, S], F32, nres=2)
        lohi = P.sbuf("lohi", [128, 2, S], BF16, nres=2)
        w1s = P.sbuf("w1s", [128, 32, 256], BF16)
        w2f = P.sbuf("w2f", [128, 2, 128], F32)
        w2s = P.sbuf("w2s", [128, 2, 128], BF16)
        hidT = P.sbuf("hidT", [128, 2, 256], BF16)
        vb = P.sbuf("vb", [128, S], BF16)
        P.op("dve", "memset", dict(ap=kcT.t[:, :], constant=0.0), [], [kcT.r0])
        P.op("dve", "memset", dict(ap=vca.t[:], constant=0.0), [], [vca.r0])
        P.op("dve", "memset", dict(ap=vca.t[:, :, 128:129], constant=1.0), [vca.r0], [vca.r0])
        P.dma("sp", vca.t[:, :, 129:193], ovl_d, writes=[vca.r0])
        for kvi in range(2):
            ch = C_KV + kvi * 2 + g
            P.dma("sp", ld.t[:, 0, :], zfm[ch], writes=[ld.r[0]])
            P.dma("sp", w1s.t[:], w1bf[kvi], writes=[w1s.r0])
            P.dma("sp", w2f.t[:], w2_d[kvi], writes=[w2f.r0])
            P.op("dve", "tensor_copy", dict(out=w2s.t[:], in_=w2f.t[:]), [w2f.r0], [w2s.r0])
            for v_ in range(2):
                P.op("dve", "tensor_tensor", dict(out=lohi.t[:, v_, :].rearrange("p (n l) -> p n l", l=16),
                                                  in0=ld.t[:, 0, :].rearrange("p (n l) -> p n l", l=16),
                                                  in1=pes[kvi].t[:, v_ * 16:(v_ + 1) * 16].unsqueeze(1).broadcast_to([128, 256, 16]),
                                                  op=ALU.add), [ld.r[0], pes[kvi].r0], [lohi.r[v_]])
            for hc in range(2):
                for l in range(32):
                    v_ = 0 if l < 16 else 1
                    mm(ps_s.t[:, hc, 0:255], w1s.t[:, l, hc * 128:(hc + 1) * 128], lohi.t[:, v_, l:l + 16 * 254 + 1:16],
                       l == 0, l == 31, [w1s.r0, lohi.r[v_]], [ps_s.r[hc]])
                P.op("act", "activation", dict(out=hidT.t[:, hc, 0:255], in_=ps_s.t[:, hc, 0:255], func=AF.Gelu_apprx_tanh,
                                               bias=b1s[kvi].t[:, hc:hc + 1]), [ps_s.r[hc], b1s[kvi].r0], [hidT.r0])
            if kvi == 0:
                for hc in range(2):
                    mm(ps_s.t[:, 0, 0:255], w2s.t[:, hc, :], hidT.t[:, hc, 0:255], hc == 0, hc == 1, [w2s.r0, hidT.r0], [ps_s.r[0]])
                P.op("dve", "tensor_scalar", dict(out=kcT.t[:, 0:255], in0=ps_s.t[:, 0, 0:255], scalar1=b2k.t[:, 0:1], scalar2=None, op0=ALU.add),
                     [ps_s.r[0], b2k.r0], [kcT.r0])
            else:
                for nch in range(2):
                    rows = 128 if nch == 0 else 127
                    for hc in range(2):
                        mm(ps_acc.t[0:rows, nch, 0:128], hidT.t[:, hc, nch * 128:nch * 128 + rows], w2s.t[:, hc, :], hc == 0, hc == 1,
                           [w2s.r0, hidT.r0], [ps_acc.r[nch]])
                    P.op("dve", "tensor_tensor", dict(out=vca.t[0:rows, nch, 0:128], in0=ps_acc.t[0:rows, nch, 0:128], in1=b2vb.t[0:rows, :], op=ALU.add),
                         [ps_acc.r[nch], b2vb.r0], [vca.r0])
        for (kch, swch, dstK) in ((C_KV + 4 + g, C_KSW + g, ksT), (C_KV + 8 + g, C_KSW + 2 + g, kwT)):
            P.dma("sp", ld.t[:, 0, :], zfm[kch], writes=[ld.r[0]])
            P.dma("sp", ld.t[:, 1, :], zfm[swch], writes=[ld.r[1]])
            if debug and g == 1 and dstK is ksT:
                DBG("g1_ld_raw", ld, [128, 2, S])
                DBG("g1_cosT", cosT, [128, S])
                DBG("g1_sinT", sinT, [128, S])
            P.op("dve", "tensor_tensor", dict(out=ld.t[:, 0, :], in0=ld.t[:, 0, :], in1=cosT.t[:, :], op=ALU.mult), [ld.r[0], cosT.r0], [ld.r[0]])
            P.op("pool", "tensor_tensor", dict(out=ld.t[:, 1, :], in0=ld.t[:, 1, :], in1=sinT.t[:, :], op=ALU.mult), [ld.r[1], sinT.r0], [ld.r[1]])
            if debug and g == 1 and dstK is ksT:
                DBG("g1_ld_mul", ld, [128, 2, S])
            P.op("dve", "tensor_tensor", dict(out=dstK.t[:, :], in0=ld.t[:, 0, :], in1=ld.t[:, 1, :], op=ALU.add), [ld.r[0], ld.r[1]], [dstK.r0])
        for (vch, dstV) in ((C_KV + 6 + g, vsa), (C_KV + 10 + g, vwa)):
            P.dma("sp", ld.t[:, 0, :], zfm[vch], writes=[ld.r[0]])
            P.op("act", "copy", dict(out=vb.t[:, :], in_=ld.t[:, 0, :]), [ld.r[0]], [vb.r0])
            for t4 in range(8):
                for a in range(4):
                    tt = t4 * 4 + a
                    P.op("pe", "transpose", dict(out=ps_trb.t[:, a * 128:(a + 1) * 128], in_=vb.t[:, tt * 128:(tt + 1) * 128], identity=identb.t[:, :]),
                         [vb.r0, identb.r0], [ps_trb.r0])
                cast(dstV.t[:, t4 * 4:(t4 + 1) * 4, 0:128], ps_trb.t[:, 0:512].rearrange("p (a d) -> p a d", a=4), [ps_trb.r0], [dstV.r0])
        P.emit_stage()
        P.pop()
        if upto == "C0":
            P.close()
            return nc

        P.push()
        zq = P.sbuf("zq", [128, 4, 512], F32, nres=4)
        zqs = P.sbuf("zqs", [128, 4, 512], F32, nres=4)
        qn = P.sbuf("qn", [128, 4, 512], BF16)
        qr = P.sbuf("qr", [128, 4, 512], BF16)
        cmbq = P.sbuf("cmbq", [128, 2, 512], BF16)
        fa = P.sbuf("fa", [128, 4, 64], F32)
        na = P.sbuf("na", [128, 4, 64], F32)
        vm = P.sbuf("vm", [128, 4, 64], F32)
        gTs = P.sbuf("gTs", [24, 512], F32)
        gts = P.sbuf("gts", [128, 4, 24], F32)
        pc = P.sbuf("pc", [128, 2, 2, 512], BF16, nres=2)
        pt = P.sbuf("pt", [128, 3, 512], BF16, nres=3)
        oacc = P.sbuf("oacc", [128, 4, 4, 128], F32)
        imp = P.sbuf("imp", [128, 4, 64], F32)
        impp = P.sbuf("impp", [128, 64], F32)
        imp2 = P.sbuf("imp2", [128, 64], F32)
        m8a = P.sbuf("m8a", [128, 8], F32)
        m8b = P.sbuf("m8b", [128, 8], F32)
        msel = P.sbuf("msel", [128, 64], F32)
        bmb = P.sbuf("bmb", [128, 64], BF16)
        biasT = P.sbuf("biasT", [64, 512], BF16)
        sm = P.sbuf("sm", [128, 8], F32)
        obb = P.sbuf("obb", [128, 4, 4, 128], BF16)
        obT = P.sbuf("obT", [128, 4, 512], BF16)
        ptr = [0]
        aset = [0]
        for qc in range(NQC):
            q0 = qc * 512
            for hq in range(4):
                P.dma("sp", zq.t[:, hq, :], zfm[C_Q + 4 * g + hq, :, q0:q0 + 512], writes=[zq.r[hq]])
                P.dma("sp", zqs.t[:, hq, :], zfm[C_QS + 4 * g + hq, :, q0:q0 + 512], writes=[zqs.r[hq]])
            P.dma("sp", cmbq.t[:], cmb_d[:, :, q0:q0 + 512], writes=[cmbq.r0])
            P.dma("sp", fa.t[:], fa_d[:, qc * 4:qc * 4 + 4, :], writes=[fa.r0])
            P.dma("sp", na.t[:], na_d[:, qc * 4:qc * 4 + 4, :], writes=[na.r0])
            P.dma("sp", vm.t[:], vm_d[:, qc * 4:qc * 4 + 4, :], writes=[vm.r0])
            P.dma("sp", gTs.t[:, :], zfm[C_NG, 0:24, q0:q0 + 512], writes=[gTs.r0])
            P.op("act", "copy", dict(out=qn.t[:], in_=zq.t[:]), zq.r, [qn.r0])
            cs = cosT.t[:, q0:q0 + 512].unsqueeze(1).broadcast_to([128, 4, 512])
            sn = sinT.t[:, q0:q0 + 512].unsqueeze(1).broadcast_to([128, 4, 512])
            P.op("dve", "tensor_tensor", dict(out=zq.t[:], in0=zq.t[:], in1=cs, op=ALU.mult), zq.r + [cosT.r0], zq.r)
            P.op("pool", "tensor_tensor", dict(out=zqs.t[:], in0=zqs.t[:], in1=sn, op=ALU.mult), zqs.r + [sinT.r0], zqs.r)
            P.op("dve", "tensor_tensor", dict(out=qr.t[:], in0=zq.t[:], in1=zqs.t[:], op=ALU.add), zq.r + zqs.r, [qr.r0])
            for qt in range(4):
                P.op("pe", "transpose", dict(out=ps_trf.t[:, qt * 24:(qt + 1) * 24], in_=gTs.t[0:24, qt * 128:(qt + 1) * 128], identity=identf.t[0:24, 0:24]),
                     [gTs.r0, identf.r0], [ps_trf.r0])
            P.op("dve", "tensor_copy", dict(out=gts.t[:].rearrange("p a c -> p (a c)"), in_=ps_trf.t[:, 0:96]), [ps_trf.r0], [gts.r0])

            def post(hh, qt, bank, off, br, first, aux=None):
                h = 4 * g + hh
                accv = ps_acc.t[:, bank, :]
                P.op("dve", "tensor_scalar", dict(out=sm.t[:, 0:1], in0=accv[:, off + 128:off + 129], scalar1=1e-30, scalar2=None, op0=ALU.max),
                     [ps_acc.r[bank]], [sm.r0])
                P.op("dve", "reciprocal", dict(out=sm.t[:, 1:2], in_=sm.t[:, 0:1]), [sm.r0], [sm.r0])
                P.op("dve", "tensor_tensor", dict(out=sm.t[:, 2:3], in0=sm.t[:, 1:2], in1=gts.t[:, qt, h * 3 + br:h * 3 + br + 1], op=ALU.mult),
                     [sm.r0, gts.r0], [sm.r0])
                if first:
                    P.op("dve", "tensor_scalar", dict(out=oacc.t[:, qt, hh, :], in0=accv[:, off:off + 128], scalar1=sm.t[:, 2:3], scalar2=None, op0=ALU.mult),
                         [ps_acc.r[bank], sm.r0], [oacc.r0])
                else:
                    P.op("dve", "scalar_tensor_tensor", dict(out=oacc.t[:, qt, hh, :], in0=accv[:, off:off + 128], scalar=sm.t[:, 2:3], in1=oacc.t[:, qt, hh, :],
                                                             op0=ALU.mult, op1=ALU.add), [ps_acc.r[bank], sm.r0, oacc.r0], [oacc.r0])
                if aux is not None:
                    if hh == 0:
                        P.op("dve", "tensor_scalar", dict(out=imp.t[:, qt, :], in0=accv[:, off + 129:off + 193], scalar1=sm.t[:, 1:2], scalar2=None, op0=ALU.mult),
                             [ps_acc.r[bank], sm.r0], [imp.r0])
                    else:
                        P.op("dve", "scalar_tensor_tensor", dict(out=imp.t[:, qt, :], in0=accv[:, off + 129:off + 193], scalar=sm.t[:, 1:2], in1=imp.t[:, qt, :],
                                                                 op0=ALU.mult, op1=ALU.add), [ps_acc.r[bank], sm.r0, imp.r0], [imp.r0])

            if (g, qc) in ((0, 0), (1, 1)):
                tg = f"g{g}q{qc}"
                DBG(tg + "_gts", gts, [128, 4, 24]); DBG(tg + "_qn", qn, [128, 4, 512], BF16); DBG(tg + "_qr", qr, [128, 4, 512], BF16)
                DBG(tg + "_kcT", kcT, [128, 256], BF16); DBG(tg + "_vca", vca, [128, 2, 193], BF16)
                DBG(tg + "_ksT", ksT, [128, S], BF16); DBG(tg + "_kwT", kwT, [128, S], BF16)
                DBG(tg + "_vsa", vsa, [128, NTT, 129], BF16); DBG(tg + "_vwa", vwa, [128, NTT, 129], BF16)
            n_nch = 2 if (q0 + 511 >= 16 * 128 + 31) else 1
            for hh in range(4):
                pcb = hh % 2
                for nch in range(n_nch):
                    sb = sbank()
                    mm(ps_s.t[:, sb, :], kcT.t[:, nch * 128:(nch + 1) * 128], qn.t[:, hh, :], True, False, [kcT.r0, qn.r0], [ps_s.r[sb]])
                    mm(ps_s.t[:, sb, :], identb.t[:, :], cmbq.t[:, nch, :], False, True, [identb.r0, cmbq.r0], [ps_s.r[sb]])
                    P.op("act", "activation", dict(out=pc.t[:, pcb, nch, :], in_=ps_s.t[:, sb, :], func=AF.Exp, scale=SCALE), [ps_s.r[sb]], [pc.r[pcb]])
                aset[0] ^= 1
                started = {}
                for qt in range(4):
                    bank = aset[0] * 2 + qt // 2
                    off = (qt % 2) * 193
                    for nch in range(n_nch):
                        mm(ps_acc.t[:, bank, off:off + 193], pc.t[:, pcb, nch, qt * 128:(qt + 1) * 128], vca.t[:, nch, :],
                           bank not in started, False, [pc.r[pcb], vca.r0], [ps_acc.r[bank]])
                        started[bank] = 1
                for qt in range(4):
                    post(hh, qt, aset[0] * 2 + qt // 2, (qt % 2) * 193, 0, True, aux=True)
            if (g, qc) in ((0, 0), (1, 1)):
                DBG(tg + "_oacc_c", oacc, [128, 4, 4, 128]); DBG(tg + "_imp", imp, [128, 4, 64])
            for qt in range(4):
                P.op("dve", "tensor_tensor", dict(out=impp.t[:, :], in0=imp.t[:, qt, :], in1=fa.t[:, qt, :], op=ALU.max), [imp.r0, fa.r0], [impp.r0])
                P.op("dve", "tensor_tensor", dict(out=impp.t[:, :], in0=impp.t[:, :], in1=na.t[:, qt, :], op=ALU.add), [impp.r0, na.r0], [impp.r0])
                P.op("dve", "max", dict(out=m8a.t[:, :], in_=impp.t[:, :]), [impp.r0], [m8a.r0])
                P.op("dve", "match_replace", dict(out=imp2.t[:, :], in_to_replace=m8a.t[:, :], in_values=impp.t[:, :], imm_value=-3.0e38),
                     [impp.r0, m8a.r0], [imp2.r0])
                P.op("dve", "max", dict(out=m8b.t[:, :], in_=imp2.t[:, :]), [imp2.r0], [m8b.r0])
                P.op("dve", "scalar_tensor_tensor", dict(out=msel.t[:, :], in0=impp.t[:, :], scalar=m8b.t[:, 7:8], in1=vm.t[:, qt, :],
                                                         op0=ALU.is_ge, op1=ALU.mult), [impp.r0, m8b.r0, vm.r0], [msel.r0])
                P.op("dve", "tensor_scalar", dict(out=bmb.t[:, :], in0=msel.t[:, :], scalar1=-NEG, scalar2=NEG, op0=ALU.mult, op1=ALU.add),
                     [msel.r0], [bmb.r0])
                P.op("pe", "transpose", dict(out=ps_trb.t[0:64, qt * 128:(qt + 1) * 128], in_=bmb.t[:, :], identity=identb.t[:, :]),
                     [bmb.r0, identb.r0], [ps_trb.r0])
            P.op("dve", "tensor_copy", dict(out=biasT.t[:, :], in_=ps_trb.t[0:64, 0:512]), [ps_trb.r0], [biasT.r0])
            if (g, qc) in ((0, 0), (1, 1)):
                DBG(tg + "_biasT", biasT, [64, 512], BF16)
            for br in (2, 1):
                for hh in range(4):
                    aset[0] ^= 1
                    started = {}
                    if br == 1:
                        kts = list(range(0, qc * 4 + 4))
                    else:
                        kts = list(range(max(0, qc * 4 - 4), qc * 4 + 4))
                    vsrc = vsa if br == 1 else vwa

                    def emit_scores(kt):
                        r = kt - qc * 4
                        sb = sbank()
                        if br == 1:
                            mm(ps_s.t[:, sb, :], ksT.t[:, kt * 128:(kt + 1) * 128], qr.t[:, hh, :], True, False, [ksT.r0, qr.r0], [ps_s.r[sb]])
                            mm(ps_s.t[:, sb, :], E_s.t[0:64, kt * 128:(kt + 1) * 128], biasT.t[0:64, :], False, r < 0, [E_s.r0, biasT.r0], [ps_s.r[sb]])
                            if r >= 0:
                                mm(ps_s.t[:, sb, :], identb.t[:, :], cb_s.t[:, r, :], False, True, [identb.r0, cb_s.r0], [ps_s.r[sb]])
                        else:
                            mm(ps_s.t[:, sb, :], kwT.t[:, kt * 128:(kt + 1) * 128], qr.t[:, hh, :], True, False, [kwT.r0, qr.r0], [ps_s.r[sb]])
                            btab = wb_s.t[:, r + 4, :] if r < 0 else cb_s.t[:, r, :]
                            mm(ps_s.t[:, sb, :], identb.t[:, :], btab, False, True, [identb.r0, cb_s.r0, wb_s.r0], [ps_s.r[sb]])
                        pb = ptr[0] % 3
                        ptr[0] += 1
                        P.op("act", "activation", dict(out=pt.t[:, pb, :], in_=ps_s.t[:, sb, :], func=AF.Exp, scale=SCALE), [ps_s.r[sb]], [pt.r[pb]])
                        return (kt, pb)

                    def emit_pv(kt, pb):
                        r = kt - qc * 4
                        for qt in range(4):
                            if r > qt:
                                continue
                            if br == 2 and r < qt - 4:
                                continue
                            bank = aset[0] * 2 + qt // 2
                            off = (qt % 2) * 129
                            mm(ps_acc.t[:, bank, off:off + 129], pt.t[:, pb, qt * 128:(qt + 1) * 128], vsrc.t[:, kt, :],
                               bank not in started, False, [pt.r[pb], vsrc.r0], [ps_acc.r[bank]])
                            started[bank] = 1

                    pend = None
                    for kt in kts:
                        cur = emit_scores(kt)
                        if pend is not None:
                            emit_pv(*pend)
                        pend = cur
                    emit_pv(*pend)
                    for qt in range(4):
                        post(hh, qt, aset[0] * 2 + qt // 2, (qt % 2) * 129, br, False)
            if (g, qc) in ((0, 0), (1, 1)):
                DBG(tg + "_oacc_w", oacc, [128, 4, 4, 128])
            P.op("act", "copy", dict(out=obb.t[:], in_=oacc.t[:]), [oacc.r0], [obb.r0])
            for hh in range(4):
                for qt in range(4):
                    P.op("pe", "transpose", dict(out=ps_trb.t[:, qt * 128:(qt + 1) * 128], in_=obb.t[:, qt, hh, :], identity=identb.t[:, :]),
                         [obb.r0, identb.r0], [ps_trb.r0])
                cast(obT.t[:, hh, :], ps_trb.t[:, 0:512], [ps_trb.r0], [obT.r0])
            for hq in range(4):
                P.dma("pool", obt_s[4 * g + hq, :, q0:q0 + 512], obT.t[:, hq, :], reads=[obT.r0])
        P.emit_stage()
        P.pop()
    P.pop()
    if upto == "C":
        P.close()
        return nc

    def rmsnorm_tile(src, gain, dst_f32, dst_bf, sq, junk_):
        P.op("act", "activation", dict(out=junk_.t[:, :], in_=src.t[:, :], func=AF.Square, accum_out=sq.t[:, 0:1]), [src.r0], [junk_.r0, sq.r0])
        P.op("act", "activation", dict(out=sq.t[:, 1:2], in_=sq.t[:, 0:1], func=AF.Sqrt, scale=1.0 / D, bias=1e-6), [sq.r0], [sq.r0])
        P.op("dve", "reciprocal", dict(out=sq.t[:, 2:3], in_=sq.t[:, 1:2]), [sq.r0], [sq.r0])
        P.op("dve", "scalar_tensor_tensor", dict(out=dst_f32.t[:, :], in0=src.t[:, :], scalar=sq.t[:, 2:3], in1=gain.t[:, :], op0=ALU.mult, op1=ALU.mult),
             [src.r0, sq.r0, gain.r0], [dst_f32.r0])
        if dst_bf is not None:
            P.op("act", "copy", dict(out=dst_bf.t[:, :], in_=dst_f32.t[:, :]), [dst_f32.r0], [dst_bf.r0])

    P.push()
    w3 = [P.sbuf(f"w3s{i}", [128, 8, D], BF16) for i in range(3)]
    for i in range(3):
        P.dma("sp", w3[i].t[:], w5bf[i], writes=[w3[i].r0])
    wA, wB, wO = w3
    obTq = P.sbuf("obTq", [128, 8, 512], BF16, nres=8)
    maTq = P.sbuf("maTq", [128, 8, 512], BF16, nres=8)
    mgT = P.sbuf("mgT", [128, 8, 512], BF16)
    g01 = P.sbuf("g01", [128, 2, 2, 512], F32, nres=2)
    tA = P.sbuf("tA", [128, 2, 512], F32, nres=2)
    tB = P.sbuf("tB", [128, 2, 512], F32, nres=2)
    xtl = P.sbuf("xtl", [128, 2, D], F32, nres=2)
    h1t = P.sbuf("h1t", [128, 2, D], F32, nres=2)
    ps_y = P.psum("ps_y", [128, 4, 512], F32, nres=4)
    ps_h = P.psum("ps_h", [128, 2, 512], F32, nres=2)
    ti = 0
    for qc in range(NQC):
        q0 = qc * 512
        for c8 in range(8):
            P.dma("sp", obTq.t[:, c8, :], obt_s[c8, :, q0:q0 + 512], writes=[obTq.r[c8]])
            P.dma("sp", maTq.t[:, c8, :], ma_s[c8, :, q0:q0 + 512], writes=[maTq.r[c8]])
        for c in range(8):
            b = c % 2
            P.dma("sp", g01.t[:, b, 0, :], zfm[C_M + c, :, q0:q0 + 512], writes=[g01.r[b]])
            P.dma("sp", g01.t[:, b, 1, :], zfm[C_M + 8 + c, :, q0:q0 + 512], writes=[g01.r[b]])
            for k in range(8):
                mm(ps_y.t[:, 2 * b, :], wA.t[:, k, c * 128:(c + 1) * 128], maTq.t[:, k, :], k == 0, k == 7, [wA.r0, maTq.r[k]], [ps_y.r[2 * b]])
            for k in range(8):
                mm(ps_y.t[:, 2 * b + 1, :], wB.t[:, k, c * 128:(c + 1) * 128], obTq.t[:, k, :], k == 0, k == 7, [wB.r0, obTq.r[k]], [ps_y.r[2 * b + 1]])
            P.op("dve", "tensor_tensor", dict(out=tA.t[:, b, :], in0=ps_y.t[:, 2 * b, :], in1=g01.t[:, b, 0, :], op=ALU.mult), [ps_y.r[2 * b], g01.r[b]], [tA.r[b]])
            P.op("dve", "tensor_tensor", dict(out=tB.t[:, b, :], in0=ps_y.t[:, 2 * b + 1, :], in1=g01.t[:, b, 1, :], op=ALU.mult), [ps_y.r[2 * b + 1], g01.r[b]], [tB.r[b]])
            P.op("pool", "tensor_tensor", dict(out=mgT.t[:, c, :], in0=tA.t[:, b, :], in1=tB.t[:, b, :], op=ALU.add), [tA.r[b], tB.r[b]], [mgT.r0])
        for qt in range(4):
            tt = qc * 4 + qt
            b = ti % 2
            ti += 1
            P.dma("sp", xtl.t[:, b, :], x_d[tt * 128:(tt + 1) * 128, :], writes=[xtl.r[b]])
            for half in range(2):
                for c in range(8):
                    mm(ps_h.t[:, half, :], mgT.t[:, c, qt * 128:(qt + 1) * 128], wO.t[:, c, half * 512:(half + 1) * 512], c == 0, c == 7, [mgT.r0, wO.r0], [ps_h.r[half]])
                P.op("dve", "tensor_tensor", dict(out=h1t.t[:, b, half * 512:(half + 1) * 512], in0=ps_h.t[:, half, :], in1=xtl.t[:, b, half * 512:(half + 1) * 512], op=ALU.add),
                     [ps_h.r[half], xtl.r[b]], [h1t.r[b]])
            P.dma("pool", h1_s[tt * 128:(tt + 1) * 128, :], h1t.t[:, b, :], reads=[h1t.r[b]])
    P.emit_stage()
    P.pop()
    if upto == "D1":
        P.close()
        return nc

    P.push()
    wQ = P.sbuf("wQ", [128, 8, D], BF16)
    wG = P.sbuf("wG", [128, 8, D], BF16)
    wPs = P.sbuf("wPs", [128, 2, D], BF16)
    P.dma("sp", wQ.t[:], w5bf[3], writes=[wQ.r0])
    P.dma("sp", wG.t[:], w5bf[4], writes=[wG.r0])
    P.dma("sp", wPs.t[:], wpbf, writes=[wPs.r0])
    kbdf = P.sbuf("kbdf", [128, 256], F32)
    kbdb = P.sbuf("kbdb", [128, 256], BF16)
    P.dma("sp", kbdf.t[:, :], kbd_d, writes=[kbdf.r0])
    P.op("dve", "tensor_copy", dict(out=kbdb.t[:, :], in_=kbdf.t[:, :]), [kbdf.r0], [kbdb.r0])
    gains = []
    for nm, gd in (("gfb", gf_d), ("gpb", gp_d), ("glb", gl_d)):
        t_ = P.sbuf(nm, [128, D], F32)
        P.dma("sp", t_.t[:, :], gd.partition_broadcast(128), writes=[t_.r0])
        gains.append(t_)
    gfb, gpb, glb = gains
    io128 = P.sbuf("io128", [128, 128], I32)
    io256 = P.sbuf("io256", [128, 256], I32)
    io16 = P.sbuf("io16", [128, 16], F32)
    P.dma("sp", io128.t[:, :], io128_d[:, 0, :], writes=[io128.r0])
    P.dma("sp", io256.t[:, :], io256_d[:, 0, :], writes=[io256.r0])
    P.dma("sp", io16.t[:, :], io16_d[:, 0, 0, :], writes=[io16.r0])
    NG = int(_os.environ.get("K_NG", "14"))

    def dbl(name, shape, dt):
        return [P.sbuf(f"{name}{i}", shape, dt) for i in range(2)]

    hh2 = [P.sbuf(f"hh{i}", [128, D], F32) for i in range(3)]
    ptl2 = [P.sbuf(f"ptl{i}", [128, 256], F32) for i in range(3)]
    xn2 = dbl("xn", [128, D], F32)
    eidx2 = dbl("eidx", [128, 128], I32)
    gte2 = dbl("gte", [128, 128], F32)
    ptb = P.sbuf("ptb", [128, 256], BF16)
    pT = P.sbuf("pT", [128, 2, 128], BF16)
    xnb = P.sbuf("xnb", [128, D], BF16)
    xnT = P.sbuf("xnT", [128, 8, 128], BF16)
    xne = P.sbuf("xne", [128, D], F32)
    xnbe = P.sbuf("xnbe", [128, D], BF16)
    xnTe = P.sbuf("xnTe", [128, 8, 128], BF16)
    qpT = P.sbuf("qpT", [128, 8, 128], BF16)
    junkF = P.sbuf("junkF", [128, D], BF16)
    junkG = P.sbuf("junkG", [128, D], BF16)
    junkE = P.sbuf("junkE", [128, D], BF16)
    sqF = P.sbuf("sqF", [128, 4], F32)
    sqE = P.sbuf("sqE", [128, 4], F32)
    s_all = P.sbuf("s_all", [128, 16, 128], F32)
    stmp = P.sbuf("stmp", [128, 256], F32)
    v12 = P.sbuf("v12", [128, 16, 16], F32)
    i12i = P.sbuf("i12i", [128, 16, 16], I32)
    i12f = P.sbuf("i12f", [128, 16, 16], F32)
    cand = P.sbuf("cand", [128, 8, 256], F32)
    oh = cand
    best = P.sbuf("best", [128, 8, 16], F32)
    posi2 = P.sbuf("posi2", [128, 8, 16], I32)
    abi = P.sbuf("abi", [128, 2, 8, 16], I32)
    abf = P.sbuf("abf", [128, 2, 8, 16], F32)
    isel = P.sbuf("isel", [128, 2, 8, 16], F32)
    ef = P.sbuf("ef", [128, 128], F32)
    bex = P.sbuf("bex", [128, 8, 16], F32)
    bsm = P.sbuf("bsm", [128, 2, 8], F32)
    dots = P.sbuf("dots", [128, 128], F32, nres=128)
    actv = P.sbuf("actv", [128, 128], F32, nres=32)
    a1 = P.sbuf("a1", [128, 128], F32, nres=32)
    ug = P.sbuf("ug", [128, NG, 2 * D], BF16, nres=NG)
    tmpv = P.sbuf("tmpv", [128, 4, D], BF16, nres=4)
    gate = P.sbuf("gate", [128, D], F32)
    tmpg = P.sbuf("tmpg", [128, 512], F32)
    ot = P.sbuf("ot", [128, D], F32)
    ps_t = P.psum("ps_t2", [128, 1024], BF16)
    ps_q = P.psum("ps_q", [128, 2, 512], F32, nres=2)
    ps_c = P.psum("ps_c", [128, 2, 512], F32, nres=2)
    ps_o = P.psum("ps_o", [128, 2, 512], F32, nres=2)
    cnt = {"gi": 0, "tvi": 0}

    def transpose8(src_bf, dst, n):
        for c in range(n):
            P.op("pe", "transpose", dict(out=ps_t.t[:, c * 128:(c + 1) * 128], in_=src_bf.t[:, c * 128:(c + 1) * 128], identity=identb.t[:, :]),
                 [src_bf.r0, identb.r0], [ps_t.r0])
        cast(dst.t[:, 0:n, :], ps_t.t[:, 0:n * 128].rearrange("p (c t) -> p c t", c=n), [ps_t.r0], [dst.r0])

    v12v = v12.t[:].rearrange("p (h c) k -> p h c k", c=2)
    i12v = i12f.t[:].rearrange("p (h c) k -> p h c k", c=2)

    def front(tt):
        par = tt % 2
        hh_, ptl, xn, eidx, gte = hh2[tt % 3], ptl2[tt % 3], xn2[par], eidx2[par], gte2[par]
        P.dma("sp", hh_.t[:, :], h1_s[tt * 128:(tt + 1) * 128, :], writes=[hh_.r0])
        P.dma("sp", ptl.t[:, :], p_d[tt * 128:(tt + 1) * 128, :], writes=[ptl.r0])
        rmsnorm_tile(hh_, gfb, xn, xnb, sqF, junkF)
        yield
        transpose8(xnb, xnT, 8)
        yield
        for hd in range(8):
            bk = hd // 4
            for k in range(8):
                mm(ps_q.t[:, bk, (hd % 4) * 128:(hd % 4 + 1) * 128], wQ.t[:, k, hd * 128:(hd + 1) * 128], xnT.t[:, k, :],
                   (k == 0 and hd % 4 == 0), False, [wQ.r0, xnT.r0], [ps_q.r[bk]])
        for bk in range(2):
            cast(qpT.t[:, bk * 4:(bk + 1) * 4, :], ps_q.t[:, bk, :].rearrange("p (a t) -> p a t", a=4), [ps_q.r[bk]], [qpT.r0])
        for rnd in range(2):
            for h4 in range(4):
                hd = rnd * 4 + h4
                bk = h4 // 2
                mm(ps_c.t[:, bk, (hd % 2) * 256:(hd % 2 + 1) * 256], qpT.t[:, hd, :], kbdb.t[:, :], hd % 2 == 0, False, [qpT.r0, kbdb.r0], [ps_c.r[bk]])
            for bk in range(2):
                cast(s_all.t[:, rnd * 8 + bk * 4:rnd * 8 + (bk + 1) * 4, :], ps_c.t[:, bk, :].rearrange("p (a n) -> p a n", a=4), [ps_c.r[bk]], [s_all.r0])
            yield
        sI = s_all.t[:].bitcast(I32)
        P.op("dve", "tensor_scalar", dict(out=sI, in0=sI, scalar1=-128, scalar2=None, op0=ALU.bitwise_and), [s_all.r0], [s_all.r0])
        P.op("dve", "tensor_tensor", dict(out=sI, in0=sI, in1=io128.t[:, :].unsqueeze(1).broadcast_to([128, 16, 128]), op=ALU.bitwise_or), [s_all.r0, io128.r0], [s_all.r0])
        yield
        for j in range(16):
            P.op("dve", "max", dict(out=v12.t[:, j, 0:8], in_=s_all.t[:, j, :]), [s_all.r0], [v12.r0])
            P.op("dve", "match_replace", dict(out=stmp.t[:, 0:128], in_to_replace=v12.t[:, j, 0:8], in_values=s_all.t[:, j, :], imm_value=-3.0e38),
                 [s_all.r0, v12.r0], [stmp.r0])
            P.op("dve", "max", dict(out=v12.t[:, j, 8:16], in_=stmp.t[:, 0:128]), [stmp.r0], [v12.r0])
            if j % 2 == 1:
                yield
        P.op("dve", "tensor_scalar", dict(out=i12i.t[:], in0=v12.t[:].bitcast(I32), scalar1=127, scalar2=None, op0=ALU.bitwise_and), [v12.r0], [i12i.r0])
        P.op("dve", "tensor_copy", dict(out=i12f.t[:], in_=i12i.t[:]), [i12i.r0], [i12f.r0])
        candv = cand.t[:].rearrange("p h (a b) -> p h a b", a=16)
        P.op("dve", "tensor_tensor", dict(out=candv, in0=v12v[:, :, 0, :].unsqueeze(3).broadcast_to([128, 8, 16, 16]),
                                          in1=v12v[:, :, 1, :].unsqueeze(2).broadcast_to([128, 8, 16, 16]), op=ALU.add), [v12.r0], [cand.r0])
        yield
        cI = cand.t[:].bitcast(I32)
        P.op("dve", "tensor_scalar", dict(out=cI, in0=cI, scalar1=-256, scalar2=None, op0=ALU.bitwise_and), [cand.r0], [cand.r0])
        P.op("dve", "tensor_tensor", dict(out=cI, in0=cI, in1=io256.t[:, :].unsqueeze(1).broadcast_to([128, 8, 256]), op=ALU.bitwise_or), [cand.r0, io256.r0], [cand.r0])
        yield
        for hd in range(8):
            P.op("dve", "max", dict(out=best.t[:, hd, 0:8], in_=cand.t[:, hd, :]), [cand.r0], [best.r0])
            P.op("dve", "match_replace", dict(out=stmp.t[:, :], in_to_replace=best.t[:, hd, 0:8], in_values=cand.t[:, hd, :], imm_value=-3.0e38),
                 [cand.r0, best.r0], [stmp.r0])
            P.op("dve", "max", dict(out=best.t[:, hd, 8:16], in_=stmp.t[:, :]), [stmp.r0], [best.r0])
            if hd % 2 == 1:
                yield
        P.op("dve", "tensor_scalar", dict(out=posi2.t[:], in0=best.t[:].bitcast(I32), scalar1=255, scalar2=None, op0=ALU.bitwise_and), [best.r0], [posi2.r0])
        P.op("dve", "tensor_scalar", dict(out=abi.t[:, 0], in0=posi2.t[:], scalar1=4, scalar2=None, op0=ALU.arith_shift_right), [posi2.r0], [abi.r0])
        P.op("dve", "tensor_scalar", dict(out=abi.t[:, 1], in0=posi2.t[:], scalar1=15, scalar2=None, op0=ALU.bitwise_and), [posi2.r0, abi.r0], [abi.r0])
        P.op("dve", "tensor_copy", dict(out=abf.t[:], in_=abi.t[:]), [abi.r0], [abf.r0])
        yield
        ohv = oh.t[:].rearrange("p h (a b) -> p h a b", a=16)
        for c in range(2):
            P.op("dve", "tensor_tensor", dict(out=ohv, in0=abf.t[:, c].unsqueeze(3).broadcast_to([128, 8, 16, 16]),
                                              in1=io16.t[:, :].unsqueeze(1).unsqueeze(1).broadcast_to([128, 8, 16, 16]), op=ALU.is_equal),
                 [abf.r0, io16.r0], [oh.r0])
            P.op("dve", "tensor_tensor", dict(out=ohv, in0=ohv, in1=i12v[:, :, c, :].unsqueeze(2).broadcast_to([128, 8, 16, 16]), op=ALU.mult),
                 [oh.r0, i12f.r0], [oh.r0])
            P.op("dve", "tensor_reduce", dict(out=isel.t[:, c], in_=ohv, axis=AX.X, op=ALU.add), [oh.r0], [isel.r0])
            yield
        P.op("dve", "scalar_tensor_tensor", dict(out=ef.t[:, :], in0=isel.t[:, 0].rearrange("p h k -> p (h k)"), scalar=128.0,
                                                 in1=isel.t[:, 1].rearrange("p h k -> p (h k)"), op0=ALU.mult, op1=ALU.add), [isel.r0], [ef.r0])
        P.op("dve", "tensor_copy", dict(out=eidx.t[:, :], in_=ef.t[:, :]), [ef.r0], [eidx.r0])
        P.op("dve", "tensor_tensor", dict(out=bex.t[:], in0=best.t[:], in1=best.t[:, :, 0:1].broadcast_to([128, 8, 16]), op=ALU.subtract), [best.r0], [bex.r0])
        P.op("act", "activation", dict(out=bex.t[:], in_=bex.t[:], func=AF.Exp), [bex.r0], [bex.r0])
        yield
        P.op("dve", "tensor_reduce", dict(out=bsm.t[:, 0], in_=bex.t[:], axis=AX.X, op=ALU.add), [bex.r0], [bsm.r0])
        P.op("dve", "reciprocal", dict(out=bsm.t[:, 1], in_=bsm.t[:, 0]), [bsm.r0], [bsm.r0])
        P.op("dve", "tensor_tensor", dict(out=gte.t[:, :].rearrange("p (h k) -> p h k", h=8), in0=bex.t[:],
                                          in1=bsm.t[:, 1].unsqueeze(2).broadcast_to([128, 8, 16]), op=ALU.mult), [bex.r0, bsm.r0], [gte.r0])
        yield

    def gather_group(tt, grp):
        par = tt % 2
        xn, eidx, gte = xn2[par], eidx2[par], gte2[par]
        bufs = []
        for j in range(4):
            slot = grp * 4 + j
            b = cnt["gi"] % NG
            cnt["gi"] += 1
            bufs.append(b)
            P.dma("pool", ug.t[:, b, :], uv_s[:, :], reads=[eidx.r0], writes=[ug.r[b]],
                  indirect=dict(out_offset=None, in_offset=bass.IndirectOffsetOnAxis(ap=eidx.t[:, slot:slot + 1], axis=0)))
            if not _os.environ.get("K_NOCONS"):
                P.op("dve", "scalar_tensor_tensor", dict(out=junkG.t[:, :], in0=ug.t[:, b, 0:D], scalar=1.0, in1=(xnb.t[:, :] if _os.environ.get("K_XNB") else xn.t[:, :]),
                                                         op0=ALU.mult, op1=ALU.mult, accum_out=dots.t[:, slot:slot + 1]),
                     [ug.r[b], xn.r0], [dots.r[slot]])
        if _os.environ.get("K_NOCONS") or _os.environ.get("K_DOTSONLY"):
            return
        if cnt.get("pend") is not None:
            finish_group(*cnt["pend"])
        gs = slice(grp * 4, grp * 4 + 4)
        P.op("act", "activation", dict(out=a1.t[:, gs], in_=dots.t[:, gs], func=AF.Gelu_apprx_tanh), dots.r[grp * 4:grp * 4 + 4], [a1.r[grp]])
        cnt["pend"] = (tt, grp, bufs)
        if grp == 31:
            finish_group(*cnt["pend"])
            cnt["pend"] = None

    def finish_group(tt, grp, bufs):
        gte = gte2[tt % 2]
        gs = slice(grp * 4, grp * 4 + 4)
        P.op("dve", "tensor_tensor", dict(out=actv.t[:, gs], in0=a1.t[:, gs], in1=gte.t[:, gs], op=ALU.mult), [a1.r[grp], gte.r0], [actv.r[grp]])
        for j in range(4):
            slot = grp * 4 + j
            b = bufs[j]
            tb = cnt["tvi"] % 4
            cnt["tvi"] += 1
            P.op("act", "activation", dict(out=tmpv.t[:, tb, :], in_=ug.t[:, b, D:2 * D], func=AF.Copy, scale=actv.t[:, slot:slot + 1]),
                 [ug.r[b], actv.r[grp]], [tmpv.r[tb]])
            for half in range(2):
                mm(ps_o.t[:, half, :], identb.t[:, :], tmpv.t[:, tb, half * 512:(half + 1) * 512], slot == 0, slot == 127,
                   [identb.r0, tmpv.r[tb]], [ps_o.r[half]])

    def epilogue(tt):
        hh_, ptl = hh2[tt % 3], ptl2[tt % 3]
        for half in range(2):
            P.op("dve", "tensor_tensor", dict(out=hh_.t[:, half * 512:(half + 1) * 512], in0=ps_o.t[:, half, :], in1=hh_.t[:, half * 512:(half + 1) * 512], op=ALU.add),
                 [ps_o.r[half], hh_.r0], [hh_.r0])
        yield
        rmsnorm_tile(hh_, gpb, xne, xnbe, sqE, junkE)
        yield
        transpose8(xnbe, xnTe, 8)
        yield
        P.op("act", "copy", dict(out=ptb.t[:, :], in_=ptl.t[:, :]), [ptl.r0], [ptb.r0])
        transpose8(ptb, pT, 2)
        yield
        for half in range(2):
            for k in range(8):
                mm(ps_q.t[:, half, :], xnTe.t[:, k, :], wG.t[:, k, half * 512:(half + 1) * 512], k == 0, k == 7, [xnTe.r0, wG.r0], [ps_q.r[half]])
            P.op("act", "activation", dict(out=gate.t[:, half * 512:(half + 1) * 512], in_=ps_q.t[:, half, :], func=AF.Sigmoid), [ps_q.r[half]], [gate.r0])
            for k in range(2):
                mm(ps_c.t[:, half, :], pT.t[:, k, :], wPs.t[:, k, half * 512:(half + 1) * 512], k == 0, k == 1, [pT.r0, wPs.r0], [ps_c.r[half]])
            P.op("dve", "tensor_tensor", dict(out=tmpg.t[:, :], in0=ps_c.t[:, half, :], in1=gate.t[:, half * 512:(half + 1) * 512], op=ALU.mult),
                 [ps_c.r[half], gate.r0], [tmpg.r0])
            P.op("dve", "tensor_tensor", dict(out=hh_.t[:, half * 512:(half + 1) * 512], in0=hh_.t[:, half * 512:(half + 1) * 512], in1=tmpg.t[:, :], op=ALU.add),
                 [hh_.r0, tmpg.r0], [hh_.r0])
            yield
        rmsnorm_tile(hh_, glb, ot, None, sqE, junkE)
        yield
        P.dma("sp", out_d[tt * 128:(tt + 1) * 128, :], ot.t[:, :], reads=[ot.r0])

    for _ in front(0):
        pass
    for tt in range(NTT):
        nxt = front(tt + 1) if tt + 1 < NTT else iter(())
        for grp in range(32):
            gather_group(tt, grp)
            if grp >= 1:
                next(nxt, None)
        for _ in nxt:
            pass
        for _ in epilogue(tt):
            pass
    P.emit_stage()
    P.pop()

    P.close()
    return nc


def kernel(**inputs):
    consts = make_consts()
    shared = prep_shared(inputs)
    shared.update(consts)
    in_maps = []
    for b in range(8):
        m = dict(shared)
        m["x"] = np.ascontiguousarray(inputs["x"][b])
        m["p"] = np.ascontiguousarray(inputs["p"][0, b])
        m["positions"] = np.ascontiguousarray(inputs["positions"][b].reshape(1, S).astype(np.int32))
        in_maps.append(m)
    nc = build_nc()
    res = run_bass_kernel_spmd(nc, in_maps, core_ids=list(range(8)))
    return np.stack([np.asarray(r["out"]) for r in res.results], axis=0).astype(np.float32)
```

```python
from contextlib import ExitStack
import os as _os
import numpy as np
import ml_dtypes
import concourse.bass as bass
import concourse.mybir as mybir
from concourse.bass_utils import run_bass_kernel_spmd

F32 = mybir.dt.float32
BF16 = mybir.dt.bfloat16
I32 = mybir.dt.int32
AF = mybir.ActivationFunctionType
ALU = mybir.AluOpType
AX = mybir.AxisListType

S = 4096
D = 1024
NQC = 8
NTT = 32
NEG = -30000.0
SCALE = 128 ** -0.5
COMPUTE = ("pe", "dve", "act", "pool")
NOSELF = tuple(x for x in _os.environ.get("K_NOSELF", "").split(",") if x)
NROT = int(_os.environ.get("K_NROT", "8"))
NCH = 65
WCOLS = NCH * 128

C_RX, C_RG, C_Q, C_QS, C_KV, C_KSW, C_M, C_NG = 0, 8, 16, 24, 32, 44, 48, 64


class Res:
    __slots__ = ("w", "r", "name")

    def __init__(self, name=""):
        self.w = None
        self.r = {}
        self.name = name


class TT:
    def __init__(self, t, nres, name):
        self.t = t
        self.r = [Res(f"{name}.{i}") for i in range(nres)]

    @property
    def r0(self):
        return self.r[0]


class Prog:
    def __init__(self, nc):
        self.nc = nc
        self.scopes = [ExitStack()]
        self.streams = {e: [] for e in ("pe", "dve", "act", "pool", "sp")}
        self.cnt = {e: 0 for e in COMPUTE}
        self.seen = {e: {} for e in self.streams}
        self.sems = {}
        for i in range(int(_os.environ.get("K_SEMPAD", "0"))):
            self.scopes[0].enter_context(nc.semaphore(f"s_pad{i}"))
        for e in COMPUTE:
            self.sems[e] = self.scopes[0].enter_context(nc.semaphore(f"s_{e}"))
        self.dq = {}
        self.dma_total = {}
        for q in ("sp", "act", "pool"):
            self.dq[q] = {"n": 0, "sems": []}
            nrot_q = int(_os.environ.get("K_NROT_" + q.upper(), "1" if q == "sp" else str(NROT)))
            self.dq[q]["nrot"] = nrot_q
            for i in range(nrot_q):
                k = ("dma", q, i)
                self.sems[k] = self.scopes[0].enter_context(nc.semaphore(f"s_dma_{q}_{i}"))
                self.dq[q]["sems"].append(k)
                self.dma_total[k] = 0
        self.n_instr = 0

    def push(self):
        self.scopes.append(ExitStack())

    def pop(self):
        self.scopes.pop().close()

    def sbuf(self, name, shape, dtype, nres=1):
        self.n_instr += 0
        self.uid = getattr(self, "uid", 0) + 1
        t = self.scopes[-1].enter_context(self.nc.sbuf_tensor(f"{name}_{self.uid}", list(shape), dtype))
        return TT(t, nres, name)

    def psum(self, name, shape, dtype, nres=1):
        self.uid = getattr(self, "uid", 0) + 1
        t = self.scopes[-1].enter_context(self.nc.psum_tensor(f"{name}_{self.uid}", list(shape), dtype))
        return TT(t, nres, name)

    def _deps(self, eng, reads, writes):
        deps = {}

        def add(tok):
            if tok is None:
                return
            k, v = tok
            if deps.get(k, 0) < v:
                deps[k] = v

        for r in reads:
            add(r.w)
        for w in writes:
            add(w.w)
            for k, v in w.r.items():
                add((k, v))
        out = []
        for k, v in deps.items():
            if k == eng and (eng == "pe" or eng in NOSELF):
                continue
            if self.seen[eng].get(k, 0) >= v:
                continue
            self.seen[eng][k] = v
            out.append((k, v))
        return out

    def _record(self, tok, reads, writes):
        k, v = tok
        for r in reads:
            if r.r.get(k, 0) < v:
                r.r[k] = v
        for w in writes:
            w.w = tok
            w.r = {}

    def op(self, eng, meth, kw, reads=(), writes=()):
        waits = self._deps(eng, reads, writes)
        self.cnt[eng] += 1
        tok = (eng, self.cnt[eng])
        self._record(tok, reads, writes)
        self.streams[eng].append((meth, kw, waits, (eng, 1)))
        self.n_instr += 1

    def dma(self, q, out, in_, reads=(), writes=(), indirect=None, **kw):
        d = self.dq[q]
        i = d["n"]
        d["n"] += 1
        sk = d["sems"][i % d["nrot"]]
        prev = self.dma_total[sk]
        waits = self._deps(q, reads, writes)
        if prev > 0 and self.seen[q].get(sk, 0) < prev:
            self.seen[q][sk] = prev
            waits.append((sk, prev))
        self.dma_total[sk] = prev + 16
        tok = (sk, prev + 16)
        self._record(tok, reads, writes)
        kk = dict(out=out, in_=in_)
        if indirect is not None:
            kk.update(indirect)
        kk.update(kw)
        self.streams[q].append(("indirect_dma_start" if indirect is not None else "dma_start", kk, waits, (sk, 16)))
        self.n_instr += 1
        return tok

    def emit_stage(self):
        targets = [(e, self.cnt[e]) for e in COMPUTE if self.cnt[e] > 0]
        targets += [(k, v) for k, v in self.dma_total.items() if v > 0]
        for e in self.streams:
            ws = []
            for k, v in targets:
                if self.seen[e].get(k, 0) < v:
                    self.seen[e][k] = v
                    ws.append((k, v))
            if ws:
                self.streams[e].append((None, None, ws, None))
        sems = self.sems
        streams = self.streams

        def run(eng_obj, items):
            for meth, kw, waits, inc in items:
                for k, v in waits:
                    eng_obj.wait_ge(sems[k], v)
                if meth is None:
                    continue
                ins = getattr(eng_obj, meth)(**kw)
                ins.then_inc(sems[inc[0]], inc[1])

        with self.nc.Block() as block:
            @block.tensor
            def _(e):
                run(e, streams["pe"])

            @block.vector
            def _(e):
                run(e, streams["dve"])

            @block.scalar
            def _(e):
                run(e, streams["act"])

            @block.gpsimd
            def _(e):
                run(e, streams["pool"])

            @block.sync
            def _(e):
                run(e, streams["sp"])
        self.streams = {e: [] for e in streams}

    def close(self):
        while self.scopes:
            self.scopes.pop().close()


def _bf(a):
    return np.ascontiguousarray(a).astype(ml_dtypes.bfloat16)


def make_consts():
    c = {}
    c["c_identb"] = _bf(np.eye(128, dtype=np.float32))
    c["c_identf"] = np.eye(128, dtype=np.float32)
    half = 64
    inv = (10000.0 ** (-np.arange(half, dtype=np.float32) / np.float32(half))).astype(np.float32)
    c["c_invf"] = np.concatenate([inv, inv]).reshape(128, 1).astype(np.float32)
    c["c_sgn"] = np.concatenate([-np.ones(64), np.ones(64)]).reshape(128, 1).astype(np.float32)
    kp = np.arange(128)[:, None]
    q = np.arange(512)[None, :]
    cb = np.zeros((128, 4, 512), np.float32)
    wb = np.zeros((128, 4, 512), np.float32)
    for r in range(4):
        k = r * 128 + kp
        cb[:, r, :] = np.where(k <= q, 0.0, NEG)
        k2 = (r - 4) * 128 + kp
        wb[:, r, :] = np.where(q - k2 < 512, 0.0, NEG)
    c["c_cb"] = _bf(cb)
    c["c_wb"] = _bf(wb)
    n = (np.arange(2)[None, :, None] * 128 + np.arange(128)[:, None, None])
    t = np.arange(S)[None, None, :]
    cmb = np.where((16 * n + 31 <= t) & (n < 255), 0.0, NEG).astype(np.float32)
    c["c_cmb"] = _bf(cmb)
    j = np.arange(64)[:, None]
    kk = np.arange(S)[None, :]
    c["c_E"] = _bf((kk // 64 == j).astype(np.float32))
    nn = np.arange(256)
    jj = np.arange(64)
    ov = ((16 * nn[:, None] < 64 * jj[None, :] + 64) & (16 * nn[:, None] + 31 >= 64 * jj[None, :]) & (nn[:, None] < 255))
    c["c_ovl"] = _bf(ov.astype(np.float32).reshape(2, 128, 64).transpose(1, 0, 2))
    tt = np.arange(S).reshape(NTT, 128)[:, :, None]
    cur = tt // 64
    j3 = np.arange(64)[None, None, :]
    forced = (j3 == 0) | (j3 == cur) | (j3 == cur - 1)
    valid = (64 * j3 <= tt)
    c["c_fa"] = np.ascontiguousarray(np.where(forced, 1000.0, 0.0).astype(np.float32).transpose(1, 0, 2))
    c["c_na"] = np.ascontiguousarray(np.where(valid, 0.0, -1e30).astype(np.float32).transpose(1, 0, 2))
    c["c_vm"] = np.ascontiguousarray(valid.astype(np.float32).transpose(1, 0, 2))
    c["c_iota128"] = np.ascontiguousarray(np.broadcast_to(np.arange(128, dtype=np.int32)[None, None, :], (128, 16, 128)))
    c["c_iota256"] = np.ascontiguousarray(np.broadcast_to(np.arange(256, dtype=np.int32)[None, None, :], (128, 8, 256)))
    c["c_iota16"] = np.ascontiguousarray(np.broadcast_to(np.arange(16, dtype=np.float32)[None, None, None, :], (128, 8, 16, 16)))
    return c


def pc_layout(v):
    return np.ascontiguousarray(np.asarray(v).reshape(8, 128).T)


def prep_shared(inp):
    m = {}
    def pk(w):
        K_, N_ = w.shape
        return np.ascontiguousarray(w.reshape(K_ // 128, 128, N_).transpose(1, 0, 2))
    m["w_in"] = pk(inp["w_in"][0])
    for k in ("w_a", "w_b", "w_out", "peer_wq", "ple_wg", "ple_wp"):
        m[k] = pk(inp[k][0])
    for k in ("peer_u", "peer_v"):
        m[k] = np.ascontiguousarray(inp[k][0])
    m["lru_wr"] = np.ascontiguousarray(inp["lru_wr"][0].transpose(1, 0, 2))
    m["lru_wi"] = np.ascontiguousarray(inp["lru_wi"][0].transpose(1, 0, 2))
    for k in ("cmp_w1_k", "cmp_w1_v"):
        m[k] = np.ascontiguousarray(inp[k][0].reshape(32, 128, 256).transpose(1, 0, 2))
    for k in ("cmp_w2_k", "cmp_w2_v"):
        m[k] = pk(inp[k][0])
    for k in ("norm_mix", "norm_ffn", "norm_ple"):
        m[k] = np.ascontiguousarray(inp[k][0].reshape(1, D))
    m["norm_final"] = np.ascontiguousarray(inp["norm_final"].reshape(1, D))
    m["conv_w"] = np.ascontiguousarray(inp["conv_w"][0].reshape(4, 8, 128).transpose(2, 1, 0))
    for k in ("conv_b", "lru_br", "lru_bi", "lru_lam"):
        m[k] = pc_layout(inp[k][0])
    for k in ("cmp_b1_k", "cmp_b1_v"):
        m[k] = np.ascontiguousarray(inp[k][0].reshape(2, 128).T)
    m["cmp_b2_k"] = np.ascontiguousarray(inp["cmp_b2_k"][0].reshape(128, 1))
    m["cmp_b2_v"] = np.ascontiguousarray(inp["cmp_b2_v"][0].reshape(1, 128))
    m["cmp_pe_k"] = np.ascontiguousarray(inp["cmp_pe_k"][0].T)
    m["cmp_pe_v"] = np.ascontiguousarray(inp["cmp_pe_v"][0].T)
    keys = inp["peer_keys"][0]
    kbd = np.zeros((128, 256), np.float32)
    kbd[0:64, 0:128] = keys[0].T
    kbd[64:128, 128:256] = keys[1].T
    m["peer_kbd"] = kbd
    return m


def win_segments():
    segs = [(0, 0, 2048), (2048, 2048, 1024)]
    d = 3072
    for h in range(8):
        segs.append((d, 2048 + h * 128 + 64, 64))
        segs.append((d + 64, 2048 + h * 128, 64))
        d += 128
    segs.append((4096, 3072, 1536))
    d = 5632
    for jkv in (2, 4):
        for g in range(2):
            s0 = 3072 + jkv * 256 + g * 128
            segs.append((d, s0 + 64, 64))
            segs.append((d + 64, s0, 64))
            d += 128
    segs.append((6144, 4632, 2048))
    segs.append((8192, 4608, 24))
    return segs


def build_nc(upto="D", debug=False):
    nc = bass.Bass("TRN2", target_bir_lowering=False)
    P = Prog(nc)
    ein = {}

    def IN(name, shape, dt=F32):
        ein[name] = nc.dram_tensor(name, list(shape), dt, kind="ExternalInput").ap()
        return ein[name]

    x_d = IN("x", [S, D])
    p_d = IN("p", [S, 256])
    pos_d = IN("positions", [1, S], I32)
    win_d = IN("w_in", [128, 8, 6680])
    w5_d = [IN(k, [128, 8, D]) for k in ("w_a", "w_b", "w_out", "peer_wq", "ple_wg")]
    wp_d = IN("ple_wp", [128, 2, D])
    u_d = IN("peer_u", [16384, D])
    v_d = IN("peer_v", [16384, D])
    wr_d = IN("lru_wr", [128, 8, 128])
    wi_d = IN("lru_wi", [128, 8, 128])
    w1_d = [IN("cmp_w1_k", [128, 32, 256]), IN("cmp_w1_v", [128, 32, 256])]
    w2_d = [IN("cmp_w2_k", [128, 2, 128]), IN("cmp_w2_v", [128, 2, 128])]
    gm_d = IN("norm_mix", [1, D])
    gf_d = IN("norm_ffn", [1, D])
    gp_d = IN("norm_ple", [1, D])
    gl_d = IN("norm_final", [1, D])
    cw_d = IN("conv_w", [128, 8, 4])
    cbias_d = IN("conv_b", [128, 8])
    br_d = IN("lru_br", [128, 8])
    bi_d = IN("lru_bi", [128, 8])
    lam_d = IN("lru_lam", [128, 8])
    b1_d = [IN("cmp_b1_k", [128, 2]), IN("cmp_b1_v", [128, 2])]
    b2k_d = IN("cmp_b2_k", [128, 1])
    b2v_d = IN("cmp_b2_v", [1, 128])
    pe_d = [IN("cmp_pe_k", [128, 32]), IN("cmp_pe_v", [128, 32])]
    kbd_d = IN("peer_kbd", [128, 256])
    identb_d = IN("c_identb", [128, 128], BF16)
    identf_d = IN("c_identf", [128, 128])
    invf_d = IN("c_invf", [128, 1])
    sgn_d = IN("c_sgn", [128, 1])
    cb_d = IN("c_cb", [128, 4, 512], BF16)
    wb_d = IN("c_wb", [128, 4, 512], BF16)
    cmb_d = IN("c_cmb", [128, 2, S], BF16)
    E_d = IN("c_E", [64, S], BF16)
    ovl_d = IN("c_ovl", [128, 2, 64], BF16)
    fa_d = IN("c_fa", [128, NTT, 64])
    na_d = IN("c_na", [128, NTT, 64])
    vm_d = IN("c_vm", [128, NTT, 64])
    io128_d = IN("c_iota128", [128, 16, 128], I32)
    io256_d = IN("c_iota256", [128, 8, 256], I32)
    io16_d = IN("c_iota16", [128, 8, 16, 16])

    out_d = nc.dram_tensor("out", [S, D], F32, kind="ExternalOutput").ap()

    dbg_n = [0]

    def DBG(tag, tt_, shape, dt=F32):
        if not debug:
            return
        dbg_n[0] += 1
        d = nc.dram_tensor(f"dbg_{tag}", list(shape), dt, kind="ExternalOutput").ap()
        P.dma("sp", d, tt_.t[:], reads=tt_.r)

    def SCR(name, shape, dt):
        kind = "ExternalOutput" if debug else "Internal"
        return nc.dram_tensor(name, list(shape), dt, kind=kind).ap()

    winbf = SCR("s_winbf", [128, 8, WCOLS], BF16)
    w5bf = [SCR(f"s_w5bf{i}", [128, 8, D], BF16) for i in range(5)]
    wpbf = SCR("s_wpbf", [128, 2, D], BF16)
    w1bf = [SCR(f"s_w1bf{i}", [128, 32, 256], BF16) for i in range(2)]
    zfm = SCR("s_zfm", [NCH, 128, S], F32)
    ma_s = SCR("s_ma", [8, 128, S], BF16)
    obt_s = SCR("s_obt", [8, 128, S], BF16)
    h1_s = SCR("s_h1", [S, D], F32)
    uv_s = nc.dram_tensor("s_uv", [16384, 2048], BF16, kind="Internal").ap()

    identb = P.sbuf("identb", [128, 128], BF16)
    identf = P.sbuf("identf", [128, 128], F32)
    P.dma("sp", identb.t[:, :], identb_d, writes=[identb.r0])
    P.dma("sp", identf.t[:, :], identf_d, writes=[identf.r0])

    cast_rr = [0]

    def cast(out, in_, reads, writes, engs=tuple(_os.environ.get("K_CAST", "dve,act").split(","))):
        e = engs[cast_rr[0] % len(engs)]
        cast_rr[0] += 1
        if e == "act":
            P.op("act", "copy", dict(out=out, in_=in_), reads, writes)
        else:
            P.op(e, "tensor_copy", dict(out=out, in_=in_), reads, writes)

    P.push()
    stg = P.sbuf("stg", [128, 2, 4096], F32, nres=2)
    stgb = P.sbuf("stgb", [128, 2, 4096], BF16, nres=2)
    P.op("dve", "memset", dict(ap=stg.t[:, 0, :], constant=0.0), [], [stg.r[0]])
    P.op("dve", "memset", dict(ap=stg.t[:, 1, :], constant=0.0), [], [stg.r[1]])
    pi = [0]

    def piece(loads, n, dst, dshape):
        b = pi[0] % 2
        pi[0] += 1
        for mk, src in loads:
            P.dma("sp", mk(stg.t[:, b, 0:n]), src, writes=[stg.r[b]])
        cast(stgb.t[:, b, 0:n], stg.t[:, b, 0:n], [stg.r[b]], [stgb.r[b]])
        P.dma("pool", dst, dshape(stgb.t[:, b, 0:n]), reads=[stgb.r[b]])

    win_v = win_d
    segs = win_segments()
    for c0 in range(0, WCOLS, 512):
        c1 = min(c0 + 512, WCOLS)
        w = c1 - c0
        loads = []
        for (dcol, scol, ncol) in segs:
            lo, hi = max(dcol, c0), min(dcol + ncol, c1)
            if lo >= hi:
                continue
            so = scol + (lo - dcol)
            loads.append((lambda a, lo=lo, hi=hi, w=w, c0=c0: a.rearrange("p (kc n) -> p kc n", kc=8)[:, :, lo - c0:hi - c0],
                          win_v[:, :, so:so + (hi - lo)]))
        if c1 == WCOLS and 8192 + 24 < WCOLS:
            pass
        piece(loads, 8 * w, winbf[:, :, c0:c1], lambda a, w=w: a.rearrange("p (kc n) -> p kc n", kc=8))
    for i in range(5):
        wv = w5_d[i]
        for hh in range(2):
            piece([(lambda a: a.rearrange("p (kc n) -> p kc n", kc=8), wv[:, :, hh * 512:(hh + 1) * 512])], 4096,
                  w5bf[i][:, :, hh * 512:(hh + 1) * 512], lambda a: a.rearrange("p (kc n) -> p kc n", kc=8))
    piece([(lambda a: a.rearrange("p (kc n) -> p kc n", kc=2), wp_d)], 2048,
          wpbf[:, :, :], lambda a: a.rearrange("p (kc n) -> p kc n", kc=2))
    for i in range(2):
        wv = w1_d[i]
        for hh in range(2):
            piece([(lambda a: a.rearrange("p (l h) -> p l h", l=16), wv[:, hh * 16:(hh + 1) * 16, :])], 4096,
                  w1bf[i][:, hh * 16:(hh + 1) * 16, :], lambda a: a.rearrange("p (l h) -> p l h", l=16))
    ust = P.sbuf("ust", [128, 2, 4096], F32, nres=2)
    uvb = P.sbuf("uvb", [128, 2, 4, 2048], BF16, nres=2)
    u_v = u_d.rearrange("(r p j) d -> r p (j d)", p=128, j=4)
    v_v = v_d.rearrange("(r p j) d -> r p (j d)", p=128, j=4)
    uv_v = uv_s.rearrange("(r p j) c -> r p (j c)", p=128, j=4)
    for rb in range(32):
        ob_ = rb % 2
        for ti_, (src_v, eng_) in enumerate(((u_v, "act"), (v_v, "dve"))):
            P.dma("sp", ust.t[:, ti_, :], src_v[rb], writes=[ust.r[ti_]])
            dst_ = uvb.t[:, ob_, :, ti_ * 1024:(ti_ + 1) * 1024]
            src_ = ust.t[:, ti_, :].rearrange("p (j d) -> p j d", j=4)
            if eng_ == "act":
                P.op("act", "copy", dict(out=dst_, in_=src_), [ust.r[ti_]], [uvb.r[ob_]])
            else:
                P.op("dve", "tensor_copy", dict(out=dst_, in_=src_), [ust.r[ti_]], [uvb.r[ob_]])
        P.dma("pool", uv_v[rb], uvb.t[:, ob_].rearrange("p j c -> p (j c)"), reads=[uvb.r[ob_]])
    P.emit_stage()
    P.pop()
    if upto == "P":
        P.close()
        return nc

    P.push()
    nT = P.sbuf("nT", [128, 8, S], BF16, nres=NTT)
    gbc = P.sbuf("gbc", [128, D], F32)
    xt = P.sbuf("xt", [128, 2, D], F32, nres=2)
    nb = P.sbuf("nb", [128, 2, D], BF16, nres=2)
    junk = P.sbuf("junk", [128, D], F32)
    ss = P.sbuf("ss", [128, NTT], F32, nres=NTT)
    rs = P.sbuf("rs", [128, NTT], F32, nres=NTT)
    tp = P.psum("tp", [128, 2, 1024], BF16, nres=2)
    P.dma("sp", gbc.t[:, :], gm_d.partition_broadcast(128), writes=[gbc.r0])
    for tt in range(NTT):
        b = tt % 2
        P.dma("sp", xt.t[:, b, :], x_d[tt * 128:(tt + 1) * 128, :], writes=[xt.r[b]])
        P.op("act", "activation", dict(out=junk.t[:, :], in_=xt.t[:, b, :], func=AF.Square, accum_out=ss.t[:, tt:tt + 1]),
             reads=[xt.r[b]], writes=[junk.r0, ss.r[tt]])
        P.op("act", "activation", dict(out=rs.t[:, tt:tt + 1], in_=ss.t[:, tt:tt + 1], func=AF.Sqrt, scale=1.0 / D, bias=1e-6),
             reads=[ss.r[tt]], writes=[rs.r[tt]])
        P.op("dve", "reciprocal", dict(out=rs.t[:, tt:tt + 1], in_=rs.t[:, tt:tt + 1]), reads=[rs.r[tt]], writes=[rs.r[tt]])
        P.op("dve", "scalar_tensor_tensor", dict(out=nb.t[:, b, :], in0=xt.t[:, b, :], scalar=rs.t[:, tt:tt + 1], in1=gbc.t[:, :],
                                                 op0=ALU.mult, op1=ALU.mult), reads=[xt.r[b], rs.r[tt], gbc.r0], writes=[nb.r[b]])
        for c in range(8):
            P.op("pe", "transpose", dict(out=tp.t[:, b, c * 128:(c + 1) * 128], in_=nb.t[:, b, c * 128:(c + 1) * 128], identity=identb.t[:, :]),
                 reads=[nb.r[b], identb.r0], writes=[tp.r[b]])
        cast(nT.t[:, :, tt * 128:(tt + 1) * 128], tp.t[:, b, :].rearrange("p (c t) -> p c t", c=8), [tp.r[b]], [nT.r[tt]])

    wbuf = P.sbuf("wbuf", [128, 2, 8, 512], BF16, nres=2)
    zst = P.sbuf("zst", [128, 2, S], F32, nres=2)
    pz = P.psum("pz", [128, 4, 512], F32, nres=4)
    nTall = list(nT.r)
    zi = 0
    pzi = 0
    for wpi, c0 in enumerate(range(0, WCOLS, 512)):
        c1 = min(c0 + 512, WCOLS)
        wb_ = wpi % 2
        P.dma("sp", wbuf.t[:, wb_, :, 0:c1 - c0], winbf[:, :, c0:c1], writes=[wbuf.r[wb_]])
        for cc in range((c1 - c0) // 128):
            ch = c0 // 128 + cc
            M = 24 if ch == C_NG else 128
            zb = zi % 2
            zi += 1
            for qc in range(NQC):
                pb = pzi % 4
                pzi += 1
                for kc in range(8):
                    P.op("pe", "matmul", dict(out=pz.t[0:M, pb, :], lhsT=wbuf.t[:, wb_, kc, cc * 128:cc * 128 + M],
                                              rhs=nT.t[:, kc, qc * 512:(qc + 1) * 512], start=(kc == 0), stop=(kc == 7),
                                              skip_group_check=True),
                         reads=[wbuf.r[wb_]] + nTall[qc * 4:(qc + 1) * 4], writes=[pz.r[pb]])
                dst = zst.t[0:M, zb, qc * 512:(qc + 1) * 512]
                src = pz.t[0:M, pb, :]
                if C_RG <= ch < C_RG + 8:
                    P.op("act", "activation", dict(out=dst, in_=src, func=AF.Gelu_apprx_tanh), [pz.r[pb]], [zst.r[zb]])
                elif ch >= C_M:
                    P.op("act", "activation", dict(out=dst, in_=src, func=AF.Sigmoid), [pz.r[pb]], [zst.r[zb]])
                else:
                    P.op("dve", "tensor_copy", dict(out=dst, in_=src), [pz.r[pb]], [zst.r[zb]])
            P.dma("pool", zfm[ch, 0:M, :], zst.t[0:M, zb, :], reads=[zst.r[zb]])
    P.emit_stage()
    P.pop()
    if upto == "A":
        P.close()
        return nc

    P.push()
    cw = P.sbuf("cw", [128, 8, 4], F32)
    cbias = P.sbuf("cbias", [128, 8], F32)
    brs = P.sbuf("brs", [128, 8], F32)
    bis = P.sbuf("bis", [128, 8], F32)
    lam = P.sbuf("lam", [128, 8], F32)
    m8 = P.sbuf("m8", [128, 8], F32)
    m16 = P.sbuf("m16", [128, 8], F32)
    wst = P.sbuf("wst", [128, 2, 8, 128], F32, nres=2)
    wrb = P.sbuf("wrb", [128, 8, 128], BF16)
    wib = P.sbuf("wib", [128, 8, 128], BF16)
    for (t_, d_) in ((cw, cw_d), (cbias, cbias_d), (brs, br_d), (bis, bi_d), (lam, lam_d)):
        P.dma("sp", t_.t[:], d_, writes=[t_.r0])
    P.dma("sp", wst.t[:, 0], wr_d, writes=[wst.r[0]])
    P.dma("sp", wst.t[:, 1], wi_d, writes=[wst.r[1]])
    P.op("dve", "tensor_copy", dict(out=wrb.t[:, :, :], in_=wst.t[:, 0]), [wst.r[0]], [wrb.r0])
    P.op("dve", "tensor_copy", dict(out=wib.t[:, :, :], in_=wst.t[:, 1]), [wst.r[1]], [wib.r0])
    P.op("act", "activation", dict(out=m8.t[:, :], in_=lam.t[:, :], func=AF.Exp, scale=-1.0), [lam.r0], [m8.r0])
    P.op("act", "activation", dict(out=m8.t[:, :], in_=m8.t[:, :], func=AF.Ln, bias=1.0), [m8.r0], [m8.r0])
    P.op("dve", "tensor_scalar", dict(out=m16.t[:, :], in0=m8.t[:, :], scalar1=-16.0, scalar2=None, op0=ALU.mult), [m8.r0], [m16.r0])
    P.op("dve", "tensor_scalar", dict(out=m8.t[:, :], in0=m8.t[:, :], scalar1=-8.0, scalar2=None, op0=ALU.mult), [m8.r0, m16.r0], [m8.r0])
    zx = P.sbuf("zx", [128, S + 3], F32)
    gz = P.sbuf("gz", [128, S], F32)
    xa = P.sbuf("xa", [128, S], F32)
    xab = P.sbuf("xab", [128, S], BF16)
    rr = P.sbuf("rr", [128, S], F32)
    ig = P.sbuf("ig", [128, S], F32)
    t1 = P.sbuf("t1", [128, S], F32)
    t2 = P.sbuf("t2", [128, S], F32)
    mab = P.sbuf("mab", [128, S], BF16)
    pg = P.psum("pg", [128, 4, 512], F32, nres=4)
    P.op("dve", "memset", dict(ap=zx.t[:, 0:3], constant=0.0), [], [zx.r0])
    pgi = 0
    for ch in range(8):
        P.dma("sp", zx.t[:, 3:S + 3], zfm[C_RX + ch], writes=[zx.r0])
        P.dma("sp", gz.t[:, :], zfm[C_RG + ch], writes=[gz.r0])
        P.op("dve", "tensor_scalar", dict(out=xa.t[:, :], in0=zx.t[:, 0:S], scalar1=cw.t[:, ch, 0:1], scalar2=cbias.t[:, ch:ch + 1],
                                          op0=ALU.mult, op1=ALU.add), [zx.r0, cw.r0, cbias.r0], [xa.r0])
        for k in range(1, 4):
            P.op("dve", "scalar_tensor_tensor", dict(out=xa.t[:, :], in0=zx.t[:, k:k + S], scalar=cw.t[:, ch, k:k + 1], in1=xa.t[:, :],
                                                     op0=ALU.mult, op1=ALU.add), [zx.r0, cw.r0, xa.r0], [xa.r0])
        P.op("act", "copy", dict(out=xab.t[:, :], in_=xa.t[:, :]), [xa.r0], [xab.r0])
        for (wgt, bia, dst) in ((wrb, brs, rr), (wib, bis, ig)):
            for qc in range(NQC):
                pb = pgi % 4
                pgi += 1
                P.op("pe", "matmul", dict(out=pg.t[:, pb, :], lhsT=wgt.t[:, ch, :], rhs=xab.t[:, qc * 512:(qc + 1) * 512], start=True, stop=True,
                                          skip_group_check=True), [wgt.r0, xab.r0], [pg.r[pb]])
                P.op("act", "activation", dict(out=dst.t[:, qc * 512:(qc + 1) * 512], in_=pg.t[:, pb, :], func=AF.Sigmoid, bias=bia.t[:, ch:ch + 1]),
                     [pg.r[pb], bia.r0], [dst.r0])
        P.op("act", "activation", dict(out=t1.t[:, :], in_=rr.t[:, :], func=AF.Exp, scale=m8.t[:, ch:ch + 1]), [rr.r0, m8.r0], [t1.r0])
        P.op("act", "activation", dict(out=t2.t[:, :], in_=rr.t[:, :], func=AF.Exp, scale=m16.t[:, ch:ch + 1]), [rr.r0, m16.r0], [t2.r0])
        P.op("act", "activation", dict(out=t2.t[:, :], in_=t2.t[:, :], func=AF.Sqrt, scale=-1.0, bias=1.0), [t2.r0], [t2.r0])
        P.op("pool", "tensor_tensor", dict(out=ig.t[:, :], in0=ig.t[:, :], in1=xa.t[:, :], op=ALU.mult), [ig.r0, xa.r0], [ig.r0])
        P.op("pool", "tensor_tensor", dict(out=ig.t[:, :], in0=ig.t[:, :], in1=t2.t[:, :], op=ALU.mult), [ig.r0, t2.r0], [ig.r0])
        P.op("dve", "tensor_tensor_scan", dict(out=rr.t[:, :], data0=t1.t[:, :], data1=ig.t[:, :], initial=0.0, op0=ALU.mult, op1=ALU.add),
             [t1.r0, ig.r0, rr.r0], [rr.r0])
        P.op("dve", "tensor_tensor", dict(out=mab.t[:, :], in0=rr.t[:, :], in1=gz.t[:, :], op=ALU.mult), [rr.r0, gz.r0], [mab.r0])
        P.dma("pool", ma_s[ch], mab.t[:, :], reads=[mab.r0])
    P.emit_stage()
    P.pop()
    if upto == "B":
        P.close()
        return nc

    P.push()
    cosT = P.sbuf("cosT", [128, S], F32)
    sinT = P.sbuf("sinT", [128, S], F32)
    P.push()
    posi = P.sbuf("posi", [128, S], I32)
    tq_ = P.sbuf("tq", [128, S], F32)
    tk_ = P.sbuf("tk", [128, S], F32)
    ki_ = P.sbuf("ki", [128, S], I32)
    invf = P.sbuf("invf", [128, 1], F32)
    sgn = P.sbuf("sgn", [128, 1], F32)
    P.dma("sp", posi.t[:, :], pos_d.partition_broadcast(128), writes=[posi.r0])
    P.dma("sp", invf.t[:, :], invf_d, writes=[invf.r0])
    P.dma("sp", sgn.t[:, :], sgn_d, writes=[sgn.r0])
    P.op("dve", "tensor_copy", dict(out=tq_.t[:, :], in_=posi.t[:, :]), [posi.r0], [tq_.r0])
    P.op("dve", "tensor_scalar", dict(out=tq_.t[:, :], in0=tq_.t[:, :], scalar1=invf.t[:, 0:1], scalar2=float(1.0 / (2 * np.pi)),
                                      op0=ALU.mult, op1=ALU.mult), [tq_.r0, invf.r0], [tq_.r0])
    TWO_PI = float(2 * np.pi * (1 - 1e-6))
    for which, dstT in (("sin", sinT), ("cos", cosT)):
        if which == "cos":
            P.op("dve", "tensor_scalar", dict(out=tq_.t[:, :], in0=tq_.t[:, :], scalar1=0.25, scalar2=None, op0=ALU.add), [tq_.r0], [tq_.r0])
        P.op("dve", "tensor_copy", dict(out=ki_.t[:, :], in_=tq_.t[:, :]), [tq_.r0], [ki_.r0])
        P.op("dve", "tensor_copy", dict(out=tk_.t[:, :], in_=ki_.t[:, :]), [ki_.r0], [tk_.r0])
        P.op("dve", "tensor_tensor", dict(out=tk_.t[:, :], in0=tq_.t[:, :], in1=tk_.t[:, :], op=ALU.subtract), [tq_.r0, tk_.r0], [tk_.r0])
        P.op("act", "activation", dict(out=dstT.t[:, :], in_=tk_.t[:, :], func=AF.Sin, scale=TWO_PI), [tk_.r0], [dstT.r0])
    P.op("dve", "tensor_scalar", dict(out=sinT.t[:, :], in0=sinT.t[:, :], scalar1=sgn.t[:, 0:1], scalar2=None, op0=ALU.mult), [sinT.r0, sgn.r0], [sinT.r0])
    P.emit_stage()
    P.pop()

    ksT = P.sbuf("ksT", [128, S], BF16)
    kwT = P.sbuf("kwT", [128, S], BF16)
    vsa = P.sbuf("vsa", [128, NTT, 129], BF16)
    vwa = P.sbuf("vwa", [128, NTT, 129], BF16)
    kcT = P.sbuf("kcT", [128, 256], BF16)
    vca = P.sbuf("vca", [128, 2, 193], BF16)
    E_s = P.sbuf("E_s", [64, S], BF16)
    cb_s = P.sbuf("cb_s", [128, 4, 512], BF16)
    wb_s = P.sbuf("wb_s", [128, 4, 512], BF16)
    b1s = [P.sbuf(f"b1s{i}", [128, 2], F32) for i in range(2)]
    pes = [P.sbuf(f"pes{i}", [128, 32], F32) for i in range(2)]
    b2k = P.sbuf("b2k", [128, 1], F32)
    b2vb = P.sbuf("b2vb", [128, 128], F32)
    ps_s = P.psum("ps_s", [128, 2, 512], F32, nres=2)
    ps_acc = P.psum("ps_acc", [128, 4, 512], F32, nres=4)
    ps_trb = P.psum("ps_trb", [128, 1024], BF16)
    ps_trf = P.psum("ps_trf", [128, 512], F32)
    P.dma("sp", E_s.t[:, :], E_d, writes=[E_s.r0])
    P.dma("sp", cb_s.t[:], cb_d, writes=[cb_s.r0])
    P.dma("sp", wb_s.t[:], wb_d, writes=[wb_s.r0])
    for i in range(2):
        P.dma("sp", b1s[i].t[:, :], b1_d[i], writes=[b1s[i].r0])
        P.dma("sp", pes[i].t[:, :], pe_d[i], writes=[pes[i].r0])
    P.dma("sp", b2k.t[:, :], b2k_d, writes=[b2k.r0])
    P.dma("sp", b2vb.t[:, :], b2v_d.partition_broadcast(128), writes=[b2vb.r0])
    P.op("dve", "memset", dict(ap=vsa.t[:, :, 128:129], constant=1.0), [], [vsa.r0])
    P.op("dve", "memset", dict(ap=vwa.t[:, :, 128:129], constant=1.0), [], [vwa.r0])
    srot = [0]

    def sbank():
        srot[0] += 1
        return srot[0] % 2

    def mm(out, lhsT, rhs, start, stop, reads, writes):
        P.op("pe", "matmul", dict(out=out, lhsT=lhsT, rhs=rhs, start=start, stop=stop, skip_group_check=True), reads, writes)

    for g in range(2):
        P.push()
        ld = P.sbuf("ld", [128, 2, S], F32, nres=2)
        lohi = P.sbuf("lohi", [128, 2, S], BF16, nres=2)
        w1s = P.sbuf("w1s", [128, 32, 256], BF16)
        w2f = P.sbuf("w2f", [128, 2, 128], F32)
        w2s = P.sbuf("w2s", [128, 2, 128], BF16)
        hidT = P.sbuf("hidT", [128, 2, 256], BF16)
        vb = P.sbuf("vb", [128, S], BF16)
        P.op("dve", "memset", dict(ap=kcT.t[:, :], constant=0.0), [], [kcT.r0])
        P.op("dve", "memset", dict(ap=vca.t[:], constant=0.0), [], [vca.r0])
        P.op("dve", "memset", dict(ap=vca.t[:, :, 128:129], constant=1.0), [vca.r0], [vca.r0])
        P.dma("sp", vca.t[:, :, 129:193], ovl_d, writes=[vca.r0])
        for kvi in range(2):
            ch = C_KV + kvi * 2 + g
            P.dma("sp", ld.t[:, 0, :], zfm[ch], writes=[ld.r[0]])
            P.dma("sp", w1s.t[:], w1bf[kvi], writes=[w1s.r0])
            P.dma("sp", w2f.t[:], w2_d[kvi], writes=[w2f.r0])
            P.op("dve", "tensor_copy", dict(out=w2s.t[:], in_=w2f.t[:]), [w2f.r0], [w2s.r0])
            for v_ in range(2):
                P.op("dve", "tensor_tensor", dict(out=lohi.t[:, v_, :].rearrange("p (n l) -> p n l", l=16),
                                                  in0=ld.t[:, 0, :].rearrange("p (n l) -> p n l", l=16),
                                                  in1=pes[kvi].t[:, v_ * 16:(v_ + 1) * 16].unsqueeze(1).broadcast_to([128, 256, 16]),
                                                  op=ALU.add), [ld.r[0], pes[kvi].r0], [lohi.r[v_]])
            for hc in range(2):
                for l in range(32):
                    v_ = 0 if l < 16 else 1
                    mm(ps_s.t[:, hc, 0:255], w1s.t[:, l, hc * 128:(hc + 1) * 128], lohi.t[:, v_, l:l + 16 * 254 + 1:16],
                       l == 0, l == 31, [w1s.r0, lohi.r[v_]], [ps_s.r[hc]])
                P.op("act", "activation", dict(out=hidT.t[:, hc, 0:255], in_=ps_s.t[:, hc, 0:255], func=AF.Gelu_apprx_tanh,
                                               bias=b1s[kvi].t[:, hc:hc + 1]), [ps_s.r[hc], b1s[kvi].r0], [hidT.r0])
            if kvi == 0:
                for hc in range(2):
                    mm(ps_s.t[:, 0, 0:255], w2s.t[:, hc, :], hidT.t[:, hc, 0:255], hc == 0, hc == 1, [w2s.r0, hidT.r0], [ps_s.r[0]])
                P.op("dve", "tensor_scalar", dict(out=kcT.t[:, 0:255], in0=ps_s.t[:, 0, 0:255], scalar1=b2k.t[:, 0:1], scalar2=None, op0=ALU.add),
                     [ps_s.r[0], b2k.r0], [kcT.r0])
            else:
                for nch in range(2):
                    rows = 128 if nch == 0 else 127
                    for hc in range(2):
                        mm(ps_acc.t[0:rows, nch, 0:128], hidT.t[:, hc, nch * 128:nch * 128 + rows], w2s.t[:, hc, :], hc == 0, hc == 1,
                           [w2s.r0, hidT.r0], [ps_acc.r[nch]])
                    P.op("dve", "tensor_tensor", dict(out=vca.t[0:rows, nch, 0:128], in0=ps_acc.t[0:rows, nch, 0:128], in1=b2vb.t[0:rows, :], op=ALU.add),
                         [ps_acc.r[nch], b2vb.r0], [vca.r0])
        for (kch, swch, dstK) in ((C_KV + 4 + g, C_KSW + g, ksT), (C_KV + 8 + g, C_KSW + 2 + g, kwT)):
            P.dma("sp", ld.t[:, 0, :], zfm[kch], writes=[ld.r[0]])
            P.dma("sp", ld.t[:, 1, :], zfm[swch], writes=[ld.r[1]])
            if debug and g == 1 and dstK is ksT:
                DBG("g1_ld_raw", ld, [128, 2, S])
                DBG("g1_cosT", cosT, [128, S])
                DBG("g1_sinT", sinT, [128, S])
            P.op("dve", "tensor_tensor", dict(out=ld.t[:, 0, :], in0=ld.t[:, 0, :], in1=cosT.t[:, :], op=ALU.mult), [ld.r[0], cosT.r0], [ld.r[0]])
            P.op("pool", "tensor_tensor", dict(out=ld.t[:, 1, :], in0=ld.t[:, 1, :], in1=sinT.t[:, :], op=ALU.mult), [ld.r[1], sinT.r0], [ld.r[1]])
            if debug and g == 1 and dstK is ksT:
                DBG("g1_ld_mul", ld, [128, 2, S])
            P.op("dve", "tensor_tensor", dict(out=dstK.t[:, :], in0=ld.t[:, 0, :], in1=ld.t[:, 1, :], op=ALU.add), [ld.r[0], ld.r[1]], [dstK.r0])
        for (vch, dstV) in ((C_KV + 6 + g, vsa), (C_KV + 10 + g, vwa)):
            P.dma("sp", ld.t[:, 0, :], zfm[vch], writes=[ld.r[0]])
            P.op("act", "copy", dict(out=vb.t[:, :], in_=ld.t[:, 0, :]), [ld.r[0]], [vb.r0])
            for t4 in range(8):
                for a in range(4):
                    tt = t4 * 4 + a
                    P.op("pe", "transpose", dict(out=ps_trb.t[:, a * 128:(a + 1) * 128], in_=vb.t[:, tt * 128:(tt + 1) * 128], identity=identb.t[:, :]),
                         [vb.r0, identb.r0], [ps_trb.r0])
                cast(dstV.t[:, t4 * 4:(t4 + 1) * 4, 0:128], ps_trb.t[:, 0:512].rearrange("p (a d) -> p a d", a=4), [ps_trb.r0], [dstV.r0])
        P.emit_stage()
        P.pop()
        if upto == "C0":
            P.close()
            return nc

        P.push()
        zq = P.sbuf("zq", [128, 4, 512], F32, nres=4)
        zqs = P.sbuf("zqs", [128, 4, 512], F32, nres=4)
        qn = P.sbuf("qn", [128, 4, 512], BF16)
        qr = P.sbuf("qr", [128, 4, 512], BF16)
        cmbq = P.sbuf("cmbq", [128, 2, 512], BF16)
        fa = P.sbuf("fa", [128, 4, 64], F32)
        na = P.sbuf("na", [128, 4, 64], F32)
        vm = P.sbuf("vm", [128, 4, 64], F32)
        gTs = P.sbuf("gTs", [24, 512], F32)
        gts = P.sbuf("gts", [128, 4, 24], F32)
        pc = P.sbuf("pc", [128, 2, 2, 512], BF16, nres=2)
        pt = P.sbuf("pt", [128, 3, 512], BF16, nres=3)
        oacc = P.sbuf("oacc", [128, 4, 4, 128], F32)
        imp = P.sbuf("imp", [128, 4, 64], F32)
        impp = P.sbuf("impp", [128, 64], F32)
        imp2 = P.sbuf("imp2", [128, 64], F32)
        m8a = P.sbuf("m8a", [128, 8], F32)
        m8b = P.sbuf("m8b", [128, 8], F32)
        msel = P.sbuf("msel", [128, 64], F32)
        bmb = P.sbuf("bmb", [128, 64], BF16)
        biasT = P.sbuf("biasT", [64, 512], BF16)
        sm = P.sbuf("sm", [128, 8], F32)
        obb = P.sbuf("obb", [128, 4, 4, 128], BF16)
        obT = P.sbuf("obT", [128, 4, 512], BF16)
        ptr = [0]
        aset = [0]
        for qc in range(NQC):
            q0 = qc * 512
            for hq in range(4):
                P.dma("sp", zq.t[:, hq, :], zfm[C_Q + 4 * g + hq, :, q0:q0 + 512], writes=[zq.r[hq]])
                P.dma("sp", zqs.t[:, hq, :], zfm[C_QS + 4 * g + hq, :, q0:q0 + 512], writes=[zqs.r[hq]])
            P.dma("sp", cmbq.t[:], cmb_d[:, :, q0:q0 + 512], writes=[cmbq.r0])
            P.dma("sp", fa.t[:], fa_d[:, qc * 4:qc * 4 + 4, :], writes=[fa.r0])
            P.dma("sp", na.t[:], na_d[:, qc * 4:qc * 4 + 4, :], writes=[na.r0])
            P.dma("sp", vm.t[:], vm_d[:, qc * 4:qc * 4 + 4, :], writes=[vm.r0])
            P.dma("sp", gTs.t[:, :], zfm[C_NG, 0:24, q0:q0 + 512], writes=[gTs.r0])
            P.op("act", "copy", dict(out=qn.t[:], in_=zq.t[:]), zq.r, [qn.r0])
            cs = cosT.t[:, q0:q0 + 512].unsqueeze(1).broadcast_to([128, 4, 512])
            sn = sinT.t[:, q0:q0 + 512].unsqueeze(1).broadcast_to([128, 4, 512])
            P.op("dve", "tensor_tensor", dict(out=zq.t[:], in0=zq.t[:], in1=cs, op=ALU.mult), zq.r + [cosT.r0], zq.r)
            P.op("pool", "tensor_tensor", dict(out=zqs.t[:], in0=zqs.t[:], in1=sn, op=ALU.mult), zqs.r + [sinT.r0], zqs.r)
            P.op("dve", "tensor_tensor", dict(out=qr.t[:], in0=zq.t[:], in1=zqs.t[:], op=ALU.add), zq.r + zqs.r, [qr.r0])
            for qt in range(4):
                P.op("pe", "transpose", dict(out=ps_trf.t[:, qt * 24:(qt + 1) * 24], in_=gTs.t[0:24, qt * 128:(qt + 1) * 128], identity=identf.t[0:24, 0:24]),
                     [gTs.r0, identf.r0], [ps_trf.r0])
            P.op("dve", "tensor_copy", dict(out=gts.t[:].rearrange("p a c -> p (a c)"), in_=ps_trf.t[:, 0:96]), [ps_trf.r0], [gts.r0])

            def post(hh, qt, bank, off, br, first, aux=None):
                h = 4 * g + hh
                accv = ps_acc.t[:, bank, :]
                P.op("dve", "tensor_scalar", dict(out=sm.t[:, 0:1], in0=accv[:, off + 128:off + 129], scalar1=1e-30, scalar2=None, op0=ALU.max),
                     [ps_acc.r[bank]], [sm.r0])
                P.op("dve", "reciprocal", dict(out=sm.t[:, 1:2], in_=sm.t[:, 0:1]), [sm.r0], [sm.r0])
                P.op("dve", "tensor_tensor", dict(out=sm.t[:, 2:3], in0=sm.t[:, 1:2], in1=gts.t[:, qt, h * 3 + br:h * 3 + br + 1], op=ALU.mult),
                     [sm.r0, gts.r0], [sm.r0])
                if first:
                    P.op("dve", "tensor_scalar", dict(out=oacc.t[:, qt, hh, :], in0=accv[:, off:off + 128], scalar1=sm.t[:, 2:3], scalar2=None, op0=ALU.mult),
                         [ps_acc.r[bank], sm.r0], [oacc.r0])
                else:
                    P.op("dve", "scalar_tensor_tensor", dict(out=oacc.t[:, qt, hh, :], in0=accv[:, off:off + 128], scalar=sm.t[:, 2:3], in1=oacc.t[:, qt, hh, :],
                                                             op0=ALU.mult, op1=ALU.add), [ps_acc.r[bank], sm.r0, oacc.r0], [oacc.r0])
                if aux is not None:
                    if hh == 0:
                        P.op("dve", "tensor_scalar", dict(out=imp.t[:, qt, :], in0=accv[:, off + 129:off + 193], scalar1=sm.t[:, 1:2], scalar2=None, op0=ALU.mult),
                             [ps_acc.r[bank], sm.r0], [imp.r0])
                    else:
                        P.op("dve", "scalar_tensor_tensor", dict(out=imp.t[:, qt, :], in0=accv[:, off + 129:off + 193], scalar=sm.t[:, 1:2], in1=imp.t[:, qt, :],
                                                                 op0=ALU.mult, op1=ALU.add), [ps_acc.r[bank], sm.r0, imp.r0], [imp.r0])

            if (g, qc) in ((0, 0), (1, 1)):
                tg = f"g{g}q{qc}"
                DBG(tg + "_gts", gts, [128, 4, 24]); DBG(tg + "_qn", qn, [128, 4, 512], BF16); DBG(tg + "_qr", qr, [128, 4, 512], BF16)
                DBG(tg + "_kcT", kcT, [128, 256], BF16); DBG(tg + "_vca", vca, [128, 2, 193], BF16)
                DBG(tg + "_ksT", ksT, [128, S], BF16); DBG(tg + "_kwT", kwT, [128, S], BF16)
                DBG(tg + "_vsa", vsa, [128, NTT, 129], BF16); DBG(tg + "_vwa", vwa, [128, NTT, 129], BF16)
            n_nch = 2 if (q0 + 511 >= 16 * 128 + 31) else 1
            for hh in range(4):
                pcb = hh % 2
                for nch in range(n_nch):
                    sb = sbank()
                    mm(ps_s.t[:, sb, :], kcT.t[:, nch * 128:(nch + 1) * 128], qn.t[:, hh, :], True, False, [kcT.r0, qn.r0], [ps_s.r[sb]])
                    mm(ps_s.t[:, sb, :], identb.t[:, :], cmbq.t[:, nch, :], False, True, [identb.r0, cmbq.r0], [ps_s.r[sb]])
                    P.op("act", "activation", dict(out=pc.t[:, pcb, nch, :], in_=ps_s.t[:, sb, :], func=AF.Exp, scale=SCALE), [ps_s.r[sb]], [pc.r[pcb]])
                aset[0] ^= 1
                started = {}
                for qt in range(4):
                    bank = aset[0] * 2 + qt // 2
                    off = (qt % 2) * 193
                    for nch in range(n_nch):
                        mm(ps_acc.t[:, bank, off:off + 193], pc.t[:, pcb, nch, qt * 128:(qt + 1) * 128], vca.t[:, nch, :],
                           bank not in started, False, [pc.r[pcb], vca.r0], [ps_acc.r[bank]])
                        started[bank] = 1
                for qt in range(4):
                    post(hh, qt, aset[0] * 2 + qt // 2, (qt % 2) * 193, 0, True, aux=True)
            if (g, qc) in ((0, 0), (1, 1)):
                DBG(tg + "_oacc_c", oacc, [128, 4, 4, 128]); DBG(tg + "_imp", imp, [128, 4, 64])
            for qt in range(4):
                P.op("dve", "tensor_tensor", dict(out=impp.t[:, :], in0=imp.t[:, qt, :], in1=fa.t[:, qt, :], op=ALU.max), [imp.r0, fa.r0], [impp.r0])
                P.op("dve", "tensor_tensor", dict(out=impp.t[:, :], in0=impp.t[:, :], in1=na.t[:, qt, :], op=ALU.add), [impp.r0, na.r0], [impp.r0])
                P.op("dve", "max", dict(out=m8a.t[:, :], in_=impp.t[:, :]), [impp.r0], [m8a.r0])
                P.op("dve", "match_replace", dict(out=imp2.t[:, :], in_to_replace=m8a.t[:, :], in_values=impp.t[:, :], imm_value=-3.0e38),
                     [impp.r0, m8a.r0], [imp2.r0])
                P.op("dve", "max", dict(out=m8b.t[:, :], in_=imp2.t[:, :]), [imp2.r0], [m8b.r0])
                P.op("dve", "scalar_tensor_tensor", dict(out=msel.t[:, :], in0=impp.t[:, :], scalar=m8b.t[:, 7:8], in1=vm.t[:, qt, :],
                                                         op0=ALU.is_ge, op1=ALU.mult), [impp.r0, m8b.r0, vm.r0], [msel.r0])
                P.op("dve", "tensor_scalar", dict(out=bmb.t[:, :], in0=msel.t[:, :], scalar1=-NEG, scalar2=NEG, op0=ALU.mult, op1=ALU.add),
                     [msel.r0], [bmb.r0])
                P.op("pe", "transpose", dict(out=ps_trb.t[0:64, qt * 128:(qt + 1) * 128], in_=bmb.t[:, :], identity=identb.t[:, :]),
                     [bmb.r0, identb.r0], [ps_trb.r0])
            P.op("dve", "tensor_copy", dict(out=biasT.t[:, :], in_=ps_trb.t[0:64, 0:512]), [ps_trb.r0], [biasT.r0])
            if (g, qc) in ((0, 0), (1, 1)):
                DBG(tg + "_biasT", biasT, [64, 512], BF16)
            for br in (2, 1):
                for hh in range(4):
                    aset[0] ^= 1
                    started = {}
                    if br == 1:
                        kts = list(range(0, qc * 4 + 4))
                    else:
                        kts = list(range(max(0, qc * 4 - 4), qc * 4 + 4))
                    vsrc = vsa if br == 1 else vwa

                    def emit_scores(kt):
                        r = kt - qc * 4
                        sb = sbank()
                        if br == 1:
                            mm(ps_s.t[:, sb, :], ksT.t[:, kt * 128:(kt + 1) * 128], qr.t[:, hh, :], True, False, [ksT.r0, qr.r0], [ps_s.r[sb]])
                            mm(ps_s.t[:, sb, :], E_s.t[0:64, kt * 128:(kt + 1) * 128], biasT.t[0:64, :], False, r < 0, [E_s.r0, biasT.r0], [ps_s.r[sb]])
                            if r >= 0:
                                mm(ps_s.t[:, sb, :], identb.t[:, :], cb_s.t[:, r, :], False, True, [identb.r0, cb_s.r0], [ps_s.r[sb]])
                        else:
                            mm(ps_s.t[:, sb, :], kwT.t[:, kt * 128:(kt + 1) * 128], qr.t[:, hh, :], True, False, [kwT.r0, qr.r0], [ps_s.r[sb]])
                            btab = wb_s.t[:, r + 4, :] if r < 0 else cb_s.t[:, r, :]
                            mm(ps_s.t[:, sb, :], identb.t[:, :], btab, False, True, [identb.r0, cb_s.r0, wb_s.r0], [ps_s.r[sb]])
                        pb = ptr[0] % 3
                        ptr[0] += 1
                        P.op("act", "activation", dict(out=pt.t[:, pb, :], in_=ps_s.t[:, sb, :], func=AF.Exp, scale=SCALE), [ps_s.r[sb]], [pt.r[pb]])
                        return (kt, pb)

                    def emit_pv(kt, pb):
                        r = kt - qc * 4
                        for qt in range(4):
                            if r > qt:
                                continue
                            if br == 2 and r < qt - 4:
                                continue
                            bank = aset[0] * 2 + qt // 2
                            off = (qt % 2) * 129
                            mm(ps_acc.t[:, bank, off:off + 129], pt.t[:, pb, qt * 128:(qt + 1) * 128], vsrc.t[:, kt, :],
                               bank not in started, False, [pt.r[pb], vsrc.r0], [ps_acc.r[bank]])
                            started[bank] = 1

                    pend = None
                    for kt in kts:
                        cur = emit_scores(kt)
                        if pend is not None:
                            emit_pv(*pend)
                        pend = cur
                    emit_pv(*pend)
                    for qt in range(4):
                        post(hh, qt, aset[0] * 2 + qt // 2, (qt % 2) * 129, br, False)
            if (g, qc) in ((0, 0), (1, 1)):
                DBG(tg + "_oacc_w", oacc, [128, 4, 4, 128])
            P.op("act", "copy", dict(out=obb.t[:], in_=oacc.t[:]), [oacc.r0], [obb.r0])
            for hh in range(4):
                for qt in range(4):
                    P.op("pe", "transpose", dict(out=ps_trb.t[:, qt * 128:(qt + 1) * 128], in_=obb.t[:, qt, hh, :], identity=identb.t[:, :]),
                         [obb.r0, identb.r0], [ps_trb.r0])
                cast(obT.t[:, hh, :], ps_trb.t[:, 0:512], [ps_trb.r0], [obT.r0])
            for hq in range(4):
                P.dma("pool", obt_s[4 * g + hq, :, q0:q0 + 512], obT.t[:, hq, :], reads=[obT.r0])
        P.emit_stage()
        P.pop()
    P.pop()
    if upto == "C":
        P.close()
        return nc

    def rmsnorm_tile(src, gain, dst_f32, dst_bf, sq, junk_):
        P.op("act", "activation", dict(out=junk_.t[:, :], in_=src.t[:, :], func=AF.Square, accum_out=sq.t[:, 0:1]), [src.r0], [junk_.r0, sq.r0])
        P.op("act", "activation", dict(out=sq.t[:, 1:2], in_=sq.t[:, 0:1], func=AF.Sqrt, scale=1.0 / D, bias=1e-6), [sq.r0], [sq.r0])
        P.op("dve", "reciprocal", dict(out=sq.t[:, 2:3], in_=sq.t[:, 1:2]), [sq.r0], [sq.r0])
        P.op("dve", "scalar_tensor_tensor", dict(out=dst_f32.t[:, :], in0=src.t[:, :], scalar=sq.t[:, 2:3], in1=gain.t[:, :], op0=ALU.mult, op1=ALU.mult),
             [src.r0, sq.r0, gain.r0], [dst_f32.r0])
        if dst_bf is not None:
            P.op("act", "copy", dict(out=dst_bf.t[:, :], in_=dst_f32.t[:, :]), [dst_f32.r0], [dst_bf.r0])

    P.push()
    w3 = [P.sbuf(f"w3s{i}", [128, 8, D], BF16) for i in range(3)]
    for i in range(3):
        P.dma("sp", w3[i].t[:], w5bf[i], writes=[w3[i].r0])
    wA, wB, wO = w3
    obTq = P.sbuf("obTq", [128, 8, 512], BF16, nres=8)
    maTq = P.sbuf("maTq", [128, 8, 512], BF16, nres=8)
    mgT = P.sbuf("mgT", [128, 8, 512], BF16)
    g01 = P.sbuf("g01", [128, 2, 2, 512], F32, nres=2)
    tA = P.sbuf("tA", [128, 2, 512], F32, nres=2)
    tB = P.sbuf("tB", [128, 2, 512], F32, nres=2)
    xtl = P.sbuf("xtl", [128, 2, D], F32, nres=2)
    h1t = P.sbuf("h1t", [128, 2, D], F32, nres=2)
    ps_y = P.psum("ps_y", [128, 4, 512], F32, nres=4)
    ps_h = P.psum("ps_h", [128, 2, 512], F32, nres=2)
    ti = 0
    for qc in range(NQC):
        q0 = qc * 512
        for c8 in range(8):
            P.dma("sp", obTq.t[:, c8, :], obt_s[c8, :, q0:q0 + 512], writes=[obTq.r[c8]])
            P.dma("sp", maTq.t[:, c8, :], ma_s[c8, :, q0:q0 + 512], writes=[maTq.r[c8]])
        for c in range(8):
            b = c % 2
            P.dma("sp", g01.t[:, b, 0, :], zfm[C_M + c, :, q0:q0 + 512], writes=[g01.r[b]])
            P.dma("sp", g01.t[:, b, 1, :], zfm[C_M + 8 + c, :, q0:q0 + 512], writes=[g01.r[b]])
            for k in range(8):
                mm(ps_y.t[:, 2 * b, :], wA.t[:, k, c * 128:(c + 1) * 128], maTq.t[:, k, :], k == 0, k == 7, [wA.r0, maTq.r[k]], [ps_y.r[2 * b]])
            for k in range(8):
                mm(ps_y.t[:, 2 * b + 1, :], wB.t[:, k, c * 128:(c + 1) * 128], obTq.t[:, k, :], k == 0, k == 7, [wB.r0, obTq.r[k]], [ps_y.r[2 * b + 1]])
            P.op("dve", "tensor_tensor", dict(out=tA.t[:, b, :], in0=ps_y.t[:, 2 * b, :], in1=g01.t[:, b, 0, :], op=ALU.mult), [ps_y.r[2 * b], g01.r[b]], [tA.r[b]])
            P.op("dve", "tensor_tensor", dict(out=tB.t[:, b, :], in0=ps_y.t[:, 2 * b + 1, :], in1=g01.t[:, b, 1, :], op=ALU.mult), [ps_y.r[2 * b + 1], g01.r[b]], [tB.r[b]])
            P.op("pool", "tensor_tensor", dict(out=mgT.t[:, c, :], in0=tA.t[:, b, :], in1=tB.t[:, b, :], op=ALU.add), [tA.r[b], tB.r[b]], [mgT.r0])
        for qt in range(4):
            tt = qc * 4 + qt
            b = ti % 2
            ti += 1
            P.dma("sp", xtl.t[:, b, :], x_d[tt * 128:(tt + 1) * 128, :], writes=[xtl.r[b]])
            for half in range(2):
                for c in range(8):
                    mm(ps_h.t[:, half, :], mgT.t[:, c, qt * 128:(qt + 1) * 128], wO.t[:, c, half * 512:(half + 1) * 512], c == 0, c == 7, [mgT.r0, wO.r0], [ps_h.r[half]])
                P.op("dve", "tensor_tensor", dict(out=h1t.t[:, b, half * 512:(half + 1) * 512], in0=ps_h.t[:, half, :], in1=xtl.t[:, b, half * 512:(half + 1) * 512], op=ALU.add),
                     [ps_h.r[half], xtl.r[b]], [h1t.r[b]])
            P.dma("pool", h1_s[tt * 128:(tt + 1) * 128, :], h1t.t[:, b, :], reads=[h1t.r[b]])
    P.emit_stage()
    P.pop()
    if upto == "D1":
        P.close()
        return nc

    P.push()
    wQ = P.sbuf("wQ", [128, 8, D], BF16)
    wG = P.sbuf("wG", [128, 8, D], BF16)
    wPs = P.sbuf("wPs", [128, 2, D], BF16)
    P.dma("sp", wQ.t[:], w5bf[3], writes=[wQ.r0])
    P.dma("sp", wG.t[:], w5bf[4], writes=[wG.r0])
    P.dma("sp", wPs.t[:], wpbf, writes=[wPs.r0])
    kbdf = P.sbuf("kbdf", [128, 256], F32)
    kbdb = P.sbuf("kbdb", [128, 256], BF16)
    P.dma("sp", kbdf.t[:, :], kbd_d, writes=[kbdf.r0])
    P.op("dve", "tensor_copy", dict(out=kbdb.t[:, :], in_=kbdf.t[:, :]), [kbdf.r0], [kbdb.r0])
    gains = []
    for nm, gd in (("gfb", gf_d), ("gpb", gp_d), ("glb", gl_d)):
        t_ = P.sbuf(nm, [128, D], F32)
        P.dma("sp", t_.t[:, :], gd.partition_broadcast(128), writes=[t_.r0])
        gains.append(t_)
    gfb, gpb, glb = gains
    io128 = P.sbuf("io128", [128, 128], I32)
    io256 = P.sbuf("io256", [128, 256], I32)
    io16 = P.sbuf("io16", [128, 16], F32)
    P.dma("sp", io128.t[:, :], io128_d[:, 0, :], writes=[io128.r0])
    P.dma("sp", io256.t[:, :], io256_d[:, 0, :], writes=[io256.r0])
    P.dma("sp", io16.t[:, :], io16_d[:, 0, 0, :], writes=[io16.r0])
    NG = int(_os.environ.get("K_NG", "13"))

    def dbl(name, shape, dt):
        return [P.sbuf(f"{name}{i}", shape, dt) for i in range(2)]

    hh2 = [P.sbuf(f"hh{i}", [128, D], F32) for i in range(3)]
    ptl2 = [P.sbuf(f"ptl{i}", [128, 256], F32) for i in range(3)]
    xn2 = dbl("xn", [128, D], F32)
    eidx2 = dbl("eidx", [128, 128], I32)
    gte2 = dbl("gte", [128, 128], F32)
    ptb = P.sbuf("ptb", [128, 256], BF16)
    pT = P.sbuf("pT", [128, 2, 128], BF16)
    xnb2 = dbl("xnb", [128, D], BF16)
    prodb = P.sbuf("prodb", [128, 4, D], BF16, nres=4)
    junkA = P.sbuf("junkA", [128, D], BF16)
    xnT = P.sbuf("xnT", [128, 8, 128], BF16)
    xne = P.sbuf("xne", [128, D], F32)
    xnbe = P.sbuf("xnbe", [128, D], BF16)
    xnTe = P.sbuf("xnTe", [128, 8, 128], BF16)
    qpT = P.sbuf("qpT", [128, 8, 128], BF16)
    junkF = P.sbuf("junkF", [128, D], BF16)
    junkG = P.sbuf("junkG", [128, D], BF16)
    junkE = P.sbuf("junkE", [128, D], BF16)
    sqF = P.sbuf("sqF", [128, 4], F32)
    sqE = P.sbuf("sqE", [128, 4], F32)
    s_all = P.sbuf("s_all", [128, 16, 128], F32)
    stmp = P.sbuf("stmp", [128, 256], F32)
    v12 = P.sbuf("v12", [128, 16, 16], F32)
    i12i = P.sbuf("i12i", [128, 16, 16], I32)
    i12f = P.sbuf("i12f", [128, 16, 16], F32)
    cand = P.sbuf("cand", [128, 8, 256], F32)
    oh = cand
    best = P.sbuf("best", [128, 8, 16], F32)
    posi2 = P.sbuf("posi2", [128, 8, 16], I32)
    abi = P.sbuf("abi", [128, 2, 8, 16], I32)
    abf = P.sbuf("abf", [128, 2, 8, 16], F32)
    isel = P.sbuf("isel", [128, 2, 8, 16], F32)
    ef = P.sbuf("ef", [128, 128], F32)
    bex = P.sbuf("bex", [128, 8, 16], F32)
    bsm = P.sbuf("bsm", [128, 2, 8], F32)
    dots = P.sbuf("dots", [128, 128], F32, nres=128)
    actv = P.sbuf("actv", [128, 128], F32, nres=32)
    a1 = P.sbuf("a1", [128, 128], F32, nres=32)
    ug = P.sbuf("ug", [128, NG, 2 * D], BF16, nres=NG)
    tmpv = P.sbuf("tmpv", [128, 4, D], BF16, nres=4)
    gate = P.sbuf("gate", [128, D], F32)
    tmpg = P.sbuf("tmpg", [128, 512], F32)
    ot = P.sbuf("ot", [128, D], F32)
    ps_t = P.psum("ps_t2", [128, 1024], BF16)
    ps_q = P.psum("ps_q", [128, 2, 512], F32, nres=2)
    ps_c = P.psum("ps_c", [128, 2, 512], F32, nres=2)
    ps_o = P.psum("ps_o", [128, 2, 512], F32, nres=2)
    cnt = {"gi": 0, "tvi": 0}

    def transpose8(src_bf, dst, n):
        for c in range(n):
            P.op("pe", "transpose", dict(out=ps_t.t[:, c * 128:(c + 1) * 128], in_=src_bf.t[:, c * 128:(c + 1) * 128], identity=identb.t[:, :]),
                 [src_bf.r0, identb.r0], [ps_t.r0])
        cast(dst.t[:, 0:n, :], ps_t.t[:, 0:n * 128].rearrange("p (c t) -> p c t", c=n), [ps_t.r0], [dst.r0])

    v12v = v12.t[:].rearrange("p (h c) k -> p h c k", c=2)
    i12v = i12f.t[:].rearrange("p (h c) k -> p h c k", c=2)

    def front(tt):
        par = tt % 2
        hh_, ptl, xn, eidx, gte = hh2[tt % 3], ptl2[tt % 3], xn2[par], eidx2[par], gte2[par]
        xnb = xnb2[par]
        P.dma("sp", hh_.t[:, :], h1_s[tt * 128:(tt + 1) * 128, :], writes=[hh_.r0])
        P.dma("sp", ptl.t[:, :], p_d[tt * 128:(tt + 1) * 128, :], writes=[ptl.r0])
        rmsnorm_tile(hh_, gfb, xn, xnb, sqF, junkF)
        yield
        transpose8(xnb, xnT, 8)
        yield
        for hd in range(8):
            bk = hd // 4
            for k in range(8):
                mm(ps_q.t[:, bk, (hd % 4) * 128:(hd % 4 + 1) * 128], wQ.t[:, k, hd * 128:(hd + 1) * 128], xnT.t[:, k, :],
                   (k == 0 and hd % 4 == 0), False, [wQ.r0, xnT.r0], [ps_q.r[bk]])
        for bk in range(2):
            cast(qpT.t[:, bk * 4:(bk + 1) * 4, :], ps_q.t[:, bk, :].rearrange("p (a t) -> p a t", a=4), [ps_q.r[bk]], [qpT.r0])
        for rnd in range(2):
            for h4 in range(4):
                hd = rnd * 4 + h4
                bk = h4 // 2
                mm(ps_c.t[:, bk, (hd % 2) * 256:(hd % 2 + 1) * 256], qpT.t[:, hd, :], kbdb.t[:, :], hd % 2 == 0, False, [qpT.r0, kbdb.r0], [ps_c.r[bk]])
            for bk in range(2):
                cast(s_all.t[:, rnd * 8 + bk * 4:rnd * 8 + (bk + 1) * 4, :], ps_c.t[:, bk, :].rearrange("p (a n) -> p a n", a=4), [ps_c.r[bk]], [s_all.r0])
            yield
        sI = s_all.t[:].bitcast(I32)
        P.op("dve", "tensor_scalar", dict(out=sI, in0=sI, scalar1=-128, scalar2=None, op0=ALU.bitwise_and), [s_all.r0], [s_all.r0])
        P.op("dve", "tensor_tensor", dict(out=sI, in0=sI, in1=io128.t[:, :].unsqueeze(1).broadcast_to([128, 16, 128]), op=ALU.bitwise_or), [s_all.r0, io128.r0], [s_all.r0])
        yield
        for j in range(16):
            P.op("dve", "max", dict(out=v12.t[:, j, 0:8], in_=s_all.t[:, j, :]), [s_all.r0], [v12.r0])
            P.op("dve", "match_replace", dict(out=stmp.t[:, 0:128], in_to_replace=v12.t[:, j, 0:8], in_values=s_all.t[:, j, :], imm_value=-3.0e38),
                 [s_all.r0, v12.r0], [stmp.r0])
            P.op("dve", "max", dict(out=v12.t[:, j, 8:16], in_=stmp.t[:, 0:128]), [stmp.r0], [v12.r0])
            if j % 2 == 1:
                yield
        P.op("dve", "tensor_scalar", dict(out=i12i.t[:], in0=v12.t[:].bitcast(I32), scalar1=127, scalar2=None, op0=ALU.bitwise_and), [v12.r0], [i12i.r0])
        P.op("dve", "tensor_copy", dict(out=i12f.t[:], in_=i12i.t[:]), [i12i.r0], [i12f.r0])
        candv = cand.t[:].rearrange("p h (a b) -> p h a b", a=16)
        P.op("dve", "tensor_tensor", dict(out=candv, in0=v12v[:, :, 0, :].unsqueeze(3).broadcast_to([128, 8, 16, 16]),
                                          in1=v12v[:, :, 1, :].unsqueeze(2).broadcast_to([128, 8, 16, 16]), op=ALU.add), [v12.r0], [cand.r0])
        yield
        cI = cand.t[:].bitcast(I32)
        P.op("dve", "tensor_scalar", dict(out=cI, in0=cI, scalar1=-256, scalar2=None, op0=ALU.bitwise_and), [cand.r0], [cand.r0])
        P.op("dve", "tensor_tensor", dict(out=cI, in0=cI, in1=io256.t[:, :].unsqueeze(1).broadcast_to([128, 8, 256]), op=ALU.bitwise_or), [cand.r0, io256.r0], [cand.r0])
        yield
        for hd in range(8):
            P.op("dve", "max", dict(out=best.t[:, hd, 0:8], in_=cand.t[:, hd, :]), [cand.r0], [best.r0])
            P.op("dve", "match_replace", dict(out=stmp.t[:, :], in_to_replace=best.t[:, hd, 0:8], in_values=cand.t[:, hd, :], imm_value=-3.0e38),
                 [cand.r0, best.r0], [stmp.r0])
            P.op("dve", "max", dict(out=best.t[:, hd, 8:16], in_=stmp.t[:, :]), [stmp.r0], [best.r0])
            if hd % 2 == 1:
                yield
        P.op("dve", "tensor_scalar", dict(out=posi2.t[:], in0=best.t[:].bitcast(I32), scalar1=255, scalar2=None, op0=ALU.bitwise_and), [best.r0], [posi2.r0])
        P.op("dve", "tensor_scalar", dict(out=abi.t[:, 0], in0=posi2.t[:], scalar1=4, scalar2=None, op0=ALU.arith_shift_right), [posi2.r0], [abi.r0])
        P.op("dve", "tensor_scalar", dict(out=abi.t[:, 1], in0=posi2.t[:], scalar1=15, scalar2=None, op0=ALU.bitwise_and), [posi2.r0, abi.r0], [abi.r0])
        P.op("dve", "tensor_copy", dict(out=abf.t[:], in_=abi.t[:]), [abi.r0], [abf.r0])
        yield
        ohv = oh.t[:].rearrange("p h (a b) -> p h a b", a=16)
        for c in range(2):
            P.op("dve", "tensor_tensor", dict(out=ohv, in0=abf.t[:, c].unsqueeze(3).broadcast_to([128, 8, 16, 16]),
                                              in1=io16.t[:, :].unsqueeze(1).unsqueeze(1).broadcast_to([128, 8, 16, 16]), op=ALU.is_equal),
                 [abf.r0, io16.r0], [oh.r0])
            P.op("dve", "tensor_tensor", dict(out=ohv, in0=ohv, in1=i12v[:, :, c, :].unsqueeze(2).broadcast_to([128, 8, 16, 16]), op=ALU.mult),
                 [oh.r0, i12f.r0], [oh.r0])
            P.op("dve", "tensor_reduce", dict(out=isel.t[:, c], in_=ohv, axis=AX.X, op=ALU.add), [oh.r0], [isel.r0])
            yield
        P.op("dve", "scalar_tensor_tensor", dict(out=ef.t[:, :], in0=isel.t[:, 0].rearrange("p h k -> p (h k)"), scalar=128.0,
                                                 in1=isel.t[:, 1].rearrange("p h k -> p (h k)"), op0=ALU.mult, op1=ALU.add), [isel.r0], [ef.r0])
        P.op("dve", "tensor_copy", dict(out=eidx.t[:, :], in_=ef.t[:, :]), [ef.r0], [eidx.r0])
        P.op("dve", "tensor_tensor", dict(out=bex.t[:], in0=best.t[:], in1=best.t[:, :, 0:1].broadcast_to([128, 8, 16]), op=ALU.subtract), [best.r0], [bex.r0])
        P.op("act", "activation", dict(out=bex.t[:], in_=bex.t[:], func=AF.Exp), [bex.r0], [bex.r0])
        yield
        P.op("dve", "tensor_reduce", dict(out=bsm.t[:, 0], in_=bex.t[:], axis=AX.X, op=ALU.add), [bex.r0], [bsm.r0])
        P.op("dve", "reciprocal", dict(out=bsm.t[:, 1], in_=bsm.t[:, 0]), [bsm.r0], [bsm.r0])
        P.op("dve", "tensor_tensor", dict(out=gte.t[:, :].rearrange("p (h k) -> p h k", h=8), in0=bex.t[:],
                                          in1=bsm.t[:, 1].unsqueeze(2).broadcast_to([128, 8, 16]), op=ALU.mult), [bex.r0, bsm.r0], [gte.r0])
        yield

    def gather_group(tt, grp):
        par = tt % 2
        xn, eidx, gte = xn2[par], eidx2[par], gte2[par]
        bufs = []
        for j in range(4):
            slot = grp * 4 + j
            b = cnt["gi"] % NG
            cnt["gi"] += 1
            bufs.append(b)
            P.dma("pool", ug.t[:, b, :], uv_s[:, :], reads=[eidx.r0], writes=[ug.r[b]],
                  indirect=dict(out_offset=None, in_offset=bass.IndirectOffsetOnAxis(ap=eidx.t[:, slot:slot + 1], axis=0)))
            if j % 2 == 0:
                P.op("dve", "scalar_tensor_tensor", dict(out=junkG.t[:, :], in0=ug.t[:, b, 0:D], scalar=1.0, in1=xn.t[:, :],
                                                         op0=ALU.mult, op1=ALU.mult, accum_out=dots.t[:, slot:slot + 1]),
                     [ug.r[b], xn.r0], [dots.r[slot]])
            else:
                pb_ = cnt.get("pbi", 0) % 4
                cnt["pbi"] = cnt.get("pbi", 0) + 1
                P.op("dve", "tensor_tensor", dict(out=prodb.t[:, pb_, :], in0=ug.t[:, b, 0:D], in1=xnb2[par].t[:, :], op=ALU.mult),
                     [ug.r[b], xnb2[par].r0], [prodb.r[pb_]])
                P.op("act", "activation", dict(out=junkA.t[:, :], in_=prodb.t[:, pb_, :], func=AF.Copy, accum_out=dots.t[:, slot:slot + 1]),
                     [prodb.r[pb_]], [dots.r[slot]])
        if _os.environ.get("K_NOCONS") or _os.environ.get("K_DOTSONLY"):
            return
        if cnt.get("pend") is not None:
            finish_group(*cnt["pend"])
        gs = slice(grp * 4, grp * 4 + 4)
        P.op("act", "activation", dict(out=a1.t[:, gs], in_=dots.t[:, gs], func=AF.Gelu_apprx_tanh), dots.r[grp * 4:grp * 4 + 4], [a1.r[grp]])
        cnt["pend"] = (tt, grp, bufs)
        if grp == 31:
            finish_group(*cnt["pend"])
            cnt["pend"] = None

    def finish_group(tt, grp, bufs):
        gte = gte2[tt % 2]
        gs = slice(grp * 4, grp * 4 + 4)
        P.op("dve", "tensor_tensor", dict(out=actv.t[:, gs], in0=a1.t[:, gs], in1=gte.t[:, gs], op=ALU.mult), [a1.r[grp], gte.r0], [actv.r[grp]])
        for j in range(4):
            slot = grp * 4 + j
            b = bufs[j]
            tb = cnt["tvi"] % 4
            cnt["tvi"] += 1
            P.op("act", "activation", dict(out=tmpv.t[:, tb, :], in_=ug.t[:, b, D:2 * D], func=AF.Copy, scale=actv.t[:, slot:slot + 1]),
                 [ug.r[b], actv.r[grp]], [tmpv.r[tb]])
            for half in range(2):
                mm(ps_o.t[:, half, :], identb.t[:, :], tmpv.t[:, tb, half * 512:(half + 1) * 512], slot == 0, slot == 127,
                   [identb.r0, tmpv.r[tb]], [ps_o.r[half]])

    def epilogue(tt):
        hh_, ptl = hh2[tt % 3], ptl2[tt % 3]
        for half in range(2):
            P.op("dve", "tensor_tensor", dict(out=hh_.t[:, half * 512:(half + 1) * 512], in0=ps_o.t[:, half, :], in1=hh_.t[:, half * 512:(half + 1) * 512], op=ALU.add),
                 [ps_o.r[half], hh_.r0], [hh_.r0])
        yield
        rmsnorm_tile(hh_, gpb, xne, xnbe, sqE, junkE)
        yield
        transpose8(xnbe, xnTe, 8)
        yield
        P.op("act", "copy", dict(out=ptb.t[:, :], in_=ptl.t[:, :]), [ptl.r0], [ptb.r0])
        transpose8(ptb, pT, 2)
        yield
        for half in range(2):
            for k in range(8):
                mm(ps_q.t[:, half, :], xnTe.t[:, k, :], wG.t[:, k, half * 512:(half + 1) * 512], k == 0, k == 7, [xnTe.r0, wG.r0], [ps_q.r[half]])
            P.op("act", "activation", dict(out=gate.t[:, half * 512:(half + 1) * 512], in_=ps_q.t[:, half, :], func=AF.Sigmoid), [ps_q.r[half]], [gate.r0])
            for k in range(2):
                mm(ps_c.t[:, half, :], pT.t[:, k, :], wPs.t[:, k, half * 512:(half + 1) * 512], k == 0, k == 1, [pT.r0, wPs.r0], [ps_c.r[half]])
            P.op("dve", "tensor_tensor", dict(out=tmpg.t[:, :], in0=ps_c.t[:, half, :], in1=gate.t[:, half * 512:(half + 1) * 512], op=ALU.mult),
                 [ps_c.r[half], gate.r0], [tmpg.r0])
            P.op("dve", "tensor_tensor", dict(out=hh_.t[:, half * 512:(half + 1) * 512], in0=hh_.t[:, half * 512:(half + 1) * 512], in1=tmpg.t[:, :], op=ALU.add),
                 [hh_.r0, tmpg.r0], [hh_.r0])
            yield
        rmsnorm_tile(hh_, glb, ot, None, sqE, junkE)
        yield
        P.dma("sp", out_d[tt * 128:(tt + 1) * 128, :], ot.t[:, :], reads=[ot.r0])

    for _ in front(0):
        pass
    for tt in range(NTT):
        nxt = front(tt + 1) if tt + 1 < NTT else iter(())
        for grp in range(32):
            gather_group(tt, grp)
            if grp >= 1:
                next(nxt, None)
        for _ in nxt:
            pass
        for _ in epilogue(tt):
            pass
    P.emit_stage()
    P.pop()

    P.close()
    return nc


def kernel(**inputs):
    consts = make_consts()
    shared = prep_shared(inputs)
    shared.update(consts)
    in_maps = []
    for b in range(8):
        m = dict(shared)
        m["x"] = np.ascontiguousarray(inputs["x"][b])
        m["p"] = np.ascontiguousarray(inputs["p"][0, b])
        m["positions"] = np.ascontiguousarray(inputs["positions"][b].reshape(1, S).astype(np.int32))
        in_maps.append(m)
    nc = build_nc()
    res = run_bass_kernel_spmd(nc, in_maps, core_ids=list(range(8)))
    return np.stack([np.asarray(r["out"]) for r in res.results], axis=0).astype(np.float32)
```

```python
from contextlib import ExitStack
import os as _os
import numpy as np
import ml_dtypes
import concourse.bass as bass
import concourse.mybir as mybir
from concourse.bass_utils import run_bass_kernel_spmd

F32 = mybir.dt.float32
BF16 = mybir.dt.bfloat16
I32 = mybir.dt.int32
AF = mybir.ActivationFunctionType
ALU = mybir.AluOpType
AX = mybir.AxisListType

S = 4096
D = 1024
NQC = 8
NTT = 32
NEG = -30000.0
SCALE = 128 ** -0.5
COMPUTE = ("pe", "dve", "act", "pool")
NOSELF = tuple(x for x in _os.environ.get("K_NOSELF", "").split(",") if x)
NROT = int(_os.environ.get("K_NROT", "8"))
NCH = 65
WCOLS = NCH * 128

C_RX, C_RG, C_Q, C_QS, C_KV, C_KSW, C_M, C_NG = 0, 8, 16, 24, 32, 44, 48, 64


class Res:
    __slots__ = ("w", "r", "name")

    def __init__(self, name=""):
        self.w = None
        self.r = {}
        self.name = name


class TT:
    def __init__(self, t, nres, name):
        self.t = t
        self.r = [Res(f"{name}.{i}") for i in range(nres)]

    @property
    def r0(self):
        return self.r[0]


class Prog:
    def __init__(self, nc):
        self.nc = nc
        self.scopes = [ExitStack()]
        self.streams = {e: [] for e in ("pe", "dve", "act", "pool", "sp")}
        self.cnt = {e: 0 for e in COMPUTE}
        self.seen = {e: {} for e in self.streams}
        self.sems = {}
        for i in range(int(_os.environ.get("K_SEMPAD", "0"))):
            self.scopes[0].enter_context(nc.semaphore(f"s_pad{i}"))
        for e in COMPUTE:
            self.sems[e] = self.scopes[0].enter_context(nc.semaphore(f"s_{e}"))
        self.dq = {}
        self.dma_total = {}
        for q in ("sp", "act", "pool"):
            self.dq[q] = {"n": 0, "sems": []}
            nrot_q = int(_os.environ.get("K_NROT_" + q.upper(), "1" if q == "sp" else str(NROT)))
            self.dq[q]["nrot"] = nrot_q
            for i in range(nrot_q):
                k = ("dma", q, i)
                self.sems[k] = self.scopes[0].enter_context(nc.semaphore(f"s_dma_{q}_{i}"))
                self.dq[q]["sems"].append(k)
                self.dma_total[k] = 0
        self.n_instr = 0

    def push(self):
        self.scopes.append(ExitStack())

    def pop(self):
        self.scopes.pop().close()

    def sbuf(self, name, shape, dtype, nres=1):
        self.n_instr += 0
        self.uid = getattr(self, "uid", 0) + 1
        t = self.scopes[-1].enter_context(self.nc.sbuf_tensor(f"{name}_{self.uid}", list(shape), dtype))
        return TT(t, nres, name)

    def psum(self, name, shape, dtype, nres=1):
        self.uid = getattr(self, "uid", 0) + 1
        t = self.scopes[-1].enter_context(self.nc.psum_tensor(f"{name}_{self.uid}", list(shape), dtype))
        return TT(t, nres, name)

    def _deps(self, eng, reads, writes):
        deps = {}

        def add(tok):
            if tok is None:
                return
            k, v = tok
            if deps.get(k, 0) < v:
                deps[k] = v

        for r in reads:
            add(r.w)
        for w in writes:
            add(w.w)
            for k, v in w.r.items():
                add((k, v))
        out = []
        for k, v in deps.items():
            if k == eng and (eng == "pe" or eng in NOSELF):
                continue
            if self.seen[eng].get(k, 0) >= v:
                continue
            self.seen[eng][k] = v
            out.append((k, v))
        return out

    def _record(self, tok, reads, writes):
        k, v = tok
        for r in reads:
            if r.r.get(k, 0) < v:
                r.r[k] = v
        for w in writes:
            w.w = tok
            w.r = {}

    def op(self, eng, meth, kw, reads=(), writes=()):
        waits = self._deps(eng, reads, writes)
        self.cnt[eng] += 1
        tok = (eng, self.cnt[eng])
        self._record(tok, reads, writes)
        self.streams[eng].append((meth, kw, waits, (eng, 1)))
        self.n_instr += 1

    def dma(self, q, out, in_, reads=(), writes=(), indirect=None, **kw):
        d = self.dq[q]
        i = d["n"]
        d["n"] += 1
        sk = d["sems"][i % d["nrot"]]
        prev = self.dma_total[sk]
        waits = self._deps(q, reads, writes)
        if prev > 0 and self.seen[q].get(sk, 0) < prev:
            self.seen[q][sk] = prev
            waits.append((sk, prev))
        self.dma_total[sk] = prev + 16
        tok = (sk, prev + 16)
        self._record(tok, reads, writes)
        kk = dict(out=out, in_=in_)
        if indirect is not None:
            kk.update(indirect)
        kk.update(kw)
        self.streams[q].append(("indirect_dma_start" if indirect is not None else "dma_start", kk, waits, (sk, 16)))
        self.n_instr += 1
        return tok

    def emit_stage(self):
        targets = [(e, self.cnt[e]) for e in COMPUTE if self.cnt[e] > 0]
        targets += [(k, v) for k, v in self.dma_total.items() if v > 0]
        for e in self.streams:
            ws = []
            for k, v in targets:
                if self.seen[e].get(k, 0) < v:
                    self.seen[e][k] = v
                    ws.append((k, v))
            if ws:
                self.streams[e].append((None, None, ws, None))
        sems = self.sems
        streams = self.streams

        def run(eng_obj, items):
            for meth, kw, waits, inc in items:
                for k, v in waits:
                    eng_obj.wait_ge(sems[k], v)
                if meth is None:
                    continue
                ins = getattr(eng_obj, meth)(**kw)
                ins.then_inc(sems[inc[0]], inc[1])

        with self.nc.Block() as block:
            @block.tensor
            def _(e):
                run(e, streams["pe"])

            @block.vector
            def _(e):
                run(e, streams["dve"])

            @block.scalar
            def _(e):
                run(e, streams["act"])

            @block.gpsimd
            def _(e):
                run(e, streams["pool"])

            @block.sync
            def _(e):
                run(e, streams["sp"])
        self.streams = {e: [] for e in streams}

    def close(self):
        while self.scopes:
            self.scopes.pop().close()


def _bf(a):
    return np.ascontiguousarray(a).astype(ml_dtypes.bfloat16)


def make_consts():
    c = {}
    c["c_identb"] = _bf(np.eye(128, dtype=np.float32))
    c["c_identf"] = np.eye(128, dtype=np.float32)
    half = 64
    inv = (10000.0 ** (-np.arange(half, dtype=np.float32) / np.float32(half))).astype(np.float32)
    c["c_invf"] = np.concatenate([inv, inv]).reshape(128, 1).astype(np.float32)
    c["c_sgn"] = np.concatenate([-np.ones(64), np.ones(64)]).reshape(128, 1).astype(np.float32)
    kp = np.arange(128)[:, None]
    q = np.arange(512)[None, :]
    cb = np.zeros((128, 4, 512), np.float32)
    wb = np.zeros((128, 4, 512), np.float32)
    for r in range(4):
        k = r * 128 + kp
        cb[:, r, :] = np.where(k <= q, 0.0, NEG)
        k2 = (r - 4) * 128 + kp
        wb[:, r, :] = np.where(q - k2 < 512, 0.0, NEG)
    c["c_cb"] = _bf(cb)
    c["c_wb"] = _bf(wb)
    n = (np.arange(2)[None, :, None] * 128 + np.arange(128)[:, None, None])
    t = np.arange(S)[None, None, :]
    cmb = np.where((16 * n + 31 <= t) & (n < 255), 0.0, NEG).astype(np.float32)
    c["c_cmb"] = _bf(cmb)
    j = np.arange(64)[:, None]
    kk = np.arange(S)[None, :]
    c["c_E"] = _bf((kk // 64 == j).astype(np.float32))
    nn = np.arange(256)
    jj = np.arange(64)
    ov = ((16 * nn[:, None] < 64 * jj[None, :] + 64) & (16 * nn[:, None] + 31 >= 64 * jj[None, :]) & (nn[:, None] < 255))
    c["c_ovl"] = _bf(ov.astype(np.float32).reshape(2, 128, 64).transpose(1, 0, 2))
    tt = np.arange(S).reshape(NTT, 128)[:, :, None]
    cur = tt // 64
    j3 = np.arange(64)[None, None, :]
    forced = (j3 == 0) | (j3 == cur) | (j3 == cur - 1)
    valid = (64 * j3 <= tt)
    c["c_fa"] = np.ascontiguousarray(np.where(forced, 1000.0, 0.0).astype(np.float32).transpose(1, 0, 2))
    c["c_na"] = np.ascontiguousarray(np.where(valid, 0.0, -1e30).astype(np.float32).transpose(1, 0, 2))
    c["c_vm"] = np.ascontiguousarray(valid.astype(np.float32).transpose(1, 0, 2))
    c["c_iota128"] = np.ascontiguousarray(np.broadcast_to(np.arange(128, dtype=np.int32)[None, None, :], (128, 16, 128)))
    c["c_iota256"] = np.ascontiguousarray(np.broadcast_to(np.arange(256, dtype=np.int32)[None, None, :], (128, 8, 256)))
    c["c_iota16"] = np.ascontiguousarray(np.broadcast_to(np.arange(16, dtype=np.float32)[None, None, None, :], (128, 8, 16, 16)))
    return c


def pc_layout(v):
    return np.ascontiguousarray(np.asarray(v).reshape(8, 128).T)


def prep_shared(inp):
    m = {}
    def pk(w):
        K_, N_ = w.shape
        return np.ascontiguousarray(w.reshape(K_ // 128, 128, N_).transpose(1, 0, 2))
    m["w_in"] = pk(inp["w_in"][0])
    for k in ("w_a", "w_b", "w_out", "peer_wq", "ple_wg", "ple_wp"):
        m[k] = pk(inp[k][0])
    for k in ("peer_u", "peer_v"):
        m[k] = np.ascontiguousarray(inp[k][0])
    m["lru_wr"] = np.ascontiguousarray(inp["lru_wr"][0].transpose(1, 0, 2))
    m["lru_wi"] = np.ascontiguousarray(inp["lru_wi"][0].transpose(1, 0, 2))
    for k in ("cmp_w1_k", "cmp_w1_v"):
        m[k] = np.ascontiguousarray(inp[k][0].reshape(32, 128, 256).transpose(1, 0, 2))
    for k in ("cmp_w2_k", "cmp_w2_v"):
        m[k] = pk(inp[k][0])
    for k in ("norm_mix", "norm_ffn", "norm_ple"):
        m[k] = np.ascontiguousarray(inp[k][0].reshape(1, D))
    m["norm_final"] = np.ascontiguousarray(inp["norm_final"].reshape(1, D))
    m["conv_w"] = np.ascontiguousarray(inp["conv_w"][0].reshape(4, 8, 128).transpose(2, 1, 0))
    for k in ("conv_b", "lru_br", "lru_bi", "lru_lam"):
        m[k] = pc_layout(inp[k][0])
    for k in ("cmp_b1_k", "cmp_b1_v"):
        m[k] = np.ascontiguousarray(inp[k][0].reshape(2, 128).T)
    m["cmp_b2_k"] = np.ascontiguousarray(inp["cmp_b2_k"][0].reshape(128, 1))
    m["cmp_b2_v"] = np.ascontiguousarray(inp["cmp_b2_v"][0].reshape(1, 128))
    m["cmp_pe_k"] = np.ascontiguousarray(inp["cmp_pe_k"][0].T)
    m["cmp_pe_v"] = np.ascontiguousarray(inp["cmp_pe_v"][0].T)
    keys = inp["peer_keys"][0]
    kbd = np.zeros((128, 256), np.float32)
    kbd[0:64, 0:128] = keys[0].T
    kbd[64:128, 128:256] = keys[1].T
    m["peer_kbd"] = kbd
    return m


def win_segments():
    segs = [(0, 0, 2048), (2048, 2048, 1024)]
    d = 3072
    for h in range(8):
        segs.append((d, 2048 + h * 128 + 64, 64))
        segs.append((d + 64, 2048 + h * 128, 64))
        d += 128
    segs.append((4096, 3072, 1536))
    d = 5632
    for jkv in (2, 4):
        for g in range(2):
            s0 = 3072 + jkv * 256 + g * 128
            segs.append((d, s0 + 64, 64))
            segs.append((d + 64, s0, 64))
            d += 128
    segs.append((6144, 4632, 2048))
    segs.append((8192, 4608, 24))
    return segs


def build_nc(upto="D", debug=False):
    nc = bass.Bass("TRN2", target_bir_lowering=False)
    P = Prog(nc)
    ein = {}

    def IN(name, shape, dt=F32):
        ein[name] = nc.dram_tensor(name, list(shape), dt, kind="ExternalInput").ap()
        return ein[name]

    x_d = IN("x", [S, D])
    p_d = IN("p", [S, 256])
    pos_d = IN("positions", [1, S], I32)
    win_d = IN("w_in", [128, 8, 6680])
    w5_d = [IN(k, [128, 8, D]) for k in ("w_a", "w_b", "w_out", "peer_wq", "ple_wg")]
    wp_d = IN("ple_wp", [128, 2, D])
    u_d = IN("peer_u", [16384, D])
    v_d = IN("peer_v", [16384, D])
    wr_d = IN("lru_wr", [128, 8, 128])
    wi_d = IN("lru_wi", [128, 8, 128])
    w1_d = [IN("cmp_w1_k", [128, 32, 256]), IN("cmp_w1_v", [128, 32, 256])]
    w2_d = [IN("cmp_w2_k", [128, 2, 128]), IN("cmp_w2_v", [128, 2, 128])]
    gm_d = IN("norm_mix", [1, D])
    gf_d = IN("norm_ffn", [1, D])
    gp_d = IN("norm_ple", [1, D])
    gl_d = IN("norm_final", [1, D])
    cw_d = IN("conv_w", [128, 8, 4])
    cbias_d = IN("conv_b", [128, 8])
    br_d = IN("lru_br", [128, 8])
    bi_d = IN("lru_bi", [128, 8])
    lam_d = IN("lru_lam", [128, 8])
    b1_d = [IN("cmp_b1_k", [128, 2]), IN("cmp_b1_v", [128, 2])]
    b2k_d = IN("cmp_b2_k", [128, 1])
    b2v_d = IN("cmp_b2_v", [1, 128])
    pe_d = [IN("cmp_pe_k", [128, 32]), IN("cmp_pe_v", [128, 32])]
    kbd_d = IN("peer_kbd", [128, 256])
    identb_d = IN("c_identb", [128, 128], BF16)
    identf_d = IN("c_identf", [128, 128])
    invf_d = IN("c_invf", [128, 1])
    sgn_d = IN("c_sgn", [128, 1])
    cb_d = IN("c_cb", [128, 4, 512], BF16)
    wb_d = IN("c_wb", [128, 4, 512], BF16)
    cmb_d = IN("c_cmb", [128, 2, S], BF16)
    E_d = IN("c_E", [64, S], BF16)
    ovl_d = IN("c_ovl", [128, 2, 64], BF16)
    fa_d = IN("c_fa", [128, NTT, 64])
    na_d = IN("c_na", [128, NTT, 64])
    vm_d = IN("c_vm", [128, NTT, 64])
    io128_d = IN("c_iota128", [128, 16, 128], I32)
    io256_d = IN("c_iota256", [128, 8, 256], I32)
    io16_d = IN("c_iota16", [128, 8, 16, 16])

    out_d = nc.dram_tensor("out", [S, D], F32, kind="ExternalOutput").ap()

    dbg_n = [0]

    def DBG(tag, tt_, shape, dt=F32):
        if not debug:
            return
        dbg_n[0] += 1
        d = nc.dram_tensor(f"dbg_{tag}", list(shape), dt, kind="ExternalOutput").ap()
        P.dma("sp", d, tt_.t[:], reads=tt_.r)

    def SCR(name, shape, dt):
        kind = "ExternalOutput" if debug else "Internal"
        return nc.dram_tensor(name, list(shape), dt, kind=kind).ap()

    winbf = SCR("s_winbf", [128, 8, WCOLS], BF16)
    w5bf = [SCR(f"s_w5bf{i}", [128, 8, D], BF16) for i in range(5)]
    wpbf = SCR("s_wpbf", [128, 2, D], BF16)
    w1bf = [SCR(f"s_w1bf{i}", [128, 32, 256], BF16) for i in range(2)]
    zfm = SCR("s_zfm", [NCH, 128, S], F32)
    ma_s = SCR("s_ma", [8, 128, S], BF16)
    obt_s = SCR("s_obt", [8, 128, S], BF16)
    h1_s = SCR("s_h1", [S, D], F32)
    uv_s = nc.dram_tensor("s_uv", [16384, 2048], BF16, kind="Internal").ap()

    identb = P.sbuf("identb", [128, 128], BF16)
    identf = P.sbuf("identf", [128, 128], F32)
    P.dma("sp", identb.t[:, :], identb_d, writes=[identb.r0])
    P.dma("sp", identf.t[:, :], identf_d, writes=[identf.r0])

    cast_rr = [0]

    def cast(out, in_, reads, writes, engs=tuple(_os.environ.get("K_CAST", "dve,act").split(","))):
        e = engs[cast_rr[0] % len(engs)]
        cast_rr[0] += 1
        if e == "act":
            P.op("act", "copy", dict(out=out, in_=in_), reads, writes)
        else:
            P.op(e, "tensor_copy", dict(out=out, in_=in_), reads, writes)

    P.push()
    stg = P.sbuf("stg", [128, 2, 4096], F32, nres=2)
    stgb = P.sbuf("stgb", [128, 2, 4096], BF16, nres=2)
    P.op("dve", "memset", dict(ap=stg.t[:, 0, :], constant=0.0), [], [stg.r[0]])
    P.op("dve", "memset", dict(ap=stg.t[:, 1, :], constant=0.0), [], [stg.r[1]])
    pi = [0]

    def piece(loads, n, dst, dshape):
        b = pi[0] % 2
        pi[0] += 1
        for mk, src in loads:
            P.dma("sp", mk(stg.t[:, b, 0:n]), src, writes=[stg.r[b]])
        cast(stgb.t[:, b, 0:n], stg.t[:, b, 0:n], [stg.r[b]], [stgb.r[b]])
        P.dma("pool", dst, dshape(stgb.t[:, b, 0:n]), reads=[stgb.r[b]])

    win_v = win_d
    segs = win_segments()
    for c0 in range(0, WCOLS, 512):
        c1 = min(c0 + 512, WCOLS)
        w = c1 - c0
        loads = []
        for (dcol, scol, ncol) in segs:
            lo, hi = max(dcol, c0), min(dcol + ncol, c1)
            if lo >= hi:
                continue
            so = scol + (lo - dcol)
            loads.append((lambda a, lo=lo, hi=hi, w=w, c0=c0: a.rearrange("p (kc n) -> p kc n", kc=8)[:, :, lo - c0:hi - c0],
                          win_v[:, :, so:so + (hi - lo)]))
        if c1 == WCOLS and 8192 + 24 < WCOLS:
            pass
        piece(loads, 8 * w, winbf[:, :, c0:c1], lambda a, w=w: a.rearrange("p (kc n) -> p kc n", kc=8))
    for i in range(5):
        wv = w5_d[i]
        for hh in range(2):
            piece([(lambda a: a.rearrange("p (kc n) -> p kc n", kc=8), wv[:, :, hh * 512:(hh + 1) * 512])], 4096,
                  w5bf[i][:, :, hh * 512:(hh + 1) * 512], lambda a: a.rearrange("p (kc n) -> p kc n", kc=8))
    piece([(lambda a: a.rearrange("p (kc n) -> p kc n", kc=2), wp_d)], 2048,
          wpbf[:, :, :], lambda a: a.rearrange("p (kc n) -> p kc n", kc=2))
    for i in range(2):
        wv = w1_d[i]
        for hh in range(2):
            piece([(lambda a: a.rearrange("p (l h) -> p l h", l=16), wv[:, hh * 16:(hh + 1) * 16, :])], 4096,
                  w1bf[i][:, hh * 16:(hh + 1) * 16, :], lambda a: a.rearrange("p (l h) -> p l h", l=16))
    P.emit_stage()
    P.pop()
    if upto == "P":
        P.close()
        return nc

    P.push()
    nT = P.sbuf("nT", [128, 8, S], BF16, nres=NTT)
    gbc = P.sbuf("gbc", [128, D], F32)
    xt = P.sbuf("xt", [128, 2, D], F32, nres=2)
    nb = P.sbuf("nb", [128, 2, D], BF16, nres=2)
    junk = P.sbuf("junk", [128, D], F32)
    ss = P.sbuf("ss", [128, NTT], F32, nres=NTT)
    rs = P.sbuf("rs", [128, NTT], F32, nres=NTT)
    tp = P.psum("tp", [128, 2, 1024], BF16, nres=2)
    P.dma("sp", gbc.t[:, :], gm_d.partition_broadcast(128), writes=[gbc.r0])
    for tt in range(NTT):
        b = tt % 2
        P.dma("sp", xt.t[:, b, :], x_d[tt * 128:(tt + 1) * 128, :], writes=[xt.r[b]])
        P.op("act", "activation", dict(out=junk.t[:, :], in_=xt.t[:, b, :], func=AF.Square, accum_out=ss.t[:, tt:tt + 1]),
             reads=[xt.r[b]], writes=[junk.r0, ss.r[tt]])
        P.op("act", "activation", dict(out=rs.t[:, tt:tt + 1], in_=ss.t[:, tt:tt + 1], func=AF.Sqrt, scale=1.0 / D, bias=1e-6),
             reads=[ss.r[tt]], writes=[rs.r[tt]])
        P.op("dve", "reciprocal", dict(out=rs.t[:, tt:tt + 1], in_=rs.t[:, tt:tt + 1]), reads=[rs.r[tt]], writes=[rs.r[tt]])
        P.op("dve", "scalar_tensor_tensor", dict(out=nb.t[:, b, :], in0=xt.t[:, b, :], scalar=rs.t[:, tt:tt + 1], in1=gbc.t[:, :],
                                                 op0=ALU.mult, op1=ALU.mult), reads=[xt.r[b], rs.r[tt], gbc.r0], writes=[nb.r[b]])
        for c in range(8):
            P.op("pe", "transpose", dict(out=tp.t[:, b, c * 128:(c + 1) * 128], in_=nb.t[:, b, c * 128:(c + 1) * 128], identity=identb.t[:, :]),
                 reads=[nb.r[b], identb.r0], writes=[tp.r[b]])
        cast(nT.t[:, :, tt * 128:(tt + 1) * 128], tp.t[:, b, :].rearrange("p (c t) -> p c t", c=8), [tp.r[b]], [nT.r[tt]])

    def uv_build():
        ust = P.sbuf("ust", [128, 2, 4096], F32, nres=2)
        uvb = P.sbuf("uvb", [128, 2, 4, 2048], BF16, nres=2)
        u_v = u_d.rearrange("(r p j) d -> r p (j d)", p=128, j=4)
        v_v = v_d.rearrange("(r p j) d -> r p (j d)", p=128, j=4)
        uv_v = uv_s.rearrange("(r p j) c -> r p (j c)", p=128, j=4)
        for rb in range(32):
            ob_ = rb % 2
            for ti_, (src_v, eng_) in enumerate(((u_v, "act"), (v_v, "dve"))):
                P.dma("sp", ust.t[:, ti_, :], src_v[rb], writes=[ust.r[ti_]])
                dst_ = uvb.t[:, ob_, :, ti_ * 1024:(ti_ + 1) * 1024]
                src_ = ust.t[:, ti_, :].rearrange("p (j d) -> p j d", j=4)
                if eng_ == "act":
                    P.op("act", "copy", dict(out=dst_, in_=src_), [ust.r[ti_]], [uvb.r[ob_]])
                else:
                    P.op("dve", "tensor_copy", dict(out=dst_, in_=src_), [ust.r[ti_]], [uvb.r[ob_]])
            P.dma("pool", uv_v[rb], uvb.t[:, ob_].rearrange("p j c -> p (j c)"), reads=[uvb.r[ob_]])
            yield

    wbuf = P.sbuf("wbuf", [128, 2, 8, 512], BF16, nres=2)
    zst = P.sbuf("zst", [128, 2, S], F32, nres=2)
    uvgen = uv_build()
    pz = P.psum("pz", [128, 4, 512], F32, nres=4)
    nTall = list(nT.r)
    zi = 0
    pzi = 0
    for wpi, c0 in enumerate(range(0, WCOLS, 512)):
        c1 = min(c0 + 512, WCOLS)
        wb_ = wpi % 2
        P.dma("sp", wbuf.t[:, wb_, :, 0:c1 - c0], winbf[:, :, c0:c1], writes=[wbuf.r[wb_]])
        for cc in range((c1 - c0) // 128):
            ch = c0 // 128 + cc
            M = 24 if ch == C_NG else 128
            zb = zi % 2
            zi += 1
            for qc in range(NQC):
                pb = pzi % 4
                pzi += 1
                for kc in range(8):
                    P.op("pe", "matmul", dict(out=pz.t[0:M, pb, :], lhsT=wbuf.t[:, wb_, kc, cc * 128:cc * 128 + M],
                                              rhs=nT.t[:, kc, qc * 512:(qc + 1) * 512], start=(kc == 0), stop=(kc == 7),
                                              skip_group_check=True),
                         reads=[wbuf.r[wb_]] + nTall[qc * 4:(qc + 1) * 4], writes=[pz.r[pb]])
                dst = zst.t[0:M, zb, qc * 512:(qc + 1) * 512]
                src = pz.t[0:M, pb, :]
                if C_RG <= ch < C_RG + 8:
                    P.op("act", "activation", dict(out=dst, in_=src, func=AF.Gelu_apprx_tanh), [pz.r[pb]], [zst.r[zb]])
                elif ch >= C_M:
                    P.op("act", "activation", dict(out=dst, in_=src, func=AF.Sigmoid), [pz.r[pb]], [zst.r[zb]])
                else:
                    P.op("dve", "tensor_copy", dict(out=dst, in_=src), [pz.r[pb]], [zst.r[zb]])
            P.dma("pool", zfm[ch, 0:M, :], zst.t[0:M, zb, :], reads=[zst.r[zb]])
            if ch % 2 == 0:
                next(uvgen, None)
    for _ in uvgen:
        pass
    P.emit_stage()
    P.pop()
    if upto == "A":
        P.close()
        return nc

    P.push()
    cw = P.sbuf("cw", [128, 8, 4], F32)
    cbias = P.sbuf("cbias", [128, 8], F32)
    brs = P.sbuf("brs", [128, 8], F32)
    bis = P.sbuf("bis", [128, 8], F32)
    lam = P.sbuf("lam", [128, 8], F32)
    m8 = P.sbuf("m8", [128, 8], F32)
    m16 = P.sbuf("m16", [128, 8], F32)
    wst = P.sbuf("wst", [128, 2, 8, 128], F32, nres=2)
    wrb = P.sbuf("wrb", [128, 8, 128], BF16)
    wib = P.sbuf("wib", [128, 8, 128], BF16)
    for (t_, d_) in ((cw, cw_d), (cbias, cbias_d), (brs, br_d), (bis, bi_d), (lam, lam_d)):
        P.dma("sp", t_.t[:], d_, writes=[t_.r0])
    P.dma("sp", wst.t[:, 0], wr_d, writes=[wst.r[0]])
    P.dma("sp", wst.t[:, 1], wi_d, writes=[wst.r[1]])
    P.op("dve", "tensor_copy", dict(out=wrb.t[:, :, :], in_=wst.t[:, 0]), [wst.r[0]], [wrb.r0])
    P.op("dve", "tensor_copy", dict(out=wib.t[:, :, :], in_=wst.t[:, 1]), [wst.r[1]], [wib.r0])
    P.op("act", "activation", dict(out=m8.t[:, :], in_=lam.t[:, :], func=AF.Exp, scale=-1.0), [lam.r0], [m8.r0])
    P.op("act", "activation", dict(out=m8.t[:, :], in_=m8.t[:, :], func=AF.Ln, bias=1.0), [m8.r0], [m8.r0])
    P.op("dve", "tensor_scalar", dict(out=m16.t[:, :], in0=m8.t[:, :], scalar1=-16.0, scalar2=None, op0=ALU.mult), [m8.r0], [m16.r0])
    P.op("dve", "tensor_scalar", dict(out=m8.t[:, :], in0=m8.t[:, :], scalar1=-8.0, scalar2=None, op0=ALU.mult), [m8.r0, m16.r0], [m8.r0])
    zx = P.sbuf("zx", [128, S + 3], F32)
    gz = P.sbuf("gz", [128, S], F32)
    xa = P.sbuf("xa", [128, S], F32)
    xab = P.sbuf("xab", [128, S], BF16)
    rr = P.sbuf("rr", [128, S], F32)
    ig = P.sbuf("ig", [128, S], F32)
    t1 = P.sbuf("t1", [128, S], F32)
    t2 = P.sbuf("t2", [128, S], F32)
    mab = P.sbuf("mab", [128, S], BF16)
    pg = P.psum("pg", [128, 4, 512], F32, nres=4)
    P.op("dve", "memset", dict(ap=zx.t[:, 0:3], constant=0.0), [], [zx.r0])
    pgi = 0
    for ch in range(8):
        P.dma("sp", zx.t[:, 3:S + 3], zfm[C_RX + ch], writes=[zx.r0])
        P.dma("sp", gz.t[:, :], zfm[C_RG + ch], writes=[gz.r0])
        P.op("dve", "tensor_scalar", dict(out=xa.t[:, :], in0=zx.t[:, 0:S], scalar1=cw.t[:, ch, 0:1], scalar2=cbias.t[:, ch:ch + 1],
                                          op0=ALU.mult, op1=ALU.add), [zx.r0, cw.r0, cbias.r0], [xa.r0])
        for k in range(1, 4):
            P.op("dve", "scalar_tensor_tensor", dict(out=xa.t[:, :], in0=zx.t[:, k:k + S], scalar=cw.t[:, ch, k:k + 1], in1=xa.t[:, :],
                                                     op0=ALU.mult, op1=ALU.add), [zx.r0, cw.r0, xa.r0], [xa.r0])
        P.op("act", "copy", dict(out=xab.t[:, :], in_=xa.t[:, :]), [xa.r0], [xab.r0])
        for (wgt, bia, dst) in ((wrb, brs, rr), (wib, bis, ig)):
            for qc in range(NQC):
                pb = pgi % 4
                pgi += 1
                P.op("pe", "matmul", dict(out=pg.t[:, pb, :], lhsT=wgt.t[:, ch, :], rhs=xab.t[:, qc * 512:(qc + 1) * 512], start=True, stop=True,
                                          skip_group_check=True), [wgt.r0, xab.r0], [pg.r[pb]])
                P.op("act", "activation", dict(out=dst.t[:, qc * 512:(qc + 1) * 512], in_=pg.t[:, pb, :], func=AF.Sigmoid, bias=bia.t[:, ch:ch + 1]),
                     [pg.r[pb], bia.r0], [dst.r0])
        P.op("act", "activation", dict(out=t1.t[:, :], in_=rr.t[:, :], func=AF.Exp, scale=m8.t[:, ch:ch + 1]), [rr.r0, m8.r0], [t1.r0])
        P.op("act", "activation", dict(out=t2.t[:, :], in_=rr.t[:, :], func=AF.Exp, scale=m16.t[:, ch:ch + 1]), [rr.r0, m16.r0], [t2.r0])
        P.op("act", "activation", dict(out=t2.t[:, :], in_=t2.t[:, :], func=AF.Sqrt, scale=-1.0, bias=1.0), [t2.r0], [t2.r0])
        P.op("pool", "tensor_tensor", dict(out=ig.t[:, :], in0=ig.t[:, :], in1=xa.t[:, :], op=ALU.mult), [ig.r0, xa.r0], [ig.r0])
        P.op("pool", "tensor_tensor", dict(out=ig.t[:, :], in0=ig.t[:, :], in1=t2.t[:, :], op=ALU.mult), [ig.r0, t2.r0], [ig.r0])
        P.op("dve", "tensor_tensor_scan", dict(out=rr.t[:, :], data0=t1.t[:, :], data1=ig.t[:, :], initial=0.0, op0=ALU.mult, op1=ALU.add),
             [t1.r0, ig.r0, rr.r0], [rr.r0])
        P.op("dve", "tensor_tensor", dict(out=mab.t[:, :], in0=rr.t[:, :], in1=gz.t[:, :], op=ALU.mult), [rr.r0, gz.r0], [mab.r0])
        P.dma("pool", ma_s[ch], mab.t[:, :], reads=[mab.r0])
    P.emit_stage()
    P.pop()
    if upto == "B":
        P.close()
        return nc

    P.push()
    cosT = P.sbuf("cosT", [128, S], F32)
    sinT = P.sbuf("sinT", [128, S], F32)
    P.push()
    posi = P.sbuf("posi", [128, S], I32)
    tq_ = P.sbuf("tq", [128, S], F32)
    tk_ = P.sbuf("tk", [128, S], F32)
    ki_ = P.sbuf("ki", [128, S], I32)
    invf = P.sbuf("invf", [128, 1], F32)
    sgn = P.sbuf("sgn", [128, 1], F32)
    P.dma("sp", posi.t[:, :], pos_d.partition_broadcast(128), writes=[posi.r0])
    P.dma("sp", invf.t[:, :], invf_d, writes=[invf.r0])
    P.dma("sp", sgn.t[:, :], sgn_d, writes=[sgn.r0])
    P.op("dve", "tensor_copy", dict(out=tq_.t[:, :], in_=posi.t[:, :]), [posi.r0], [tq_.r0])
    P.op("dve", "tensor_scalar", dict(out=tq_.t[:, :], in0=tq_.t[:, :], scalar1=invf.t[:, 0:1], scalar2=float(1.0 / (2 * np.pi)),
                                      op0=ALU.mult, op1=ALU.mult), [tq_.r0, invf.r0], [tq_.r0])
    TWO_PI = float(2 * np.pi * (1 - 1e-6))
    for which, dstT in (("sin", sinT), ("cos", cosT)):
        if which == "cos":
            P.op("dve", "tensor_scalar", dict(out=tq_.t[:, :], in0=tq_.t[:, :], scalar1=0.25, scalar2=None, op0=ALU.add), [tq_.r0], [tq_.r0])
        P.op("dve", "tensor_copy", dict(out=ki_.t[:, :], in_=tq_.t[:, :]), [tq_.r0], [ki_.r0])
        P.op("dve", "tensor_copy", dict(out=tk_.t[:, :], in_=ki_.t[:, :]), [ki_.r0], [tk_.r0])
        P.op("dve", "tensor_tensor", dict(out=tk_.t[:, :], in0=tq_.t[:, :], in1=tk_.t[:, :], op=ALU.subtract), [tq_.r0, tk_.r0], [tk_.r0])
        P.op("act", "activation", dict(out=dstT.t[:, :], in_=tk_.t[:, :], func=AF.Sin, scale=TWO_PI), [tk_.r0], [dstT.r0])
    P.op("dve", "tensor_scalar", dict(out=sinT.t[:, :], in0=sinT.t[:, :], scalar1=sgn.t[:, 0:1], scalar2=None, op0=ALU.mult), [sinT.r0, sgn.r0], [sinT.r0])
    P.emit_stage()
    P.pop()

    ksT = P.sbuf("ksT", [128, S], BF16)
    kwT = P.sbuf("kwT", [128, S], BF16)
    vsa = P.sbuf("vsa", [128, NTT, 129], BF16)
    vwa = P.sbuf("vwa", [128, NTT, 129], BF16)
    kcT = P.sbuf("kcT", [128, 256], BF16)
    vca = P.sbuf("vca", [128, 2, 193], BF16)
    E_s = P.sbuf("E_s", [64, S], BF16)
    cb_s = P.sbuf("cb_s", [128, 4, 512], BF16)
    wb_s = P.sbuf("wb_s", [128, 4, 512], BF16)
    b1s = [P.sbuf(f"b1s{i}", [128, 2], F32) for i in range(2)]
    pes = [P.sbuf(f"pes{i}", [128, 32], F32) for i in range(2)]
    b2k = P.sbuf("b2k", [128, 1], F32)
    b2vb = P.sbuf("b2vb", [128, 128], F32)
    ps_s = P.psum("ps_s", [128, 2, 512], F32, nres=2)
    ps_acc = P.psum("ps_acc", [128, 4, 512], F32, nres=4)
    ps_trb = P.psum("ps_trb", [128, 1024], BF16)
    ps_trf = P.psum("ps_trf", [128, 512], F32)
    P.dma("sp", E_s.t[:, :], E_d, writes=[E_s.r0])
    P.dma("sp", cb_s.t[:], cb_d, writes=[cb_s.r0])
    P.dma("sp", wb_s.t[:], wb_d, writes=[wb_s.r0])
    for i in range(2):
        P.dma("sp", b1s[i].t[:, :], b1_d[i], writes=[b1s[i].r0])
        P.dma("sp", pes[i].t[:, :], pe_d[i], writes=[pes[i].r0])
    P.dma("sp", b2k.t[:, :], b2k_d, writes=[b2k.r0])
    P.dma("sp", b2vb.t[:, :], b2v_d.partition_broadcast(128), writes=[b2vb.r0])
    P.op("dve", "memset", dict(ap=vsa.t[:, :, 128:129], constant=1.0), [], [vsa.r0])
    P.op("dve", "memset", dict(ap=vwa.t[:, :, 128:129], constant=1.0), [], [vwa.r0])
    srot = [0]

    def sbank():
        srot[0] += 1
        return srot[0] % 2

    def mm(out, lhsT, rhs, start, stop, reads, writes):
        P.op("pe", "matmul", dict(out=out, lhsT=lhsT, rhs=rhs, start=start, stop=stop, skip_group_check=True), reads, writes)

    for g in range(2):
        P.push()
        ld = P.sbuf("ld", [128, 2, S], F32, nres=2)
        lohi = P.sbuf("lohi", [128, 2, S], BF16, nres=2)
        w1s = P.sbuf("w1s", [128, 32, 256], BF16)
        w2f = P.sbuf("w2f", [128, 2, 128], F32)
        w2s = P.sbuf("w2s", [128, 2, 128], BF16)
        hidT = P.sbuf("hidT", [128, 2, 256], BF16)
        vb = P.sbuf("vb", [128, S], BF16)
        P.op("dve", "memset", dict(ap=kcT.t[:, :], constant=0.0), [], [kcT.r0])
        P.op("dve", "memset", dict(ap=vca.t[:], constant=0.0), [], [vca.r0])
        P.op("dve", "memset", dict(ap=vca.t[:, :, 128:129], constant=1.0), [vca.r0], [vca.r0])
        P.dma("sp", vca.t[:, :, 129:193], ovl_d, writes=[vca.r0])
        for kvi in range(2):
            ch = C_KV + kvi * 2 + g
            P.dma("sp", ld.t[:, 0, :], zfm[ch], writes=[ld.r[0]])
            P.dma("sp", w1s.t[:], w1bf[kvi], writes=[w1s.r0])
            P.dma("sp", w2f.t[:], w2_d[kvi], writes=[w2f.r0])
            P.op("dve", "tensor_copy", dict(out=w2s.t[:], in_=w2f.t[:]), [w2f.r0], [w2s.r0])
            for v_ in range(2):
                P.op("dve", "tensor_tensor", dict(out=lohi.t[:, v_, :].rearrange("p (n l) -> p n l", l=16),
                                                  in0=ld.t[:, 0, :].rearrange("p (n l) -> p n l", l=16),
                                                  in1=pes[kvi].t[:, v_ * 16:(v_ + 1) * 16].unsqueeze(1).broadcast_to([128, 256, 16]),
                                                  op=ALU.add), [ld.r[0], pes[kvi].r0], [lohi.r[v_]])
            for hc in range(2):
                for l in range(32):
                    v_ = 0 if l < 16 else 1
                    mm(ps_s.t[:, hc, 0:255], w1s.t[:, l, hc * 128:(hc + 1) * 128], lohi.t[:, v_, l:l + 16 * 254 + 1:16],
                       l == 0, l == 31, [w1s.r0, lohi.r[v_]], [ps_s.r[hc]])
                P.op("act", "activation", dict(out=hidT.t[:, hc, 0:255], in_=ps_s.t[:, hc, 0:255], func=AF.Gelu_apprx_tanh,
                                               bias=b1s[kvi].t[:, hc:hc + 1]), [ps_s.r[hc], b1s[kvi].r0], [hidT.r0])
            if kvi == 0:
                for hc in range(2):
                    mm(ps_s.t[:, 0, 0:255], w2s.t[:, hc, :], hidT.t[:, hc, 0:255], hc == 0, hc == 1, [w2s.r0, hidT.r0], [ps_s.r[0]])
                P.op("dve", "tensor_scalar", dict(out=kcT.t[:, 0:255], in0=ps_s.t[:, 0, 0:255], scalar1=b2k.t[:, 0:1], scalar2=None, op0=ALU.add),
                     [ps_s.r[0], b2k.r0], [kcT.r0])
            else:
                for nch in range(2):
                    rows = 128 if nch == 0 else 127
                    for hc in range(2):
                        mm(ps_acc.t[0:rows, nch, 0:128], hidT.t[:, hc, nch * 128:nch * 128 + rows], w2s.t[:, hc, :], hc == 0, hc == 1,
                           [w2s.r0, hidT.r0], [ps_acc.r[nch]])
                    P.op("dve", "tensor_tensor", dict(out=vca.t[0:rows, nch, 0:128], in0=ps_acc.t[0:rows, nch, 0:128], in1=b2vb.t[0:rows, :], op=ALU.add),
                         [ps_acc.r[nch], b2vb.r0], [vca.r0])
        for (kch, swch, dstK) in ((C_KV + 4 + g, C_KSW + g, ksT), (C_KV + 8 + g, C_KSW + 2 + g, kwT)):
            P.dma("sp", ld.t[:, 0, :], zfm[kch], writes=[ld.r[0]])
            P.dma("sp", ld.t[:, 1, :], zfm[swch], writes=[ld.r[1]])
            if debug and g == 1 and dstK is ksT:
                DBG("g1_ld_raw", ld, [128, 2, S])
                DBG("g1_cosT", cosT, [128, S])
                DBG("g1_sinT", sinT, [128, S])
            P.op("dve", "tensor_tensor", dict(out=ld.t[:, 0, :], in0=ld.t[:, 0, :], in1=cosT.t[:, :], op=ALU.mult), [ld.r[0], cosT.r0], [ld.r[0]])
            P.op("pool", "tensor_tensor", dict(out=ld.t[:, 1, :], in0=ld.t[:, 1, :], in1=sinT.t[:, :], op=ALU.mult), [ld.r[1], sinT.r0], [ld.r[1]])
            if debug and g == 1 and dstK is ksT:
                DBG("g1_ld_mul", ld, [128, 2, S])
            P.op("dve", "tensor_tensor", dict(out=dstK.t[:, :], in0=ld.t[:, 0, :], in1=ld.t[:, 1, :], op=ALU.add), [ld.r[0], ld.r[1]], [dstK.r0])
        for (vch, dstV) in ((C_KV + 6 + g, vsa), (C_KV + 10 + g, vwa)):
            P.dma("sp", ld.t[:, 0, :], zfm[vch], writes=[ld.r[0]])
            P.op("act", "copy", dict(out=vb.t[:, :], in_=ld.t[:, 0, :]), [ld.r[0]], [vb.r0])
            for t4 in range(8):
                for a in range(4):
                    tt = t4 * 4 + a
                    P.op("pe", "transpose", dict(out=ps_trb.t[:, a * 128:(a + 1) * 128], in_=vb.t[:, tt * 128:(tt + 1) * 128], identity=identb.t[:, :]),
                         [vb.r0, identb.r0], [ps_trb.r0])
                cast(dstV.t[:, t4 * 4:(t4 + 1) * 4, 0:128], ps_trb.t[:, 0:512].rearrange("p (a d) -> p a d", a=4), [ps_trb.r0], [dstV.r0])
        P.emit_stage()
        P.pop()
        if upto == "C0":
            P.close()
            return nc

        P.push()
        zq = P.sbuf("zq", [128, 4, 512], F32, nres=4)
        zqs = P.sbuf("zqs", [128, 4, 512], F32, nres=4)
        qn = P.sbuf("qn", [128, 4, 512], BF16)
        qr = P.sbuf("qr", [128, 4, 512], BF16)
        cmbq = P.sbuf("cmbq", [128, 2, 512], BF16)
        fa = P.sbuf("fa", [128, 4, 64], F32)
        na = P.sbuf("na", [128, 4, 64], F32)
        vm = P.sbuf("vm", [128, 4, 64], F32)
        gTs = P.sbuf("gTs", [24, 512], F32)
        gts = P.sbuf("gts", [128, 4, 24], F32)
        pc = P.sbuf("pc", [128, 2, 2, 512], BF16, nres=2)
        pt = P.sbuf("pt", [128, 3, 512], BF16, nres=3)
        oacc = P.sbuf("oacc", [128, 4, 4, 128], F32)
        imp = P.sbuf("imp", [128, 4, 64], F32)
        impp = P.sbuf("impp", [128, 64], F32)
        imp2 = P.sbuf("imp2", [128, 64], F32)
        m8a = P.sbuf("m8a", [128, 8], F32)
        m8b = P.sbuf("m8b", [128, 8], F32)
        msel = P.sbuf("msel", [128, 64], F32)
        bmb = P.sbuf("bmb", [128, 64], BF16)
        biasT = P.sbuf("biasT", [64, 512], BF16)
        sm = P.sbuf("sm", [128, 8], F32)
        obb = P.sbuf("obb", [128, 4, 4, 128], BF16)
        obT = P.sbuf("obT", [128, 4, 512], BF16)
        ptr = [0]
        aset = [0]
        for qc in range(NQC):
            q0 = qc * 512
            for hq in range(4):
                P.dma("sp", zq.t[:, hq, :], zfm[C_Q + 4 * g + hq, :, q0:q0 + 512], writes=[zq.r[hq]])
                P.dma("sp", zqs.t[:, hq, :], zfm[C_QS + 4 * g + hq, :, q0:q0 + 512], writes=[zqs.r[hq]])
            P.dma("sp", cmbq.t[:], cmb_d[:, :, q0:q0 + 512], writes=[cmbq.r0])
            P.dma("sp", fa.t[:], fa_d[:, qc * 4:qc * 4 + 4, :], writes=[fa.r0])
            P.dma("sp", na.t[:], na_d[:, qc * 4:qc * 4 + 4, :], writes=[na.r0])
            P.dma("sp", vm.t[:], vm_d[:, qc * 4:qc * 4 + 4, :], writes=[vm.r0])
            P.dma("sp", gTs.t[:, :], zfm[C_NG, 0:24, q0:q0 + 512], writes=[gTs.r0])
            P.op("act", "copy", dict(out=qn.t[:], in_=zq.t[:]), zq.r, [qn.r0])
            cs = cosT.t[:, q0:q0 + 512].unsqueeze(1).broadcast_to([128, 4, 512])
            sn = sinT.t[:, q0:q0 + 512].unsqueeze(1).broadcast_to([128, 4, 512])
            P.op("dve", "tensor_tensor", dict(out=zq.t[:], in0=zq.t[:], in1=cs, op=ALU.mult), zq.r + [cosT.r0], zq.r)
            P.op("pool", "tensor_tensor", dict(out=zqs.t[:], in0=zqs.t[:], in1=sn, op=ALU.mult), zqs.r + [sinT.r0], zqs.r)
            P.op("dve", "tensor_tensor", dict(out=qr.t[:], in0=zq.t[:], in1=zqs.t[:], op=ALU.add), zq.r + zqs.r, [qr.r0])
            for qt in range(4):
                P.op("pe", "transpose", dict(out=ps_trf.t[:, qt * 24:(qt + 1) * 24], in_=gTs.t[0:24, qt * 128:(qt + 1) * 128], identity=identf.t[0:24, 0:24]),
                     [gTs.r0, identf.r0], [ps_trf.r0])
            P.op("dve", "tensor_copy", dict(out=gts.t[:].rearrange("p a c -> p (a c)"), in_=ps_trf.t[:, 0:96]), [ps_trf.r0], [gts.r0])

            def post(hh, qt, bank, off, br, first, aux=None):
                h = 4 * g + hh
                accv = ps_acc.t[:, bank, :]
                P.op("dve", "tensor_scalar", dict(out=sm.t[:, 0:1], in0=accv[:, off + 128:off + 129], scalar1=1e-30, scalar2=None, op0=ALU.max),
                     [ps_acc.r[bank]], [sm.r0])
                P.op("dve", "reciprocal", dict(out=sm.t[:, 1:2], in_=sm.t[:, 0:1]), [sm.r0], [sm.r0])
                P.op("dve", "tensor_tensor", dict(out=sm.t[:, 2:3], in0=sm.t[:, 1:2], in1=gts.t[:, qt, h * 3 + br:h * 3 + br + 1], op=ALU.mult),
                     [sm.r0, gts.r0], [sm.r0])
                if first:
                    P.op("dve", "tensor_scalar", dict(out=oacc.t[:, qt, hh, :], in0=accv[:, off:off + 128], scalar1=sm.t[:, 2:3], scalar2=None, op0=ALU.mult),
                         [ps_acc.r[bank], sm.r0], [oacc.r0])
                else:
                    P.op("dve", "scalar_tensor_tensor", dict(out=oacc.t[:, qt, hh, :], in0=accv[:, off:off + 128], scalar=sm.t[:, 2:3], in1=oacc.t[:, qt, hh, :],
                                                             op0=ALU.mult, op1=ALU.add), [ps_acc.r[bank], sm.r0, oacc.r0], [oacc.r0])
                if aux is not None:
                    if hh == 0:
                        P.op("dve", "tensor_scalar", dict(out=imp.t[:, qt, :], in0=accv[:, off + 129:off + 193], scalar1=sm.t[:, 1:2], scalar2=None, op0=ALU.mult),
                             [ps_acc.r[bank], sm.r0], [imp.r0])
                    else:
                        P.op("dve", "scalar_tensor_tensor", dict(out=imp.t[:, qt, :], in0=accv[:, off + 129:off + 193], scalar=sm.t[:, 1:2], in1=imp.t[:, qt, :],
                                                                 op0=ALU.mult, op1=ALU.add), [ps_acc.r[bank], sm.r0, imp.r0], [imp.r0])

            if (g, qc) in ((0, 0), (1, 1)):
                tg = f"g{g}q{qc}"
                DBG(tg + "_gts", gts, [128, 4, 24]); DBG(tg + "_qn", qn, [128, 4, 512], BF16); DBG(tg + "_qr", qr, [128, 4, 512], BF16)
                DBG(tg + "_kcT", kcT, [128, 256], BF16); DBG(tg + "_vca", vca, [128, 2, 193], BF16)
                DBG(tg + "_ksT", ksT, [128, S], BF16); DBG(tg + "_kwT", kwT, [128, S], BF16)
                DBG(tg + "_vsa", vsa, [128, NTT, 129], BF16); DBG(tg + "_vwa", vwa, [128, NTT, 129], BF16)
            n_nch = 2 if (q0 + 511 >= 16 * 128 + 31) else 1
            for hh in range(4):
                pcb = hh % 2
                for nch in range(n_nch):
                    sb = sbank()
                    mm(ps_s.t[:, sb, :], kcT.t[:, nch * 128:(nch + 1) * 128], qn.t[:, hh, :], True, False, [kcT.r0, qn.r0], [ps_s.r[sb]])
                    mm(ps_s.t[:, sb, :], identb.t[:, :], cmbq.t[:, nch, :], False, True, [identb.r0, cmbq.r0], [ps_s.r[sb]])
                    P.op("act", "activation", dict(out=pc.t[:, pcb, nch, :], in_=ps_s.t[:, sb, :], func=AF.Exp, scale=SCALE), [ps_s.r[sb]], [pc.r[pcb]])
                aset[0] ^= 1
                started = {}
                for qt in range(4):
                    bank = aset[0] * 2 + qt // 2
                    off = (qt % 2) * 193
                    for nch in range(n_nch):
                        mm(ps_acc.t[:, bank, off:off + 193], pc.t[:, pcb, nch, qt * 128:(qt + 1) * 128], vca.t[:, nch, :],
                           bank not in started, False, [pc.r[pcb], vca.r0], [ps_acc.r[bank]])
                        started[bank] = 1
                for qt in range(4):
                    post(hh, qt, aset[0] * 2 + qt // 2, (qt % 2) * 193, 0, True, aux=True)
            if (g, qc) in ((0, 0), (1, 1)):
                DBG(tg + "_oacc_c", oacc, [128, 4, 4, 128]); DBG(tg + "_imp", imp, [128, 4, 64])
            for qt in range(4):
                P.op("dve", "tensor_tensor", dict(out=impp.t[:, :], in0=imp.t[:, qt, :], in1=fa.t[:, qt, :], op=ALU.max), [imp.r0, fa.r0], [impp.r0])
                P.op("dve", "tensor_tensor", dict(out=impp.t[:, :], in0=impp.t[:, :], in1=na.t[:, qt, :], op=ALU.add), [impp.r0, na.r0], [impp.r0])
                P.op("dve", "max", dict(out=m8a.t[:, :], in_=impp.t[:, :]), [impp.r0], [m8a.r0])
                P.op("dve", "match_replace", dict(out=imp2.t[:, :], in_to_replace=m8a.t[:, :], in_values=impp.t[:, :], imm_value=-3.0e38),
                     [impp.r0, m8a.r0], [imp2.r0])
                P.op("dve", "max", dict(out=m8b.t[:, :], in_=imp2.t[:, :]), [imp2.r0], [m8b.r0])
                P.op("dve", "scalar_tensor_tensor", dict(out=msel.t[:, :], in0=impp.t[:, :], scalar=m8b.t[:, 7:8], in1=vm.t[:, qt, :],
                                                         op0=ALU.is_ge, op1=ALU.mult), [impp.r0, m8b.r0, vm.r0], [msel.r0])
                P.op("dve", "tensor_scalar", dict(out=bmb.t[:, :], in0=msel.t[:, :], scalar1=-NEG, scalar2=NEG, op0=ALU.mult, op1=ALU.add),
                     [msel.r0], [bmb.r0])
                P.op("pe", "transpose", dict(out=ps_trb.t[0:64, qt * 128:(qt + 1) * 128], in_=bmb.t[:, :], identity=identb.t[:, :]),
                     [bmb.r0, identb.r0], [ps_trb.r0])
            P.op("dve", "tensor_copy", dict(out=biasT.t[:, :], in_=ps_trb.t[0:64, 0:512]), [ps_trb.r0], [biasT.r0])
            if (g, qc) in ((0, 0), (1, 1)):
                DBG(tg + "_biasT", biasT, [64, 512], BF16)
            for br in (2, 1):
                for hh in range(4):
                    aset[0] ^= 1
                    started = {}
                    if br == 1:
                        kts = list(range(0, qc * 4 + 4))
                    else:
                        kts = list(range(max(0, qc * 4 - 4), qc * 4 + 4))
                    vsrc = vsa if br == 1 else vwa

                    def emit_scores(kt):
                        r = kt - qc * 4
                        sb = sbank()
                        if br == 1:
                            mm(ps_s.t[:, sb, :], ksT.t[:, kt * 128:(kt + 1) * 128], qr.t[:, hh, :], True, False, [ksT.r0, qr.r0], [ps_s.r[sb]])
                            mm(ps_s.t[:, sb, :], E_s.t[0:64, kt * 128:(kt + 1) * 128], biasT.t[0:64, :], False, r < 0, [E_s.r0, biasT.r0], [ps_s.r[sb]])
                            if r >= 0:
                                mm(ps_s.t[:, sb, :], identb.t[:, :], cb_s.t[:, r, :], False, True, [identb.r0, cb_s.r0], [ps_s.r[sb]])
                        else:
                            mm(ps_s.t[:, sb, :], kwT.t[:, kt * 128:(kt + 1) * 128], qr.t[:, hh, :], True, False, [kwT.r0, qr.r0], [ps_s.r[sb]])
                            btab = wb_s.t[:, r + 4, :] if r < 0 else cb_s.t[:, r, :]
                            mm(ps_s.t[:, sb, :], identb.t[:, :], btab, False, True, [identb.r0, cb_s.r0, wb_s.r0], [ps_s.r[sb]])
                        pb = ptr[0] % 3
                        ptr[0] += 1
                        P.op("act", "activation", dict(out=pt.t[:, pb, :], in_=ps_s.t[:, sb, :], func=AF.Exp, scale=SCALE), [ps_s.r[sb]], [pt.r[pb]])
                        return (kt, pb)

                    def emit_pv(kt, pb):
                        r = kt - qc * 4
                        for qt in range(4):
                            if r > qt:
                                continue
                            if br == 2 and r < qt - 4:
                                continue
                            bank = aset[0] * 2 + qt // 2
                            off = (qt % 2) * 129
                            mm(ps_acc.t[:, bank, off:off + 129], pt.t[:, pb, qt * 128:(qt + 1) * 128], vsrc.t[:, kt, :],
                               bank not in started, False, [pt.r[pb], vsrc.r0], [ps_acc.r[bank]])
                            started[bank] = 1

                    pend = None
                    for kt in kts:
                        cur = emit_scores(kt)
                        if pend is not None:
                            emit_pv(*pend)
                        pend = cur
                    emit_pv(*pend)
                    for qt in range(4):
                        post(hh, qt, aset[0] * 2 + qt // 2, (qt % 2) * 129, br, False)
            if (g, qc) in ((0, 0), (1, 1)):
                DBG(tg + "_oacc_w", oacc, [128, 4, 4, 128])
            P.op("act", "copy", dict(out=obb.t[:], in_=oacc.t[:]), [oacc.r0], [obb.r0])
            for hh in range(4):
                for qt in range(4):
                    P.op("pe", "transpose", dict(out=ps_trb.t[:, qt * 128:(qt + 1) * 128], in_=obb.t[:, qt, hh, :], identity=identb.t[:, :]),
                         [obb.r0, identb.r0], [ps_trb.r0])
                cast(obT.t[:, hh, :], ps_trb.t[:, 0:512], [ps_trb.r0], [obT.r0])
            for hq in range(4):
                P.dma("pool", obt_s[4 * g + hq, :, q0:q0 + 512], obT.t[:, hq, :], reads=[obT.r0])
        P.emit_stage()
        P.pop()
    P.pop()
    if upto == "C":
        P.close()
        return nc

    def rmsnorm_tile(src, gain, dst_f32, dst_bf, sq, junk_):
        P.op("act", "activation", dict(out=junk_.t[:, :], in_=src.t[:, :], func=AF.Square, accum_out=sq.t[:, 0:1]), [src.r0], [junk_.r0, sq.r0])
        P.op("act", "activation", dict(out=sq.t[:, 1:2], in_=sq.t[:, 0:1], func=AF.Sqrt, scale=1.0 / D, bias=1e-6), [sq.r0], [sq.r0])
        P.op("dve", "reciprocal", dict(out=sq.t[:, 2:3], in_=sq.t[:, 1:2]), [sq.r0], [sq.r0])
        P.op("dve", "scalar_tensor_tensor", dict(out=dst_f32.t[:, :], in0=src.t[:, :], scalar=sq.t[:, 2:3], in1=gain.t[:, :], op0=ALU.mult, op1=ALU.mult),
             [src.r0, sq.r0, gain.r0], [dst_f32.r0])
        if dst_bf is not None:
            P.op("act", "copy", dict(out=dst_bf.t[:, :], in_=dst_f32.t[:, :]), [dst_f32.r0], [dst_bf.r0])

    P.push()
    w3 = [P.sbuf(f"w3s{i}", [128, 8, D], BF16) for i in range(3)]
    for i in range(3):
        P.dma("sp", w3[i].t[:], w5bf[i], writes=[w3[i].r0])
    wA, wB, wO = w3
    obTq = P.sbuf("obTq", [128, 8, 512], BF16, nres=8)
    maTq = P.sbuf("maTq", [128, 8, 512], BF16, nres=8)
    mgT = P.sbuf("mgT", [128, 8, 512], BF16)
    g01 = P.sbuf("g01", [128, 2, 2, 512], F32, nres=2)
    tA = P.sbuf("tA", [128, 2, 512], F32, nres=2)
    tB = P.sbuf("tB", [128, 2, 512], F32, nres=2)
    xtl = P.sbuf("xtl", [128, 2, D], F32, nres=2)
    h1t = P.sbuf("h1t", [128, 2, D], F32, nres=2)
    ps_y = P.psum("ps_y", [128, 4, 512], F32, nres=4)
    ps_h = P.psum("ps_h", [128, 2, 512], F32, nres=2)
    ti = 0
    for qc in range(NQC):
        q0 = qc * 512
        for c8 in range(8):
            P.dma("sp", obTq.t[:, c8, :], obt_s[c8, :, q0:q0 + 512], writes=[obTq.r[c8]])
            P.dma("sp", maTq.t[:, c8, :], ma_s[c8, :, q0:q0 + 512], writes=[maTq.r[c8]])
        for c in range(8):
            b = c % 2
            P.dma("sp", g01.t[:, b, 0, :], zfm[C_M + c, :, q0:q0 + 512], writes=[g01.r[b]])
            P.dma("sp", g01.t[:, b, 1, :], zfm[C_M + 8 + c, :, q0:q0 + 512], writes=[g01.r[b]])
            for k in range(8):
                mm(ps_y.t[:, 2 * b, :], wA.t[:, k, c * 128:(c + 1) * 128], maTq.t[:, k, :], k == 0, k == 7, [wA.r0, maTq.r[k]], [ps_y.r[2 * b]])
            for k in range(8):
                mm(ps_y.t[:, 2 * b + 1, :], wB.t[:, k, c * 128:(c + 1) * 128], obTq.t[:, k, :], k == 0, k == 7, [wB.r0, obTq.r[k]], [ps_y.r[2 * b + 1]])
            P.op("dve", "tensor_tensor", dict(out=tA.t[:, b, :], in0=ps_y.t[:, 2 * b, :], in1=g01.t[:, b, 0, :], op=ALU.mult), [ps_y.r[2 * b], g01.r[b]], [tA.r[b]])
            P.op("dve", "tensor_tensor", dict(out=tB.t[:, b, :], in0=ps_y.t[:, 2 * b + 1, :], in1=g01.t[:, b, 1, :], op=ALU.mult), [ps_y.r[2 * b + 1], g01.r[b]], [tB.r[b]])
            P.op("pool", "tensor_tensor", dict(out=mgT.t[:, c, :], in0=tA.t[:, b, :], in1=tB.t[:, b, :], op=ALU.add), [tA.r[b], tB.r[b]], [mgT.r0])
        for qt in range(4):
            tt = qc * 4 + qt
            b = ti % 2
            ti += 1
            P.dma("sp", xtl.t[:, b, :], x_d[tt * 128:(tt + 1) * 128, :], writes=[xtl.r[b]])
            for half in range(2):
                for c in range(8):
                    mm(ps_h.t[:, half, :], mgT.t[:, c, qt * 128:(qt + 1) * 128], wO.t[:, c, half * 512:(half + 1) * 512], c == 0, c == 7, [mgT.r0, wO.r0], [ps_h.r[half]])
                P.op("dve", "tensor_tensor", dict(out=h1t.t[:, b, half * 512:(half + 1) * 512], in0=ps_h.t[:, half, :], in1=xtl.t[:, b, half * 512:(half + 1) * 512], op=ALU.add),
                     [ps_h.r[half], xtl.r[b]], [h1t.r[b]])
            P.dma("pool", h1_s[tt * 128:(tt + 1) * 128, :], h1t.t[:, b, :], reads=[h1t.r[b]])
    P.emit_stage()
    P.pop()
    if upto == "D1":
        P.close()
        return nc

    P.push()
    wQ = P.sbuf("wQ", [128, 8, D], BF16)
    wG = P.sbuf("wG", [128, 8, D], BF16)
    wPs = P.sbuf("wPs", [128, 2, D], BF16)
    P.dma("sp", wQ.t[:], w5bf[3], writes=[wQ.r0])
    P.dma("sp", wG.t[:], w5bf[4], writes=[wG.r0])
    P.dma("sp", wPs.t[:], wpbf, writes=[wPs.r0])
    kbdf = P.sbuf("kbdf", [128, 256], F32)
    kbdb = P.sbuf("kbdb", [128, 256], BF16)
    P.dma("sp", kbdf.t[:, :], kbd_d, writes=[kbdf.r0])
    P.op("dve", "tensor_copy", dict(out=kbdb.t[:, :], in_=kbdf.t[:, :]), [kbdf.r0], [kbdb.r0])
    gains = []
    for nm, gd in (("gfb", gf_d), ("gpb", gp_d), ("glb", gl_d)):
        t_ = P.sbuf(nm, [128, D], F32)
        P.dma("sp", t_.t[:, :], gd.partition_broadcast(128), writes=[t_.r0])
        gains.append(t_)
    gfb, gpb, glb = gains
    io128 = P.sbuf("io128", [128, 128], I32)
    io256 = P.sbuf("io256", [128, 256], I32)
    io16 = P.sbuf("io16", [128, 16], F32)
    P.dma("sp", io128.t[:, :], io128_d[:, 0, :], writes=[io128.r0])
    P.dma("sp", io256.t[:, :], io256_d[:, 0, :], writes=[io256.r0])
    P.dma("sp", io16.t[:, :], io16_d[:, 0, 0, :], writes=[io16.r0])
    NG = int(_os.environ.get("K_NG", "13"))

    def dbl(name, shape, dt):
        return [P.sbuf(f"{name}{i}", shape, dt) for i in range(2)]

    hh2 = [P.sbuf(f"hh{i}", [128, D], F32) for i in range(3)]
    ptl2 = [P.sbuf(f"ptl{i}", [128, 256], F32) for i in range(3)]
    xn2 = dbl("xn", [128, D], F32)
    eidx2 = dbl("eidx", [128, 128], I32)
    gte2 = dbl("gte", [128, 128], F32)
    ptb = P.sbuf("ptb", [128, 256], BF16)
    pT = P.sbuf("pT", [128, 2, 128], BF16)
    xnb2 = dbl("xnb", [128, D], BF16)
    prodb = P.sbuf("prodb", [128, 4, D], BF16, nres=4)
    junkA = P.sbuf("junkA", [128, D], BF16)
    xnT = P.sbuf("xnT", [128, 8, 128], BF16)
    xne = P.sbuf("xne", [128, D], F32)
    xnbe = P.sbuf("xnbe", [128, D], BF16)
    xnTe = P.sbuf("xnTe", [128, 8, 128], BF16)
    qpT = P.sbuf("qpT", [128, 8, 128], BF16)
    junkF = P.sbuf("junkF", [128, D], BF16)
    junkG = P.sbuf("junkG", [128, D], BF16)
    junkE = P.sbuf("junkE", [128, D], BF16)
    sqF = P.sbuf("sqF", [128, 4], F32)
    sqE = P.sbuf("sqE", [128, 4], F32)
    s_all = P.sbuf("s_all", [128, 16, 128], F32)
    stmp = P.sbuf("stmp", [128, 256], F32)
    v12 = P.sbuf("v12", [128, 16, 16], F32)
    i12i = P.sbuf("i12i", [128, 16, 16], I32)
    i12f = P.sbuf("i12f", [128, 16, 16], F32)
    cand = P.sbuf("cand", [128, 8, 256], F32)
    oh = cand
    best = P.sbuf("best", [128, 8, 16], F32)
    posi2 = P.sbuf("posi2", [128, 8, 16], I32)
    abi = P.sbuf("abi", [128, 2, 8, 16], I32)
    abf = P.sbuf("abf", [128, 2, 8, 16], F32)
    isel = P.sbuf("isel", [128, 2, 8, 16], F32)
    ef = P.sbuf("ef", [128, 128], F32)
    bex = P.sbuf("bex", [128, 8, 16], F32)
    bsm = P.sbuf("bsm", [128, 2, 8], F32)
    dots = P.sbuf("dots", [128, 128], F32, nres=128)
    actv = P.sbuf("actv", [128, 128], F32, nres=32)
    a1 = P.sbuf("a1", [128, 128], F32, nres=32)
    ug = P.sbuf("ug", [128, NG, 2 * D], BF16, nres=NG)
    tmpv = P.sbuf("tmpv", [128, 4, D], BF16, nres=4)
    gate = P.sbuf("gate", [128, D], F32)
    tmpg = P.sbuf("tmpg", [128, 512], F32)
    ot = P.sbuf("ot", [128, D], F32)
    ps_t = P.psum("ps_t2", [128, 1024], BF16)
    ps_q = P.psum("ps_q", [128, 2, 512], F32, nres=2)
    ps_c = P.psum("ps_c", [128, 2, 512], F32, nres=2)
    ps_o = P.psum("ps_o", [128, 2, 512], F32, nres=2)
    cnt = {"gi": 0, "tvi": 0}

    def transpose8(src_bf, dst, n):
        for c in range(n):
            P.op("pe", "transpose", dict(out=ps_t.t[:, c * 128:(c + 1) * 128], in_=src_bf.t[:, c * 128:(c + 1) * 128], identity=identb.t[:, :]),
                 [src_bf.r0, identb.r0], [ps_t.r0])
        cast(dst.t[:, 0:n, :], ps_t.t[:, 0:n * 128].rearrange("p (c t) -> p c t", c=n), [ps_t.r0], [dst.r0])

    v12v = v12.t[:].rearrange("p (h c) k -> p h c k", c=2)
    i12v = i12f.t[:].rearrange("p (h c) k -> p h c k", c=2)

    def front(tt):
        par = tt % 2
        hh_, ptl, xn, eidx, gte = hh2[tt % 3], ptl2[tt % 3], xn2[par], eidx2[par], gte2[par]
        xnb = xnb2[par]
        P.dma("sp", hh_.t[:, :], h1_s[tt * 128:(tt + 1) * 128, :], writes=[hh_.r0])
        P.dma("sp", ptl.t[:, :], p_d[tt * 128:(tt + 1) * 128, :], writes=[ptl.r0])
        rmsnorm_tile(hh_, gfb, xn, xnb, sqF, junkF)
        yield
        transpose8(xnb, xnT, 8)
        yield
        for hd in range(8):
            bk = hd // 4
            for k in range(8):
                mm(ps_q.t[:, bk, (hd % 4) * 128:(hd % 4 + 1) * 128], wQ.t[:, k, hd * 128:(hd + 1) * 128], xnT.t[:, k, :],
                   (k == 0 and hd % 4 == 0), False, [wQ.r0, xnT.r0], [ps_q.r[bk]])
        for bk in range(2):
            cast(qpT.t[:, bk * 4:(bk + 1) * 4, :], ps_q.t[:, bk, :].rearrange("p (a t) -> p a t", a=4), [ps_q.r[bk]], [qpT.r0])
        for rnd in range(2):
            for h4 in range(4):
                hd = rnd * 4 + h4
                bk = h4 // 2
                mm(ps_c.t[:, bk, (hd % 2) * 256:(hd % 2 + 1) * 256], qpT.t[:, hd, :], kbdb.t[:, :], hd % 2 == 0, False, [qpT.r0, kbdb.r0], [ps_c.r[bk]])
            for bk in range(2):
                cast(s_all.t[:, rnd * 8 + bk * 4:rnd * 8 + (bk + 1) * 4, :], ps_c.t[:, bk, :].rearrange("p (a n) -> p a n", a=4), [ps_c.r[bk]], [s_all.r0])
            yield
        sI = s_all.t[:].bitcast(I32)
        P.op("dve", "tensor_scalar", dict(out=sI, in0=sI, scalar1=-128, scalar2=None, op0=ALU.bitwise_and), [s_all.r0], [s_all.r0])
        P.op("dve", "tensor_tensor", dict(out=sI, in0=sI, in1=io128.t[:, :].unsqueeze(1).broadcast_to([128, 16, 128]), op=ALU.bitwise_or), [s_all.r0, io128.r0], [s_all.r0])
        yield
        for j in range(16):
            P.op("dve", "max", dict(out=v12.t[:, j, 0:8], in_=s_all.t[:, j, :]), [s_all.r0], [v12.r0])
            P.op("dve", "match_replace", dict(out=stmp.t[:, 0:128], in_to_replace=v12.t[:, j, 0:8], in_values=s_all.t[:, j, :], imm_value=-3.0e38),
                 [s_all.r0, v12.r0], [stmp.r0])
            P.op("dve", "max", dict(out=v12.t[:, j, 8:16], in_=stmp.t[:, 0:128]), [stmp.r0], [v12.r0])
            if j % 2 == 1:
                yield
        P.op("dve", "tensor_scalar", dict(out=i12i.t[:], in0=v12.t[:].bitcast(I32), scalar1=127, scalar2=None, op0=ALU.bitwise_and), [v12.r0], [i12i.r0])
        P.op("dve", "tensor_copy", dict(out=i12f.t[:], in_=i12i.t[:]), [i12i.r0], [i12f.r0])
        candv = cand.t[:].rearrange("p h (a b) -> p h a b", a=16)
        P.op("dve", "tensor_tensor", dict(out=candv, in0=v12v[:, :, 0, :].unsqueeze(3).broadcast_to([128, 8, 16, 16]),
                                          in1=v12v[:, :, 1, :].unsqueeze(2).broadcast_to([128, 8, 16, 16]), op=ALU.add), [v12.r0], [cand.r0])
        yield
        cI = cand.t[:].bitcast(I32)
        P.op("dve", "tensor_scalar", dict(out=cI, in0=cI, scalar1=-256, scalar2=None, op0=ALU.bitwise_and), [cand.r0], [cand.r0])
        P.op("dve", "tensor_tensor", dict(out=cI, in0=cI, in1=io256.t[:, :].unsqueeze(1).broadcast_to([128, 8, 256]), op=ALU.bitwise_or), [cand.r0, io256.r0], [cand.r0])
        yield
        for hd in range(8):
            P.op("dve", "max", dict(out=best.t[:, hd, 0:8], in_=cand.t[:, hd, :]), [cand.r0], [best.r0])
            P.op("dve", "match_replace", dict(out=stmp.t[:, :], in_to_replace=best.t[:, hd, 0:8], in_values=cand.t[:, hd, :], imm_value=-3.0e38),
                 [cand.r0, best.r0], [stmp.r0])
            P.op("dve", "max", dict(out=best.t[:, hd, 8:16], in_=stmp.t[:, :]), [stmp.r0], [best.r0])
            if hd % 2 == 1:
                yield
        P.op("dve", "tensor_scalar", dict(out=posi2.t[:], in0=best.t[:].bitcast(I32), scalar1=255, scalar2=None, op0=ALU.bitwise_and), [best.r0], [posi2.r0])
        P.op("dve", "tensor_scalar", dict(out=abi.t[:, 0], in0=posi2.t[:], scalar1=4, scalar2=None, op0=ALU.arith_shift_right), [posi2.r0], [abi.r0])
        P.op("dve", "tensor_scalar", dict(out=abi.t[:, 1], in0=posi2.t[:], scalar1=15, scalar2=None, op0=ALU.bitwise_and), [posi2.r0, abi.r0], [abi.r0])
        P.op("dve", "tensor_copy", dict(out=abf.t[:], in_=abi.t[:]), [abi.r0], [abf.r0])
        yield
        ohv = oh.t[:].rearrange("p h (a b) -> p h a b", a=16)
        for c in range(2):
            P.op("dve", "tensor_tensor", dict(out=ohv, in0=abf.t[:, c].unsqueeze(3).broadcast_to([128, 8, 16, 16]),
                                              in1=io16.t[:, :].unsqueeze(1).unsqueeze(1).broadcast_to([128, 8, 16, 16]), op=ALU.is_equal),
                 [abf.r0, io16.r0], [oh.r0])
            P.op("dve", "tensor_tensor", dict(out=ohv, in0=ohv, in1=i12v[:, :, c, :].unsqueeze(2).broadcast_to([128, 8, 16, 16]), op=ALU.mult),
                 [oh.r0, i12f.r0], [oh.r0])
            P.op("dve", "tensor_reduce", dict(out=isel.t[:, c], in_=ohv, axis=AX.X, op=ALU.add), [oh.r0], [isel.r0])
            yield
        P.op("dve", "scalar_tensor_tensor", dict(out=ef.t[:, :], in0=isel.t[:, 0].rearrange("p h k -> p (h k)"), scalar=128.0,
                                                 in1=isel.t[:, 1].rearrange("p h k -> p (h k)"), op0=ALU.mult, op1=ALU.add), [isel.r0], [ef.r0])
        P.op("dve", "tensor_copy", dict(out=eidx.t[:, :], in_=ef.t[:, :]), [ef.r0], [eidx.r0])
        P.op("dve", "tensor_tensor", dict(out=bex.t[:], in0=best.t[:], in1=best.t[:, :, 0:1].broadcast_to([128, 8, 16]), op=ALU.subtract), [best.r0], [bex.r0])
        P.op("act", "activation", dict(out=bex.t[:], in_=bex.t[:], func=AF.Exp), [bex.r0], [bex.r0])
        yield
        P.op("dve", "tensor_reduce", dict(out=bsm.t[:, 0], in_=bex.t[:], axis=AX.X, op=ALU.add), [bex.r0], [bsm.r0])
        P.op("dve", "reciprocal", dict(out=bsm.t[:, 1], in_=bsm.t[:, 0]), [bsm.r0], [bsm.r0])
        P.op("dve", "tensor_tensor", dict(out=gte.t[:, :].rearrange("p (h k) -> p h k", h=8), in0=bex.t[:],
                                          in1=bsm.t[:, 1].unsqueeze(2).broadcast_to([128, 8, 16]), op=ALU.mult), [bex.r0, bsm.r0], [gte.r0])
        yield

    def gather_group(tt, grp):
        par = tt % 2
        xn, eidx, gte = xn2[par], eidx2[par], gte2[par]
        bufs = []
        for j in range(4):
            slot = grp * 4 + j
            b = cnt["gi"] % NG
            cnt["gi"] += 1
            bufs.append(b)
            P.dma("pool", ug.t[:, b, :], uv_s[:, :], reads=[eidx.r0], writes=[ug.r[b]],
                  indirect=dict(out_offset=None, in_offset=bass.IndirectOffsetOnAxis(ap=eidx.t[:, slot:slot + 1], axis=0)))
            if j % 2 == 0:
                P.op("dve", "scalar_tensor_tensor", dict(out=junkG.t[:, :], in0=ug.t[:, b, 0:D], scalar=1.0, in1=xn.t[:, :],
                                                         op0=ALU.mult, op1=ALU.mult, accum_out=dots.t[:, slot:slot + 1]),
                     [ug.r[b], xn.r0], [dots.r[slot]])
            else:
                pb_ = cnt.get("pbi", 0) % 4
                cnt["pbi"] = cnt.get("pbi", 0) + 1
                P.op("dve", "tensor_tensor", dict(out=prodb.t[:, pb_, :], in0=ug.t[:, b, 0:D], in1=xnb2[par].t[:, :], op=ALU.mult),
                     [ug.r[b], xnb2[par].r0], [prodb.r[pb_]])
                P.op("act", "activation", dict(out=junkA.t[:, :], in_=prodb.t[:, pb_, :], func=AF.Copy, accum_out=dots.t[:, slot:slot + 1]),
                     [prodb.r[pb_]], [dots.r[slot]])
        if _os.environ.get("K_NOCONS") or _os.environ.get("K_DOTSONLY"):
            return
        if cnt.get("pend") is not None:
            finish_group(*cnt["pend"])
        gs = slice(grp * 4, grp * 4 + 4)
        P.op("act", "activation", dict(out=a1.t[:, gs], in_=dots.t[:, gs], func=AF.Gelu_apprx_tanh), dots.r[grp * 4:grp * 4 + 4], [a1.r[grp]])
        cnt["pend"] = (tt, grp, bufs)
        if grp == 31:
            finish_group(*cnt["pend"])
            cnt["pend"] = None

    def finish_group(tt, grp, bufs):
        gte = gte2[tt % 2]
        gs = slice(grp * 4, grp * 4 + 4)
        P.op("dve", "tensor_tensor", dict(out=actv.t[:, gs], in0=a1.t[:, gs], in1=gte.t[:, gs], op=ALU.mult), [a1.r[grp], gte.r0], [actv.r[grp]])
        for j in range(4):
            slot = grp * 4 + j
            b = bufs[j]
            tb = cnt["tvi"] % 4
            cnt["tvi"] += 1
            P.op("act", "activation", dict(out=tmpv.t[:, tb, :], in_=ug.t[:, b, D:2 * D], func=AF.Copy, scale=actv.t[:, slot:slot + 1]),
                 [ug.r[b], actv.r[grp]], [tmpv.r[tb]])
            for half in range(2):
                mm(ps_o.t[:, half, :], identb.t[:, :], tmpv.t[:, tb, half * 512:(half + 1) * 512], slot == 0, slot == 127,
                   [identb.r0, tmpv.r[tb]], [ps_o.r[half]])

    def epilogue(tt):
        hh_, ptl = hh2[tt % 3], ptl2[tt % 3]
        for half in range(2):
            P.op("dve", "tensor_tensor", dict(out=hh_.t[:, half * 512:(half + 1) * 512], in0=ps_o.t[:, half, :], in1=hh_.t[:, half * 512:(half + 1) * 512], op=ALU.add),
                 [ps_o.r[half], hh_.r0], [hh_.r0])
        yield
        rmsnorm_tile(hh_, gpb, xne, xnbe, sqE, junkE)
        yield
        transpose8(xnbe, xnTe, 8)
        yield
        P.op("act", "copy", dict(out=ptb.t[:, :], in_=ptl.t[:, :]), [ptl.r0], [ptb.r0])
        transpose8(ptb, pT, 2)
        yield
        for half in range(2):
            for k in range(8):
                mm(ps_q.t[:, half, :], xnTe.t[:, k, :], wG.t[:, k, half * 512:(half + 1) * 512], k == 0, k == 7, [xnTe.r0, wG.r0], [ps_q.r[half]])
            P.op("act", "activation", dict(out=gate.t[:, half * 512:(half + 1) * 512], in_=ps_q.t[:, half, :], func=AF.Sigmoid), [ps_q.r[half]], [gate.r0])
            for k in range(2):
                mm(ps_c.t[:, half, :], pT.t[:, k, :], wPs.t[:, k, half * 512:(half + 1) * 512], k == 0, k == 1, [pT.r0, wPs.r0], [ps_c.r[half]])
            P.op("dve", "tensor_tensor", dict(out=tmpg.t[:, :], in0=ps_c.t[:, half, :], in1=gate.t[:, half * 512:(half + 1) * 512], op=ALU.mult),
                 [ps_c.r[half], gate.r0], [tmpg.r0])
            P.op("dve", "tensor_tensor", dict(out=hh_.t[:, half * 512:(half + 1) * 512], in0=hh_.t[:, half * 512:(half + 1) * 512], in1=tmpg.t[:, :], op=ALU.add),
                 [hh_.r0, tmpg.r0], [hh_.r0])
            yield
        rmsnorm_tile(hh_, glb, ot, None, sqE, junkE)
        yield
        P.dma("sp", out_d[tt * 128:(tt + 1) * 128, :], ot.t[:, :], reads=[ot.r0])

    for _ in front(0):
        pass
    ep = iter(())
    EPG = int(_os.environ.get("K_EPG", "8"))
    for tt in range(NTT):
        nxt = front(tt + 1) if tt + 1 < NTT else iter(())
        for grp in range(32):
            gather_group(tt, grp)
            if grp == EPG:
                for _ in ep:
                    pass
            if grp >= 1:
                next(nxt, None)
        for _ in nxt:
            pass
        ep = epilogue(tt)
        next(ep)
    for _ in ep:
        pass
    P.emit_stage()
    P.pop()

    P.close()
    return nc


def kernel(**inputs):
    consts = make_consts()
    shared = prep_shared(inputs)
    shared.update(consts)
    in_maps = []
    for b in range(8):
        m = dict(shared)
        m["x"] = np.ascontiguousarray(inputs["x"][b])
        m["p"] = np.ascontiguousarray(inputs["p"][0, b])
        m["positions"] = np.ascontiguousarray(inputs["positions"][b].reshape(1, S).astype(np.int32))
        in_maps.append(m)
    nc = build_nc()
    res = run_bass_kernel_spmd(nc, in_maps, core_ids=list(range(8)))
    return np.stack([np.asarray(r["out"]) for r in res.results], axis=0).astype(np.float32)
```

```python
from contextlib import ExitStack
import os as _os
import numpy as np
import ml_dtypes
import concourse.bass as bass
import concourse.mybir as mybir
from concourse.bass_utils import run_bass_kernel_spmd

F32 = mybir.dt.float32
BF16 = mybir.dt.bfloat16
I32 = mybir.dt.int32
AF = mybir.ActivationFunctionType
ALU = mybir.AluOpType
AX = mybir.AxisListType

S = 4096
D = 1024
NQC = 8
NTT = 32
NEG = -30000.0
SCALE = 128 ** -0.5
COMPUTE = ("pe", "dve", "act", "pool")
NOSELF = tuple(x for x in _os.environ.get("K_NOSELF", "").split(",") if x)
NROT = int(_os.environ.get("K_NROT", "8"))
NCH = 65
WCOLS = NCH * 128

C_RX, C_RG, C_Q, C_QS, C_KV, C_KSW, C_M, C_NG = 0, 8, 16, 24, 32, 44, 48, 64


class Res:
    __slots__ = ("w", "r", "name")

    def __init__(self, name=""):
        self.w = None
        self.r = {}
        self.name = name


class TT:
    def __init__(self, t, nres, name):
        self.t = t
        self.r = [Res(f"{name}.{i}") for i in range(nres)]

    @property
    def r0(self):
        return self.r[0]


class Prog:
    def __init__(self, nc):
        self.nc = nc
        self.scopes = [ExitStack()]
        self.streams = {e: [] for e in ("pe", "dve", "act", "pool", "sp")}
        self.cnt = {e: 0 for e in COMPUTE}
        self.seen = {e: {} for e in self.streams}
        self.sems = {}
        for i in range(int(_os.environ.get("K_SEMPAD", "0"))):
            self.scopes[0].enter_context(nc.semaphore(f"s_pad{i}"))
        for e in COMPUTE:
            self.sems[e] = self.scopes[0].enter_context(nc.semaphore(f"s_{e}"))
        self.dq = {}
        self.dma_total = {}
        for q in ("sp", "act", "pool"):
            self.dq[q] = {"n": 0, "sems": []}
            nrot_q = int(_os.environ.get("K_NROT_" + q.upper(), "1" if q == "sp" else str(NROT)))
            self.dq[q]["nrot"] = nrot_q
            for i in range(nrot_q):
                k = ("dma", q, i)
                self.sems[k] = self.scopes[0].enter_context(nc.semaphore(f"s_dma_{q}_{i}"))
                self.dq[q]["sems"].append(k)
                self.dma_total[k] = 0
        self.n_instr = 0

    def push(self):
        self.scopes.append(ExitStack())

    def pop(self):
        self.scopes.pop().close()

    def sbuf(self, name, shape, dtype, nres=1):
        self.n_instr += 0
        self.uid = getattr(self, "uid", 0) + 1
        t = self.scopes[-1].enter_context(self.nc.sbuf_tensor(f"{name}_{self.uid}", list(shape), dtype))
        return TT(t, nres, name)

    def psum(self, name, shape, dtype, nres=1):
        self.uid = getattr(self, "uid", 0) + 1
        t = self.scopes[-1].enter_context(self.nc.psum_tensor(f"{name}_{self.uid}", list(shape), dtype))
        return TT(t, nres, name)

    def _deps(self, eng, reads, writes):
        deps = {}

        def add(tok):
            if tok is None:
                return
            k, v = tok
            if deps.get(k, 0) < v:
                deps[k] = v

        for r in reads:
            add(r.w)
        for w in writes:
            add(w.w)
            for k, v in w.r.items():
                add((k, v))
        out = []
        for k, v in deps.items():
            if k == eng and (eng == "pe" or eng in NOSELF):
                continue
            if self.seen[eng].get(k, 0) >= v:
                continue
            self.seen[eng][k] = v
            out.append((k, v))
        return out

    def _record(self, tok, reads, writes):
        k, v = tok
        for r in reads:
            if r.r.get(k, 0) < v:
                r.r[k] = v
        for w in writes:
            w.w = tok
            w.r = {}

    def op(self, eng, meth, kw, reads=(), writes=()):
        waits = self._deps(eng, reads, writes)
        self.cnt[eng] += 1
        tok = (eng, self.cnt[eng])
        self._record(tok, reads, writes)
        self.streams[eng].append((meth, kw, waits, (eng, 1)))
        self.n_instr += 1

    def dma(self, q, out, in_, reads=(), writes=(), indirect=None, **kw):
        d = self.dq[q]
        i = d["n"]
        d["n"] += 1
        sk = d["sems"][i % d["nrot"]]
        prev = self.dma_total[sk]
        waits = self._deps(q, reads, writes)
        if prev > 0 and self.seen[q].get(sk, 0) < prev:
            self.seen[q][sk] = prev
            waits.append((sk, prev))
        self.dma_total[sk] = prev + 16
        tok = (sk, prev + 16)
        self._record(tok, reads, writes)
        kk = dict(out=out, in_=in_)
        if indirect is not None:
            kk.update(indirect)
        kk.update(kw)
        self.streams[q].append(("indirect_dma_start" if indirect is not None else "dma_start", kk, waits, (sk, 16)))
        self.n_instr += 1
        return tok

    def emit_stage(self):
        targets = [(e, self.cnt[e]) for e in COMPUTE if self.cnt[e] > 0]
        targets += [(k, v) for k, v in self.dma_total.items() if v > 0]
        for e in self.streams:
            ws = []
            for k, v in targets:
                if self.seen[e].get(k, 0) < v:
                    self.seen[e][k] = v
                    ws.append((k, v))
            if ws:
                self.streams[e].append((None, None, ws, None))
        sems = self.sems
        streams = self.streams

        def run(eng_obj, items):
            for meth, kw, waits, inc in items:
                for k, v in waits:
                    eng_obj.wait_ge(sems[k], v)
                if meth is None:
                    continue
                ins = getattr(eng_obj, meth)(**kw)
                ins.then_inc(sems[inc[0]], inc[1])

        with self.nc.Block() as block:
            @block.tensor
            def _(e):
                run(e, streams["pe"])

            @block.vector
            def _(e):
                run(e, streams["dve"])

            @block.scalar
            def _(e):
                run(e, streams["act"])

            @block.gpsimd
            def _(e):
                run(e, streams["pool"])

            @block.sync
            def _(e):
                run(e, streams["sp"])
        self.streams = {e: [] for e in streams}

    def close(self):
        while self.scopes:
            self.scopes.pop().close()


def _bf(a):
    return np.ascontiguousarray(a).astype(ml_dtypes.bfloat16)


def make_consts():
    c = {}
    c["c_identb"] = _bf(np.eye(128, dtype=np.float32))
    c["c_identf"] = np.eye(128, dtype=np.float32)
    half = 64
    inv = (10000.0 ** (-np.arange(half, dtype=np.float32) / np.float32(half))).astype(np.float32)
    c["c_invf"] = np.concatenate([inv, inv]).reshape(128, 1).astype(np.float32)
    c["c_sgn"] = np.concatenate([-np.ones(64), np.ones(64)]).reshape(128, 1).astype(np.float32)
    kp = np.arange(128)[:, None]
    q = np.arange(512)[None, :]
    cb = np.zeros((128, 4, 512), np.float32)
    wb = np.zeros((128, 4, 512), np.float32)
    for r in range(4):
        k = r * 128 + kp
        cb[:, r, :] = np.where(k <= q, 0.0, NEG)
        k2 = (r - 4) * 128 + kp
        wb[:, r, :] = np.where(q - k2 < 512, 0.0, NEG)
    c["c_cb"] = _bf(cb)
    c["c_wb"] = _bf(wb)
    n = (np.arange(2)[None, :, None] * 128 + np.arange(128)[:, None, None])
    t = np.arange(S)[None, None, :]
    cmb = np.where((16 * n + 31 <= t) & (n < 255), 0.0, NEG).astype(np.float32)
    c["c_cmb"] = _bf(cmb)
    j = np.arange(64)[:, None]
    kk = np.arange(S)[None, :]
    c["c_E"] = _bf(np.concatenate([(kk // 64 == j).astype(np.float32), np.zeros((64, S), np.float32)], 0))
    nn = np.arange(256)
    jj = np.arange(64)
    ov = ((16 * nn[:, None] < 64 * jj[None, :] + 64) & (16 * nn[:, None] + 31 >= 64 * jj[None, :]) & (nn[:, None] < 255))
    c["c_ovl"] = _bf(ov.astype(np.float32).reshape(2, 128, 64).transpose(1, 0, 2))
    tt = np.arange(S).reshape(NTT, 128)[:, :, None]
    cur = tt // 64
    j3 = np.arange(64)[None, None, :]
    forced = (j3 == 0) | (j3 == cur) | (j3 == cur - 1)
    valid = (64 * j3 <= tt)
    c["c_fa"] = np.ascontiguousarray(np.where(forced, 1000.0, 0.0).astype(np.float32).transpose(1, 0, 2))
    c["c_na"] = np.ascontiguousarray(np.where(valid, 0.0, -1e30).astype(np.float32).transpose(1, 0, 2))
    c["c_vm"] = np.ascontiguousarray(valid.astype(np.float32).transpose(1, 0, 2))
    c["c_iota128"] = np.ascontiguousarray(np.broadcast_to(np.arange(128, dtype=np.int32)[None, None, :], (128, 16, 128)))
    c["c_iota256"] = np.ascontiguousarray(np.broadcast_to(np.arange(256, dtype=np.int32)[None, None, :], (128, 8, 256)))
    c["c_iota16"] = np.ascontiguousarray(np.broadcast_to(np.arange(16, dtype=np.float32)[None, None, None, :], (128, 8, 16, 16)))
    return c


def pc_layout(v):
    return np.ascontiguousarray(np.asarray(v).reshape(8, 128).T)


def prep_shared(inp):
    m = {}
    def pk(w):
        K_, N_ = w.shape
        return np.ascontiguousarray(w.reshape(K_ // 128, 128, N_).transpose(1, 0, 2))
    m["w_in"] = pk(inp["w_in"][0])
    for k in ("w_a", "w_b", "w_out", "peer_wq", "ple_wg", "ple_wp"):
        m[k] = pk(inp[k][0])
    for k in ("peer_u", "peer_v"):
        m[k] = np.ascontiguousarray(inp[k][0])
    m["lru_wr"] = np.ascontiguousarray(inp["lru_wr"][0].transpose(1, 0, 2))
    m["lru_wi"] = np.ascontiguousarray(inp["lru_wi"][0].transpose(1, 0, 2))
    for k in ("cmp_w1_k", "cmp_w1_v"):
        m[k] = np.ascontiguousarray(inp[k][0].reshape(32, 128, 256).transpose(1, 0, 2))
    for k in ("cmp_w2_k", "cmp_w2_v"):
        m[k] = pk(inp[k][0])
    for k in ("norm_mix", "norm_ffn", "norm_ple"):
        m[k] = np.ascontiguousarray(inp[k][0].reshape(1, D))
    m["norm_final"] = np.ascontiguousarray(inp["norm_final"].reshape(1, D))
    m["conv_w"] = np.ascontiguousarray(inp["conv_w"][0].reshape(4, 8, 128).transpose(2, 1, 0))
    for k in ("conv_b", "lru_br", "lru_bi", "lru_lam"):
        m[k] = pc_layout(inp[k][0])
    for k in ("cmp_b1_k", "cmp_b1_v"):
        m[k] = np.ascontiguousarray(inp[k][0].reshape(2, 128).T)
    m["cmp_b2_k"] = np.ascontiguousarray(inp["cmp_b2_k"][0].reshape(128, 1))
    m["cmp_b2_v"] = np.ascontiguousarray(inp["cmp_b2_v"][0].reshape(1, 128))
    m["cmp_pe_k"] = np.ascontiguousarray(inp["cmp_pe_k"][0].T)
    m["cmp_pe_v"] = np.ascontiguousarray(inp["cmp_pe_v"][0].T)
    keys = inp["peer_keys"][0]
    kbd = np.zeros((128, 256), np.float32)
    kbd[0:64, 0:128] = keys[0].T
    kbd[64:128, 128:256] = keys[1].T
    m["peer_kbd"] = kbd
    return m


def win_segments():
    segs = [(0, 0, 2048), (2048, 2048, 1024)]
    d = 3072
    for h in range(8):
        segs.append((d, 2048 + h * 128 + 64, 64))
        segs.append((d + 64, 2048 + h * 128, 64))
        d += 128
    segs.append((4096, 3072, 1536))
    d = 5632
    for jkv in (2, 4):
        for g in range(2):
            s0 = 3072 + jkv * 256 + g * 128
            segs.append((d, s0 + 64, 64))
            segs.append((d + 64, s0, 64))
            d += 128
    segs.append((6144, 4632, 2048))
    segs.append((8192, 4608, 24))
    return segs


def build_nc(upto="D", debug=False):
    nc = bass.Bass("TRN2", target_bir_lowering=False)
    P = Prog(nc)
    ein = {}

    def IN(name, shape, dt=F32):
        ein[name] = nc.dram_tensor(name, list(shape), dt, kind="ExternalInput").ap()
        return ein[name]

    x_d = IN("x", [S, D])
    p_d = IN("p", [S, 256])
    pos_d = IN("positions", [1, S], I32)
    win_d = IN("w_in", [128, 8, 6680])
    w5_d = [IN(k, [128, 8, D]) for k in ("w_a", "w_b", "w_out", "peer_wq", "ple_wg")]
    wp_d = IN("ple_wp", [128, 2, D])
    u_d = IN("peer_u", [16384, D])
    v_d = IN("peer_v", [16384, D])
    wr_d = IN("lru_wr", [128, 8, 128])
    wi_d = IN("lru_wi", [128, 8, 128])
    w1_d = [IN("cmp_w1_k", [128, 32, 256]), IN("cmp_w1_v", [128, 32, 256])]
    w2_d = [IN("cmp_w2_k", [128, 2, 128]), IN("cmp_w2_v", [128, 2, 128])]
    gm_d = IN("norm_mix", [1, D])
    gf_d = IN("norm_ffn", [1, D])
    gp_d = IN("norm_ple", [1, D])
    gl_d = IN("norm_final", [1, D])
    cw_d = IN("conv_w", [128, 8, 4])
    cbias_d = IN("conv_b", [128, 8])
    br_d = IN("lru_br", [128, 8])
    bi_d = IN("lru_bi", [128, 8])
    lam_d = IN("lru_lam", [128, 8])
    b1_d = [IN("cmp_b1_k", [128, 2]), IN("cmp_b1_v", [128, 2])]
    b2k_d = IN("cmp_b2_k", [128, 1])
    b2v_d = IN("cmp_b2_v", [1, 128])
    pe_d = [IN("cmp_pe_k", [128, 32]), IN("cmp_pe_v", [128, 32])]
    kbd_d = IN("peer_kbd", [128, 256])
    identb_d = IN("c_identb", [128, 128], BF16)
    identf_d = IN("c_identf", [128, 128])
    invf_d = IN("c_invf", [128, 1])
    sgn_d = IN("c_sgn", [128, 1])
    cb_d = IN("c_cb", [128, 4, 512], BF16)
    wb_d = IN("c_wb", [128, 4, 512], BF16)
    cmb_d = IN("c_cmb", [128, 2, S], BF16)
    E_d = IN("c_E", [128, S], BF16)
    ovl_d = IN("c_ovl", [128, 2, 64], BF16)
    fa_d = IN("c_fa", [128, NTT, 64])
    na_d = IN("c_na", [128, NTT, 64])
    vm_d = IN("c_vm", [128, NTT, 64])
    io128_d = IN("c_iota128", [128, 16, 128], I32)
    io256_d = IN("c_iota256", [128, 8, 256], I32)
    io16_d = IN("c_iota16", [128, 8, 16, 16])

    out_d = nc.dram_tensor("out", [S, D], F32, kind="ExternalOutput").ap()

    dbg_n = [0]

    def DBG(tag, tt_, shape, dt=F32):
        if not debug:
            return
        dbg_n[0] += 1
        d = nc.dram_tensor(f"dbg_{tag}", list(shape), dt, kind="ExternalOutput").ap()
        P.dma("sp", d, tt_.t[:], reads=tt_.r)

    def SCR(name, shape, dt):
        kind = "ExternalOutput" if debug else "Internal"
        return nc.dram_tensor(name, list(shape), dt, kind=kind).ap()

    winbf = SCR("s_winbf", [128, 8, WCOLS], BF16)
    w5bf = [SCR(f"s_w5bf{i}", [128, 8, D], BF16) for i in range(5)]
    wpbf = SCR("s_wpbf", [128, 2, D], BF16)
    w1bf = [SCR(f"s_w1bf{i}", [128, 32, 256], BF16) for i in range(2)]
    zfm = SCR("s_zfm", [NCH, 128, S], F32)
    ma_s = SCR("s_ma", [8, 128, S], BF16)
    obt_s = SCR("s_obt", [8, 128, S], BF16)
    h1_s = SCR("s_h1", [S, D], F32)
    uv_s = nc.dram_tensor("s_uv", [16384, 2048], BF16, kind="Internal").ap()

    identb = P.sbuf("identb", [128, 128], BF16)
    identf = P.sbuf("identf", [128, 128], F32)
    P.dma("sp", identb.t[:, :], identb_d, writes=[identb.r0])
    P.dma("sp", identf.t[:, :], identf_d, writes=[identf.r0])

    cast_rr = [0]

    def cast(out, in_, reads, writes, engs=tuple(_os.environ.get("K_CAST", "dve,act").split(","))):
        e = engs[cast_rr[0] % len(engs)]
        cast_rr[0] += 1
        if e == "act":
            P.op("act", "copy", dict(out=out, in_=in_), reads, writes)
        else:
            P.op(e, "tensor_copy", dict(out=out, in_=in_), reads, writes)

    P.push()
    stg = P.sbuf("stg", [128, 2, 4096], F32, nres=2)
    stgb = P.sbuf("stgb", [128, 2, 4096], BF16, nres=2)
    P.op("dve", "memset", dict(ap=stg.t[:, 0, :], constant=0.0), [], [stg.r[0]])
    P.op("dve", "memset", dict(ap=stg.t[:, 1, :], constant=0.0), [], [stg.r[1]])
    pi = [0]

    def piece(loads, n, dst, dshape):
        b = pi[0] % 2
        pi[0] += 1
        for mk, src in loads:
            P.dma("sp", mk(stg.t[:, b, 0:n]), src, writes=[stg.r[b]])
        cast(stgb.t[:, b, 0:n], stg.t[:, b, 0:n], [stg.r[b]], [stgb.r[b]])
        P.dma("pool", dst, dshape(stgb.t[:, b, 0:n]), reads=[stgb.r[b]])

    win_v = win_d
    segs = win_segments()
    for c0 in range(0, WCOLS, 512):
        c1 = min(c0 + 512, WCOLS)
        w = c1 - c0
        loads = []
        for (dcol, scol, ncol) in segs:
            lo, hi = max(dcol, c0), min(dcol + ncol, c1)
            if lo >= hi:
                continue
            so = scol + (lo - dcol)
            loads.append((lambda a, lo=lo, hi=hi, w=w, c0=c0: a.rearrange("p (kc n) -> p kc n", kc=8)[:, :, lo - c0:hi - c0],
                          win_v[:, :, so:so + (hi - lo)]))
        if c1 == WCOLS and 8192 + 24 < WCOLS:
            pass
        piece(loads, 8 * w, winbf[:, :, c0:c1], lambda a, w=w: a.rearrange("p (kc n) -> p kc n", kc=8))
    for i in range(5):
        wv = w5_d[i]
        for hh in range(2):
            piece([(lambda a: a.rearrange("p (kc n) -> p kc n", kc=8), wv[:, :, hh * 512:(hh + 1) * 512])], 4096,
                  w5bf[i][:, :, hh * 512:(hh + 1) * 512], lambda a: a.rearrange("p (kc n) -> p kc n", kc=8))
    piece([(lambda a: a.rearrange("p (kc n) -> p kc n", kc=2), wp_d)], 2048,
          wpbf[:, :, :], lambda a: a.rearrange("p (kc n) -> p kc n", kc=2))
    for i in range(2):
        wv = w1_d[i]
        for hh in range(2):
            piece([(lambda a: a.rearrange("p (l h) -> p l h", l=16), wv[:, hh * 16:(hh + 1) * 16, :])], 4096,
                  w1bf[i][:, hh * 16:(hh + 1) * 16, :], lambda a: a.rearrange("p (l h) -> p l h", l=16))
    P.emit_stage()
    P.pop()
    if upto == "P":
        P.close()
        return nc

    P.push()
    nT = P.sbuf("nT", [128, 8, S], BF16, nres=NTT)
    gbc = P.sbuf("gbc", [128, D], F32)
    xt = P.sbuf("xt", [128, 2, D], F32, nres=2)
    nb = P.sbuf("nb", [128, 2, D], BF16, nres=2)
    junk = P.sbuf("junk", [128, D], F32)
    ss = P.sbuf("ss", [128, NTT], F32, nres=NTT)
    rs = P.sbuf("rs", [128, NTT], F32, nres=NTT)
    tp = P.psum("tp", [128, 2, 1024], BF16, nres=2)
    P.dma("sp", gbc.t[:, :], gm_d.partition_broadcast(128), writes=[gbc.r0])
    for tt in range(NTT):
        b = tt % 2
        P.dma("sp", xt.t[:, b, :], x_d[tt * 128:(tt + 1) * 128, :], writes=[xt.r[b]])
        P.op("act", "activation", dict(out=junk.t[:, :], in_=xt.t[:, b, :], func=AF.Square, accum_out=ss.t[:, tt:tt + 1]),
             reads=[xt.r[b]], writes=[junk.r0, ss.r[tt]])
        P.op("act", "activation", dict(out=rs.t[:, tt:tt + 1], in_=ss.t[:, tt:tt + 1], func=AF.Sqrt, scale=1.0 / D, bias=1e-6),
             reads=[ss.r[tt]], writes=[rs.r[tt]])
        P.op("dve", "reciprocal", dict(out=rs.t[:, tt:tt + 1], in_=rs.t[:, tt:tt + 1]), reads=[rs.r[tt]], writes=[rs.r[tt]])
        P.op("dve", "scalar_tensor_tensor", dict(out=nb.t[:, b, :], in0=xt.t[:, b, :], scalar=rs.t[:, tt:tt + 1], in1=gbc.t[:, :],
                                                 op0=ALU.mult, op1=ALU.mult), reads=[xt.r[b], rs.r[tt], gbc.r0], writes=[nb.r[b]])
        for c in range(8):
            P.op("pe", "transpose", dict(out=tp.t[:, b, c * 128:(c + 1) * 128], in_=nb.t[:, b, c * 128:(c + 1) * 128], identity=identb.t[:, :]),
                 reads=[nb.r[b], identb.r0], writes=[tp.r[b]])
        cast(nT.t[:, :, tt * 128:(tt + 1) * 128], tp.t[:, b, :].rearrange("p (c t) -> p c t", c=8), [tp.r[b]], [nT.r[tt]])

    def uv_build():
        ust = P.sbuf("ust", [128, 2, 4096], F32, nres=2)
        uvb = P.sbuf("uvb", [128, 2, 4, 2048], BF16, nres=2)
        u_v = u_d.rearrange("(r p j) d -> r p (j d)", p=128, j=4)
        v_v = v_d.rearrange("(r p j) d -> r p (j d)", p=128, j=4)
        uv_v = uv_s.rearrange("(r p j) c -> r p (j c)", p=128, j=4)
        for rb in range(32):
            ob_ = rb % 2
            for ti_, (src_v, eng_) in enumerate(((u_v, "act"), (v_v, "dve"))):
                P.dma("sp", ust.t[:, ti_, :], src_v[rb], writes=[ust.r[ti_]])
                dst_ = uvb.t[:, ob_, :, ti_ * 1024:(ti_ + 1) * 1024]
                src_ = ust.t[:, ti_, :].rearrange("p (j d) -> p j d", j=4)
                if eng_ == "act":
                    P.op("act", "copy", dict(out=dst_, in_=src_), [ust.r[ti_]], [uvb.r[ob_]])
                else:
                    P.op("dve", "tensor_copy", dict(out=dst_, in_=src_), [ust.r[ti_]], [uvb.r[ob_]])
            P.dma("pool", uv_v[rb], uvb.t[:, ob_].rearrange("p j c -> p (j c)"), reads=[uvb.r[ob_]])
            yield

    wbuf = P.sbuf("wbuf", [128, 2, 8, 512], BF16, nres=2)
    zst = P.sbuf("zst", [128, 2, S], F32, nres=2)
    uvgen = uv_build()
    pz = P.psum("pz", [128, 4, 512], F32, nres=4)
    nTall = list(nT.r)
    zi = 0
    pzi = 0
    for wpi, c0 in enumerate(range(0, WCOLS, 512)):
        c1 = min(c0 + 512, WCOLS)
        wb_ = wpi % 2
        P.dma("sp", wbuf.t[:, wb_, :, 0:c1 - c0], winbf[:, :, c0:c1], writes=[wbuf.r[wb_]])
        for cc in range((c1 - c0) // 128):
            ch = c0 // 128 + cc
            M = 24 if ch == C_NG else 128
            zb = zi % 2
            zi += 1
            for qc in range(NQC):
                pb = pzi % 4
                pzi += 1
                for kc in range(8):
                    P.op("pe", "matmul", dict(out=pz.t[0:M, pb, :], lhsT=wbuf.t[:, wb_, kc, cc * 128:cc * 128 + M],
                                              rhs=nT.t[:, kc, qc * 512:(qc + 1) * 512], start=(kc == 0), stop=(kc == 7),
                                              skip_group_check=True),
                         reads=[wbuf.r[wb_]] + nTall[qc * 4:(qc + 1) * 4], writes=[pz.r[pb]])
                dst = zst.t[0:M, zb, qc * 512:(qc + 1) * 512]
                src = pz.t[0:M, pb, :]
                if C_RG <= ch < C_RG + 8:
                    P.op("act", "activation", dict(out=dst, in_=src, func=AF.Gelu_apprx_tanh), [pz.r[pb]], [zst.r[zb]])
                elif ch >= C_M:
                    P.op("act", "activation", dict(out=dst, in_=src, func=AF.Sigmoid), [pz.r[pb]], [zst.r[zb]])
                else:
                    P.op("dve", "tensor_copy", dict(out=dst, in_=src), [pz.r[pb]], [zst.r[zb]])
            P.dma("pool", zfm[ch, 0:M, :], zst.t[0:M, zb, :], reads=[zst.r[zb]])
            if ch % 2 == 0:
                next(uvgen, None)
    for _ in uvgen:
        pass
    P.emit_stage()
    P.pop()
    if upto == "A":
        P.close()
        return nc

    P.push()
    cw = P.sbuf("cw", [128, 8, 4], F32)
    cbias = P.sbuf("cbias", [128, 8], F32)
    brs = P.sbuf("brs", [128, 8], F32)
    bis = P.sbuf("bis", [128, 8], F32)
    lam = P.sbuf("lam", [128, 8], F32)
    m8 = P.sbuf("m8", [128, 8], F32)
    m16 = P.sbuf("m16", [128, 8], F32)
    wst = P.sbuf("wst", [128, 2, 8, 128], F32, nres=2)
    wrb = P.sbuf("wrb", [128, 8, 128], BF16)
    wib = P.sbuf("wib", [128, 8, 128], BF16)
    for (t_, d_) in ((cw, cw_d), (cbias, cbias_d), (brs, br_d), (bis, bi_d), (lam, lam_d)):
        P.dma("sp", t_.t[:], d_, writes=[t_.r0])
    P.dma("sp", wst.t[:, 0], wr_d, writes=[wst.r[0]])
    P.dma("sp", wst.t[:, 1], wi_d, writes=[wst.r[1]])
    P.op("dve", "tensor_copy", dict(out=wrb.t[:, :, :], in_=wst.t[:, 0]), [wst.r[0]], [wrb.r0])
    P.op("dve", "tensor_copy", dict(out=wib.t[:, :, :], in_=wst.t[:, 1]), [wst.r[1]], [wib.r0])
    P.op("act", "activation", dict(out=m8.t[:, :], in_=lam.t[:, :], func=AF.Exp, scale=-1.0), [lam.r0], [m8.r0])
    P.op("act", "activation", dict(out=m8.t[:, :], in_=m8.t[:, :], func=AF.Ln, bias=1.0), [m8.r0], [m8.r0])
    P.op("dve", "tensor_scalar", dict(out=m16.t[:, :], in0=m8.t[:, :], scalar1=-16.0, scalar2=None, op0=ALU.mult), [m8.r0], [m16.r0])
    P.op("dve", "tensor_scalar", dict(out=m8.t[:, :], in0=m8.t[:, :], scalar1=-8.0, scalar2=None, op0=ALU.mult), [m8.r0, m16.r0], [m8.r0])
    zx = P.sbuf("zx", [128, S + 3], F32)
    gz = P.sbuf("gz", [128, S], F32)
    xa = P.sbuf("xa", [128, S], F32)
    xab = P.sbuf("xab", [128, S], BF16)
    rr = P.sbuf("rr", [128, S], F32)
    ig = P.sbuf("ig", [128, S], F32)
    t1 = P.sbuf("t1", [128, S], F32)
    t2 = P.sbuf("t2", [128, S], F32)
    mab = P.sbuf("mab", [128, S], BF16)
    pg = P.psum("pg", [128, 4, 512], F32, nres=4)
    P.op("dve", "memset", dict(ap=zx.t[:, 0:3], constant=0.0), [], [zx.r0])
    pgi = 0
    for ch in range(8):
        P.dma("sp", zx.t[:, 3:S + 3], zfm[C_RX + ch], writes=[zx.r0])
        P.dma("sp", gz.t[:, :], zfm[C_RG + ch], writes=[gz.r0])
        P.op("dve", "tensor_scalar", dict(out=xa.t[:, :], in0=zx.t[:, 0:S], scalar1=cw.t[:, ch, 0:1], scalar2=cbias.t[:, ch:ch + 1],
                                          op0=ALU.mult, op1=ALU.add), [zx.r0, cw.r0, cbias.r0], [xa.r0])
        for k in range(1, 4):
            P.op("dve", "scalar_tensor_tensor", dict(out=xa.t[:, :], in0=zx.t[:, k:k + S], scalar=cw.t[:, ch, k:k + 1], in1=xa.t[:, :],
                                                     op0=ALU.mult, op1=ALU.add), [zx.r0, cw.r0, xa.r0], [xa.r0])
        P.op("act", "copy", dict(out=xab.t[:, :], in_=xa.t[:, :]), [xa.r0], [xab.r0])
        for (wgt, bia, dst) in ((wrb, brs, rr), (wib, bis, ig)):
            for qc in range(NQC):
                pb = pgi % 4
                pgi += 1
                P.op("pe", "matmul", dict(out=pg.t[:, pb, :], lhsT=wgt.t[:, ch, :], rhs=xab.t[:, qc * 512:(qc + 1) * 512], start=True, stop=True,
                                          skip_group_check=True), [wgt.r0, xab.r0], [pg.r[pb]])
                P.op("act", "activation", dict(out=dst.t[:, qc * 512:(qc + 1) * 512], in_=pg.t[:, pb, :], func=AF.Sigmoid, bias=bia.t[:, ch:ch + 1]),
                     [pg.r[pb], bia.r0], [dst.r0])
        P.op("act", "activation", dict(out=t1.t[:, :], in_=rr.t[:, :], func=AF.Exp, scale=m8.t[:, ch:ch + 1]), [rr.r0, m8.r0], [t1.r0])
        P.op("act", "activation", dict(out=t2.t[:, :], in_=rr.t[:, :], func=AF.Exp, scale=m16.t[:, ch:ch + 1]), [rr.r0, m16.r0], [t2.r0])
        P.op("act", "activation", dict(out=t2.t[:, :], in_=t2.t[:, :], func=AF.Sqrt, scale=-1.0, bias=1.0), [t2.r0], [t2.r0])
        P.op("pool", "tensor_tensor", dict(out=ig.t[:, :], in0=ig.t[:, :], in1=xa.t[:, :], op=ALU.mult), [ig.r0, xa.r0], [ig.r0])
        P.op("pool", "tensor_tensor", dict(out=ig.t[:, :], in0=ig.t[:, :], in1=t2.t[:, :], op=ALU.mult), [ig.r0, t2.r0], [ig.r0])
        P.op("dve", "tensor_tensor_scan", dict(out=rr.t[:, :], data0=t1.t[:, :], data1=ig.t[:, :], initial=0.0, op0=ALU.mult, op1=ALU.add),
             [t1.r0, ig.r0, rr.r0], [rr.r0])
        P.op("dve", "tensor_tensor", dict(out=mab.t[:, :], in0=rr.t[:, :], in1=gz.t[:, :], op=ALU.mult), [rr.r0, gz.r0], [mab.r0])
        P.dma("pool", ma_s[ch], mab.t[:, :], reads=[mab.r0])
    P.emit_stage()
    P.pop()
    if upto == "B":
        P.close()
        return nc

    P.push()
    cosT = P.sbuf("cosT", [128, S], F32)
    sinT = P.sbuf("sinT", [128, S], F32)
    P.push()
    posi = P.sbuf("posi", [128, S], I32)
    tq_ = P.sbuf("tq", [128, S], F32)
    tk_ = P.sbuf("tk", [128, S], F32)
    ki_ = P.sbuf("ki", [128, S], I32)
    invf = P.sbuf("invf", [128, 1], F32)
    sgn = P.sbuf("sgn", [128, 1], F32)
    P.dma("sp", posi.t[:, :], pos_d.partition_broadcast(128), writes=[posi.r0])
    P.dma("sp", invf.t[:, :], invf_d, writes=[invf.r0])
    P.dma("sp", sgn.t[:, :], sgn_d, writes=[sgn.r0])
    P.op("dve", "tensor_copy", dict(out=tq_.t[:, :], in_=posi.t[:, :]), [posi.r0], [tq_.r0])
    P.op("dve", "tensor_scalar", dict(out=tq_.t[:, :], in0=tq_.t[:, :], scalar1=invf.t[:, 0:1], scalar2=float(1.0 / (2 * np.pi)),
                                      op0=ALU.mult, op1=ALU.mult), [tq_.r0, invf.r0], [tq_.r0])
    TWO_PI = float(2 * np.pi * (1 - 1e-6))
    for which, dstT in (("sin", sinT), ("cos", cosT)):
        if which == "cos":
            P.op("dve", "tensor_scalar", dict(out=tq_.t[:, :], in0=tq_.t[:, :], scalar1=0.25, scalar2=None, op0=ALU.add), [tq_.r0], [tq_.r0])
        P.op("dve", "tensor_copy", dict(out=ki_.t[:, :], in_=tq_.t[:, :]), [tq_.r0], [ki_.r0])
        P.op("dve", "tensor_copy", dict(out=tk_.t[:, :], in_=ki_.t[:, :]), [ki_.r0], [tk_.r0])
        P.op("dve", "tensor_tensor", dict(out=tk_.t[:, :], in0=tq_.t[:, :], in1=tk_.t[:, :], op=ALU.subtract), [tq_.r0, tk_.r0], [tk_.r0])
        P.op("act", "activation", dict(out=dstT.t[:, :], in_=tk_.t[:, :], func=AF.Sin, scale=TWO_PI), [tk_.r0], [dstT.r0])
    P.op("dve", "tensor_scalar", dict(out=sinT.t[:, :], in0=sinT.t[:, :], scalar1=sgn.t[:, 0:1], scalar2=None, op0=ALU.mult), [sinT.r0, sgn.r0], [sinT.r0])
    P.emit_stage()
    P.pop()

    ksT = P.sbuf("ksT", [128, S], BF16)
    kwT = P.sbuf("kwT", [128, S], BF16)
    vsa = P.sbuf("vsa", [128, NTT, 129], BF16)
    vwa = P.sbuf("vwa", [128, NTT, 129], BF16)
    kcT = P.sbuf("kcT", [128, 256], BF16)
    vca = P.sbuf("vca", [128, 2, 193], BF16)
    E_s = P.sbuf("E_s", [128, S], BF16)
    cb_s = P.sbuf("cb_s", [128, 4, 512], BF16)
    wb_s = P.sbuf("wb_s", [128, 4, 512], BF16)
    b1s = [P.sbuf(f"b1s{i}", [128, 2], F32) for i in range(2)]
    pes = [P.sbuf(f"pes{i}", [128, 32], F32) for i in range(2)]
    b2k = P.sbuf("b2k", [128, 1], F32)
    b2vb = P.sbuf("b2vb", [128, 128], F32)
    ps_s = P.psum("ps_s", [128, 2, 512], F32, nres=2)
    ps_acc = P.psum("ps_acc", [128, 4, 512], F32, nres=4)
    ps_trb = P.psum("ps_trb", [128, 1024], BF16)
    ps_trf = P.psum("ps_trf", [128, 512], F32)
    P.dma("sp", E_s.t[:, :], E_d, writes=[E_s.r0])
    P.dma("sp", cb_s.t[:], cb_d, writes=[cb_s.r0])
    P.dma("sp", wb_s.t[:], wb_d, writes=[wb_s.r0])
    for i in range(2):
        P.dma("sp", b1s[i].t[:, :], b1_d[i], writes=[b1s[i].r0])
        P.dma("sp", pes[i].t[:, :], pe_d[i], writes=[pes[i].r0])
    P.dma("sp", b2k.t[:, :], b2k_d, writes=[b2k.r0])
    P.dma("sp", b2vb.t[:, :], b2v_d.partition_broadcast(128), writes=[b2vb.r0])
    P.op("dve", "memset", dict(ap=vsa.t[:, :, 128:129], constant=1.0), [], [vsa.r0])
    P.op("dve", "memset", dict(ap=vwa.t[:, :, 128:129], constant=1.0), [], [vwa.r0])
    srot = [0]

    def sbank():
        srot[0] += 1
        return srot[0] % 2

    def mm(out, lhsT, rhs, start, stop, reads, writes):
        P.op("pe", "matmul", dict(out=out, lhsT=lhsT, rhs=rhs, start=start, stop=stop, skip_group_check=True), reads, writes)

    for g in range(2):
        P.push()
        ld = P.sbuf("ld", [128, 2, S], F32, nres=2)
        lohi = P.sbuf("lohi", [128, 2, S], BF16, nres=2)
        w1s = P.sbuf("w1s", [128, 32, 256], BF16)
        w2f = P.sbuf("w2f", [128, 2, 128], F32)
        w2s = P.sbuf("w2s", [128, 2, 128], BF16)
        hidT = P.sbuf("hidT", [128, 2, 256], BF16)
        vb = P.sbuf("vb", [128, S], BF16)
        P.op("dve", "memset", dict(ap=kcT.t[:, :], constant=0.0), [], [kcT.r0])
        P.op("dve", "memset", dict(ap=vca.t[:], constant=0.0), [], [vca.r0])
        P.op("dve", "memset", dict(ap=vca.t[:, :, 128:129], constant=1.0), [vca.r0], [vca.r0])
        P.dma("sp", vca.t[:, :, 129:193], ovl_d, writes=[vca.r0])
        for kvi in range(2):
            ch = C_KV + kvi * 2 + g
            P.dma("sp", ld.t[:, 0, :], zfm[ch], writes=[ld.r[0]])
            P.dma("sp", w1s.t[:], w1bf[kvi], writes=[w1s.r0])
            P.dma("sp", w2f.t[:], w2_d[kvi], writes=[w2f.r0])
            P.op("dve", "tensor_copy", dict(out=w2s.t[:], in_=w2f.t[:]), [w2f.r0], [w2s.r0])
            for v_ in range(2):
                P.op("dve", "tensor_tensor", dict(out=lohi.t[:, v_, :].rearrange("p (n l) -> p n l", l=16),
                                                  in0=ld.t[:, 0, :].rearrange("p (n l) -> p n l", l=16),
                                                  in1=pes[kvi].t[:, v_ * 16:(v_ + 1) * 16].unsqueeze(1).broadcast_to([128, 256, 16]),
                                                  op=ALU.add), [ld.r[0], pes[kvi].r0], [lohi.r[v_]])
            for hc in range(2):
                for l in range(32):
                    v_ = 0 if l < 16 else 1
                    mm(ps_s.t[:, hc, 0:255], w1s.t[:, l, hc * 128:(hc + 1) * 128], lohi.t[:, v_, l:l + 16 * 254 + 1:16],
                       l == 0, l == 31, [w1s.r0, lohi.r[v_]], [ps_s.r[hc]])
                P.op("act", "activation", dict(out=hidT.t[:, hc, 0:255], in_=ps_s.t[:, hc, 0:255], func=AF.Gelu_apprx_tanh,
                                               bias=b1s[kvi].t[:, hc:hc + 1]), [ps_s.r[hc], b1s[kvi].r0], [hidT.r0])
            if kvi == 0:
                for hc in range(2):
                    mm(ps_s.t[:, 0, 0:255], w2s.t[:, hc, :], hidT.t[:, hc, 0:255], hc == 0, hc == 1, [w2s.r0, hidT.r0], [ps_s.r[0]])
                P.op("dve", "tensor_scalar", dict(out=kcT.t[:, 0:255], in0=ps_s.t[:, 0, 0:255], scalar1=b2k.t[:, 0:1], scalar2=None, op0=ALU.add),
                     [ps_s.r[0], b2k.r0], [kcT.r0])
            else:
                for nch in range(2):
                    rows = 128 if nch == 0 else 127
                    for hc in range(2):
                        mm(ps_acc.t[0:rows, nch, 0:128], hidT.t[:, hc, nch * 128:nch * 128 + rows], w2s.t[:, hc, :], hc == 0, hc == 1,
                           [w2s.r0, hidT.r0], [ps_acc.r[nch]])
                    P.op("dve", "tensor_tensor", dict(out=vca.t[0:rows, nch, 0:128], in0=ps_acc.t[0:rows, nch, 0:128], in1=b2vb.t[0:rows, :], op=ALU.add),
                         [ps_acc.r[nch], b2vb.r0], [vca.r0])
        for (kch, swch, dstK) in ((C_KV + 4 + g, C_KSW + g, ksT), (C_KV + 8 + g, C_KSW + 2 + g, kwT)):
            P.dma("sp", ld.t[:, 0, :], zfm[kch], writes=[ld.r[0]])
            P.dma("sp", ld.t[:, 1, :], zfm[swch], writes=[ld.r[1]])
            if debug and g == 1 and dstK is ksT:
                DBG("g1_ld_raw", ld, [128, 2, S])
                DBG("g1_cosT", cosT, [128, S])
                DBG("g1_sinT", sinT, [128, S])
            P.op("dve", "tensor_tensor", dict(out=ld.t[:, 0, :], in0=ld.t[:, 0, :], in1=cosT.t[:, :], op=ALU.mult), [ld.r[0], cosT.r0], [ld.r[0]])
            P.op("pool", "tensor_tensor", dict(out=ld.t[:, 1, :], in0=ld.t[:, 1, :], in1=sinT.t[:, :], op=ALU.mult), [ld.r[1], sinT.r0], [ld.r[1]])
            if debug and g == 1 and dstK is ksT:
                DBG("g1_ld_mul", ld, [128, 2, S])
            P.op("dve", "tensor_tensor", dict(out=dstK.t[:, :], in0=ld.t[:, 0, :], in1=ld.t[:, 1, :], op=ALU.add), [ld.r[0], ld.r[1]], [dstK.r0])
        for (vch, dstV) in ((C_KV + 6 + g, vsa), (C_KV + 10 + g, vwa)):
            P.dma("sp", ld.t[:, 0, :], zfm[vch], writes=[ld.r[0]])
            P.op("act", "copy", dict(out=vb.t[:, :], in_=ld.t[:, 0, :]), [ld.r[0]], [vb.r0])
            for t4 in range(8):
                for a in range(4):
                    tt = t4 * 4 + a
                    P.op("pe", "transpose", dict(out=ps_trb.t[:, a * 128:(a + 1) * 128], in_=vb.t[:, tt * 128:(tt + 1) * 128], identity=identb.t[:, :]),
                         [vb.r0, identb.r0], [ps_trb.r0])
                cast(dstV.t[:, t4 * 4:(t4 + 1) * 4, 0:128], ps_trb.t[:, 0:512].rearrange("p (a d) -> p a d", a=4), [ps_trb.r0], [dstV.r0])
        P.emit_stage()
        P.pop()
        if upto == "C0":
            P.close()
            return nc

        P.push()
        zq = P.sbuf("zq", [128, 4, 512], F32, nres=4)
        zqs = P.sbuf("zqs", [128, 4, 512], F32, nres=4)
        qn = P.sbuf("qn", [128, 4, 512], BF16)
        qr = P.sbuf("qr", [128, 4, 512], BF16)
        cmbq = P.sbuf("cmbq", [128, 2, 512], BF16)
        fa = P.sbuf("fa", [128, 4, 64], F32)
        na = P.sbuf("na", [128, 4, 64], F32)
        vm = P.sbuf("vm", [128, 4, 64], F32)
        gTs = P.sbuf("gTs", [24, 512], F32)
        gts = P.sbuf("gts", [128, 4, 24], F32)
        pc = P.sbuf("pc", [128, 2, 2, 512], BF16, nres=2)
        pt = P.sbuf("pt", [128, 3, 512], BF16, nres=3)
        oacc = P.sbuf("oacc", [128, 4, 4, 128], F32)
        imp = P.sbuf("imp", [128, 4, 64], F32)
        impp = P.sbuf("impp", [128, 64], F32)
        imp2 = P.sbuf("imp2", [128, 64], F32)
        m8a = P.sbuf("m8a", [128, 8], F32)
        m8b = P.sbuf("m8b", [128, 8], F32)
        msel = P.sbuf("msel", [128, 64], F32)
        bmb = P.sbuf("bmb", [128, 64], BF16)
        biasT = P.sbuf("biasT", [128, 512], BF16)
        P.op("dve", "memset", dict(ap=biasT.t[:, :], constant=0.0), [], [biasT.r0])
        sm = P.sbuf("sm", [128, 8], F32)
        obb = P.sbuf("obb", [128, 4, 4, 128], BF16)
        obT = P.sbuf("obT", [128, 4, 512], BF16)
        ptr = [0]
        aset = [0]
        for qc in range(NQC):
            q0 = qc * 512
            for hq in range(4):
                P.dma("sp", zq.t[:, hq, :], zfm[C_Q + 4 * g + hq, :, q0:q0 + 512], writes=[zq.r[hq]])
                P.dma("sp", zqs.t[:, hq, :], zfm[C_QS + 4 * g + hq, :, q0:q0 + 512], writes=[zqs.r[hq]])
            P.dma("sp", cmbq.t[:], cmb_d[:, :, q0:q0 + 512], writes=[cmbq.r0])
            P.dma("sp", fa.t[:], fa_d[:, qc * 4:qc * 4 + 4, :], writes=[fa.r0])
            P.dma("sp", na.t[:], na_d[:, qc * 4:qc * 4 + 4, :], writes=[na.r0])
            P.dma("sp", vm.t[:], vm_d[:, qc * 4:qc * 4 + 4, :], writes=[vm.r0])
            P.dma("sp", gTs.t[:, :], zfm[C_NG, 0:24, q0:q0 + 512], writes=[gTs.r0])
            P.op("act", "copy", dict(out=qn.t[:], in_=zq.t[:]), zq.r, [qn.r0])
            cs = cosT.t[:, q0:q0 + 512].unsqueeze(1).broadcast_to([128, 4, 512])
            sn = sinT.t[:, q0:q0 + 512].unsqueeze(1).broadcast_to([128, 4, 512])
            P.op("dve", "tensor_tensor", dict(out=zq.t[:], in0=zq.t[:], in1=cs, op=ALU.mult), zq.r + [cosT.r0], zq.r)
            P.op("pool", "tensor_tensor", dict(out=zqs.t[:], in0=zqs.t[:], in1=sn, op=ALU.mult), zqs.r + [sinT.r0], zqs.r)
            P.op("dve", "tensor_tensor", dict(out=qr.t[:], in0=zq.t[:], in1=zqs.t[:], op=ALU.add), zq.r + zqs.r, [qr.r0])
            for qt in range(4):
                P.op("pe", "transpose", dict(out=ps_trf.t[:, qt * 24:(qt + 1) * 24], in_=gTs.t[0:24, qt * 128:(qt + 1) * 128], identity=identf.t[0:24, 0:24]),
                     [gTs.r0, identf.r0], [ps_trf.r0])
            P.op("dve", "tensor_copy", dict(out=gts.t[:].rearrange("p a c -> p (a c)"), in_=ps_trf.t[:, 0:96]), [ps_trf.r0], [gts.r0])

            def post(hh, qt, bank, off, br, first, aux=None):
                h = 4 * g + hh
                accv = ps_acc.t[:, bank, :]
                P.op("dve", "tensor_scalar", dict(out=sm.t[:, 0:1], in0=accv[:, off + 128:off + 129], scalar1=1e-30, scalar2=None, op0=ALU.max),
                     [ps_acc.r[bank]], [sm.r0])
                P.op("dve", "reciprocal", dict(out=sm.t[:, 1:2], in_=sm.t[:, 0:1]), [sm.r0], [sm.r0])
                P.op("dve", "tensor_tensor", dict(out=sm.t[:, 2:3], in0=sm.t[:, 1:2], in1=gts.t[:, qt, h * 3 + br:h * 3 + br + 1], op=ALU.mult),
                     [sm.r0, gts.r0], [sm.r0])
                if first:
                    P.op("dve", "tensor_scalar", dict(out=oacc.t[:, qt, hh, :], in0=accv[:, off:off + 128], scalar1=sm.t[:, 2:3], scalar2=None, op0=ALU.mult),
                         [ps_acc.r[bank], sm.r0], [oacc.r0])
                else:
                    P.op("dve", "scalar_tensor_tensor", dict(out=oacc.t[:, qt, hh, :], in0=accv[:, off:off + 128], scalar=sm.t[:, 2:3], in1=oacc.t[:, qt, hh, :],
                                                             op0=ALU.mult, op1=ALU.add), [ps_acc.r[bank], sm.r0, oacc.r0], [oacc.r0])
                if aux is not None:
                    if hh == 0:
                        P.op("dve", "tensor_scalar", dict(out=imp.t[:, qt, :], in0=accv[:, off + 129:off + 193], scalar1=sm.t[:, 1:2], scalar2=None, op0=ALU.mult),
                             [ps_acc.r[bank], sm.r0], [imp.r0])
                    else:
                        P.op("dve", "scalar_tensor_tensor", dict(out=imp.t[:, qt, :], in0=accv[:, off + 129:off + 193], scalar=sm.t[:, 1:2], in1=imp.t[:, qt, :],
                                                                 op0=ALU.mult, op1=ALU.add), [ps_acc.r[bank], sm.r0, imp.r0], [imp.r0])

            if (g, qc) in ((0, 0), (1, 1)):
                tg = f"g{g}q{qc}"
                DBG(tg + "_gts", gts, [128, 4, 24]); DBG(tg + "_qn", qn, [128, 4, 512], BF16); DBG(tg + "_qr", qr, [128, 4, 512], BF16)
                DBG(tg + "_kcT", kcT, [128, 256], BF16); DBG(tg + "_vca", vca, [128, 2, 193], BF16)
                DBG(tg + "_ksT", ksT, [128, S], BF16); DBG(tg + "_kwT", kwT, [128, S], BF16)
                DBG(tg + "_vsa", vsa, [128, NTT, 129], BF16); DBG(tg + "_vwa", vwa, [128, NTT, 129], BF16)
            n_nch = 2 if (q0 + 511 >= 16 * 128 + 31) else 1
            for hh in range(4):
                pcb = hh % 2
                for nch in range(n_nch):
                    sb = sbank()
                    mm(ps_s.t[:, sb, :], kcT.t[:, nch * 128:(nch + 1) * 128], qn.t[:, hh, :], True, False, [kcT.r0, qn.r0], [ps_s.r[sb]])
                    mm(ps_s.t[:, sb, :], identb.t[:, :], cmbq.t[:, nch, :], False, True, [identb.r0, cmbq.r0], [ps_s.r[sb]])
                    P.op("act", "activation", dict(out=pc.t[:, pcb, nch, :], in_=ps_s.t[:, sb, :], func=AF.Exp, scale=SCALE), [ps_s.r[sb]], [pc.r[pcb]])
                aset[0] ^= 1
                started = {}
                for qt in range(4):
                    bank = aset[0] * 2 + qt // 2
                    off = (qt % 2) * 193
                    for nch in range(n_nch):
                        mm(ps_acc.t[:, bank, off:off + 193], pc.t[:, pcb, nch, qt * 128:(qt + 1) * 128], vca.t[:, nch, :],
                           bank not in started, False, [pc.r[pcb], vca.r0], [ps_acc.r[bank]])
                        started[bank] = 1
                for qt in range(4):
                    post(hh, qt, aset[0] * 2 + qt // 2, (qt % 2) * 193, 0, True, aux=True)
            if (g, qc) in ((0, 0), (1, 1)):
                DBG(tg + "_oacc_c", oacc, [128, 4, 4, 128]); DBG(tg + "_imp", imp, [128, 4, 64])
            for qt in range(4):
                P.op("dve", "tensor_tensor", dict(out=impp.t[:, :], in0=imp.t[:, qt, :], in1=fa.t[:, qt, :], op=ALU.max), [imp.r0, fa.r0], [impp.r0])
                P.op("dve", "tensor_tensor", dict(out=impp.t[:, :], in0=impp.t[:, :], in1=na.t[:, qt, :], op=ALU.add), [impp.r0, na.r0], [impp.r0])
                P.op("dve", "max", dict(out=m8a.t[:, :], in_=impp.t[:, :]), [impp.r0], [m8a.r0])
                P.op("dve", "match_replace", dict(out=imp2.t[:, :], in_to_replace=m8a.t[:, :], in_values=impp.t[:, :], imm_value=-3.0e38),
                     [impp.r0, m8a.r0], [imp2.r0])
                P.op("dve", "max", dict(out=m8b.t[:, :], in_=imp2.t[:, :]), [imp2.r0], [m8b.r0])
                P.op("dve", "scalar_tensor_tensor", dict(out=msel.t[:, :], in0=impp.t[:, :], scalar=m8b.t[:, 7:8], in1=vm.t[:, qt, :],
                                                         op0=ALU.is_ge, op1=ALU.mult), [impp.r0, m8b.r0, vm.r0], [msel.r0])
                P.op("dve", "tensor_scalar", dict(out=bmb.t[:, :], in0=msel.t[:, :], scalar1=-NEG, scalar2=NEG, op0=ALU.mult, op1=ALU.add),
                     [msel.r0], [bmb.r0])
                P.op("pe", "transpose", dict(out=ps_trb.t[0:64, qt * 128:(qt + 1) * 128], in_=bmb.t[:, :], identity=identb.t[:, :]),
                     [bmb.r0, identb.r0], [ps_trb.r0])
            P.op("dve", "tensor_copy", dict(out=biasT.t[0:64, :], in_=ps_trb.t[0:64, 0:512]), [ps_trb.r0], [biasT.r0])
            if (g, qc) in ((0, 0), (1, 1)):
                DBG(tg + "_biasT", biasT, [128, 512], BF16)
            for br in (2, 1):
                for hh in range(4):
                    aset[0] ^= 1
                    started = {}
                    if br == 1:
                        kts = list(range(0, qc * 4 + 4))
                    else:
                        kts = list(range(max(0, qc * 4 - 4), qc * 4 + 4))
                    vsrc = vsa if br == 1 else vwa

                    def emit_scores(kt):
                        r = kt - qc * 4
                        sb = sbank()
                        if br == 1:
                            mm(ps_s.t[:, sb, :], ksT.t[:, kt * 128:(kt + 1) * 128], qr.t[:, hh, :], True, False, [ksT.r0, qr.r0], [ps_s.r[sb]])
                            mm(ps_s.t[:, sb, :], E_s.t[:, kt * 128:(kt + 1) * 128], biasT.t[:, :], False, r < 0, [E_s.r0, biasT.r0], [ps_s.r[sb]])
                            if r >= 0:
                                mm(ps_s.t[:, sb, :], identb.t[:, :], cb_s.t[:, r, :], False, True, [identb.r0, cb_s.r0], [ps_s.r[sb]])
                        else:
                            mm(ps_s.t[:, sb, :], kwT.t[:, kt * 128:(kt + 1) * 128], qr.t[:, hh, :], True, False, [kwT.r0, qr.r0], [ps_s.r[sb]])
                            btab = wb_s.t[:, r + 4, :] if r < 0 else cb_s.t[:, r, :]
                            mm(ps_s.t[:, sb, :], identb.t[:, :], btab, False, True, [identb.r0, cb_s.r0, wb_s.r0], [ps_s.r[sb]])
                        pb = ptr[0] % 3
                        ptr[0] += 1
                        P.op("act", "activation", dict(out=pt.t[:, pb, :], in_=ps_s.t[:, sb, :], func=AF.Exp, scale=SCALE), [ps_s.r[sb]], [pt.r[pb]])
                        return (kt, pb)

                    def emit_pv(kt, pb):
                        r = kt - qc * 4
                        for qt in range(4):
                            if r > qt:
                                continue
                            if br == 2 and r < qt - 4:
                                continue
                            bank = aset[0] * 2 + qt // 2
                            off = (qt % 2) * 129
                            mm(ps_acc.t[:, bank, off:off + 129], pt.t[:, pb, qt * 128:(qt + 1) * 128], vsrc.t[:, kt, :],
                               bank not in started, False, [pt.r[pb], vsrc.r0], [ps_acc.r[bank]])
                            started[bank] = 1

                    pend = None
                    for kt in kts:
                        cur = emit_scores(kt)
                        if pend is not None:
                            emit_pv(*pend)
                        pend = cur
                    emit_pv(*pend)
                    for qt in range(4):
                        post(hh, qt, aset[0] * 2 + qt // 2, (qt % 2) * 129, br, False)
            if (g, qc) in ((0, 0), (1, 1)):
                DBG(tg + "_oacc_w", oacc, [128, 4, 4, 128])
            P.op("act", "copy", dict(out=obb.t[:], in_=oacc.t[:]), [oacc.r0], [obb.r0])
            for hh in range(4):
                for qt in range(4):
                    P.op("pe", "transpose", dict(out=ps_trb.t[:, qt * 128:(qt + 1) * 128], in_=obb.t[:, qt, hh, :], identity=identb.t[:, :]),
                         [obb.r0, identb.r0], [ps_trb.r0])
                cast(obT.t[:, hh, :], ps_trb.t[:, 0:512], [ps_trb.r0], [obT.r0])
            for hq in range(4):
                P.dma("pool", obt_s[4 * g + hq, :, q0:q0 + 512], obT.t[:, hq, :], reads=[obT.r0])
        P.emit_stage()
        P.pop()
    P.pop()
    if upto == "C":
        P.close()
        return nc

    def rmsnorm_tile(src, gain, dst_f32, dst_bf, sq, junk_):
        P.op("act", "activation", dict(out=junk_.t[:, :], in_=src.t[:, :], func=AF.Square, accum_out=sq.t[:, 0:1]), [src.r0], [junk_.r0, sq.r0])
        P.op("act", "activation", dict(out=sq.t[:, 1:2], in_=sq.t[:, 0:1], func=AF.Sqrt, scale=1.0 / D, bias=1e-6), [sq.r0], [sq.r0])
        P.op("dve", "reciprocal", dict(out=sq.t[:, 2:3], in_=sq.t[:, 1:2]), [sq.r0], [sq.r0])
        P.op("dve", "scalar_tensor_tensor", dict(out=dst_f32.t[:, :], in0=src.t[:, :], scalar=sq.t[:, 2:3], in1=gain.t[:, :], op0=ALU.mult, op1=ALU.mult),
             [src.r0, sq.r0, gain.r0], [dst_f32.r0])
        if dst_bf is not None:
            P.op("act", "copy", dict(out=dst_bf.t[:, :], in_=dst_f32.t[:, :]), [dst_f32.r0], [dst_bf.r0])

    P.push()
    w3 = [P.sbuf(f"w3s{i}", [128, 8, D], BF16) for i in range(3)]
    for i in range(3):
        P.dma("sp", w3[i].t[:], w5bf[i], writes=[w3[i].r0])
    wA, wB, wO = w3
    obTq = P.sbuf("obTq", [128, 8, 512], BF16, nres=8)
    maTq = P.sbuf("maTq", [128, 8, 512], BF16, nres=8)
    mgT = P.sbuf("mgT", [128, 8, 512], BF16)
    g01 = P.sbuf("g01", [128, 2, 16, 512], F32, nres=2)
    tA = P.sbuf("tA", [128, 2, 512], F32, nres=2)
    tB = P.sbuf("tB", [128, 2, 512], F32, nres=2)
    xtl = P.sbuf("xtl", [128, 2, D], F32, nres=2)
    h1t = P.sbuf("h1t", [128, 2, D], F32, nres=2)
    ps_y = P.psum("ps_y", [128, 4, 512], F32, nres=4)
    ps_h = P.psum("ps_h", [128, 2, 512], F32, nres=2)
    ti = 0
    for qc in range(NQC):
        q0 = qc * 512
        gb = qc % 2
        P.dma("sp", obTq.t[:], obt_s[:, :, q0:q0 + 512].rearrange("c p t -> p c t"), writes=list(obTq.r))
        P.dma("sp", maTq.t[:], ma_s[:, :, q0:q0 + 512].rearrange("c p t -> p c t"), writes=list(maTq.r))
        P.dma("pool", g01.t[:, gb], zfm[C_M:C_M + 16, :, q0:q0 + 512].rearrange("c p t -> p c t"), writes=[g01.r[gb]])
        for c in range(8):
            b = c % 2
            for k in range(8):
                mm(ps_y.t[:, 2 * b, :], wA.t[:, k, c * 128:(c + 1) * 128], maTq.t[:, k, :], k == 0, k == 7, [wA.r0, maTq.r[k]], [ps_y.r[2 * b]])
            for k in range(8):
                mm(ps_y.t[:, 2 * b + 1, :], wB.t[:, k, c * 128:(c + 1) * 128], obTq.t[:, k, :], k == 0, k == 7, [wB.r0, obTq.r[k]], [ps_y.r[2 * b + 1]])
            P.op("dve", "tensor_tensor", dict(out=tA.t[:, b, :], in0=ps_y.t[:, 2 * b, :], in1=g01.t[:, gb, c, :], op=ALU.mult), [ps_y.r[2 * b], g01.r[gb]], [tA.r[b]])
            P.op("dve", "tensor_tensor", dict(out=tB.t[:, b, :], in0=ps_y.t[:, 2 * b + 1, :], in1=g01.t[:, gb, 8 + c, :], op=ALU.mult), [ps_y.r[2 * b + 1], g01.r[gb]], [tB.r[b]])
            P.op("pool", "tensor_tensor", dict(out=mgT.t[:, c, :], in0=tA.t[:, b, :], in1=tB.t[:, b, :], op=ALU.add), [tA.r[b], tB.r[b]], [mgT.r0])
        for qt in range(4):
            tt = qc * 4 + qt
            b = ti % 2
            ti += 1
            P.dma("sp", xtl.t[:, b, :], x_d[tt * 128:(tt + 1) * 128, :], writes=[xtl.r[b]])
            for half in range(2):
                for c in range(8):
                    mm(ps_h.t[:, half, :], mgT.t[:, c, qt * 128:(qt + 1) * 128], wO.t[:, c, half * 512:(half + 1) * 512], c == 0, c == 7, [mgT.r0, wO.r0], [ps_h.r[half]])
                P.op("dve", "tensor_tensor", dict(out=h1t.t[:, b, half * 512:(half + 1) * 512], in0=ps_h.t[:, half, :], in1=xtl.t[:, b, half * 512:(half + 1) * 512], op=ALU.add),
                     [ps_h.r[half], xtl.r[b]], [h1t.r[b]])
            P.dma("pool", h1_s[tt * 128:(tt + 1) * 128, :], h1t.t[:, b, :], reads=[h1t.r[b]])
    P.emit_stage()
    P.pop()
    if upto == "D1":
        P.close()
        return nc

    P.push()
    wQ = P.sbuf("wQ", [128, 8, D], BF16)
    wG = P.sbuf("wG", [128, 8, D], BF16)
    wPs = P.sbuf("wPs", [128, 2, D], BF16)
    P.dma("sp", wQ.t[:], w5bf[3], writes=[wQ.r0])
    P.dma("sp", wG.t[:], w5bf[4], writes=[wG.r0])
    P.dma("sp", wPs.t[:], wpbf, writes=[wPs.r0])
    kbdf = P.sbuf("kbdf", [128, 256], F32)
    kbdb = P.sbuf("kbdb", [128, 256], BF16)
    P.dma("sp", kbdf.t[:, :], kbd_d, writes=[kbdf.r0])
    P.op("dve", "tensor_copy", dict(out=kbdb.t[:, :], in_=kbdf.t[:, :]), [kbdf.r0], [kbdb.r0])
    gains = []
    for nm, gd in (("gfb", gf_d), ("gpb", gp_d), ("glb", gl_d)):
        t_ = P.sbuf(nm, [128, D], F32)
        P.dma("sp", t_.t[:, :], gd.partition_broadcast(128), writes=[t_.r0])
        gains.append(t_)
    gfb, gpb, glb = gains
    io128 = P.sbuf("io128", [128, 128], I32)
    io256 = P.sbuf("io256", [128, 256], I32)
    io16 = P.sbuf("io16", [128, 16], F32)
    P.dma("sp", io128.t[:, :], io128_d[:, 0, :], writes=[io128.r0])
    P.dma("sp", io256.t[:, :], io256_d[:, 0, :], writes=[io256.r0])
    P.dma("sp", io16.t[:, :], io16_d[:, 0, 0, :], writes=[io16.r0])
    NG = int(_os.environ.get("K_NG", "13"))

    def dbl(name, shape, dt):
        return [P.sbuf(f"{name}{i}", shape, dt) for i in range(2)]

    hh2 = [P.sbuf(f"hh{i}", [128, D], F32) for i in range(3)]
    ptl2 = [P.sbuf(f"ptl{i}", [128, 256], F32) for i in range(3)]
    xn2 = dbl("xn", [128, D], F32)
    eidx2 = dbl("eidx", [128, 128], I32)
    gte2 = dbl("gte", [128, 128], F32)
    ptb = P.sbuf("ptb", [128, 256], BF16)
    pT = P.sbuf("pT", [128, 2, 128], BF16)
    xnb2 = dbl("xnb", [128, D], BF16)
    prodb = P.sbuf("prodb", [128, 4, D], BF16, nres=4)
    junkA = P.sbuf("junkA", [128, D], BF16)
    xnT = P.sbuf("xnT", [128, 8, 128], BF16)
    xne = P.sbuf("xne", [128, D], F32)
    xnbe = P.sbuf("xnbe", [128, D], BF16)
    xnTe = P.sbuf("xnTe", [128, 8, 128], BF16)
    qpT = P.sbuf("qpT", [128, 8, 128], BF16)
    junkF = P.sbuf("junkF", [128, D], BF16)
    junkG = P.sbuf("junkG", [128, D], BF16)
    junkE = P.sbuf("junkE", [128, D], BF16)
    sqF = P.sbuf("sqF", [128, 4], F32)
    sqE = P.sbuf("sqE", [128, 4], F32)
    s_all = P.sbuf("s_all", [128, 16, 128], F32)
    stmp = P.sbuf("stmp", [128, 256], F32)
    v12 = P.sbuf("v12", [128, 16, 16], F32)
    i12i = P.sbuf("i12i", [128, 16, 16], I32)
    i12f = P.sbuf("i12f", [128, 16, 16], F32)
    cand = P.sbuf("cand", [128, 8, 256], F32)
    oh = cand
    best = P.sbuf("best", [128, 8, 16], F32)
    posi2 = P.sbuf("posi2", [128, 8, 16], I32)
    abi = P.sbuf("abi", [128, 2, 8, 16], I32)
    abf = P.sbuf("abf", [128, 2, 8, 16], F32)
    isel = P.sbuf("isel", [128, 2, 8, 16], F32)
    ef = P.sbuf("ef", [128, 128], F32)
    bex = P.sbuf("bex", [128, 8, 16], F32)
    bsm = P.sbuf("bsm", [128, 2, 8], F32)
    dots = P.sbuf("dots", [128, 128], F32, nres=128)
    actv = P.sbuf("actv", [128, 128], F32, nres=32)
    a1 = P.sbuf("a1", [128, 128], F32, nres=32)
    ug = P.sbuf("ug", [128, NG, 2 * D], BF16, nres=NG)
    tmpv = P.sbuf("tmpv", [128, 4, D], BF16, nres=4)
    gate = P.sbuf("gate", [128, D], F32)
    tmpg = P.sbuf("tmpg", [128, 512], F32)
    ot = P.sbuf("ot", [128, D], F32)
    ps_t = P.psum("ps_t2", [128, 1024], BF16)
    ps_q = P.psum("ps_q", [128, 2, 512], F32, nres=2)
    ps_c = P.psum("ps_c", [128, 2, 512], F32, nres=2)
    ps_o = P.psum("ps_o", [128, 2, 512], F32, nres=2)
    cnt = {"gi": 0, "tvi": 0}

    def transpose8(src_bf, dst, n):
        for c in range(n):
            P.op("pe", "transpose", dict(out=ps_t.t[:, c * 128:(c + 1) * 128], in_=src_bf.t[:, c * 128:(c + 1) * 128], identity=identb.t[:, :]),
                 [src_bf.r0, identb.r0], [ps_t.r0])
        cast(dst.t[:, 0:n, :], ps_t.t[:, 0:n * 128].rearrange("p (c t) -> p c t", c=n), [ps_t.r0], [dst.r0])

    v12v = v12.t[:].rearrange("p (h c) k -> p h c k", c=2)
    i12v = i12f.t[:].rearrange("p (h c) k -> p h c k", c=2)

    def front(tt):
        par = tt % 2
        hh_, ptl, xn, eidx, gte = hh2[tt % 3], ptl2[tt % 3], xn2[par], eidx2[par], gte2[par]
        xnb = xnb2[par]
        P.dma("sp", hh_.t[:, :], h1_s[tt * 128:(tt + 1) * 128, :], writes=[hh_.r0])
        P.dma("sp", ptl.t[:, :], p_d[tt * 128:(tt + 1) * 128, :], writes=[ptl.r0])
        rmsnorm_tile(hh_, gfb, xn, xnb, sqF, junkF)
        yield
        transpose8(xnb, xnT, 8)
        yield
        for hd in range(8):
            bk = hd // 4
            for k in range(8):
                mm(ps_q.t[:, bk, (hd % 4) * 128:(hd % 4 + 1) * 128], wQ.t[:, k, hd * 128:(hd + 1) * 128], xnT.t[:, k, :],
                   (k == 0 and hd % 4 == 0), False, [wQ.r0, xnT.r0], [ps_q.r[bk]])
        for bk in range(2):
            cast(qpT.t[:, bk * 4:(bk + 1) * 4, :], ps_q.t[:, bk, :].rearrange("p (a t) -> p a t", a=4), [ps_q.r[bk]], [qpT.r0])
        for rnd in range(2):
            for h4 in range(4):
                hd = rnd * 4 + h4
                bk = h4 // 2
                mm(ps_c.t[:, bk, (hd % 2) * 256:(hd % 2 + 1) * 256], qpT.t[:, hd, :], kbdb.t[:, :], hd % 2 == 0, False, [qpT.r0, kbdb.r0], [ps_c.r[bk]])
            for bk in range(2):
                cast(s_all.t[:, rnd * 8 + bk * 4:rnd * 8 + (bk + 1) * 4, :], ps_c.t[:, bk, :].rearrange("p (a n) -> p a n", a=4), [ps_c.r[bk]], [s_all.r0])
            yield
        sI = s_all.t[:].bitcast(I32)
        P.op("dve", "tensor_scalar", dict(out=sI, in0=sI, scalar1=-128, scalar2=None, op0=ALU.bitwise_and), [s_all.r0], [s_all.r0])
        P.op("dve", "tensor_tensor", dict(out=sI, in0=sI, in1=io128.t[:, :].unsqueeze(1).broadcast_to([128, 16, 128]), op=ALU.bitwise_or), [s_all.r0, io128.r0], [s_all.r0])
        yield
        for j in range(16):
            P.op("dve", "max", dict(out=v12.t[:, j, 0:8], in_=s_all.t[:, j, :]), [s_all.r0], [v12.r0])
            P.op("dve", "match_replace", dict(out=stmp.t[:, 0:128], in_to_replace=v12.t[:, j, 0:8], in_values=s_all.t[:, j, :], imm_value=-3.0e38),
                 [s_all.r0, v12.r0], [stmp.r0])
            P.op("dve", "max", dict(out=v12.t[:, j, 8:16], in_=stmp.t[:, 0:128]), [stmp.r0], [v12.r0])
            if j % 2 == 1:
                yield
        P.op("dve", "tensor_scalar", dict(out=i12i.t[:], in0=v12.t[:].bitcast(I32), scalar1=127, scalar2=None, op0=ALU.bitwise_and), [v12.r0], [i12i.r0])
        P.op("dve", "tensor_copy", dict(out=i12f.t[:], in_=i12i.t[:]), [i12i.r0], [i12f.r0])
        candv = cand.t[:].rearrange("p h (a b) -> p h a b", a=16)
        P.op("dve", "tensor_tensor", dict(out=candv, in0=v12v[:, :, 0, :].unsqueeze(3).broadcast_to([128, 8, 16, 16]),
                                          in1=v12v[:, :, 1, :].unsqueeze(2).broadcast_to([128, 8, 16, 16]), op=ALU.add), [v12.r0], [cand.r0])
        yield
        cI = cand.t[:].bitcast(I32)
        P.op("dve", "tensor_scalar", dict(out=cI, in0=cI, scalar1=-256, scalar2=None, op0=ALU.bitwise_and), [cand.r0], [cand.r0])
        P.op("dve", "tensor_tensor", dict(out=cI, in0=cI, in1=io256.t[:, :].unsqueeze(1).broadcast_to([128, 8, 256]), op=ALU.bitwise_or), [cand.r0, io256.r0], [cand.r0])
        yield
        for hd in range(8):
            P.op("dve", "max", dict(out=best.t[:, hd, 0:8], in_=cand.t[:, hd, :]), [cand.r0], [best.r0])
            P.op("dve", "match_replace", dict(out=stmp.t[:, :], in_to_replace=best.t[:, hd, 0:8], in_values=cand.t[:, hd, :], imm_value=-3.0e38),
                 [cand.r0, best.r0], [stmp.r0])
            P.op("dve", "max", dict(out=best.t[:, hd, 8:16], in_=stmp.t[:, :]), [stmp.r0], [best.r0])
            if hd % 2 == 1:
                yield
        P.op("dve", "tensor_scalar", dict(out=posi2.t[:], in0=best.t[:].bitcast(I32), scalar1=255, scalar2=None, op0=ALU.bitwise_and), [best.r0], [posi2.r0])
        P.op("dve", "tensor_scalar", dict(out=abi.t[:, 0], in0=posi2.t[:], scalar1=4, scalar2=None, op0=ALU.arith_shift_right), [posi2.r0], [abi.r0])
        P.op("dve", "tensor_scalar", dict(out=abi.t[:, 1], in0=posi2.t[:], scalar1=15, scalar2=None, op0=ALU.bitwise_and), [posi2.r0, abi.r0], [abi.r0])
        P.op("dve", "tensor_copy", dict(out=abf.t[:], in_=abi.t[:]), [abi.r0], [abf.r0])
        yield
        ohv = oh.t[:].rearrange("p h (a b) -> p h a b", a=16)
        for c in range(2):
            P.op("dve", "tensor_tensor", dict(out=ohv, in0=abf.t[:, c].unsqueeze(3).broadcast_to([128, 8, 16, 16]),
                                              in1=io16.t[:, :].unsqueeze(1).unsqueeze(1).broadcast_to([128, 8, 16, 16]), op=ALU.is_equal),
                 [abf.r0, io16.r0], [oh.r0])
            P.op("dve", "tensor_tensor", dict(out=ohv, in0=ohv, in1=i12v[:, :, c, :].unsqueeze(2).broadcast_to([128, 8, 16, 16]), op=ALU.mult),
                 [oh.r0, i12f.r0], [oh.r0])
            P.op("dve", "tensor_reduce", dict(out=isel.t[:, c], in_=ohv, axis=AX.X, op=ALU.add), [oh.r0], [isel.r0])
            yield
        P.op("dve", "scalar_tensor_tensor", dict(out=ef.t[:, :], in0=isel.t[:, 0].rearrange("p h k -> p (h k)"), scalar=128.0,
                                                 in1=isel.t[:, 1].rearrange("p h k -> p (h k)"), op0=ALU.mult, op1=ALU.add), [isel.r0], [ef.r0])
        P.op("dve", "tensor_copy", dict(out=eidx.t[:, :], in_=ef.t[:, :]), [ef.r0], [eidx.r0])
        P.op("dve", "tensor_tensor", dict(out=bex.t[:], in0=best.t[:], in1=best.t[:, :, 0:1].broadcast_to([128, 8, 16]), op=ALU.subtract), [best.r0], [bex.r0])
        P.op("act", "activation", dict(out=bex.t[:], in_=bex.t[:], func=AF.Exp), [bex.r0], [bex.r0])
        yield
        P.op("dve", "tensor_reduce", dict(out=bsm.t[:, 0], in_=bex.t[:], axis=AX.X, op=ALU.add), [bex.r0], [bsm.r0])
        P.op("dve", "reciprocal", dict(out=bsm.t[:, 1], in_=bsm.t[:, 0]), [bsm.r0], [bsm.r0])
        P.op("dve", "tensor_tensor", dict(out=gte.t[:, :].rearrange("p (h k) -> p h k", h=8), in0=bex.t[:],
                                          in1=bsm.t[:, 1].unsqueeze(2).broadcast_to([128, 8, 16]), op=ALU.mult), [bex.r0, bsm.r0], [gte.r0])
        yield

    def gather_group(tt, grp):
        par = tt % 2
        xn, eidx, gte = xn2[par], eidx2[par], gte2[par]
        bufs = []
        for j in range(4):
            slot = grp * 4 + j
            b = cnt["gi"] % NG
            cnt["gi"] += 1
            bufs.append(b)
            P.dma("pool", ug.t[:, b, :], uv_s[:, :], reads=[eidx.r0], writes=[ug.r[b]],
                  indirect=dict(out_offset=None, in_offset=bass.IndirectOffsetOnAxis(ap=eidx.t[:, slot:slot + 1], axis=0)))
            if j % 2 == 0:
                P.op("dve", "scalar_tensor_tensor", dict(out=junkG.t[:, :], in0=ug.t[:, b, 0:D], scalar=1.0, in1=xn.t[:, :],
                                                         op0=ALU.mult, op1=ALU.mult, accum_out=dots.t[:, slot:slot + 1]),
                     [ug.r[b], xn.r0], [dots.r[slot]])
            else:
                pb_ = cnt.get("pbi", 0) % 4
                cnt["pbi"] = cnt.get("pbi", 0) + 1
                P.op("dve", "tensor_tensor", dict(out=prodb.t[:, pb_, :], in0=ug.t[:, b, 0:D], in1=xnb2[par].t[:, :], op=ALU.mult),
                     [ug.r[b], xnb2[par].r0], [prodb.r[pb_]])
                P.op("act", "activation", dict(out=junkA.t[:, :], in_=prodb.t[:, pb_, :], func=AF.Copy, accum_out=dots.t[:, slot:slot + 1]),
                     [prodb.r[pb_]], [dots.r[slot]])
        if _os.environ.get("K_NOCONS") or _os.environ.get("K_DOTSONLY"):
            return
        if cnt.get("pend") is not None:
            finish_group(*cnt["pend"])
        gs = slice(grp * 4, grp * 4 + 4)
        P.op("act", "activation", dict(out=a1.t[:, gs], in_=dots.t[:, gs], func=AF.Gelu_apprx_tanh), dots.r[grp * 4:grp * 4 + 4], [a1.r[grp]])
        cnt["pend"] = (tt, grp, bufs)
        if grp == 31:
            finish_group(*cnt["pend"])
            cnt["pend"] = None

    def finish_group(tt, grp, bufs):
        gte = gte2[tt % 2]
        gs = slice(grp * 4, grp * 4 + 4)
        P.op("dve", "tensor_tensor", dict(out=actv.t[:, gs], in0=a1.t[:, gs], in1=gte.t[:, gs], op=ALU.mult), [a1.r[grp], gte.r0], [actv.r[grp]])
        for j in range(4):
            slot = grp * 4 + j
            b = bufs[j]
            tb = cnt["tvi"] % 4
            cnt["tvi"] += 1
            P.op("act", "activation", dict(out=tmpv.t[:, tb, :], in_=ug.t[:, b, D:2 * D], func=AF.Copy, scale=actv.t[:, slot:slot + 1]),
                 [ug.r[b], actv.r[grp]], [tmpv.r[tb]])
            for half in range(2):
                mm(ps_o.t[:, half, :], identb.t[:, :], tmpv.t[:, tb, half * 512:(half + 1) * 512], slot == 0, slot == 127,
                   [identb.r0, tmpv.r[tb]], [ps_o.r[half]])

    def epilogue(tt):
        hh_, ptl = hh2[tt % 3], ptl2[tt % 3]
        for half in range(2):
            P.op("dve", "tensor_tensor", dict(out=hh_.t[:, half * 512:(half + 1) * 512], in0=ps_o.t[:, half, :], in1=hh_.t[:, half * 512:(half + 1) * 512], op=ALU.add),
                 [ps_o.r[half], hh_.r0], [hh_.r0])
        yield
        rmsnorm_tile(hh_, gpb, xne, xnbe, sqE, junkE)
        yield
        transpose8(xnbe, xnTe, 8)
        yield
        P.op("act", "copy", dict(out=ptb.t[:, :], in_=ptl.t[:, :]), [ptl.r0], [ptb.r0])
        transpose8(ptb, pT, 2)
        yield
        for half in range(2):
            for k in range(8):
                mm(ps_q.t[:, half, :], xnTe.t[:, k, :], wG.t[:, k, half * 512:(half + 1) * 512], k == 0, k == 7, [xnTe.r0, wG.r0], [ps_q.r[half]])
            P.op("act", "activation", dict(out=gate.t[:, half * 512:(half + 1) * 512], in_=ps_q.t[:, half, :], func=AF.Sigmoid), [ps_q.r[half]], [gate.r0])
            for k in range(2):
                mm(ps_c.t[:, half, :], pT.t[:, k, :], wPs.t[:, k, half * 512:(half + 1) * 512], k == 0, k == 1, [pT.r0, wPs.r0], [ps_c.r[half]])
            P.op("dve", "tensor_tensor", dict(out=tmpg.t[:, :], in0=ps_c.t[:, half, :], in1=gate.t[:, half * 512:(half + 1) * 512], op=ALU.mult),
                 [ps_c.r[half], gate.r0], [tmpg.r0])
            P.op("dve", "tensor_tensor", dict(out=hh_.t[:, half * 512:(half + 1) * 512], in0=hh_.t[:, half * 512:(half + 1) * 512], in1=tmpg.t[:, :], op=ALU.add),
                 [hh_.r0, tmpg.r0], [hh_.r0])
            yield
        rmsnorm_tile(hh_, glb, ot, None, sqE, junkE)
        yield
        P.dma("sp", out_d[tt * 128:(tt + 1) * 128, :], ot.t[:, :], reads=[ot.r0])

    for _ in front(0):
        pass
    ep = iter(())
    EPG = int(_os.environ.get("K_EPG", "8"))
    for tt in range(NTT):
        nxt = front(tt + 1) if tt + 1 < NTT else iter(())
        for grp in range(32):
            gather_group(tt, grp)
            if grp == EPG:
                for _ in ep:
                    pass
            if grp >= 1:
                next(nxt, None)
        for _ in nxt:
            pass
        ep = epilogue(tt)
        next(ep)
    for _ in ep:
        pass
    P.emit_stage()
    P.pop()

    P.close()
    return nc


def kernel(**inputs):
    consts = make_consts()
    shared = prep_shared(inputs)
    shared.update(consts)
    in_maps = []
    for b in range(8):
        m = dict(shared)
        m["x"] = np.ascontiguousarray(inputs["x"][b])
        m["p"] = np.ascontiguousarray(inputs["p"][0, b])
        m["positions"] = np.ascontiguousarray(inputs["positions"][b].reshape(1, S).astype(np.int32))
        in_maps.append(m)
    nc = build_nc()
    res = run_bass_kernel_spmd(nc, in_maps, core_ids=list(range(8)))
    return np.stack([np.asarray(r["out"]) for r in res.results], axis=0).astype(np.float32)
```

```python
from contextlib import ExitStack
import os as _os
import numpy as np
import ml_dtypes
import concourse.bass as bass
import concourse.mybir as mybir
from concourse.bass_utils import run_bass_kernel_spmd

F32 = mybir.dt.float32
BF16 = mybir.dt.bfloat16
I32 = mybir.dt.int32
AF = mybir.ActivationFunctionType
ALU = mybir.AluOpType
AX = mybir.AxisListType

S = 4096
D = 1024
NQC = 8
NTT = 32
NEG = -30000.0
SCALE = 128 ** -0.5
COMPUTE = ("pe", "dve", "act", "pool")
NOSELF = tuple(x for x in _os.environ.get("K_NOSELF", "").split(",") if x)
NROT = int(_os.environ.get("K_NROT", "8"))
NCH = 65
WCOLS = NCH * 128

C_RX, C_RG, C_Q, C_QS, C_KV, C_KSW, C_M, C_NG = 0, 8, 16, 24, 32, 44, 48, 64


class Res:
    __slots__ = ("w", "r", "name")

    def __init__(self, name=""):
        self.w = None
        self.r = {}
        self.name = name


class TT:
    def __init__(self, t, nres, name):
        self.t = t
        self.r = [Res(f"{name}.{i}") for i in range(nres)]

    @property
    def r0(self):
        return self.r[0]


class Prog:
    def __init__(self, nc):
        self.nc = nc
        self.scopes = [ExitStack()]
        self.streams = {e: [] for e in ("pe", "dve", "act", "pool", "sp")}
        self.cnt = {e: 0 for e in COMPUTE}
        self.seen = {e: {} for e in self.streams}
        self.sems = {}
        for i in range(int(_os.environ.get("K_SEMPAD", "0"))):
            self.scopes[0].enter_context(nc.semaphore(f"s_pad{i}"))
        for e in COMPUTE:
            self.sems[e] = self.scopes[0].enter_context(nc.semaphore(f"s_{e}"))
        self.dq = {}
        self.dma_total = {}
        for q in ("sp", "act", "pool"):
            self.dq[q] = {"n": 0, "sems": []}
            nrot_q = int(_os.environ.get("K_NROT_" + q.upper(), "1" if q == "sp" else str(NROT)))
            self.dq[q]["nrot"] = nrot_q
            for i in range(nrot_q):
                k = ("dma", q, i)
                self.sems[k] = self.scopes[0].enter_context(nc.semaphore(f"s_dma_{q}_{i}"))
                self.dq[q]["sems"].append(k)
                self.dma_total[k] = 0
        self.n_instr = 0

    def push(self):
        self.scopes.append(ExitStack())

    def pop(self):
        self.scopes.pop().close()

    def sbuf(self, name, shape, dtype, nres=1):
        self.n_instr += 0
        self.uid = getattr(self, "uid", 0) + 1
        t = self.scopes[-1].enter_context(self.nc.sbuf_tensor(f"{name}_{self.uid}", list(shape), dtype))
        return TT(t, nres, name)

    def psum(self, name, shape, dtype, nres=1):
        self.uid = getattr(self, "uid", 0) + 1
        t = self.scopes[-1].enter_context(self.nc.psum_tensor(f"{name}_{self.uid}", list(shape), dtype))
        return TT(t, nres, name)

    def _deps(self, eng, reads, writes):
        deps = {}

        def add(tok):
            if tok is None:
                return
            k, v = tok
            if deps.get(k, 0) < v:
                deps[k] = v

        for r in reads:
            add(r.w)
        for w in writes:
            add(w.w)
            for k, v in w.r.items():
                add((k, v))
        out = []
        for k, v in deps.items():
            if k == eng and (eng == "pe" or eng in NOSELF):
                continue
            if self.seen[eng].get(k, 0) >= v:
                continue
            self.seen[eng][k] = v
            out.append((k, v))
        return out

    def _record(self, tok, reads, writes):
        k, v = tok
        for r in reads:
            if r.r.get(k, 0) < v:
                r.r[k] = v
        for w in writes:
            w.w = tok
            w.r = {}

    def op(self, eng, meth, kw, reads=(), writes=()):
        waits = self._deps(eng, reads, writes)
        self.cnt[eng] += 1
        tok = (eng, self.cnt[eng])
        self._record(tok, reads, writes)
        self.streams[eng].append((meth, kw, waits, (eng, 1)))
        self.n_instr += 1

    def dma(self, q, out, in_, reads=(), writes=(), indirect=None, **kw):
        d = self.dq[q]
        i = d["n"]
        d["n"] += 1
        sk = d["sems"][i % d["nrot"]]
        prev = self.dma_total[sk]
        waits = self._deps(q, reads, writes)
        if prev > 0 and self.seen[q].get(sk, 0) < prev:
            self.seen[q][sk] = prev
            waits.append((sk, prev))
        self.dma_total[sk] = prev + 16
        tok = (sk, prev + 16)
        self._record(tok, reads, writes)
        kk = dict(out=out, in_=in_)
        if indirect is not None:
            kk.update(indirect)
        kk.update(kw)
        self.streams[q].append(("indirect_dma_start" if indirect is not None else "dma_start", kk, waits, (sk, 16)))
        self.n_instr += 1
        return tok

    def emit_stage(self):
        targets = [(e, self.cnt[e]) for e in COMPUTE if self.cnt[e] > 0]
        targets += [(k, v) for k, v in self.dma_total.items() if v > 0]
        for e in self.streams:
            ws = []
            for k, v in targets:
                if self.seen[e].get(k, 0) < v:
                    self.seen[e][k] = v
                    ws.append((k, v))
            if ws:
                self.streams[e].append((None, None, ws, None))
        sems = self.sems
        streams = self.streams

        def run(eng_obj, items):
            for meth, kw, waits, inc in items:
                for k, v in waits:
                    eng_obj.wait_ge(sems[k], v)
                if meth is None:
                    continue
                ins = getattr(eng_obj, meth)(**kw)
                ins.then_inc(sems[inc[0]], inc[1])

        with self.nc.Block() as block:
            @block.tensor
            def _(e):
                run(e, streams["pe"])

            @block.vector
            def _(e):
                run(e, streams["dve"])

            @block.scalar
            def _(e):
                run(e, streams["act"])

            @block.gpsimd
            def _(e):
                run(e, streams["pool"])

            @block.sync
            def _(e):
                run(e, streams["sp"])
        self.streams = {e: [] for e in streams}

    def close(self):
        while self.scopes:
            self.scopes.pop().close()


def _bf(a):
    return np.ascontiguousarray(a).astype(ml_dtypes.bfloat16)


def make_consts():
    c = {}
    c["c_identb"] = _bf(np.eye(128, dtype=np.float32))
    c["c_identf"] = np.eye(128, dtype=np.float32)
    half = 64
    inv = (10000.0 ** (-np.arange(half, dtype=np.float32) / np.float32(half))).astype(np.float32)
    c["c_invf"] = np.concatenate([inv, inv]).reshape(128, 1).astype(np.float32)
    c["c_sgn"] = np.concatenate([-np.ones(64), np.ones(64)]).reshape(128, 1).astype(np.float32)
    kp = np.arange(128)[:, None]
    q = np.arange(512)[None, :]
    cb = np.zeros((128, 4, 512), np.float32)
    wb = np.zeros((128, 4, 512), np.float32)
    for r in range(4):
        k = r * 128 + kp
        cb[:, r, :] = np.where(k <= q, 0.0, NEG)
        k2 = (r - 4) * 128 + kp
        wb[:, r, :] = np.where(q - k2 < 512, 0.0, NEG)
    c["c_cb"] = _bf(cb)
    c["c_wb"] = _bf(wb)
    n = (np.arange(2)[None, :, None] * 128 + np.arange(128)[:, None, None])
    t = np.arange(S)[None, None, :]
    cmb = np.where((16 * n + 31 <= t) & (n < 255), 0.0, NEG).astype(np.float32)
    c["c_cmb"] = _bf(cmb)
    j = np.arange(64)[:, None]
    kk = np.arange(S)[None, :]
    c["c_E"] = _bf(np.concatenate([(kk // 64 == j).astype(np.float32), np.zeros((64, S), np.float32)], 0))
    nn = np.arange(256)
    jj = np.arange(64)
    ov = ((16 * nn[:, None] < 64 * jj[None, :] + 64) & (16 * nn[:, None] + 31 >= 64 * jj[None, :]) & (nn[:, None] < 255))
    c["c_ovl"] = _bf(ov.astype(np.float32).reshape(2, 128, 64).transpose(1, 0, 2))
    tt = np.arange(S).reshape(NTT, 128)[:, :, None]
    cur = tt // 64
    j3 = np.arange(64)[None, None, :]
    forced = (j3 == 0) | (j3 == cur) | (j3 == cur - 1)
    valid = (64 * j3 <= tt)
    c["c_fa"] = np.ascontiguousarray(np.where(forced, 1000.0, 0.0).astype(np.float32).transpose(1, 0, 2))
    c["c_na"] = np.ascontiguousarray(np.where(valid, 0.0, -1e30).astype(np.float32).transpose(1, 0, 2))
    c["c_vm"] = np.ascontiguousarray(valid.astype(np.float32).transpose(1, 0, 2))
    c["c_iota128"] = np.ascontiguousarray(np.broadcast_to(np.arange(128, dtype=np.int32)[None, None, :], (128, 16, 128)))
    c["c_iota256"] = np.ascontiguousarray(np.broadcast_to(np.arange(256, dtype=np.int32)[None, None, :], (128, 8, 256)))
    c["c_iota16"] = np.ascontiguousarray(np.broadcast_to(np.arange(16, dtype=np.float32)[None, None, None, :], (128, 8, 16, 16)))
    return c


def pc_layout(v):
    return np.ascontiguousarray(np.asarray(v).reshape(8, 128).T)


def prep_shared(inp):
    m = {}
    def pk(w):
        K_, N_ = w.shape
        return np.ascontiguousarray(w.reshape(K_ // 128, 128, N_).transpose(1, 0, 2))
    m["w_in"] = pk(inp["w_in"][0])
    for k in ("w_a", "w_b", "w_out", "peer_wq", "ple_wg", "ple_wp"):
        m[k] = pk(inp[k][0])
    for k in ("peer_u", "peer_v"):
        m[k] = np.ascontiguousarray(inp[k][0])
    m["lru_wr"] = np.ascontiguousarray(inp["lru_wr"][0].transpose(1, 0, 2))
    m["lru_wi"] = np.ascontiguousarray(inp["lru_wi"][0].transpose(1, 0, 2))
    for k in ("cmp_w1_k", "cmp_w1_v"):
        m[k] = np.ascontiguousarray(inp[k][0].reshape(32, 128, 256).transpose(1, 0, 2))
    for k in ("cmp_w2_k", "cmp_w2_v"):
        m[k] = pk(inp[k][0])
    for k in ("norm_mix", "norm_ffn", "norm_ple"):
        m[k] = np.ascontiguousarray(inp[k][0].reshape(1, D))
    m["norm_final"] = np.ascontiguousarray(inp["norm_final"].reshape(1, D))
    m["conv_w"] = np.ascontiguousarray(inp["conv_w"][0].reshape(4, 8, 128).transpose(2, 1, 0))
    for k in ("conv_b", "lru_br", "lru_bi", "lru_lam"):
        m[k] = pc_layout(inp[k][0])
    for k in ("cmp_b1_k", "cmp_b1_v"):
        m[k] = np.ascontiguousarray(inp[k][0].reshape(2, 128).T)
    m["cmp_b2_k"] = np.ascontiguousarray(inp["cmp_b2_k"][0].reshape(128, 1))
    m["cmp_b2_v"] = np.ascontiguousarray(inp["cmp_b2_v"][0].reshape(1, 128))
    m["cmp_pe_k"] = np.ascontiguousarray(inp["cmp_pe_k"][0].T)
    m["cmp_pe_v"] = np.ascontiguousarray(inp["cmp_pe_v"][0].T)
    keys = inp["peer_keys"][0]
    kbd = np.zeros((128, 256), np.float32)
    kbd[0:64, 0:128] = keys[0].T
    kbd[64:128, 128:256] = keys[1].T
    m["peer_kbd"] = kbd
    return m


def win_segments():
    segs = [(0, 0, 2048), (2048, 2048, 1024)]
    d = 3072
    for h in range(8):
        segs.append((d, 2048 + h * 128 + 64, 64))
        segs.append((d + 64, 2048 + h * 128, 64))
        d += 128
    segs.append((4096, 3072, 1536))
    d = 5632
    for jkv in (2, 4):
        for g in range(2):
            s0 = 3072 + jkv * 256 + g * 128
            segs.append((d, s0 + 64, 64))
            segs.append((d + 64, s0, 64))
            d += 128
    segs.append((6144, 4632, 2048))
    segs.append((8192, 4608, 24))
    return segs


def build_nc(upto="D", debug=False):
    nc = bass.Bass("TRN2", target_bir_lowering=False)
    P = Prog(nc)
    ein = {}

    def IN(name, shape, dt=F32):
        ein[name] = nc.dram_tensor(name, list(shape), dt, kind="ExternalInput").ap()
        return ein[name]

    x_d = IN("x", [S, D])
    p_d = IN("p", [S, 256])
    pos_d = IN("positions", [1, S], I32)
    win_d = IN("w_in", [128, 8, 6680])
    w5_d = [IN(k, [128, 8, D]) for k in ("w_a", "w_b", "w_out", "peer_wq", "ple_wg")]
    wp_d = IN("ple_wp", [128, 2, D])
    u_d = IN("peer_u", [16384, D])
    v_d = IN("peer_v", [16384, D])
    wr_d = IN("lru_wr", [128, 8, 128])
    wi_d = IN("lru_wi", [128, 8, 128])
    w1_d = [IN("cmp_w1_k", [128, 32, 256]), IN("cmp_w1_v", [128, 32, 256])]
    w2_d = [IN("cmp_w2_k", [128, 2, 128]), IN("cmp_w2_v", [128, 2, 128])]
    gm_d = IN("norm_mix", [1, D])
    gf_d = IN("norm_ffn", [1, D])
    gp_d = IN("norm_ple", [1, D])
    gl_d = IN("norm_final", [1, D])
    cw_d = IN("conv_w", [128, 8, 4])
    cbias_d = IN("conv_b", [128, 8])
    br_d = IN("lru_br", [128, 8])
    bi_d = IN("lru_bi", [128, 8])
    lam_d = IN("lru_lam", [128, 8])
    b1_d = [IN("cmp_b1_k", [128, 2]), IN("cmp_b1_v", [128, 2])]
    b2k_d = IN("cmp_b2_k", [128, 1])
    b2v_d = IN("cmp_b2_v", [1, 128])
    pe_d = [IN("cmp_pe_k", [128, 32]), IN("cmp_pe_v", [128, 32])]
    kbd_d = IN("peer_kbd", [128, 256])
    identb_d = IN("c_identb", [128, 128], BF16)
    identf_d = IN("c_identf", [128, 128])
    invf_d = IN("c_invf", [128, 1])
    sgn_d = IN("c_sgn", [128, 1])
    cb_d = IN("c_cb", [128, 4, 512], BF16)
    wb_d = IN("c_wb", [128, 4, 512], BF16)
    cmb_d = IN("c_cmb", [128, 2, S], BF16)
    E_d = IN("c_E", [128, S], BF16)
    ovl_d = IN("c_ovl", [128, 2, 64], BF16)
    fa_d = IN("c_fa", [128, NTT, 64])
    na_d = IN("c_na", [128, NTT, 64])
    vm_d = IN("c_vm", [128, NTT, 64])
    io128_d = IN("c_iota128", [128, 16, 128], I32)
    io256_d = IN("c_iota256", [128, 8, 256], I32)
    io16_d = IN("c_iota16", [128, 8, 16, 16])

    out_d = nc.dram_tensor("out", [S, D], F32, kind="ExternalOutput").ap()

    dbg_n = [0]

    def DBG(tag, tt_, shape, dt=F32):
        if not debug:
            return
        dbg_n[0] += 1
        d = nc.dram_tensor(f"dbg_{tag}", list(shape), dt, kind="ExternalOutput").ap()
        P.dma("sp", d, tt_.t[:], reads=tt_.r)

    def SCR(name, shape, dt):
        kind = "ExternalOutput" if debug else "Internal"
        return nc.dram_tensor(name, list(shape), dt, kind=kind).ap()

    winbf = SCR("s_winbf", [128, 8, WCOLS], BF16)
    w5bf = [SCR(f"s_w5bf{i}", [128, 8, D], BF16) for i in range(5)]
    wpbf = SCR("s_wpbf", [128, 2, D], BF16)
    w1bf = [SCR(f"s_w1bf{i}", [128, 32, 256], BF16) for i in range(2)]
    zfm = SCR("s_zfm", [NCH, 128, S], F32)
    ma_s = SCR("s_ma", [8, 128, S], BF16)
    obt_s = SCR("s_obt", [8, 128, S], BF16)
    h1_s = SCR("s_h1", [S, D], F32)
    uv_s = nc.dram_tensor("s_uv", [16384, 2048], BF16, kind="Internal").ap()

    identb = P.sbuf("identb", [128, 128], BF16)
    identf = P.sbuf("identf", [128, 128], F32)
    P.dma("sp", identb.t[:, :], identb_d, writes=[identb.r0])
    P.dma("sp", identf.t[:, :], identf_d, writes=[identf.r0])

    cast_rr = [0]

    def cast(out, in_, reads, writes, engs=tuple(_os.environ.get("K_CAST", "dve,act").split(","))):
        e = engs[cast_rr[0] % len(engs)]
        cast_rr[0] += 1
        if e == "act":
            P.op("act", "copy", dict(out=out, in_=in_), reads, writes)
        else:
            P.op(e, "tensor_copy", dict(out=out, in_=in_), reads, writes)

    P.push()
    stg = P.sbuf("stg", [128, 2, 4096], F32, nres=2)
    stgb = P.sbuf("stgb", [128, 2, 4096], BF16, nres=2)
    P.op("dve", "memset", dict(ap=stg.t[:, 0, :], constant=0.0), [], [stg.r[0]])
    P.op("dve", "memset", dict(ap=stg.t[:, 1, :], constant=0.0), [], [stg.r[1]])
    pi = [0]

    def piece(loads, n, dst, dshape):
        b = pi[0] % 2
        pi[0] += 1
        for mk, src in loads:
            P.dma("sp", mk(stg.t[:, b, 0:n]), src, writes=[stg.r[b]])
        cast(stgb.t[:, b, 0:n], stg.t[:, b, 0:n], [stg.r[b]], [stgb.r[b]])
        P.dma("pool", dst, dshape(stgb.t[:, b, 0:n]), reads=[stgb.r[b]])

    win_v = win_d
    segs = win_segments()
    for c0 in range(0, WCOLS, 512):
        c1 = min(c0 + 512, WCOLS)
        w = c1 - c0
        loads = []
        for (dcol, scol, ncol) in segs:
            lo, hi = max(dcol, c0), min(dcol + ncol, c1)
            if lo >= hi:
                continue
            so = scol + (lo - dcol)
            loads.append((lambda a, lo=lo, hi=hi, w=w, c0=c0: a.rearrange("p (kc n) -> p kc n", kc=8)[:, :, lo - c0:hi - c0],
                          win_v[:, :, so:so + (hi - lo)]))
        if c1 == WCOLS and 8192 + 24 < WCOLS:
            pass
        piece(loads, 8 * w, winbf[:, :, c0:c1], lambda a, w=w: a.rearrange("p (kc n) -> p kc n", kc=8))
    for i in range(5):
        wv = w5_d[i]
        for hh in range(2):
            piece([(lambda a: a.rearrange("p (kc n) -> p kc n", kc=8), wv[:, :, hh * 512:(hh + 1) * 512])], 4096,
                  w5bf[i][:, :, hh * 512:(hh + 1) * 512], lambda a: a.rearrange("p (kc n) -> p kc n", kc=8))
    piece([(lambda a: a.rearrange("p (kc n) -> p kc n", kc=2), wp_d)], 2048,
          wpbf[:, :, :], lambda a: a.rearrange("p (kc n) -> p kc n", kc=2))
    for i in range(2):
        wv = w1_d[i]
        for hh in range(2):
            piece([(lambda a: a.rearrange("p (l h) -> p l h", l=16), wv[:, hh * 16:(hh + 1) * 16, :])], 4096,
                  w1bf[i][:, hh * 16:(hh + 1) * 16, :], lambda a: a.rearrange("p (l h) -> p l h", l=16))
    P.emit_stage()
    P.pop()
    if upto == "P":
        P.close()
        return nc

    P.push()
    nT = P.sbuf("nT", [128, 8, S], BF16, nres=NTT)
    gbc = P.sbuf("gbc", [128, D], F32)
    xt = P.sbuf("xt", [128, 2, D], F32, nres=2)
    nb = P.sbuf("nb", [128, 2, D], BF16, nres=2)
    junk = P.sbuf("junk", [128, D], F32)
    ss = P.sbuf("ss", [128, NTT], F32, nres=NTT)
    rs = P.sbuf("rs", [128, NTT], F32, nres=NTT)
    tp = P.psum("tp", [128, 2, 1024], BF16, nres=2)
    P.dma("sp", gbc.t[:, :], gm_d.partition_broadcast(128), writes=[gbc.r0])
    for tt in range(NTT):
        b = tt % 2
        P.dma("sp", xt.t[:, b, :], x_d[tt * 128:(tt + 1) * 128, :], writes=[xt.r[b]])
        P.op("act", "activation", dict(out=junk.t[:, :], in_=xt.t[:, b, :], func=AF.Square, accum_out=ss.t[:, tt:tt + 1]),
             reads=[xt.r[b]], writes=[junk.r0, ss.r[tt]])
        P.op("act", "activation", dict(out=rs.t[:, tt:tt + 1], in_=ss.t[:, tt:tt + 1], func=AF.Sqrt, scale=1.0 / D, bias=1e-6),
             reads=[ss.r[tt]], writes=[rs.r[tt]])
        P.op("dve", "reciprocal", dict(out=rs.t[:, tt:tt + 1], in_=rs.t[:, tt:tt + 1]), reads=[rs.r[tt]], writes=[rs.r[tt]])
        P.op("dve", "scalar_tensor_tensor", dict(out=nb.t[:, b, :], in0=xt.t[:, b, :], scalar=rs.t[:, tt:tt + 1], in1=gbc.t[:, :],
                                                 op0=ALU.mult, op1=ALU.mult), reads=[xt.r[b], rs.r[tt], gbc.r0], writes=[nb.r[b]])
        for c in range(8):
            P.op("pe", "transpose", dict(out=tp.t[:, b, c * 128:(c + 1) * 128], in_=nb.t[:, b, c * 128:(c + 1) * 128], identity=identb.t[:, :]),
                 reads=[nb.r[b], identb.r0], writes=[tp.r[b]])
        cast(nT.t[:, :, tt * 128:(tt + 1) * 128], tp.t[:, b, :].rearrange("p (c t) -> p c t", c=8), [tp.r[b]], [nT.r[tt]])

    def uv_build():
        ust = P.sbuf("ust", [128, 2, 4096], F32, nres=2)
        uvb = P.sbuf("uvb", [128, 2, 4, 2048], BF16, nres=2)
        u_v = u_d.rearrange("(r p j) d -> r p (j d)", p=128, j=4)
        v_v = v_d.rearrange("(r p j) d -> r p (j d)", p=128, j=4)
        uv_v = uv_s.rearrange("(r p j) c -> r p (j c)", p=128, j=4)
        for rb in range(32):
            ob_ = rb % 2
            for ti_, (src_v, eng_) in enumerate(((u_v, "act"), (v_v, "dve"))):
                P.dma("sp", ust.t[:, ti_, :], src_v[rb], writes=[ust.r[ti_]])
                dst_ = uvb.t[:, ob_, :, ti_ * 1024:(ti_ + 1) * 1024]
                src_ = ust.t[:, ti_, :].rearrange("p (j d) -> p j d", j=4)
                if eng_ == "act":
                    P.op("act", "copy", dict(out=dst_, in_=src_), [ust.r[ti_]], [uvb.r[ob_]])
                else:
                    P.op("dve", "tensor_copy", dict(out=dst_, in_=src_), [ust.r[ti_]], [uvb.r[ob_]])
            P.dma("pool", uv_v[rb], uvb.t[:, ob_].rearrange("p j c -> p (j c)"), reads=[uvb.r[ob_]])
            yield

    wbuf = P.sbuf("wbuf", [128, 2, 8, 512], BF16, nres=2)
    zst = P.sbuf("zst", [128, 2, S], F32, nres=2)
    uvgen = uv_build()
    pz = P.psum("pz", [128, 4, 512], F32, nres=4)
    nTall = list(nT.r)
    zi = 0
    pzi = 0
    for wpi, c0 in enumerate(range(0, WCOLS, 512)):
        c1 = min(c0 + 512, WCOLS)
        wb_ = wpi % 2
        P.dma("sp", wbuf.t[:, wb_, :, 0:c1 - c0], winbf[:, :, c0:c1], writes=[wbuf.r[wb_]])
        for cc in range((c1 - c0) // 128):
            ch = c0 // 128 + cc
            M = 24 if ch == C_NG else 128
            zb = zi % 2
            zi += 1
            for qc in range(NQC):
                pb = pzi % 4
                pzi += 1
                for kc in range(8):
                    P.op("pe", "matmul", dict(out=pz.t[0:M, pb, :], lhsT=wbuf.t[:, wb_, kc, cc * 128:cc * 128 + M],
                                              rhs=nT.t[:, kc, qc * 512:(qc + 1) * 512], start=(kc == 0), stop=(kc == 7),
                                              skip_group_check=True),
                         reads=[wbuf.r[wb_]] + nTall[qc * 4:(qc + 1) * 4], writes=[pz.r[pb]])
                dst = zst.t[0:M, zb, qc * 512:(qc + 1) * 512]
                src = pz.t[0:M, pb, :]
                if C_RG <= ch < C_RG + 8:
                    P.op("act", "activation", dict(out=dst, in_=src, func=AF.Gelu_apprx_tanh), [pz.r[pb]], [zst.r[zb]])
                elif ch >= C_M:
                    P.op("act", "activation", dict(out=dst, in_=src, func=AF.Sigmoid), [pz.r[pb]], [zst.r[zb]])
                else:
                    P.op("dve", "tensor_copy", dict(out=dst, in_=src), [pz.r[pb]], [zst.r[zb]])
            P.dma("pool", zfm[ch, 0:M, :], zst.t[0:M, zb, :], reads=[zst.r[zb]])
            if ch % 2 == 0:
                next(uvgen, None)
    for _ in uvgen:
        pass
    P.emit_stage()
    P.pop()
    if upto == "A":
        P.close()
        return nc

    P.push()
    cw = P.sbuf("cw", [128, 8, 4], F32)
    cbias = P.sbuf("cbias", [128, 8], F32)
    brs = P.sbuf("brs", [128, 8], F32)
    bis = P.sbuf("bis", [128, 8], F32)
    lam = P.sbuf("lam", [128, 8], F32)
    m8 = P.sbuf("m8", [128, 8], F32)
    m16 = P.sbuf("m16", [128, 8], F32)
    wst = P.sbuf("wst", [128, 2, 8, 128], F32, nres=2)
    wrb = P.sbuf("wrb", [128, 8, 128], BF16)
    wib = P.sbuf("wib", [128, 8, 128], BF16)
    for (t_, d_) in ((cw, cw_d), (cbias, cbias_d), (brs, br_d), (bis, bi_d), (lam, lam_d)):
        P.dma("sp", t_.t[:], d_, writes=[t_.r0])
    P.dma("sp", wst.t[:, 0], wr_d, writes=[wst.r[0]])
    P.dma("sp", wst.t[:, 1], wi_d, writes=[wst.r[1]])
    P.op("dve", "tensor_copy", dict(out=wrb.t[:, :, :], in_=wst.t[:, 0]), [wst.r[0]], [wrb.r0])
    P.op("dve", "tensor_copy", dict(out=wib.t[:, :, :], in_=wst.t[:, 1]), [wst.r[1]], [wib.r0])
    P.op("act", "activation", dict(out=m8.t[:, :], in_=lam.t[:, :], func=AF.Exp, scale=-1.0), [lam.r0], [m8.r0])
    P.op("act", "activation", dict(out=m8.t[:, :], in_=m8.t[:, :], func=AF.Ln, bias=1.0), [m8.r0], [m8.r0])
    P.op("dve", "tensor_scalar", dict(out=m16.t[:, :], in0=m8.t[:, :], scalar1=-16.0, scalar2=None, op0=ALU.mult), [m8.r0], [m16.r0])
    P.op("dve", "tensor_scalar", dict(out=m8.t[:, :], in0=m8.t[:, :], scalar1=-8.0, scalar2=None, op0=ALU.mult), [m8.r0, m16.r0], [m8.r0])
    zx = P.sbuf("zx", [128, S + 3], F32)
    gz = P.sbuf("gz", [128, S], F32)
    xa = P.sbuf("xa", [128, S], F32)
    xab = P.sbuf("xab", [128, S], BF16)
    rr = P.sbuf("rr", [128, S], F32)
    ig = P.sbuf("ig", [128, S], F32)
    t1 = P.sbuf("t1", [128, S], F32)
    t2 = P.sbuf("t2", [128, S], F32)
    mab = P.sbuf("mab", [128, S], BF16)
    pg = P.psum("pg", [128, 4, 512], F32, nres=4)
    P.op("dve", "memset", dict(ap=zx.t[:, 0:3], constant=0.0), [], [zx.r0])
    pgi = 0
    for ch in range(8):
        P.dma("sp", zx.t[:, 3:S + 3], zfm[C_RX + ch], writes=[zx.r0])
        P.dma("sp", gz.t[:, :], zfm[C_RG + ch], writes=[gz.r0])
        P.op("dve", "tensor_scalar", dict(out=xa.t[:, :], in0=zx.t[:, 0:S], scalar1=cw.t[:, ch, 0:1], scalar2=cbias.t[:, ch:ch + 1],
                                          op0=ALU.mult, op1=ALU.add), [zx.r0, cw.r0, cbias.r0], [xa.r0])
        for k in range(1, 4):
            P.op("dve", "scalar_tensor_tensor", dict(out=xa.t[:, :], in0=zx.t[:, k:k + S], scalar=cw.t[:, ch, k:k + 1], in1=xa.t[:, :],
                                                     op0=ALU.mult, op1=ALU.add), [zx.r0, cw.r0, xa.r0], [xa.r0])
        P.op("act", "copy", dict(out=xab.t[:, :], in_=xa.t[:, :]), [xa.r0], [xab.r0])
        for (wgt, bia, dst) in ((wrb, brs, rr), (wib, bis, ig)):
            for qc in range(NQC):
                pb = pgi % 4
                pgi += 1
                P.op("pe", "matmul", dict(out=pg.t[:, pb, :], lhsT=wgt.t[:, ch, :], rhs=xab.t[:, qc * 512:(qc + 1) * 512], start=True, stop=True,
                                          skip_group_check=True), [wgt.r0, xab.r0], [pg.r[pb]])
                P.op("act", "activation", dict(out=dst.t[:, qc * 512:(qc + 1) * 512], in_=pg.t[:, pb, :], func=AF.Sigmoid, bias=bia.t[:, ch:ch + 1]),
                     [pg.r[pb], bia.r0], [dst.r0])
        P.op("act", "activation", dict(out=t1.t[:, :], in_=rr.t[:, :], func=AF.Exp, scale=m8.t[:, ch:ch + 1]), [rr.r0, m8.r0], [t1.r0])
        P.op("act", "activation", dict(out=t2.t[:, :], in_=rr.t[:, :], func=AF.Exp, scale=m16.t[:, ch:ch + 1]), [rr.r0, m16.r0], [t2.r0])
        P.op("act", "activation", dict(out=t2.t[:, :], in_=t2.t[:, :], func=AF.Sqrt, scale=-1.0, bias=1.0), [t2.r0], [t2.r0])
        P.op("pool", "tensor_tensor", dict(out=ig.t[:, :], in0=ig.t[:, :], in1=xa.t[:, :], op=ALU.mult), [ig.r0, xa.r0], [ig.r0])
        P.op("pool", "tensor_tensor", dict(out=ig.t[:, :], in0=ig.t[:, :], in1=t2.t[:, :], op=ALU.mult), [ig.r0, t2.r0], [ig.r0])
        P.op("dve", "tensor_tensor_scan", dict(out=rr.t[:, :], data0=t1.t[:, :], data1=ig.t[:, :], initial=0.0, op0=ALU.mult, op1=ALU.add),
             [t1.r0, ig.r0, rr.r0], [rr.r0])
        P.op("dve", "tensor_tensor", dict(out=mab.t[:, :], in0=rr.t[:, :], in1=gz.t[:, :], op=ALU.mult), [rr.r0, gz.r0], [mab.r0])
        P.dma("pool", ma_s[ch], mab.t[:, :], reads=[mab.r0])
    P.emit_stage()
    P.pop()
    if upto == "B":
        P.close()
        return nc

    P.push()
    cosT = P.sbuf("cosT", [128, S], F32)
    sinT = P.sbuf("sinT", [128, S], F32)
    P.push()
    posi = P.sbuf("posi", [128, S], I32)
    tq_ = P.sbuf("tq", [128, S], F32)
    tk_ = P.sbuf("tk", [128, S], F32)
    ki_ = P.sbuf("ki", [128, S], I32)
    invf = P.sbuf("invf", [128, 1], F32)
    sgn = P.sbuf("sgn", [128, 1], F32)
    P.dma("sp", posi.t[:, :], pos_d.partition_broadcast(128), writes=[posi.r0])
    P.dma("sp", invf.t[:, :], invf_d, writes=[invf.r0])
    P.dma("sp", sgn.t[:, :], sgn_d, writes=[sgn.r0])
    P.op("dve", "tensor_copy", dict(out=tq_.t[:, :], in_=posi.t[:, :]), [posi.r0], [tq_.r0])
    P.op("dve", "tensor_scalar", dict(out=tq_.t[:, :], in0=tq_.t[:, :], scalar1=invf.t[:, 0:1], scalar2=float(1.0 / (2 * np.pi)),
                                      op0=ALU.mult, op1=ALU.mult), [tq_.r0, invf.r0], [tq_.r0])
    TWO_PI = float(2 * np.pi * (1 - 1e-6))
    for which, dstT in (("sin", sinT), ("cos", cosT)):
        if which == "cos":
            P.op("dve", "tensor_scalar", dict(out=tq_.t[:, :], in0=tq_.t[:, :], scalar1=0.25, scalar2=None, op0=ALU.add), [tq_.r0], [tq_.r0])
        P.op("dve", "tensor_copy", dict(out=ki_.t[:, :], in_=tq_.t[:, :]), [tq_.r0], [ki_.r0])
        P.op("dve", "tensor_copy", dict(out=tk_.t[:, :], in_=ki_.t[:, :]), [ki_.r0], [tk_.r0])
        P.op("dve", "tensor_tensor", dict(out=tk_.t[:, :], in0=tq_.t[:, :], in1=tk_.t[:, :], op=ALU.subtract), [tq_.r0, tk_.r0], [tk_.r0])
        P.op("act", "activation", dict(out=dstT.t[:, :], in_=tk_.t[:, :], func=AF.Sin, scale=TWO_PI), [tk_.r0], [dstT.r0])
    P.op("dve", "tensor_scalar", dict(out=sinT.t[:, :], in0=sinT.t[:, :], scalar1=sgn.t[:, 0:1], scalar2=None, op0=ALU.mult), [sinT.r0, sgn.r0], [sinT.r0])
    P.emit_stage()
    P.pop()

    ksT = P.sbuf("ksT", [128, S], BF16)
    kwT = P.sbuf("kwT", [128, S], BF16)
    vsa = P.sbuf("vsa", [128, NTT, 129], BF16)
    vwa = P.sbuf("vwa", [128, NTT, 129], BF16)
    kcT = P.sbuf("kcT", [128, 256], BF16)
    vca = P.sbuf("vca", [128, 2, 193], BF16)
    E_s = P.sbuf("E_s", [128, S], BF16)
    cb_s = P.sbuf("cb_s", [128, 4, 512], BF16)
    wb_s = P.sbuf("wb_s", [128, 4, 512], BF16)
    b1s = [P.sbuf(f"b1s{i}", [128, 2], F32) for i in range(2)]
    pes = [P.sbuf(f"pes{i}", [128, 32], F32) for i in range(2)]
    b2k = P.sbuf("b2k", [128, 1], F32)
    b2vb = P.sbuf("b2vb", [128, 128], F32)
    ps_s = P.psum("ps_s", [128, 2, 512], F32, nres=2)
    ps_acc = P.psum("ps_acc", [128, 4, 512], F32, nres=4)
    ps_trb = P.psum("ps_trb", [128, 1024], BF16)
    ps_trf = P.psum("ps_trf", [128, 512], F32)
    P.dma("sp", E_s.t[:, :], E_d, writes=[E_s.r0])
    P.dma("sp", cb_s.t[:], cb_d, writes=[cb_s.r0])
    P.dma("sp", wb_s.t[:], wb_d, writes=[wb_s.r0])
    for i in range(2):
        P.dma("sp", b1s[i].t[:, :], b1_d[i], writes=[b1s[i].r0])
        P.dma("sp", pes[i].t[:, :], pe_d[i], writes=[pes[i].r0])
    P.dma("sp", b2k.t[:, :], b2k_d, writes=[b2k.r0])
    P.dma("sp", b2vb.t[:, :], b2v_d.partition_broadcast(128), writes=[b2vb.r0])
    P.op("dve", "memset", dict(ap=vsa.t[:, :, 128:129], constant=1.0), [], [vsa.r0])
    P.op("dve", "memset", dict(ap=vwa.t[:, :, 128:129], constant=1.0), [], [vwa.r0])
    srot = [0]

    def sbank():
        srot[0] += 1
        return srot[0] % 2

    def mm(out, lhsT, rhs, start, stop, reads, writes):
        P.op("pe", "matmul", dict(out=out, lhsT=lhsT, rhs=rhs, start=start, stop=stop, skip_group_check=True), reads, writes)

    for g in range(2):
        P.push()
        ld = P.sbuf("ld", [128, 2, S], F32, nres=2)
        lohi = P.sbuf("lohi", [128, 2, S], BF16, nres=2)
        w1s = P.sbuf("w1s", [128, 32, 256], BF16)
        w2f = P.sbuf("w2f", [128, 2, 128], F32)
        w2s = P.sbuf("w2s", [128, 2, 128], BF16)
        hidT = P.sbuf("hidT", [128, 2, 256], BF16)
        vb = P.sbuf("vb", [128, S], BF16)
        P.op("dve", "memset", dict(ap=kcT.t[:, :], constant=0.0), [], [kcT.r0])
        P.op("dve", "memset", dict(ap=vca.t[:], constant=0.0), [], [vca.r0])
        P.op("dve", "memset", dict(ap=vca.t[:, :, 128:129], constant=1.0), [vca.r0], [vca.r0])
        P.dma("sp", vca.t[:, :, 129:193], ovl_d, writes=[vca.r0])
        for kvi in range(2):
            ch = C_KV + kvi * 2 + g
            P.dma("sp", ld.t[:, 0, :], zfm[ch], writes=[ld.r[0]])
            P.dma("sp", w1s.t[:], w1bf[kvi], writes=[w1s.r0])
            P.dma("sp", w2f.t[:], w2_d[kvi], writes=[w2f.r0])
            P.op("dve", "tensor_copy", dict(out=w2s.t[:], in_=w2f.t[:]), [w2f.r0], [w2s.r0])
            for v_ in range(2):
                P.op("dve", "tensor_tensor", dict(out=lohi.t[:, v_, :].rearrange("p (n l) -> p n l", l=16),
                                                  in0=ld.t[:, 0, :].rearrange("p (n l) -> p n l", l=16),
                                                  in1=pes[kvi].t[:, v_ * 16:(v_ + 1) * 16].unsqueeze(1).broadcast_to([128, 256, 16]),
                                                  op=ALU.add), [ld.r[0], pes[kvi].r0], [lohi.r[v_]])
            for hc in range(2):
                for l in range(32):
                    v_ = 0 if l < 16 else 1
                    mm(ps_s.t[:, hc, 0:255], w1s.t[:, l, hc * 128:(hc + 1) * 128], lohi.t[:, v_, l:l + 16 * 254 + 1:16],
                       l == 0, l == 31, [w1s.r0, lohi.r[v_]], [ps_s.r[hc]])
                P.op("act", "activation", dict(out=hidT.t[:, hc, 0:255], in_=ps_s.t[:, hc, 0:255], func=AF.Gelu_apprx_tanh,
                                               bias=b1s[kvi].t[:, hc:hc + 1]), [ps_s.r[hc], b1s[kvi].r0], [hidT.r0])
            if kvi == 0:
                for hc in range(2):
                    mm(ps_s.t[:, 0, 0:255], w2s.t[:, hc, :], hidT.t[:, hc, 0:255], hc == 0, hc == 1, [w2s.r0, hidT.r0], [ps_s.r[0]])
                P.op("dve", "tensor_scalar", dict(out=kcT.t[:, 0:255], in0=ps_s.t[:, 0, 0:255], scalar1=b2k.t[:, 0:1], scalar2=None, op0=ALU.add),
                     [ps_s.r[0], b2k.r0], [kcT.r0])
            else:
                for nch in range(2):
                    rows = 128 if nch == 0 else 127
                    for hc in range(2):
                        mm(ps_acc.t[0:rows, nch, 0:128], hidT.t[:, hc, nch * 128:nch * 128 + rows], w2s.t[:, hc, :], hc == 0, hc == 1,
                           [w2s.r0, hidT.r0], [ps_acc.r[nch]])
                    P.op("dve", "tensor_tensor", dict(out=vca.t[0:rows, nch, 0:128], in0=ps_acc.t[0:rows, nch, 0:128], in1=b2vb.t[0:rows, :], op=ALU.add),
                         [ps_acc.r[nch], b2vb.r0], [vca.r0])
        for (kch, swch, dstK) in ((C_KV + 4 + g, C_KSW + g, ksT), (C_KV + 8 + g, C_KSW + 2 + g, kwT)):
            P.dma("sp", ld.t[:, 0, :], zfm[kch], writes=[ld.r[0]])
            P.dma("sp", ld.t[:, 1, :], zfm[swch], writes=[ld.r[1]])
            if debug and g == 1 and dstK is ksT:
                DBG("g1_ld_raw", ld, [128, 2, S])
                DBG("g1_cosT", cosT, [128, S])
                DBG("g1_sinT", sinT, [128, S])
            P.op("dve", "tensor_tensor", dict(out=ld.t[:, 0, :], in0=ld.t[:, 0, :], in1=cosT.t[:, :], op=ALU.mult), [ld.r[0], cosT.r0], [ld.r[0]])
            P.op("pool", "tensor_tensor", dict(out=ld.t[:, 1, :], in0=ld.t[:, 1, :], in1=sinT.t[:, :], op=ALU.mult), [ld.r[1], sinT.r0], [ld.r[1]])
            if debug and g == 1 and dstK is ksT:
                DBG("g1_ld_mul", ld, [128, 2, S])
            P.op("dve", "tensor_tensor", dict(out=dstK.t[:, :], in0=ld.t[:, 0, :], in1=ld.t[:, 1, :], op=ALU.add), [ld.r[0], ld.r[1]], [dstK.r0])
        for (vch, dstV) in ((C_KV + 6 + g, vsa), (C_KV + 10 + g, vwa)):
            P.dma("sp", ld.t[:, 0, :], zfm[vch], writes=[ld.r[0]])
            P.op("act", "copy", dict(out=vb.t[:, :], in_=ld.t[:, 0, :]), [ld.r[0]], [vb.r0])
            for t4 in range(8):
                for a in range(4):
                    tt = t4 * 4 + a
                    P.op("pe", "transpose", dict(out=ps_trb.t[:, a * 128:(a + 1) * 128], in_=vb.t[:, tt * 128:(tt + 1) * 128], identity=identb.t[:, :]),
                         [vb.r0, identb.r0], [ps_trb.r0])
                cast(dstV.t[:, t4 * 4:(t4 + 1) * 4, 0:128], ps_trb.t[:, 0:512].rearrange("p (a d) -> p a d", a=4), [ps_trb.r0], [dstV.r0])
        P.emit_stage()
        P.pop()
        if upto == "C0":
            P.close()
            return nc

        P.push()
        zq = P.sbuf("zq", [128, 4, 512], F32, nres=4)
        zqs = P.sbuf("zqs", [128, 4, 512], F32, nres=4)
        qn = P.sbuf("qn", [128, 4, 512], BF16)
        qr = P.sbuf("qr", [128, 4, 512], BF16)
        cmbq = P.sbuf("cmbq", [128, 2, 512], BF16)
        fa = P.sbuf("fa", [128, 4, 64], F32)
        na = P.sbuf("na", [128, 4, 64], F32)
        vm = P.sbuf("vm", [128, 4, 64], F32)
        gTs = P.sbuf("gTs", [24, 512], F32)
        gts = P.sbuf("gts", [128, 4, 24], F32)
        pc = P.sbuf("pc", [128, 2, 2, 512], BF16, nres=2)
        pt = P.sbuf("pt", [128, 3, 512], BF16, nres=3)
        oacc = P.sbuf("oacc", [128, 4, 4, 128], F32)
        imp = P.sbuf("imp", [128, 4, 64], F32)
        impp = P.sbuf("impp", [128, 64], F32)
        imp2 = P.sbuf("imp2", [128, 64], F32)
        m8a = P.sbuf("m8a", [128, 8], F32)
        m8b = P.sbuf("m8b", [128, 8], F32)
        msel = P.sbuf("msel", [128, 64], F32)
        bmb = P.sbuf("bmb", [128, 64], BF16)
        biasT = P.sbuf("biasT", [128, 512], BF16)
        P.op("dve", "memset", dict(ap=biasT.t[:, :], constant=0.0), [], [biasT.r0])
        sm = P.sbuf("sm", [128, 8], F32)
        obb = P.sbuf("obb", [128, 4, 4, 128], BF16)
        obT = P.sbuf("obT", [128, 4, 512], BF16)
        ptr = [0]
        aset = [0]
        for qc in range(NQC):
            q0 = qc * 512
            for hq in range(4):
                P.dma("sp", zq.t[:, hq, :], zfm[C_Q + 4 * g + hq, :, q0:q0 + 512], writes=[zq.r[hq]])
                P.dma("sp", zqs.t[:, hq, :], zfm[C_QS + 4 * g + hq, :, q0:q0 + 512], writes=[zqs.r[hq]])
            P.dma("sp", cmbq.t[:], cmb_d[:, :, q0:q0 + 512], writes=[cmbq.r0])
            P.dma("sp", fa.t[:], fa_d[:, qc * 4:qc * 4 + 4, :], writes=[fa.r0])
            P.dma("sp", na.t[:], na_d[:, qc * 4:qc * 4 + 4, :], writes=[na.r0])
            P.dma("sp", vm.t[:], vm_d[:, qc * 4:qc * 4 + 4, :], writes=[vm.r0])
            P.dma("sp", gTs.t[:, :], zfm[C_NG, 0:24, q0:q0 + 512], writes=[gTs.r0])
            P.op("act", "copy", dict(out=qn.t[:], in_=zq.t[:]), zq.r, [qn.r0])
            cs = cosT.t[:, q0:q0 + 512].unsqueeze(1).broadcast_to([128, 4, 512])
            sn = sinT.t[:, q0:q0 + 512].unsqueeze(1).broadcast_to([128, 4, 512])
            P.op("dve", "tensor_tensor", dict(out=zq.t[:], in0=zq.t[:], in1=cs, op=ALU.mult), zq.r + [cosT.r0], zq.r)
            P.op("pool", "tensor_tensor", dict(out=zqs.t[:], in0=zqs.t[:], in1=sn, op=ALU.mult), zqs.r + [sinT.r0], zqs.r)
            P.op("dve", "tensor_tensor", dict(out=qr.t[:], in0=zq.t[:], in1=zqs.t[:], op=ALU.add), zq.r + zqs.r, [qr.r0])
            for qt in range(4):
                P.op("pe", "transpose", dict(out=ps_trf.t[:, qt * 24:(qt + 1) * 24], in_=gTs.t[0:24, qt * 128:(qt + 1) * 128], identity=identf.t[0:24, 0:24]),
                     [gTs.r0, identf.r0], [ps_trf.r0])
            P.op("dve", "tensor_copy", dict(out=gts.t[:].rearrange("p a c -> p (a c)"), in_=ps_trf.t[:, 0:96]), [ps_trf.r0], [gts.r0])

            def post(hh, qt, bank, off, br, first, aux=None):
                h = 4 * g + hh
                accv = ps_acc.t[:, bank, :]
                P.op("dve", "tensor_scalar", dict(out=sm.t[:, 0:1], in0=accv[:, off + 128:off + 129], scalar1=1e-30, scalar2=None, op0=ALU.max),
                     [ps_acc.r[bank]], [sm.r0])
                P.op("dve", "reciprocal", dict(out=sm.t[:, 1:2], in_=sm.t[:, 0:1]), [sm.r0], [sm.r0])
                P.op("dve", "tensor_tensor", dict(out=sm.t[:, 2:3], in0=sm.t[:, 1:2], in1=gts.t[:, qt, h * 3 + br:h * 3 + br + 1], op=ALU.mult),
                     [sm.r0, gts.r0], [sm.r0])
                if first:
                    P.op("dve", "tensor_scalar", dict(out=oacc.t[:, qt, hh, :], in0=accv[:, off:off + 128], scalar1=sm.t[:, 2:3], scalar2=None, op0=ALU.mult),
                         [ps_acc.r[bank], sm.r0], [oacc.r0])
                else:
                    P.op("dve", "scalar_tensor_tensor", dict(out=oacc.t[:, qt, hh, :], in0=accv[:, off:off + 128], scalar=sm.t[:, 2:3], in1=oacc.t[:, qt, hh, :],
                                                             op0=ALU.mult, op1=ALU.add), [ps_acc.r[bank], sm.r0, oacc.r0], [oacc.r0])
                if aux is not None:
                    if hh == 0:
                        P.op("dve", "tensor_scalar", dict(out=imp.t[:, qt, :], in0=accv[:, off + 129:off + 193], scalar1=sm.t[:, 1:2], scalar2=None, op0=ALU.mult),
                             [ps_acc.r[bank], sm.r0], [imp.r0])
                    else:
                        P.op("dve", "scalar_tensor_tensor", dict(out=imp.t[:, qt, :], in0=accv[:, off + 129:off + 193], scalar=sm.t[:, 1:2], in1=imp.t[:, qt, :],
                                                                 op0=ALU.mult, op1=ALU.add), [ps_acc.r[bank], sm.r0, imp.r0], [imp.r0])

            if (g, qc) in ((0, 0), (1, 1)):
                tg = f"g{g}q{qc}"
                DBG(tg + "_gts", gts, [128, 4, 24]); DBG(tg + "_qn", qn, [128, 4, 512], BF16); DBG(tg + "_qr", qr, [128, 4, 512], BF16)
                DBG(tg + "_kcT", kcT, [128, 256], BF16); DBG(tg + "_vca", vca, [128, 2, 193], BF16)
                DBG(tg + "_ksT", ksT, [128, S], BF16); DBG(tg + "_kwT", kwT, [128, S], BF16)
                DBG(tg + "_vsa", vsa, [128, NTT, 129], BF16); DBG(tg + "_vwa", vwa, [128, NTT, 129], BF16)
            n_nch = 2 if (q0 + 511 >= 16 * 128 + 31) else 1
            for hh in range(4):
                pcb = hh % 2
                for nch in range(n_nch):
                    sb = sbank()
                    mm(ps_s.t[:, sb, :], kcT.t[:, nch * 128:(nch + 1) * 128], qn.t[:, hh, :], True, False, [kcT.r0, qn.r0], [ps_s.r[sb]])
                    mm(ps_s.t[:, sb, :], identb.t[:, :], cmbq.t[:, nch, :], False, True, [identb.r0, cmbq.r0], [ps_s.r[sb]])
                    P.op("act", "activation", dict(out=pc.t[:, pcb, nch, :], in_=ps_s.t[:, sb, :], func=AF.Exp, scale=SCALE), [ps_s.r[sb]], [pc.r[pcb]])
                aset[0] ^= 1
                started = {}
                for qt in range(4):
                    bank = aset[0] * 2 + qt // 2
                    off = (qt % 2) * 193
                    for nch in range(n_nch):
                        mm(ps_acc.t[:, bank, off:off + 193], pc.t[:, pcb, nch, qt * 128:(qt + 1) * 128], vca.t[:, nch, :],
                           bank not in started, False, [pc.r[pcb], vca.r0], [ps_acc.r[bank]])
                        started[bank] = 1
                for qt in range(4):
                    post(hh, qt, aset[0] * 2 + qt // 2, (qt % 2) * 193, 0, True, aux=True)
            if (g, qc) in ((0, 0), (1, 1)):
                DBG(tg + "_oacc_c", oacc, [128, 4, 4, 128]); DBG(tg + "_imp", imp, [128, 4, 64])
            for qt in range(4):
                P.op("dve", "tensor_tensor", dict(out=impp.t[:, :], in0=imp.t[:, qt, :], in1=fa.t[:, qt, :], op=ALU.max), [imp.r0, fa.r0], [impp.r0])
                P.op("dve", "tensor_tensor", dict(out=impp.t[:, :], in0=impp.t[:, :], in1=na.t[:, qt, :], op=ALU.add), [impp.r0, na.r0], [impp.r0])
                P.op("dve", "max", dict(out=m8a.t[:, :], in_=impp.t[:, :]), [impp.r0], [m8a.r0])
                P.op("dve", "match_replace", dict(out=imp2.t[:, :], in_to_replace=m8a.t[:, :], in_values=impp.t[:, :], imm_value=-3.0e38),
                     [impp.r0, m8a.r0], [imp2.r0])
                P.op("dve", "max", dict(out=m8b.t[:, :], in_=imp2.t[:, :]), [imp2.r0], [m8b.r0])
                P.op("dve", "scalar_tensor_tensor", dict(out=msel.t[:, :], in0=impp.t[:, :], scalar=m8b.t[:, 7:8], in1=vm.t[:, qt, :],
                                                         op0=ALU.is_ge, op1=ALU.mult), [impp.r0, m8b.r0, vm.r0], [msel.r0])
                P.op("dve", "tensor_scalar", dict(out=bmb.t[:, :], in0=msel.t[:, :], scalar1=-NEG, scalar2=NEG, op0=ALU.mult, op1=ALU.add),
                     [msel.r0], [bmb.r0])
                P.op("pe", "transpose", dict(out=ps_trb.t[0:64, qt * 128:(qt + 1) * 128], in_=bmb.t[:, :], identity=identb.t[:, :]),
                     [bmb.r0, identb.r0], [ps_trb.r0])
            P.op("dve", "tensor_copy", dict(out=biasT.t[0:64, :], in_=ps_trb.t[0:64, 0:512]), [ps_trb.r0], [biasT.r0])
            if (g, qc) in ((0, 0), (1, 1)):
                DBG(tg + "_biasT", biasT, [128, 512], BF16)
            for br in (2, 1):
                for hh in range(4):
                    aset[0] ^= 1
                    started = {}
                    if br == 1:
                        kts = list(range(0, qc * 4 + 4))
                    else:
                        kts = list(range(max(0, qc * 4 - 4), qc * 4 + 4))
                    vsrc = vsa if br == 1 else vwa

                    def emit_scores(kt):
                        r = kt - qc * 4
                        sb = sbank()
                        if br == 1:
                            mm(ps_s.t[:, sb, :], ksT.t[:, kt * 128:(kt + 1) * 128], qr.t[:, hh, :], True, False, [ksT.r0, qr.r0], [ps_s.r[sb]])
                            mm(ps_s.t[:, sb, :], E_s.t[:, kt * 128:(kt + 1) * 128], biasT.t[:, :], False, r < 0, [E_s.r0, biasT.r0], [ps_s.r[sb]])
                            if r >= 0:
                                mm(ps_s.t[:, sb, :], identb.t[:, :], cb_s.t[:, r, :], False, True, [identb.r0, cb_s.r0], [ps_s.r[sb]])
                        else:
                            mm(ps_s.t[:, sb, :], kwT.t[:, kt * 128:(kt + 1) * 128], qr.t[:, hh, :], True, False, [kwT.r0, qr.r0], [ps_s.r[sb]])
                            btab = wb_s.t[:, r + 4, :] if r < 0 else cb_s.t[:, r, :]
                            mm(ps_s.t[:, sb, :], identb.t[:, :], btab, False, True, [identb.r0, cb_s.r0, wb_s.r0], [ps_s.r[sb]])
                        pb = ptr[0] % 3
                        ptr[0] += 1
                        P.op("act", "activation", dict(out=pt.t[:, pb, :], in_=ps_s.t[:, sb, :], func=AF.Exp, scale=SCALE), [ps_s.r[sb]], [pt.r[pb]])
                        return (kt, pb)

                    def emit_pv(kt, pb):
                        r = kt - qc * 4
                        for qt in range(4):
                            if r > qt:
                                continue
                            if br == 2 and r < qt - 4:
                                continue
                            bank = aset[0] * 2 + qt // 2
                            off = (qt % 2) * 129
                            mm(ps_acc.t[:, bank, off:off + 129], pt.t[:, pb, qt * 128:(qt + 1) * 128], vsrc.t[:, kt, :],
                               bank not in started, False, [pt.r[pb], vsrc.r0], [ps_acc.r[bank]])
                            started[bank] = 1

                    pend = None
                    for kt in kts:
                        cur = emit_scores(kt)
                        if pend is not None:
                            emit_pv(*pend)
                        pend = cur
                    emit_pv(*pend)
                    for qt in range(4):
                        post(hh, qt, aset[0] * 2 + qt // 2, (qt % 2) * 129, br, False)
            if (g, qc) in ((0, 0), (1, 1)):
                DBG(tg + "_oacc_w", oacc, [128, 4, 4, 128])
            P.op("act", "copy", dict(out=obb.t[:], in_=oacc.t[:]), [oacc.r0], [obb.r0])
            for hh in range(4):
                for qt in range(4):
                    P.op("pe", "transpose", dict(out=ps_trb.t[:, qt * 128:(qt + 1) * 128], in_=obb.t[:, qt, hh, :], identity=identb.t[:, :]),
                         [obb.r0, identb.r0], [ps_trb.r0])
                cast(obT.t[:, hh, :], ps_trb.t[:, 0:512], [ps_trb.r0], [obT.r0])
            for hq in range(4):
                P.dma("pool", obt_s[4 * g + hq, :, q0:q0 + 512], obT.t[:, hq, :], reads=[obT.r0])
        P.emit_stage()
        P.pop()
    P.pop()
    if upto == "C":
        P.close()
        return nc

    def rmsnorm_tile(src, gain, dst_f32, dst_bf, sq, junk_):
        P.op("act", "activation", dict(out=junk_.t[:, :], in_=src.t[:, :], func=AF.Square, accum_out=sq.t[:, 0:1]), [src.r0], [junk_.r0, sq.r0])
        P.op("act", "activation", dict(out=sq.t[:, 1:2], in_=sq.t[:, 0:1], func=AF.Sqrt, scale=1.0 / D, bias=1e-6), [sq.r0], [sq.r0])
        P.op("dve", "reciprocal", dict(out=sq.t[:, 2:3], in_=sq.t[:, 1:2]), [sq.r0], [sq.r0])
        P.op("dve", "scalar_tensor_tensor", dict(out=dst_f32.t[:, :], in0=src.t[:, :], scalar=sq.t[:, 2:3], in1=gain.t[:, :], op0=ALU.mult, op1=ALU.mult),
             [src.r0, sq.r0, gain.r0], [dst_f32.r0])
        if dst_bf is not None:
            P.op("act", "copy", dict(out=dst_bf.t[:, :], in_=dst_f32.t[:, :]), [dst_f32.r0], [dst_bf.r0])

    P.push()
    w3 = [P.sbuf(f"w3s{i}", [128, 8, D], BF16) for i in range(3)]
    for i in range(3):
        P.dma("sp", w3[i].t[:], w5bf[i], writes=[w3[i].r0])
    wA, wB, wO = w3
    obTq = P.sbuf("obTq", [128, 8, 512], BF16, nres=8)
    maTq = P.sbuf("maTq", [128, 8, 512], BF16, nres=8)
    mgT = P.sbuf("mgT", [128, 8, 512], BF16)
    g01 = P.sbuf("g01", [128, 2, 16, 512], F32, nres=2)
    tA = P.sbuf("tA", [128, 2, 512], F32, nres=2)
    tB = P.sbuf("tB", [128, 2, 512], F32, nres=2)
    xtl = P.sbuf("xtl", [128, 2, D], F32, nres=2)
    h1t = P.sbuf("h1t", [128, 2, D], F32, nres=2)
    ps_y = P.psum("ps_y", [128, 4, 512], F32, nres=4)
    ps_h = P.psum("ps_h", [128, 2, 512], F32, nres=2)
    ti = 0
    for qc in range(NQC):
        q0 = qc * 512
        gb = qc % 2
        P.dma("sp", obTq.t[:], obt_s[:, :, q0:q0 + 512].rearrange("c p t -> p c t"), writes=list(obTq.r))
        P.dma("sp", maTq.t[:], ma_s[:, :, q0:q0 + 512].rearrange("c p t -> p c t"), writes=list(maTq.r))
        P.dma("pool", g01.t[:, gb], zfm[C_M:C_M + 16, :, q0:q0 + 512].rearrange("c p t -> p c t"), writes=[g01.r[gb]])
        for c in range(8):
            b = c % 2
            for k in range(8):
                mm(ps_y.t[:, 2 * b, :], wA.t[:, k, c * 128:(c + 1) * 128], maTq.t[:, k, :], k == 0, k == 7, [wA.r0, maTq.r[k]], [ps_y.r[2 * b]])
            for k in range(8):
                mm(ps_y.t[:, 2 * b + 1, :], wB.t[:, k, c * 128:(c + 1) * 128], obTq.t[:, k, :], k == 0, k == 7, [wB.r0, obTq.r[k]], [ps_y.r[2 * b + 1]])
            P.op("dve", "tensor_tensor", dict(out=tA.t[:, b, :], in0=ps_y.t[:, 2 * b, :], in1=g01.t[:, gb, c, :], op=ALU.mult), [ps_y.r[2 * b], g01.r[gb]], [tA.r[b]])
            P.op("dve", "tensor_tensor", dict(out=tB.t[:, b, :], in0=ps_y.t[:, 2 * b + 1, :], in1=g01.t[:, gb, 8 + c, :], op=ALU.mult), [ps_y.r[2 * b + 1], g01.r[gb]], [tB.r[b]])
            P.op("pool", "tensor_tensor", dict(out=mgT.t[:, c, :], in0=tA.t[:, b, :], in1=tB.t[:, b, :], op=ALU.add), [tA.r[b], tB.r[b]], [mgT.r0])
        for qt in range(4):
            tt = qc * 4 + qt
            b = ti % 2
            ti += 1
            P.dma("sp", xtl.t[:, b, :], x_d[tt * 128:(tt + 1) * 128, :], writes=[xtl.r[b]])
            for half in range(2):
                for c in range(8):
                    mm(ps_h.t[:, half, :], mgT.t[:, c, qt * 128:(qt + 1) * 128], wO.t[:, c, half * 512:(half + 1) * 512], c == 0, c == 7, [mgT.r0, wO.r0], [ps_h.r[half]])
                P.op("dve", "tensor_tensor", dict(out=h1t.t[:, b, half * 512:(half + 1) * 512], in0=ps_h.t[:, half, :], in1=xtl.t[:, b, half * 512:(half + 1) * 512], op=ALU.add),
                     [ps_h.r[half], xtl.r[b]], [h1t.r[b]])
            P.dma("pool", h1_s[tt * 128:(tt + 1) * 128, :], h1t.t[:, b, :], reads=[h1t.r[b]])
    P.emit_stage()
    P.pop()
    if upto == "D1":
        P.close()
        return nc

    P.push()
    wQ = P.sbuf("wQ", [128, 8, D], BF16)
    wG = P.sbuf("wG", [128, 8, D], BF16)
    wPs = P.sbuf("wPs", [128, 2, D], BF16)
    P.dma("sp", wQ.t[:], w5bf[3], writes=[wQ.r0])
    P.dma("sp", wG.t[:], w5bf[4], writes=[wG.r0])
    P.dma("sp", wPs.t[:], wpbf, writes=[wPs.r0])
    kbdf = P.sbuf("kbdf", [128, 256], F32)
    kbdb = P.sbuf("kbdb", [128, 256], BF16)
    P.dma("sp", kbdf.t[:, :], kbd_d, writes=[kbdf.r0])
    P.op("dve", "tensor_copy", dict(out=kbdb.t[:, :], in_=kbdf.t[:, :]), [kbdf.r0], [kbdb.r0])
    gains = []
    for nm, gd in (("gfb", gf_d), ("gpb", gp_d), ("glb", gl_d)):
        t_ = P.sbuf(nm, [128, D], F32)
        P.dma("sp", t_.t[:, :], gd.partition_broadcast(128), writes=[t_.r0])
        gains.append(t_)
    gfb, gpb, glb = gains
    io128 = P.sbuf("io128", [128, 128], I32)
    io256 = P.sbuf("io256", [128, 256], I32)
    io16 = P.sbuf("io16", [128, 16], F32)
    P.dma("sp", io128.t[:, :], io128_d[:, 0, :], writes=[io128.r0])
    P.dma("sp", io256.t[:, :], io256_d[:, 0, :], writes=[io256.r0])
    P.dma("sp", io16.t[:, :], io16_d[:, 0, 0, :], writes=[io16.r0])
    NG = int(_os.environ.get("K_NG", "15"))

    def dbl(name, shape, dt):
        return [P.sbuf(f"{name}{i}", shape, dt) for i in range(2)]

    hh2 = [P.sbuf(f"hh{i}", [128, D], F32) for i in range(3)]
    ptl2 = [P.sbuf(f"ptl{i}", [128, 256], F32) for i in range(3)]
    xn2 = dbl("xn", [128, D], F32)
    eidx2 = dbl("eidx", [128, 128], I32)
    gte2 = dbl("gte", [128, 128], F32)
    ptb = P.sbuf("ptb", [128, 256], BF16)
    pT = P.sbuf("pT", [128, 2, 128], BF16)
    xnb2 = dbl("xnb", [128, D], BF16)
    prodb = P.sbuf("prodb", [128, 4, D], BF16, nres=4)
    junkA = P.sbuf("junkA", [128, D], BF16)
    xnT = P.sbuf("xnT", [128, 8, 128], BF16)
    xne = P.sbuf("xne", [128, D], F32)
    xnbe = P.sbuf("xnbe", [128, D], BF16)
    xnTe = P.sbuf("xnTe", [128, 8, 128], BF16)
    qpT = P.sbuf("qpT", [128, 8, 128], BF16)
    junkF = P.sbuf("junkF", [128, D], BF16)
    junkE = P.sbuf("junkE", [128, D], BF16)
    sqF = P.sbuf("sqF", [128, 4], F32)
    sqE = P.sbuf("sqE", [128, 4], F32)
    s_all = P.sbuf("s_all", [128, 16, 128], F32)
    stmp = P.sbuf("stmp", [128, 256], F32)
    v12 = P.sbuf("v12", [128, 16, 16], F32)
    i12i = P.sbuf("i12i", [128, 16, 16], I32)
    i12f = P.sbuf("i12f", [128, 16, 16], F32)
    cand = P.sbuf("cand", [128, 8, 256], F32)
    oh = cand
    best = P.sbuf("best", [128, 8, 16], F32)
    posi2 = P.sbuf("posi2", [128, 8, 16], I32)
    abi = P.sbuf("abi", [128, 2, 8, 16], I32)
    abf = P.sbuf("abf", [128, 2, 8, 16], F32)
    isel = P.sbuf("isel", [128, 2, 8, 16], F32)
    ef = P.sbuf("ef", [128, 128], F32)
    bex = P.sbuf("bex", [128, 8, 16], F32)
    bsm = P.sbuf("bsm", [128, 2, 8], F32)
    dots = P.sbuf("dots", [128, 128], F32, nres=128)
    a1 = P.sbuf("a1", [128, 128], F32, nres=32)
    ug = P.sbuf("ug", [128, NG, 2 * D], BF16, nres=NG)
    gate = P.sbuf("gate", [128, D], F32)
    tmpg = P.sbuf("tmpg", [128, 512], F32)
    ot = P.sbuf("ot", [128, D], F32)
    ps_t = P.psum("ps_t2", [128, 1024], BF16)
    ps_q = P.psum("ps_q", [128, 2, 512], F32, nres=2)
    ps_c = P.psum("ps_c", [128, 2, 512], F32, nres=2)
    ps_o = P.psum("ps_o", [128, 2, 512], F32, nres=2)
    cnt = {"gi": 0, "tvi": 0}

    def transpose8(src_bf, dst, n):
        for c in range(n):
            P.op("pe", "transpose", dict(out=ps_t.t[:, c * 128:(c + 1) * 128], in_=src_bf.t[:, c * 128:(c + 1) * 128], identity=identb.t[:, :]),
                 [src_bf.r0, identb.r0], [ps_t.r0])
        cast(dst.t[:, 0:n, :], ps_t.t[:, 0:n * 128].rearrange("p (c t) -> p c t", c=n), [ps_t.r0], [dst.r0])

    v12v = v12.t[:].rearrange("p (h c) k -> p h c k", c=2)
    i12v = i12f.t[:].rearrange("p (h c) k -> p h c k", c=2)

    def front(tt):
        par = tt % 2
        hh_, ptl, xn, eidx, gte = hh2[tt % 3], ptl2[tt % 3], xn2[par], eidx2[par], gte2[par]
        xnb = xnb2[par]
        P.dma("sp", hh_.t[:, :], h1_s[tt * 128:(tt + 1) * 128, :], writes=[hh_.r0])
        P.dma("sp", ptl.t[:, :], p_d[tt * 128:(tt + 1) * 128, :], writes=[ptl.r0])
        rmsnorm_tile(hh_, gfb, xn, xnb, sqF, junkF)
        yield
        transpose8(xnb, xnT, 8)
        yield
        for hd in range(8):
            bk = hd // 4
            for k in range(8):
                mm(ps_q.t[:, bk, (hd % 4) * 128:(hd % 4 + 1) * 128], wQ.t[:, k, hd * 128:(hd + 1) * 128], xnT.t[:, k, :],
                   (k == 0 and hd % 4 == 0), False, [wQ.r0, xnT.r0], [ps_q.r[bk]])
        for bk in range(2):
            cast(qpT.t[:, bk * 4:(bk + 1) * 4, :], ps_q.t[:, bk, :].rearrange("p (a t) -> p a t", a=4), [ps_q.r[bk]], [qpT.r0])
        for rnd in range(2):
            for h4 in range(4):
                hd = rnd * 4 + h4
                bk = h4 // 2
                mm(ps_c.t[:, bk, (hd % 2) * 256:(hd % 2 + 1) * 256], qpT.t[:, hd, :], kbdb.t[:, :], hd % 2 == 0, False, [qpT.r0, kbdb.r0], [ps_c.r[bk]])
            for bk in range(2):
                cast(s_all.t[:, rnd * 8 + bk * 4:rnd * 8 + (bk + 1) * 4, :], ps_c.t[:, bk, :].rearrange("p (a n) -> p a n", a=4), [ps_c.r[bk]], [s_all.r0])
            yield
        sI = s_all.t[:].bitcast(I32)
        P.op("dve", "tensor_scalar", dict(out=sI, in0=sI, scalar1=-128, scalar2=None, op0=ALU.bitwise_and), [s_all.r0], [s_all.r0])
        P.op("dve", "tensor_tensor", dict(out=sI, in0=sI, in1=io128.t[:, :].unsqueeze(1).broadcast_to([128, 16, 128]), op=ALU.bitwise_or), [s_all.r0, io128.r0], [s_all.r0])
        yield
        for j in range(16):
            P.op("dve", "max", dict(out=v12.t[:, j, 0:8], in_=s_all.t[:, j, :]), [s_all.r0], [v12.r0])
            P.op("dve", "match_replace", dict(out=stmp.t[:, 0:128], in_to_replace=v12.t[:, j, 0:8], in_values=s_all.t[:, j, :], imm_value=-3.0e38),
                 [s_all.r0, v12.r0], [stmp.r0])
            P.op("dve", "max", dict(out=v12.t[:, j, 8:16], in_=stmp.t[:, 0:128]), [stmp.r0], [v12.r0])
            if j % 2 == 1:
                yield
        P.op("dve", "tensor_scalar", dict(out=i12i.t[:], in0=v12.t[:].bitcast(I32), scalar1=127, scalar2=None, op0=ALU.bitwise_and), [v12.r0], [i12i.r0])
        P.op("dve", "tensor_copy", dict(out=i12f.t[:], in_=i12i.t[:]), [i12i.r0], [i12f.r0])
        candv = cand.t[:].rearrange("p h (a b) -> p h a b", a=16)
        P.op("dve", "tensor_tensor", dict(out=candv, in0=v12v[:, :, 0, :].unsqueeze(3).broadcast_to([128, 8, 16, 16]),
                                          in1=v12v[:, :, 1, :].unsqueeze(2).broadcast_to([128, 8, 16, 16]), op=ALU.add), [v12.r0], [cand.r0])
        yield
        cI = cand.t[:].bitcast(I32)
        P.op("dve", "tensor_scalar", dict(out=cI, in0=cI, scalar1=-256, scalar2=None, op0=ALU.bitwise_and), [cand.r0], [cand.r0])
        P.op("dve", "tensor_tensor", dict(out=cI, in0=cI, in1=io256.t[:, :].unsqueeze(1).broadcast_to([128, 8, 256]), op=ALU.bitwise_or), [cand.r0, io256.r0], [cand.r0])
        yield
        for hd in range(8):
            P.op("dve", "max", dict(out=best.t[:, hd, 0:8], in_=cand.t[:, hd, :]), [cand.r0], [best.r0])
            P.op("dve", "match_replace", dict(out=stmp.t[:, :], in_to_replace=best.t[:, hd, 0:8], in_values=cand.t[:, hd, :], imm_value=-3.0e38),
                 [cand.r0, best.r0], [stmp.r0])
            P.op("dve", "max", dict(out=best.t[:, hd, 8:16], in_=stmp.t[:, :]), [stmp.r0], [best.r0])
            if hd % 2 == 1:
                yield
        P.op("dve", "tensor_scalar", dict(out=posi2.t[:], in0=best.t[:].bitcast(I32), scalar1=255, scalar2=None, op0=ALU.bitwise_and), [best.r0], [posi2.r0])
        P.op("dve", "tensor_scalar", dict(out=abi.t[:, 0], in0=posi2.t[:], scalar1=4, scalar2=None, op0=ALU.arith_shift_right), [posi2.r0], [abi.r0])
        P.op("dve", "tensor_scalar", dict(out=abi.t[:, 1], in0=posi2.t[:], scalar1=15, scalar2=None, op0=ALU.bitwise_and), [posi2.r0, abi.r0], [abi.r0])
        P.op("dve", "tensor_copy", dict(out=abf.t[:], in_=abi.t[:]), [abi.r0], [abf.r0])
        yield
        ohv = oh.t[:].rearrange("p h (a b) -> p h a b", a=16)
        for c in range(2):
            P.op("dve", "tensor_tensor", dict(out=ohv, in0=abf.t[:, c].unsqueeze(3).broadcast_to([128, 8, 16, 16]),
                                              in1=io16.t[:, :].unsqueeze(1).unsqueeze(1).broadcast_to([128, 8, 16, 16]), op=ALU.is_equal),
                 [abf.r0, io16.r0], [oh.r0])
            P.op("dve", "tensor_tensor", dict(out=ohv, in0=ohv, in1=i12v[:, :, c, :].unsqueeze(2).broadcast_to([128, 8, 16, 16]), op=ALU.mult),
                 [oh.r0, i12f.r0], [oh.r0])
            P.op("dve", "tensor_reduce", dict(out=isel.t[:, c], in_=ohv, axis=AX.X, op=ALU.add), [oh.r0], [isel.r0])
            yield
        P.op("dve", "scalar_tensor_tensor", dict(out=ef.t[:, :], in0=isel.t[:, 0].rearrange("p h k -> p (h k)"), scalar=128.0,
                                                 in1=isel.t[:, 1].rearrange("p h k -> p (h k)"), op0=ALU.mult, op1=ALU.add), [isel.r0], [ef.r0])
        P.op("dve", "tensor_copy", dict(out=eidx.t[:, :], in_=ef.t[:, :]), [ef.r0], [eidx.r0])
        P.op("dve", "tensor_tensor", dict(out=bex.t[:], in0=best.t[:], in1=best.t[:, :, 0:1].broadcast_to([128, 8, 16]), op=ALU.subtract), [best.r0], [bex.r0])
        P.op("act", "activation", dict(out=bex.t[:], in_=bex.t[:], func=AF.Exp), [bex.r0], [bex.r0])
        yield
        P.op("dve", "tensor_reduce", dict(out=bsm.t[:, 0], in_=bex.t[:], axis=AX.X, op=ALU.add), [bex.r0], [bsm.r0])
        P.op("dve", "reciprocal", dict(out=bsm.t[:, 1], in_=bsm.t[:, 0]), [bsm.r0], [bsm.r0])
        P.op("dve", "tensor_tensor", dict(out=gte.t[:, :].rearrange("p (h k) -> p h k", h=8), in0=bex.t[:],
                                          in1=bsm.t[:, 1].unsqueeze(2).broadcast_to([128, 8, 16]), op=ALU.mult), [bex.r0, bsm.r0], [gte.r0])
        yield

    diagb = P.sbuf("diagb", [128, 4, 128], BF16, nres=4)

    def gather_group(tt, grp):
        par = tt % 2
        xn, eidx, gte = xn2[par], eidx2[par], gte2[par]
        bufs = []
        for j in range(4):
            slot = grp * 4 + j
            b = cnt["gi"] % NG
            cnt["gi"] += 1
            bufs.append(b)
            P.dma("pool", ug.t[:, b, :], uv_s[:, :], reads=[eidx.r0], writes=[ug.r[b]],
                  indirect=dict(out_offset=None, in_offset=bass.IndirectOffsetOnAxis(ap=eidx.t[:, slot:slot + 1], axis=0)))
            pb_ = cnt.get("pbi", 0) % 4
            cnt["pbi"] = cnt.get("pbi", 0) + 1
            P.op("dve", "tensor_tensor", dict(out=prodb.t[:, pb_, :], in0=ug.t[:, b, 0:D], in1=xnb2[par].t[:, :], op=ALU.mult),
                 [ug.r[b], xnb2[par].r0], [prodb.r[pb_]])
            P.op("act", "activation", dict(out=junkA.t[:, :], in_=prodb.t[:, pb_, :], func=AF.Copy, accum_out=dots.t[:, slot:slot + 1]),
                 [prodb.r[pb_]], [dots.r[slot]])
        if cnt.get("pend") is not None:
            finish_group(*cnt["pend"])
        gs = slice(grp * 4, grp * 4 + 4)
        P.op("act", "activation", dict(out=a1.t[:, gs], in_=dots.t[:, gs], func=AF.Gelu_apprx_tanh), dots.r[grp * 4:grp * 4 + 4], [a1.r[grp]])
        cnt["pend"] = (tt, grp, bufs)
        if grp == 31:
            finish_group(*cnt["pend"])
            cnt["pend"] = None

    def finish_group(tt, grp, bufs):
        gte = gte2[tt % 2]
        for j in range(4):
            slot = grp * 4 + j
            b = bufs[j]
            db = cnt["tvi"] % 4
            cnt["tvi"] += 1
            P.op("dve", "tensor_scalar", dict(out=diagb.t[:, db, :], in0=identb.t[:, :], scalar1=a1.t[:, slot:slot + 1], scalar2=gte.t[:, slot:slot + 1],
                                              op0=ALU.mult, op1=ALU.mult), [identb.r0, a1.r[grp], gte.r0], [diagb.r[db]])
            for half in range(2):
                mm(ps_o.t[:, half, :], diagb.t[:, db, :], ug.t[:, b, D + half * 512:D + (half + 1) * 512], slot == 0, slot == 127,
                   [diagb.r[db], ug.r[b]], [ps_o.r[half]])

    def epilogue(tt):
        hh_, ptl = hh2[tt % 3], ptl2[tt % 3]
        for half in range(2):
            P.op("dve", "tensor_tensor", dict(out=hh_.t[:, half * 512:(half + 1) * 512], in0=ps_o.t[:, half, :], in1=hh_.t[:, half * 512:(half + 1) * 512], op=ALU.add),
                 [ps_o.r[half], hh_.r0], [hh_.r0])
        yield
        rmsnorm_tile(hh_, gpb, xne, xnbe, sqE, junkE)
        yield
        transpose8(xnbe, xnTe, 8)
        yield
        P.op("act", "copy", dict(out=ptb.t[:, :], in_=ptl.t[:, :]), [ptl.r0], [ptb.r0])
        transpose8(ptb, pT, 2)
        yield
        for half in range(2):
            for k in range(8):
                mm(ps_q.t[:, half, :], xnTe.t[:, k, :], wG.t[:, k, half * 512:(half + 1) * 512], k == 0, k == 7, [xnTe.r0, wG.r0], [ps_q.r[half]])
            P.op("act", "activation", dict(out=gate.t[:, half * 512:(half + 1) * 512], in_=ps_q.t[:, half, :], func=AF.Sigmoid), [ps_q.r[half]], [gate.r0])
            for k in range(2):
                mm(ps_c.t[:, half, :], pT.t[:, k, :], wPs.t[:, k, half * 512:(half + 1) * 512], k == 0, k == 1, [pT.r0, wPs.r0], [ps_c.r[half]])
            P.op("dve", "tensor_tensor", dict(out=tmpg.t[:, :], in0=ps_c.t[:, half, :], in1=gate.t[:, half * 512:(half + 1) * 512], op=ALU.mult),
                 [ps_c.r[half], gate.r0], [tmpg.r0])
            P.op("dve", "tensor_tensor", dict(out=hh_.t[:, half * 512:(half + 1) * 512], in0=hh_.t[:, half * 512:(half + 1) * 512], in1=tmpg.t[:, :], op=ALU.add),
                 [hh_.r0, tmpg.r0], [hh_.r0])
            yield
        rmsnorm_tile(hh_, glb, ot, None, sqE, junkE)
        yield
        P.dma("sp", out_d[tt * 128:(tt + 1) * 128, :], ot.t[:, :], reads=[ot.r0])

    for _ in front(0):
        pass
    ep = iter(())
    EPG = int(_os.environ.get("K_EPG", "8"))
    for tt in range(NTT):
        nxt = front(tt + 1) if tt + 1 < NTT else iter(())
        for grp in range(32):
            gather_group(tt, grp)
            if grp == EPG:
                for _ in ep:
                    pass
            if grp >= 1:
                next(nxt, None)
        for _ in nxt:
            pass
        ep = epilogue(tt)
        next(ep)
    for _ in ep:
        pass
    P.emit_stage()
    P.pop()

    P.close()
    return nc


def kernel(**inputs):
    consts = make_consts()
    shared = prep_shared(inputs)
    shared.update(consts)
    in_maps = []
    for b in range(8):
        m = dict(shared)
        m["x"] = np.ascontiguousarray(inputs["x"][b])
        m["p"] = np.ascontiguousarray(inputs["p"][0, b])
        m["positions"] = np.ascontiguousarray(inputs["positions"][b].reshape(1, S).astype(np.int32))
        in_maps.append(m)
    nc = build_nc()
    res = run_bass_kernel_spmd(nc, in_maps, core_ids=list(range(8)))
    return np.stack([np.asarray(r["out"]) for r in res.results], axis=0).astype(np.float32)
```

```python
from contextlib import ExitStack
import os as _os
import numpy as np
import ml_dtypes
import concourse.bass as bass
import concourse.mybir as mybir
from concourse.bass_utils import run_bass_kernel_spmd

F32 = mybir.dt.float32
BF16 = mybir.dt.bfloat16
I32 = mybir.dt.int32
AF = mybir.ActivationFunctionType
ALU = mybir.AluOpType
AX = mybir.AxisListType

S = 4096
D = 1024
NQC = 8
NTT = 32
NEG = -30000.0
SCALE = 128 ** -0.5
COMPUTE = ("pe", "dve", "act", "pool")
NOSELF = tuple(x for x in _os.environ.get("K_NOSELF", "").split(",") if x)
NROT = int(_os.environ.get("K_NROT", "8"))
NCH = 65
WCOLS = NCH * 128

C_RX, C_RG, C_Q, C_QS, C_KV, C_KSW, C_M, C_NG = 0, 8, 16, 24, 32, 44, 48, 64


class Res:
    __slots__ = ("w", "r", "name")

    def __init__(self, name=""):
        self.w = None
        self.r = {}
        self.name = name


class TT:
    def __init__(self, t, nres, name):
        self.t = t
        self.r = [Res(f"{name}.{i}") for i in range(nres)]

    @property
    def r0(self):
        return self.r[0]


class Prog:
    def __init__(self, nc):
        self.nc = nc
        self.scopes = [ExitStack()]
        self.streams = {e: [] for e in ("pe", "dve", "act", "pool", "sp")}
        self.cnt = {e: 0 for e in COMPUTE}
        self.seen = {e: {} for e in self.streams}
        self.sems = {}
        for i in range(int(_os.environ.get("K_SEMPAD", "0"))):
            self.scopes[0].enter_context(nc.semaphore(f"s_pad{i}"))
        for e in COMPUTE:
            self.sems[e] = self.scopes[0].enter_context(nc.semaphore(f"s_{e}"))
        self.dq = {}
        self.dma_total = {}
        for q in ("sp", "act", "pool"):
            self.dq[q] = {"n": 0, "sems": []}
            nrot_q = int(_os.environ.get("K_NROT_" + q.upper(), "1" if q == "sp" else str(NROT)))
            self.dq[q]["nrot"] = nrot_q
            for i in range(nrot_q):
                k = ("dma", q, i)
                self.sems[k] = self.scopes[0].enter_context(nc.semaphore(f"s_dma_{q}_{i}"))
                self.dq[q]["sems"].append(k)
                self.dma_total[k] = 0
        self.n_instr = 0

    def push(self):
        self.scopes.append(ExitStack())

    def pop(self):
        self.scopes.pop().close()

    def sbuf(self, name, shape, dtype, nres=1):
        self.n_instr += 0
        self.uid = getattr(self, "uid", 0) + 1
        t = self.scopes[-1].enter_context(self.nc.sbuf_tensor(f"{name}_{self.uid}", list(shape), dtype))
        return TT(t, nres, name)

    def psum(self, name, shape, dtype, nres=1):
        self.uid = getattr(self, "uid", 0) + 1
        t = self.scopes[-1].enter_context(self.nc.psum_tensor(f"{name}_{self.uid}", list(shape), dtype))
        return TT(t, nres, name)

    def _deps(self, eng, reads, writes):
        deps = {}

        def add(tok):
            if tok is None:
                return
            k, v = tok
            if deps.get(k, 0) < v:
                deps[k] = v

        for r in reads:
            add(r.w)
        for w in writes:
            add(w.w)
            for k, v in w.r.items():
                add((k, v))
        out = []
        for k, v in deps.items():
            if k == eng and (eng == "pe" or eng in NOSELF):
                continue
            if self.seen[eng].get(k, 0) >= v:
                continue
            self.seen[eng][k] = v
            out.append((k, v))
        return out

    def _record(self, tok, reads, writes):
        k, v = tok
        for r in reads:
            if r.r.get(k, 0) < v:
                r.r[k] = v
        for w in writes:
            w.w = tok
            w.r = {}

    def op(self, eng, meth, kw, reads=(), writes=()):
        waits = self._deps(eng, reads, writes)
        self.cnt[eng] += 1
        tok = (eng, self.cnt[eng])
        self._record(tok, reads, writes)
        self.streams[eng].append((meth, kw, waits, (eng, 1)))
        self.n_instr += 1

    def dma(self, q, out, in_, reads=(), writes=(), indirect=None, **kw):
        d = self.dq[q]
        i = d["n"]
        d["n"] += 1
        sk = d["sems"][i % d["nrot"]]
        prev = self.dma_total[sk]
        waits = self._deps(q, reads, writes)
        if prev > 0 and self.seen[q].get(sk, 0) < prev:
            self.seen[q][sk] = prev
            waits.append((sk, prev))
        self.dma_total[sk] = prev + 16
        tok = (sk, prev + 16)
        self._record(tok, reads, writes)
        kk = dict(out=out, in_=in_)
        if indirect is not None:
            kk.update(indirect)
        kk.update(kw)
        self.streams[q].append(("indirect_dma_start" if indirect is not None else "dma_start", kk, waits, (sk, 16)))
        self.n_instr += 1
        return tok

    def emit_stage(self):
        targets = [(e, self.cnt[e]) for e in COMPUTE if self.cnt[e] > 0]
        targets += [(k, v) for k, v in self.dma_total.items() if v > 0]
        for e in self.streams:
            ws = []
            for k, v in targets:
                if self.seen[e].get(k, 0) < v:
                    self.seen[e][k] = v
                    ws.append((k, v))
            if ws:
                self.streams[e].append((None, None, ws, None))
        sems = self.sems
        streams = self.streams

        def run(eng_obj, items):
            for meth, kw, waits, inc in items:
                for k, v in waits:
                    eng_obj.wait_ge(sems[k], v)
                if meth is None:
                    continue
                ins = getattr(eng_obj, meth)(**kw)
                ins.then_inc(sems[inc[0]], inc[1])

        with self.nc.Block() as block:
            @block.tensor
            def _(e):
                run(e, streams["pe"])

            @block.vector
            def _(e):
                run(e, streams["dve"])

            @block.scalar
            def _(e):
                run(e, streams["act"])

            @block.gpsimd
            def _(e):
                run(e, streams["pool"])

            @block.sync
            def _(e):
                run(e, streams["sp"])
        self.streams = {e: [] for e in streams}

    def close(self):
        while self.scopes:
            self.scopes.pop().close()


def _bf(a):
    return np.ascontiguousarray(a).astype(ml_dtypes.bfloat16)


def make_consts():
    c = {}
    c["c_identb"] = _bf(np.eye(128, dtype=np.float32))
    c["c_identf"] = np.eye(128, dtype=np.float32)
    half = 64
    inv = (10000.0 ** (-np.arange(half, dtype=np.float32) / np.float32(half))).astype(np.float32)
    c["c_invf"] = np.concatenate([inv, inv]).reshape(128, 1).astype(np.float32)
    c["c_sgn"] = np.concatenate([-np.ones(64), np.ones(64)]).reshape(128, 1).astype(np.float32)
    kp = np.arange(128)[:, None]
    q = np.arange(512)[None, :]
    cb = np.zeros((128, 4, 512), np.float32)
    wb = np.zeros((128, 4, 512), np.float32)
    for r in range(4):
        k = r * 128 + kp
        cb[:, r, :] = np.where(k <= q, 0.0, NEG)
        k2 = (r - 4) * 128 + kp
        wb[:, r, :] = np.where(q - k2 < 512, 0.0, NEG)
    c["c_cb"] = _bf(cb)
    c["c_wb"] = _bf(wb)
    n = (np.arange(2)[None, :, None] * 128 + np.arange(128)[:, None, None])
    t = np.arange(S)[None, None, :]
    cmb = np.where((16 * n + 31 <= t) & (n < 255), 0.0, NEG).astype(np.float32)
    c["c_cmb"] = _bf(cmb)
    j = np.arange(64)[:, None]
    kk = np.arange(S)[None, :]
    c["c_E"] = _bf(np.concatenate([(kk // 64 == j).astype(np.float32), np.zeros((64, S), np.float32)], 0))
    nn = np.arange(256)
    jj = np.arange(64)
    ov = ((16 * nn[:, None] < 64 * jj[None, :] + 64) & (16 * nn[:, None] + 31 >= 64 * jj[None, :]) & (nn[:, None] < 255))
    c["c_ovl"] = _bf(ov.astype(np.float32).reshape(2, 128, 64).transpose(1, 0, 2))
    tt = np.arange(S).reshape(NTT, 128)[:, :, None]
    cur = tt // 64
    j3 = np.arange(64)[None, None, :]
    forced = (j3 == 0) | (j3 == cur) | (j3 == cur - 1)
    valid = (64 * j3 <= tt)
    c["c_fa"] = np.ascontiguousarray(np.where(forced, 1000.0, 0.0).astype(np.float32).transpose(1, 0, 2))
    c["c_na"] = np.ascontiguousarray(np.where(valid, 0.0, -1e30).astype(np.float32).transpose(1, 0, 2))
    c["c_vm"] = np.ascontiguousarray(valid.astype(np.float32).transpose(1, 0, 2))
    c["c_iota128"] = np.ascontiguousarray(np.broadcast_to(np.arange(128, dtype=np.int32)[None, None, :], (128, 16, 128)))
    c["c_iota256"] = np.ascontiguousarray(np.broadcast_to(np.arange(256, dtype=np.int32)[None, None, :], (128, 8, 256)))
    c["c_iota16"] = np.ascontiguousarray(np.broadcast_to(np.arange(16, dtype=np.float32)[None, None, None, :], (128, 8, 16, 16)))
    return c


def pc_layout(v):
    return np.ascontiguousarray(np.asarray(v).reshape(8, 128).T)


def prep_shared(inp):
    m = {}
    def pk(w):
        K_, N_ = w.shape
        return np.ascontiguousarray(w.reshape(K_ // 128, 128, N_).transpose(1, 0, 2))
    m["w_in"] = pk(inp["w_in"][0])
    for k in ("w_a", "w_b", "w_out", "peer_wq", "ple_wg", "ple_wp"):
        m[k] = pk(inp[k][0])
    for k in ("peer_u", "peer_v"):
        m[k] = np.ascontiguousarray(inp[k][0])
    m["lru_wr"] = np.ascontiguousarray(inp["lru_wr"][0].transpose(1, 0, 2))
    m["lru_wi"] = np.ascontiguousarray(inp["lru_wi"][0].transpose(1, 0, 2))
    for k in ("cmp_w1_k", "cmp_w1_v"):
        m[k] = np.ascontiguousarray(inp[k][0].reshape(32, 128, 256).transpose(1, 0, 2))
    for k in ("cmp_w2_k", "cmp_w2_v"):
        m[k] = pk(inp[k][0])
    for k in ("norm_mix", "norm_ffn", "norm_ple"):
        m[k] = np.ascontiguousarray(inp[k][0].reshape(1, D))
    m["norm_final"] = np.ascontiguousarray(inp["norm_final"].reshape(1, D))
    m["conv_w"] = np.ascontiguousarray(inp["conv_w"][0].reshape(4, 8, 128).transpose(2, 1, 0))
    for k in ("conv_b", "lru_br", "lru_bi", "lru_lam"):
        m[k] = pc_layout(inp[k][0])
    for k in ("cmp_b1_k", "cmp_b1_v"):
        m[k] = np.ascontiguousarray(inp[k][0].reshape(2, 128).T)
    m["cmp_b2_k"] = np.ascontiguousarray(inp["cmp_b2_k"][0].reshape(128, 1))
    m["cmp_b2_v"] = np.ascontiguousarray(inp["cmp_b2_v"][0].reshape(1, 128))
    m["cmp_pe_k"] = np.ascontiguousarray(inp["cmp_pe_k"][0].T)
    m["cmp_pe_v"] = np.ascontiguousarray(inp["cmp_pe_v"][0].T)
    keys = inp["peer_keys"][0]
    kbd = np.zeros((128, 256), np.float32)
    kbd[0:64, 0:128] = keys[0].T
    kbd[64:128, 128:256] = keys[1].T
    m["peer_kbd"] = kbd
    return m


def win_segments():
    segs = [(0, 0, 2048), (2048, 2048, 1024)]
    d = 3072
    for h in range(8):
        segs.append((d, 2048 + h * 128 + 64, 64))
        segs.append((d + 64, 2048 + h * 128, 64))
        d += 128
    segs.append((4096, 3072, 1536))
    d = 5632
    for jkv in (2, 4):
        for g in range(2):
            s0 = 3072 + jkv * 256 + g * 128
            segs.append((d, s0 + 64, 64))
            segs.append((d + 64, s0, 64))
            d += 128
    segs.append((6144, 4632, 2048))
    segs.append((8192, 4608, 24))
    return segs


def build_nc(upto="D", debug=False):
    nc = bass.Bass("TRN2", target_bir_lowering=False)
    P = Prog(nc)
    ein = {}

    def IN(name, shape, dt=F32):
        ein[name] = nc.dram_tensor(name, list(shape), dt, kind="ExternalInput").ap()
        return ein[name]

    x_d = IN("x", [S, D])
    p_d = IN("p", [S, 256])
    pos_d = IN("positions", [1, S], I32)
    win_d = IN("w_in", [128, 8, 6680])
    w5_d = [IN(k, [128, 8, D]) for k in ("w_a", "w_b", "w_out", "peer_wq", "ple_wg")]
    wp_d = IN("ple_wp", [128, 2, D])
    u_d = IN("peer_u", [16384, D])
    v_d = IN("peer_v", [16384, D])
    wr_d = IN("lru_wr", [128, 8, 128])
    wi_d = IN("lru_wi", [128, 8, 128])
    w1_d = [IN("cmp_w1_k", [128, 32, 256]), IN("cmp_w1_v", [128, 32, 256])]
    w2_d = [IN("cmp_w2_k", [128, 2, 128]), IN("cmp_w2_v", [128, 2, 128])]
    gm_d = IN("norm_mix", [1, D])
    gf_d = IN("norm_ffn", [1, D])
    gp_d = IN("norm_ple", [1, D])
    gl_d = IN("norm_final", [1, D])
    cw_d = IN("conv_w", [128, 8, 4])
    cbias_d = IN("conv_b", [128, 8])
    br_d = IN("lru_br", [128, 8])
    bi_d = IN("lru_bi", [128, 8])
    lam_d = IN("lru_lam", [128, 8])
    b1_d = [IN("cmp_b1_k", [128, 2]), IN("cmp_b1_v", [128, 2])]
    b2k_d = IN("cmp_b2_k", [128, 1])
    b2v_d = IN("cmp_b2_v", [1, 128])
    pe_d = [IN("cmp_pe_k", [128, 32]), IN("cmp_pe_v", [128, 32])]
    kbd_d = IN("peer_kbd", [128, 256])
    identb_d = IN("c_identb", [128, 128], BF16)
    identf_d = IN("c_identf", [128, 128])
    invf_d = IN("c_invf", [128, 1])
    sgn_d = IN("c_sgn", [128, 1])
    cb_d = IN("c_cb", [128, 4, 512], BF16)
    wb_d = IN("c_wb", [128, 4, 512], BF16)
    cmb_d = IN("c_cmb", [128, 2, S], BF16)
    E_d = IN("c_E", [128, S], BF16)
    ovl_d = IN("c_ovl", [128, 2, 64], BF16)
    fa_d = IN("c_fa", [128, NTT, 64])
    na_d = IN("c_na", [128, NTT, 64])
    vm_d = IN("c_vm", [128, NTT, 64])
    io128_d = IN("c_iota128", [128, 16, 128], I32)
    io256_d = IN("c_iota256", [128, 8, 256], I32)
    io16_d = IN("c_iota16", [128, 8, 16, 16])

    out_d = nc.dram_tensor("out", [S, D], F32, kind="ExternalOutput").ap()

    dbg_n = [0]

    def DBG(tag, tt_, shape, dt=F32):
        if not debug:
            return
        dbg_n[0] += 1
        d = nc.dram_tensor(f"dbg_{tag}", list(shape), dt, kind="ExternalOutput").ap()
        P.dma("sp", d, tt_.t[:], reads=tt_.r)

    def SCR(name, shape, dt):
        kind = "ExternalOutput" if debug else "Internal"
        return nc.dram_tensor(name, list(shape), dt, kind=kind).ap()

    winbf = SCR("s_winbf", [128, 8, WCOLS], BF16)
    w5bf = [SCR(f"s_w5bf{i}", [128, 8, D], BF16) for i in range(5)]
    wpbf = SCR("s_wpbf", [128, 2, D], BF16)
    w1bf = [SCR(f"s_w1bf{i}", [128, 32, 256], BF16) for i in range(2)]
    zfm = SCR("s_zfm", [NCH, 128, S], F32)
    ma_s = SCR("s_ma", [8, 128, S], BF16)
    obt_s = SCR("s_obt", [8, 128, S], BF16)
    h1_s = SCR("s_h1", [S, D], F32)
    uv_s = nc.dram_tensor("s_uv", [16384, 2048], BF16, kind="Internal").ap()

    identb = P.sbuf("identb", [128, 128], BF16)
    identf = P.sbuf("identf", [128, 128], F32)
    P.dma("sp", identb.t[:, :], identb_d, writes=[identb.r0])
    P.dma("sp", identf.t[:, :], identf_d, writes=[identf.r0])

    cast_rr = [0]

    def cast(out, in_, reads, writes, engs=tuple(_os.environ.get("K_CAST", "dve,act").split(","))):
        e = engs[cast_rr[0] % len(engs)]
        cast_rr[0] += 1
        if e == "act":
            P.op("act", "copy", dict(out=out, in_=in_), reads, writes)
        else:
            P.op(e, "tensor_copy", dict(out=out, in_=in_), reads, writes)

    P.push()
    stg = P.sbuf("stg", [128, 2, 4096], F32, nres=2)
    stgb = P.sbuf("stgb", [128, 2, 4096], BF16, nres=2)
    P.op("dve", "memset", dict(ap=stg.t[:, 0, :], constant=0.0), [], [stg.r[0]])
    P.op("dve", "memset", dict(ap=stg.t[:, 1, :], constant=0.0), [], [stg.r[1]])
    pi = [0]

    def piece(loads, n, dst, dshape):
        b = pi[0] % 2
        pi[0] += 1
        for mk, src in loads:
            P.dma("sp", mk(stg.t[:, b, 0:n]), src, writes=[stg.r[b]])
        cast(stgb.t[:, b, 0:n], stg.t[:, b, 0:n], [stg.r[b]], [stgb.r[b]])
        P.dma("pool", dst, dshape(stgb.t[:, b, 0:n]), reads=[stgb.r[b]])

    win_v = win_d
    segs = win_segments()
    for c0 in range(0, WCOLS, 512):
        c1 = min(c0 + 512, WCOLS)
        w = c1 - c0
        loads = []
        for (dcol, scol, ncol) in segs:
            lo, hi = max(dcol, c0), min(dcol + ncol, c1)
            if lo >= hi:
                continue
            so = scol + (lo - dcol)
            loads.append((lambda a, lo=lo, hi=hi, w=w, c0=c0: a.rearrange("p (kc n) -> p kc n", kc=8)[:, :, lo - c0:hi - c0],
                          win_v[:, :, so:so + (hi - lo)]))
        if c1 == WCOLS and 8192 + 24 < WCOLS:
            pass
        piece(loads, 8 * w, winbf[:, :, c0:c1], lambda a, w=w: a.rearrange("p (kc n) -> p kc n", kc=8))
    for i in range(5):
        wv = w5_d[i]
        for hh in range(2):
            piece([(lambda a: a.rearrange("p (kc n) -> p kc n", kc=8), wv[:, :, hh * 512:(hh + 1) * 512])], 4096,
                  w5bf[i][:, :, hh * 512:(hh + 1) * 512], lambda a: a.rearrange("p (kc n) -> p kc n", kc=8))
    piece([(lambda a: a.rearrange("p (kc n) -> p kc n", kc=2), wp_d)], 2048,
          wpbf[:, :, :], lambda a: a.rearrange("p (kc n) -> p kc n", kc=2))
    for i in range(2):
        wv = w1_d[i]
        for hh in range(2):
            piece([(lambda a: a.rearrange("p (l h) -> p l h", l=16), wv[:, hh * 16:(hh + 1) * 16, :])], 4096,
                  w1bf[i][:, hh * 16:(hh + 1) * 16, :], lambda a: a.rearrange("p (l h) -> p l h", l=16))
    P.emit_stage()
    P.pop()
    if upto == "P":
        P.close()
        return nc

    P.push()
    nT = P.sbuf("nT", [128, 8, S], BF16, nres=NTT)
    gbc = P.sbuf("gbc", [128, D], F32)
    xt = P.sbuf("xt", [128, 2, D], F32, nres=2)
    nb = P.sbuf("nb", [128, 2, D], BF16, nres=2)
    junk = P.sbuf("junk", [128, D], F32)
    ss = P.sbuf("ss", [128, NTT], F32, nres=NTT)
    rs = P.sbuf("rs", [128, NTT], F32, nres=NTT)
    tp = P.psum("tp", [128, 2, 1024], BF16, nres=2)
    P.dma("sp", gbc.t[:, :], gm_d.partition_broadcast(128), writes=[gbc.r0])
    for tt in range(NTT):
        b = tt % 2
        P.dma("sp", xt.t[:, b, :], x_d[tt * 128:(tt + 1) * 128, :], writes=[xt.r[b]])
        P.op("act", "activation", dict(out=junk.t[:, :], in_=xt.t[:, b, :], func=AF.Square, accum_out=ss.t[:, tt:tt + 1]),
             reads=[xt.r[b]], writes=[junk.r0, ss.r[tt]])
        P.op("act", "activation", dict(out=rs.t[:, tt:tt + 1], in_=ss.t[:, tt:tt + 1], func=AF.Sqrt, scale=1.0 / D, bias=1e-6),
             reads=[ss.r[tt]], writes=[rs.r[tt]])
        P.op("dve", "reciprocal", dict(out=rs.t[:, tt:tt + 1], in_=rs.t[:, tt:tt + 1]), reads=[rs.r[tt]], writes=[rs.r[tt]])
        P.op("dve", "scalar_tensor_tensor", dict(out=nb.t[:, b, :], in0=xt.t[:, b, :], scalar=rs.t[:, tt:tt + 1], in1=gbc.t[:, :],
                                                 op0=ALU.mult, op1=ALU.mult), reads=[xt.r[b], rs.r[tt], gbc.r0], writes=[nb.r[b]])
        for c in range(8):
            P.op("pe", "transpose", dict(out=tp.t[:, b, c * 128:(c + 1) * 128], in_=nb.t[:, b, c * 128:(c + 1) * 128], identity=identb.t[:, :]),
                 reads=[nb.r[b], identb.r0], writes=[tp.r[b]])
        cast(nT.t[:, :, tt * 128:(tt + 1) * 128], tp.t[:, b, :].rearrange("p (c t) -> p c t", c=8), [tp.r[b]], [nT.r[tt]])

    def uv_build():
        ust = P.sbuf("ust", [128, 2, 4096], F32, nres=2)
        uvb = P.sbuf("uvb", [128, 2, 4, 2048], BF16, nres=2)
        u_v = u_d.rearrange("(r p j) d -> r p (j d)", p=128, j=4)
        v_v = v_d.rearrange("(r p j) d -> r p (j d)", p=128, j=4)
        uv_v = uv_s.rearrange("(r p j) c -> r p (j c)", p=128, j=4)
        for rb in range(32):
            ob_ = rb % 2
            for ti_, (src_v, eng_) in enumerate(((u_v, "act"), (v_v, "dve"))):
                P.dma("sp", ust.t[:, ti_, :], src_v[rb], writes=[ust.r[ti_]])
                dst_ = uvb.t[:, ob_, :, ti_ * 1024:(ti_ + 1) * 1024]
                src_ = ust.t[:, ti_, :].rearrange("p (j d) -> p j d", j=4)
                if eng_ == "act":
                    P.op("act", "copy", dict(out=dst_, in_=src_), [ust.r[ti_]], [uvb.r[ob_]])
                else:
                    P.op("dve", "tensor_copy", dict(out=dst_, in_=src_), [ust.r[ti_]], [uvb.r[ob_]])
            P.dma("pool", uv_v[rb], uvb.t[:, ob_].rearrange("p j c -> p (j c)"), reads=[uvb.r[ob_]])
            yield

    wbuf = P.sbuf("wbuf", [128, 2, 8, 512], BF16, nres=2)
    zst = P.sbuf("zst", [128, 2, S], F32, nres=2)
    uvgen = uv_build()
    pz = P.psum("pz", [128, 4, 512], F32, nres=4)
    nTall = list(nT.r)
    zi = 0
    pzi = 0
    for wpi, c0 in enumerate(range(0, WCOLS, 512)):
        c1 = min(c0 + 512, WCOLS)
        wb_ = wpi % 2
        P.dma("sp", wbuf.t[:, wb_, :, 0:c1 - c0], winbf[:, :, c0:c1], writes=[wbuf.r[wb_]])
        for cc in range((c1 - c0) // 128):
            ch = c0 // 128 + cc
            M = 24 if ch == C_NG else 128
            zb = zi % 2
            zi += 1
            for qc in range(NQC):
                pb = pzi % 4
                pzi += 1
                for kc in range(8):
                    P.op("pe", "matmul", dict(out=pz.t[0:M, pb, :], lhsT=wbuf.t[:, wb_, kc, cc * 128:cc * 128 + M],
                                              rhs=nT.t[:, kc, qc * 512:(qc + 1) * 512], start=(kc == 0), stop=(kc == 7),
                                              skip_group_check=True),
                         reads=[wbuf.r[wb_]] + nTall[qc * 4:(qc + 1) * 4], writes=[pz.r[pb]])
                dst = zst.t[0:M, zb, qc * 512:(qc + 1) * 512]
                src = pz.t[0:M, pb, :]
                if C_RG <= ch < C_RG + 8:
                    P.op("act", "activation", dict(out=dst, in_=src, func=AF.Gelu_apprx_tanh), [pz.r[pb]], [zst.r[zb]])
                elif ch >= C_M:
                    P.op("act", "activation", dict(out=dst, in_=src, func=AF.Sigmoid), [pz.r[pb]], [zst.r[zb]])
                else:
                    P.op("dve", "tensor_copy", dict(out=dst, in_=src), [pz.r[pb]], [zst.r[zb]])
            P.dma("pool", zfm[ch, 0:M, :], zst.t[0:M, zb, :], reads=[zst.r[zb]])
            if ch % 2 == 0:
                next(uvgen, None)
    for _ in uvgen:
        pass
    P.emit_stage()
    P.pop()
    if upto == "A":
        P.close()
        return nc

    P.push()
    cw = P.sbuf("cw", [128, 8, 4], F32)
    cbias = P.sbuf("cbias", [128, 8], F32)
    brs = P.sbuf("brs", [128, 8], F32)
    bis = P.sbuf("bis", [128, 8], F32)
    lam = P.sbuf("lam", [128, 8], F32)
    m8 = P.sbuf("m8", [128, 8], F32)
    m16 = P.sbuf("m16", [128, 8], F32)
    wst = P.sbuf("wst", [128, 2, 8, 128], F32, nres=2)
    wrb = P.sbuf("wrb", [128, 8, 128], BF16)
    wib = P.sbuf("wib", [128, 8, 128], BF16)
    for (t_, d_) in ((cw, cw_d), (cbias, cbias_d), (brs, br_d), (bis, bi_d), (lam, lam_d)):
        P.dma("sp", t_.t[:], d_, writes=[t_.r0])
    P.dma("sp", wst.t[:, 0], wr_d, writes=[wst.r[0]])
    P.dma("sp", wst.t[:, 1], wi_d, writes=[wst.r[1]])
    P.op("dve", "tensor_copy", dict(out=wrb.t[:, :, :], in_=wst.t[:, 0]), [wst.r[0]], [wrb.r0])
    P.op("dve", "tensor_copy", dict(out=wib.t[:, :, :], in_=wst.t[:, 1]), [wst.r[1]], [wib.r0])
    P.op("act", "activation", dict(out=m8.t[:, :], in_=lam.t[:, :], func=AF.Exp, scale=-1.0), [lam.r0], [m8.r0])
    P.op("act", "activation", dict(out=m8.t[:, :], in_=m8.t[:, :], func=AF.Ln, bias=1.0), [m8.r0], [m8.r0])
    P.op("dve", "tensor_scalar", dict(out=m16.t[:, :], in0=m8.t[:, :], scalar1=-16.0, scalar2=None, op0=ALU.mult), [m8.r0], [m16.r0])
    P.op("dve", "tensor_scalar", dict(out=m8.t[:, :], in0=m8.t[:, :], scalar1=-8.0, scalar2=None, op0=ALU.mult), [m8.r0, m16.r0], [m8.r0])
    zx = P.sbuf("zx", [128, S + 3], F32)
    gz = P.sbuf("gz", [128, S], F32)
    xa = P.sbuf("xa", [128, S], F32)
    xab = P.sbuf("xab", [128, S], BF16)
    rr = P.sbuf("rr", [128, S], F32)
    ig = P.sbuf("ig", [128, S], F32)
    t1 = P.sbuf("t1", [128, S], F32)
    t2 = P.sbuf("t2", [128, S], F32)
    mab = P.sbuf("mab", [128, S], BF16)
    pg = P.psum("pg", [128, 4, 512], F32, nres=4)
    P.op("dve", "memset", dict(ap=zx.t[:, 0:3], constant=0.0), [], [zx.r0])
    pgi = 0
    for ch in range(8):
        P.dma("sp", zx.t[:, 3:S + 3], zfm[C_RX + ch], writes=[zx.r0])
        P.dma("sp", gz.t[:, :], zfm[C_RG + ch], writes=[gz.r0])
        P.op("dve", "tensor_scalar", dict(out=xa.t[:, :], in0=zx.t[:, 0:S], scalar1=cw.t[:, ch, 0:1], scalar2=cbias.t[:, ch:ch + 1],
                                          op0=ALU.mult, op1=ALU.add), [zx.r0, cw.r0, cbias.r0], [xa.r0])
        for k in range(1, 4):
            P.op("dve", "scalar_tensor_tensor", dict(out=xa.t[:, :], in0=zx.t[:, k:k + S], scalar=cw.t[:, ch, k:k + 1], in1=xa.t[:, :],
                                                     op0=ALU.mult, op1=ALU.add), [zx.r0, cw.r0, xa.r0], [xa.r0])
        P.op("act", "copy", dict(out=xab.t[:, :], in_=xa.t[:, :]), [xa.r0], [xab.r0])
        for (wgt, bia, dst) in ((wrb, brs, rr), (wib, bis, ig)):
            for qc in range(NQC):
                pb = pgi % 4
                pgi += 1
                P.op("pe", "matmul", dict(out=pg.t[:, pb, :], lhsT=wgt.t[:, ch, :], rhs=xab.t[:, qc * 512:(qc + 1) * 512], start=True, stop=True,
                                          skip_group_check=True), [wgt.r0, xab.r0], [pg.r[pb]])
                P.op("act", "activation", dict(out=dst.t[:, qc * 512:(qc + 1) * 512], in_=pg.t[:, pb, :], func=AF.Sigmoid, bias=bia.t[:, ch:ch + 1]),
                     [pg.r[pb], bia.r0], [dst.r0])
        P.op("act", "activation", dict(out=t1.t[:, :], in_=rr.t[:, :], func=AF.Exp, scale=m8.t[:, ch:ch + 1]), [rr.r0, m8.r0], [t1.r0])
        P.op("act", "activation", dict(out=t2.t[:, :], in_=rr.t[:, :], func=AF.Exp, scale=m16.t[:, ch:ch + 1]), [rr.r0, m16.r0], [t2.r0])
        P.op("act", "activation", dict(out=t2.t[:, :], in_=t2.t[:, :], func=AF.Sqrt, scale=-1.0, bias=1.0), [t2.r0], [t2.r0])
        P.op("pool", "tensor_tensor", dict(out=ig.t[:, :], in0=ig.t[:, :], in1=xa.t[:, :], op=ALU.mult), [ig.r0, xa.r0], [ig.r0])
        P.op("pool", "tensor_tensor", dict(out=ig.t[:, :], in0=ig.t[:, :], in1=t2.t[:, :], op=ALU.mult), [ig.r0, t2.r0], [ig.r0])
        P.op("dve", "tensor_tensor_scan", dict(out=rr.t[:, :], data0=t1.t[:, :], data1=ig.t[:, :], initial=0.0, op0=ALU.mult, op1=ALU.add),
             [t1.r0, ig.r0, rr.r0], [rr.r0])
        P.op("dve", "tensor_tensor", dict(out=mab.t[:, :], in0=rr.t[:, :], in1=gz.t[:, :], op=ALU.mult), [rr.r0, gz.r0], [mab.r0])
        P.dma("pool", ma_s[ch], mab.t[:, :], reads=[mab.r0])
    P.emit_stage()
    P.pop()
    if upto == "B":
        P.close()
        return nc

    P.push()
    cosT = P.sbuf("cosT", [128, S], F32)
    sinT = P.sbuf("sinT", [128, S], F32)
    P.push()
    posi = P.sbuf("posi", [128, S], I32)
    tq_ = P.sbuf("tq", [128, S], F32)
    tk_ = P.sbuf("tk", [128, S], F32)
    ki_ = P.sbuf("ki", [128, S], I32)
    invf = P.sbuf("invf", [128, 1], F32)
    sgn = P.sbuf("sgn", [128, 1], F32)
    P.dma("sp", posi.t[:, :], pos_d.partition_broadcast(128), writes=[posi.r0])
    P.dma("sp", invf.t[:, :], invf_d, writes=[invf.r0])
    P.dma("sp", sgn.t[:, :], sgn_d, writes=[sgn.r0])
    P.op("dve", "tensor_copy", dict(out=tq_.t[:, :], in_=posi.t[:, :]), [posi.r0], [tq_.r0])
    P.op("dve", "tensor_scalar", dict(out=tq_.t[:, :], in0=tq_.t[:, :], scalar1=invf.t[:, 0:1], scalar2=float(1.0 / (2 * np.pi)),
                                      op0=ALU.mult, op1=ALU.mult), [tq_.r0, invf.r0], [tq_.r0])
    TWO_PI = float(2 * np.pi * (1 - 1e-6))
    for which, dstT in (("sin", sinT), ("cos", cosT)):
        if which == "cos":
            P.op("dve", "tensor_scalar", dict(out=tq_.t[:, :], in0=tq_.t[:, :], scalar1=0.25, scalar2=None, op0=ALU.add), [tq_.r0], [tq_.r0])
        P.op("dve", "tensor_copy", dict(out=ki_.t[:, :], in_=tq_.t[:, :]), [tq_.r0], [ki_.r0])
        P.op("dve", "tensor_copy", dict(out=tk_.t[:, :], in_=ki_.t[:, :]), [ki_.r0], [tk_.r0])
        P.op("dve", "tensor_tensor", dict(out=tk_.t[:, :], in0=tq_.t[:, :], in1=tk_.t[:, :], op=ALU.subtract), [tq_.r0, tk_.r0], [tk_.r0])
        P.op("act", "activation", dict(out=dstT.t[:, :], in_=tk_.t[:, :], func=AF.Sin, scale=TWO_PI), [tk_.r0], [dstT.r0])
    P.op("dve", "tensor_scalar", dict(out=sinT.t[:, :], in0=sinT.t[:, :], scalar1=sgn.t[:, 0:1], scalar2=None, op0=ALU.mult), [sinT.r0, sgn.r0], [sinT.r0])
    P.emit_stage()
    P.pop()

    ksT = P.sbuf("ksT", [128, S], BF16)
    kwT = P.sbuf("kwT", [128, S], BF16)
    vsa = P.sbuf("vsa", [128, NTT, 129], BF16)
    vwa = P.sbuf("vwa", [128, NTT, 129], BF16)
    kcT = P.sbuf("kcT", [128, 256], BF16)
    vca = P.sbuf("vca", [128, 2, 193], BF16)
    E_s = P.sbuf("E_s", [128, S], BF16)
    cb_s = P.sbuf("cb_s", [128, 4, 512], BF16)
    wb_s = P.sbuf("wb_s", [128, 4, 512], BF16)
    b1s = [P.sbuf(f"b1s{i}", [128, 2], F32) for i in range(2)]
    pes = [P.sbuf(f"pes{i}", [128, 32], F32) for i in range(2)]
    b2k = P.sbuf("b2k", [128, 1], F32)
    b2vb = P.sbuf("b2vb", [128, 128], F32)
    ps_s = P.psum("ps_s", [128, 2, 512], F32, nres=2)
    ps_acc = P.psum("ps_acc", [128, 4, 512], F32, nres=4)
    ps_trb = P.psum("ps_trb", [128, 1024], BF16)
    ps_trf = P.psum("ps_trf", [128, 512], F32)
    P.dma("sp", E_s.t[:, :], E_d, writes=[E_s.r0])
    P.dma("sp", cb_s.t[:], cb_d, writes=[cb_s.r0])
    P.dma("sp", wb_s.t[:], wb_d, writes=[wb_s.r0])
    for i in range(2):
        P.dma("sp", b1s[i].t[:, :], b1_d[i], writes=[b1s[i].r0])
        P.dma("sp", pes[i].t[:, :], pe_d[i], writes=[pes[i].r0])
    P.dma("sp", b2k.t[:, :], b2k_d, writes=[b2k.r0])
    P.dma("sp", b2vb.t[:, :], b2v_d.partition_broadcast(128), writes=[b2vb.r0])
    P.op("dve", "memset", dict(ap=vsa.t[:, :, 128:129], constant=1.0), [], [vsa.r0])
    P.op("dve", "memset", dict(ap=vwa.t[:, :, 128:129], constant=1.0), [], [vwa.r0])
    srot = [0]

    def sbank():
        srot[0] += 1
        return srot[0] % 2

    def mm(out, lhsT, rhs, start, stop, reads, writes):
        P.op("pe", "matmul", dict(out=out, lhsT=lhsT, rhs=rhs, start=start, stop=stop, skip_group_check=True), reads, writes)

    for g in range(2):
        P.push()
        ld = P.sbuf("ld", [128, 2, S], F32, nres=2)
        lohi = P.sbuf("lohi", [128, 2, S], BF16, nres=2)
        w1s = P.sbuf("w1s", [128, 32, 256], BF16)
        w2f = P.sbuf("w2f", [128, 2, 128], F32)
        w2s = P.sbuf("w2s", [128, 2, 128], BF16)
        hidT = P.sbuf("hidT", [128, 2, 256], BF16)
        vb = P.sbuf("vb", [128, S], BF16)
        P.op("dve", "memset", dict(ap=kcT.t[:, :], constant=0.0), [], [kcT.r0])
        P.op("dve", "memset", dict(ap=vca.t[:], constant=0.0), [], [vca.r0])
        P.op("dve", "memset", dict(ap=vca.t[:, :, 128:129], constant=1.0), [vca.r0], [vca.r0])
        P.dma("sp", vca.t[:, :, 129:193], ovl_d, writes=[vca.r0])
        for kvi in range(2):
            ch = C_KV + kvi * 2 + g
            P.dma("sp", ld.t[:, 0, :], zfm[ch], writes=[ld.r[0]])
            P.dma("sp", w1s.t[:], w1bf[kvi], writes=[w1s.r0])
            P.dma("sp", w2f.t[:], w2_d[kvi], writes=[w2f.r0])
            P.op("dve", "tensor_copy", dict(out=w2s.t[:], in_=w2f.t[:]), [w2f.r0], [w2s.r0])
            for v_ in range(2):
                P.op("dve", "tensor_tensor", dict(out=lohi.t[:, v_, :].rearrange("p (n l) -> p n l", l=16),
                                                  in0=ld.t[:, 0, :].rearrange("p (n l) -> p n l", l=16),
                                                  in1=pes[kvi].t[:, v_ * 16:(v_ + 1) * 16].unsqueeze(1).broadcast_to([128, 256, 16]),
                                                  op=ALU.add), [ld.r[0], pes[kvi].r0], [lohi.r[v_]])
            for hc in range(2):
                for l in range(32):
                    v_ = 0 if l < 16 else 1
                    mm(ps_s.t[:, hc, 0:255], w1s.t[:, l, hc * 128:(hc + 1) * 128], lohi.t[:, v_, l:l + 16 * 254 + 1:16],
                       l == 0, l == 31, [w1s.r0, lohi.r[v_]], [ps_s.r[hc]])
                P.op("act", "activation", dict(out=hidT.t[:, hc, 0:255], in_=ps_s.t[:, hc, 0:255], func=AF.Gelu_apprx_tanh,
                                               bias=b1s[kvi].t[:, hc:hc + 1]), [ps_s.r[hc], b1s[kvi].r0], [hidT.r0])
            if kvi == 0:
                for hc in range(2):
                    mm(ps_s.t[:, 0, 0:255], w2s.t[:, hc, :], hidT.t[:, hc, 0:255], hc == 0, hc == 1, [w2s.r0, hidT.r0], [ps_s.r[0]])
                P.op("dve", "tensor_scalar", dict(out=kcT.t[:, 0:255], in0=ps_s.t[:, 0, 0:255], scalar1=b2k.t[:, 0:1], scalar2=None, op0=ALU.add),
                     [ps_s.r[0], b2k.r0], [kcT.r0])
            else:
                for nch in range(2):
                    rows = 128 if nch == 0 else 127
                    for hc in range(2):
                        mm(ps_acc.t[0:rows, nch, 0:128], hidT.t[:, hc, nch * 128:nch * 128 + rows], w2s.t[:, hc, :], hc == 0, hc == 1,
                           [w2s.r0, hidT.r0], [ps_acc.r[nch]])
                    P.op("dve", "tensor_tensor", dict(out=vca.t[0:rows, nch, 0:128], in0=ps_acc.t[0:rows, nch, 0:128], in1=b2vb.t[0:rows, :], op=ALU.add),
                         [ps_acc.r[nch], b2vb.r0], [vca.r0])
        for (kch, swch, dstK) in ((C_KV + 4 + g, C_KSW + g, ksT), (C_KV + 8 + g, C_KSW + 2 + g, kwT)):
            P.dma("sp", ld.t[:, 0, :], zfm[kch], writes=[ld.r[0]])
            P.dma("sp", ld.t[:, 1, :], zfm[swch], writes=[ld.r[1]])
            if debug and g == 1 and dstK is ksT:
                DBG("g1_ld_raw", ld, [128, 2, S])
                DBG("g1_cosT", cosT, [128, S])
                DBG("g1_sinT", sinT, [128, S])
            P.op("dve", "tensor_tensor", dict(out=ld.t[:, 0, :], in0=ld.t[:, 0, :], in1=cosT.t[:, :], op=ALU.mult), [ld.r[0], cosT.r0], [ld.r[0]])
            P.op("pool", "tensor_tensor", dict(out=ld.t[:, 1, :], in0=ld.t[:, 1, :], in1=sinT.t[:, :], op=ALU.mult), [ld.r[1], sinT.r0], [ld.r[1]])
            if debug and g == 1 and dstK is ksT:
                DBG("g1_ld_mul", ld, [128, 2, S])
            P.op("dve", "tensor_tensor", dict(out=dstK.t[:, :], in0=ld.t[:, 0, :], in1=ld.t[:, 1, :], op=ALU.add), [ld.r[0], ld.r[1]], [dstK.r0])
        for (vch, dstV) in ((C_KV + 6 + g, vsa), (C_KV + 10 + g, vwa)):
            P.dma("sp", ld.t[:, 0, :], zfm[vch], writes=[ld.r[0]])
            P.op("act", "copy", dict(out=vb.t[:, :], in_=ld.t[:, 0, :]), [ld.r[0]], [vb.r0])
            for t4 in range(8):
                for a in range(4):
                    tt = t4 * 4 + a
                    P.op("pe", "transpose", dict(out=ps_trb.t[:, a * 128:(a + 1) * 128], in_=vb.t[:, tt * 128:(tt + 1) * 128], identity=identb.t[:, :]),
                         [vb.r0, identb.r0], [ps_trb.r0])
                cast(dstV.t[:, t4 * 4:(t4 + 1) * 4, 0:128], ps_trb.t[:, 0:512].rearrange("p (a d) -> p a d", a=4), [ps_trb.r0], [dstV.r0])
        P.emit_stage()
        P.pop()
        if upto == "C0":
            P.close()
            return nc

        P.push()
        zq = P.sbuf("zq", [128, 4, 512], F32, nres=4)
        zqs = P.sbuf("zqs", [128, 4, 512], F32, nres=4)
        qn = P.sbuf("qn", [128, 4, 512], BF16)
        qr = P.sbuf("qr", [128, 4, 512], BF16)
        cmbq = P.sbuf("cmbq", [128, 2, 512], BF16)
        fa = P.sbuf("fa", [128, 4, 64], F32)
        na = P.sbuf("na", [128, 4, 64], F32)
        vm = P.sbuf("vm", [128, 4, 64], F32)
        gTs = P.sbuf("gTs", [24, 512], F32)
        gts = P.sbuf("gts", [128, 4, 24], F32)
        pc = P.sbuf("pc", [128, 2, 2, 512], BF16, nres=2)
        pt = P.sbuf("pt", [128, 3, 512], BF16, nres=3)
        oacc = P.sbuf("oacc", [128, 4, 4, 128], F32)
        imp = P.sbuf("imp", [128, 4, 64], F32)
        impp = P.sbuf("impp", [128, 64], F32)
        imp2 = P.sbuf("imp2", [128, 64], F32)
        m8a = P.sbuf("m8a", [128, 8], F32)
        m8b = P.sbuf("m8b", [128, 8], F32)
        msel = P.sbuf("msel", [128, 64], F32)
        bmb = P.sbuf("bmb", [128, 64], BF16)
        biasT = P.sbuf("biasT", [128, 512], BF16)
        P.op("dve", "memset", dict(ap=biasT.t[:, :], constant=0.0), [], [biasT.r0])
        sm = P.sbuf("sm", [128, 8], F32)
        obb = P.sbuf("obb", [128, 4, 4, 128], BF16)
        obT = P.sbuf("obT", [128, 4, 512], BF16)
        ptr = [0]
        aset = [0]
        for qc in range(NQC):
            q0 = qc * 512
            P.dma("sp", zq.t[:], zfm[C_Q + 4 * g:C_Q + 4 * g + 4, :, q0:q0 + 512].rearrange("c p t -> p c t"), writes=list(zq.r))
            P.dma("sp", zqs.t[:], zfm[C_QS + 4 * g:C_QS + 4 * g + 4, :, q0:q0 + 512].rearrange("c p t -> p c t"), writes=list(zqs.r))
            P.dma("sp", cmbq.t[:], cmb_d[:, :, q0:q0 + 512], writes=[cmbq.r0])
            P.dma("sp", fa.t[:], fa_d[:, qc * 4:qc * 4 + 4, :], writes=[fa.r0])
            P.dma("sp", na.t[:], na_d[:, qc * 4:qc * 4 + 4, :], writes=[na.r0])
            P.dma("sp", vm.t[:], vm_d[:, qc * 4:qc * 4 + 4, :], writes=[vm.r0])
            P.dma("sp", gTs.t[:, :], zfm[C_NG, 0:24, q0:q0 + 512], writes=[gTs.r0])
            P.op("act", "copy", dict(out=qn.t[:], in_=zq.t[:]), zq.r, [qn.r0])
            cs = cosT.t[:, q0:q0 + 512].unsqueeze(1).broadcast_to([128, 4, 512])
            sn = sinT.t[:, q0:q0 + 512].unsqueeze(1).broadcast_to([128, 4, 512])
            P.op("dve", "tensor_tensor", dict(out=zq.t[:], in0=zq.t[:], in1=cs, op=ALU.mult), zq.r + [cosT.r0], zq.r)
            P.op("pool", "tensor_tensor", dict(out=zqs.t[:], in0=zqs.t[:], in1=sn, op=ALU.mult), zqs.r + [sinT.r0], zqs.r)
            P.op("dve", "tensor_tensor", dict(out=qr.t[:], in0=zq.t[:], in1=zqs.t[:], op=ALU.add), zq.r + zqs.r, [qr.r0])
            for qt in range(4):
                P.op("pe", "transpose", dict(out=ps_trf.t[:, qt * 24:(qt + 1) * 24], in_=gTs.t[0:24, qt * 128:(qt + 1) * 128], identity=identf.t[0:24, 0:24]),
                     [gTs.r0, identf.r0], [ps_trf.r0])
            P.op("dve", "tensor_copy", dict(out=gts.t[:].rearrange("p a c -> p (a c)"), in_=ps_trf.t[:, 0:96]), [ps_trf.r0], [gts.r0])

            def post(hh, qt, bank, off, br, first, aux=None):
                h = 4 * g + hh
                accv = ps_acc.t[:, bank, :]
                P.op("dve", "tensor_scalar", dict(out=sm.t[:, 0:1], in0=accv[:, off + 128:off + 129], scalar1=1e-30, scalar2=None, op0=ALU.max),
                     [ps_acc.r[bank]], [sm.r0])
                P.op("dve", "reciprocal", dict(out=sm.t[:, 1:2], in_=sm.t[:, 0:1]), [sm.r0], [sm.r0])
                P.op("dve", "tensor_tensor", dict(out=sm.t[:, 2:3], in0=sm.t[:, 1:2], in1=gts.t[:, qt, h * 3 + br:h * 3 + br + 1], op=ALU.mult),
                     [sm.r0, gts.r0], [sm.r0])
                if first:
                    P.op("dve", "tensor_scalar", dict(out=oacc.t[:, qt, hh, :], in0=accv[:, off:off + 128], scalar1=sm.t[:, 2:3], scalar2=None, op0=ALU.mult),
                         [ps_acc.r[bank], sm.r0], [oacc.r0])
                else:
                    P.op("dve", "scalar_tensor_tensor", dict(out=oacc.t[:, qt, hh, :], in0=accv[:, off:off + 128], scalar=sm.t[:, 2:3], in1=oacc.t[:, qt, hh, :],
                                                             op0=ALU.mult, op1=ALU.add), [ps_acc.r[bank], sm.r0, oacc.r0], [oacc.r0])
                if aux is not None:
                    if hh == 0:
                        P.op("dve", "tensor_scalar", dict(out=imp.t[:, qt, :], in0=accv[:, off + 129:off + 193], scalar1=sm.t[:, 1:2], scalar2=None, op0=ALU.mult),
                             [ps_acc.r[bank], sm.r0], [imp.r0])
                    else:
                        P.op("dve", "scalar_tensor_tensor", dict(out=imp.t[:, qt, :], in0=accv[:, off + 129:off + 193], scalar=sm.t[:, 1:2], in1=imp.t[:, qt, :],
                                                                 op0=ALU.mult, op1=ALU.add), [ps_acc.r[bank], sm.r0, imp.r0], [imp.r0])

            if (g, qc) in ((0, 0), (1, 1)):
                tg = f"g{g}q{qc}"
                DBG(tg + "_gts", gts, [128, 4, 24]); DBG(tg + "_qn", qn, [128, 4, 512], BF16); DBG(tg + "_qr", qr, [128, 4, 512], BF16)
                DBG(tg + "_kcT", kcT, [128, 256], BF16); DBG(tg + "_vca", vca, [128, 2, 193], BF16)
                DBG(tg + "_ksT", ksT, [128, S], BF16); DBG(tg + "_kwT", kwT, [128, S], BF16)
                DBG(tg + "_vsa", vsa, [128, NTT, 129], BF16); DBG(tg + "_vwa", vwa, [128, NTT, 129], BF16)
            n_nch = 2 if (q0 + 511 >= 16 * 128 + 31) else 1
            for hh in range(4):
                pcb = hh % 2
                for nch in range(n_nch):
                    sb = sbank()
                    mm(ps_s.t[:, sb, :], kcT.t[:, nch * 128:(nch + 1) * 128], qn.t[:, hh, :], True, False, [kcT.r0, qn.r0], [ps_s.r[sb]])
                    mm(ps_s.t[:, sb, :], identb.t[:, :], cmbq.t[:, nch, :], False, True, [identb.r0, cmbq.r0], [ps_s.r[sb]])
                    P.op("act", "activation", dict(out=pc.t[:, pcb, nch, :], in_=ps_s.t[:, sb, :], func=AF.Exp, scale=SCALE), [ps_s.r[sb]], [pc.r[pcb]])
                aset[0] ^= 1
                started = {}
                for qt in range(4):
                    bank = aset[0] * 2 + qt // 2
                    off = (qt % 2) * 193
                    for nch in range(n_nch):
                        mm(ps_acc.t[:, bank, off:off + 193], pc.t[:, pcb, nch, qt * 128:(qt + 1) * 128], vca.t[:, nch, :],
                           bank not in started, False, [pc.r[pcb], vca.r0], [ps_acc.r[bank]])
                        started[bank] = 1
                for qt in range(4):
                    post(hh, qt, aset[0] * 2 + qt // 2, (qt % 2) * 193, 0, True, aux=True)
            if (g, qc) in ((0, 0), (1, 1)):
                DBG(tg + "_oacc_c", oacc, [128, 4, 4, 128]); DBG(tg + "_imp", imp, [128, 4, 64])
            for qt in range(4):
                P.op("dve", "tensor_tensor", dict(out=impp.t[:, :], in0=imp.t[:, qt, :], in1=fa.t[:, qt, :], op=ALU.max), [imp.r0, fa.r0], [impp.r0])
                P.op("dve", "tensor_tensor", dict(out=impp.t[:, :], in0=impp.t[:, :], in1=na.t[:, qt, :], op=ALU.add), [impp.r0, na.r0], [impp.r0])
                P.op("dve", "max", dict(out=m8a.t[:, :], in_=impp.t[:, :]), [impp.r0], [m8a.r0])
                P.op("dve", "match_replace", dict(out=imp2.t[:, :], in_to_replace=m8a.t[:, :], in_values=impp.t[:, :], imm_value=-3.0e38),
                     [impp.r0, m8a.r0], [imp2.r0])
                P.op("dve", "max", dict(out=m8b.t[:, :], in_=imp2.t[:, :]), [imp2.r0], [m8b.r0])
                P.op("dve", "scalar_tensor_tensor", dict(out=msel.t[:, :], in0=impp.t[:, :], scalar=m8b.t[:, 7:8], in1=vm.t[:, qt, :],
                                                         op0=ALU.is_ge, op1=ALU.mult), [impp.r0, m8b.r0, vm.r0], [msel.r0])
                P.op("dve", "tensor_scalar", dict(out=bmb.t[:, :], in0=msel.t[:, :], scalar1=-NEG, scalar2=NEG, op0=ALU.mult, op1=ALU.add),
                     [msel.r0], [bmb.r0])
                P.op("pe", "transpose", dict(out=ps_trb.t[0:64, qt * 128:(qt + 1) * 128], in_=bmb.t[:, :], identity=identb.t[:, :]),
                     [bmb.r0, identb.r0], [ps_trb.r0])
            P.op("dve", "tensor_copy", dict(out=biasT.t[0:64, :], in_=ps_trb.t[0:64, 0:512]), [ps_trb.r0], [biasT.r0])
            if (g, qc) in ((0, 0), (1, 1)):
                DBG(tg + "_biasT", biasT, [128, 512], BF16)
            for br in (2, 1):
                for hh in range(4):
                    aset[0] ^= 1
                    started = {}
                    if br == 1:
                        kts = list(range(0, qc * 4 + 4))
                    else:
                        kts = list(range(max(0, qc * 4 - 4), qc * 4 + 4))
                    vsrc = vsa if br == 1 else vwa

                    def emit_scores(kt):
                        r = kt - qc * 4
                        sb = sbank()
                        if br == 1:
                            mm(ps_s.t[:, sb, :], ksT.t[:, kt * 128:(kt + 1) * 128], qr.t[:, hh, :], True, False, [ksT.r0, qr.r0], [ps_s.r[sb]])
                            mm(ps_s.t[:, sb, :], E_s.t[:, kt * 128:(kt + 1) * 128], biasT.t[:, :], False, r < 0, [E_s.r0, biasT.r0], [ps_s.r[sb]])
                            if r >= 0:
                                mm(ps_s.t[:, sb, :], identb.t[:, :], cb_s.t[:, r, :], False, True, [identb.r0, cb_s.r0], [ps_s.r[sb]])
                        else:
                            mm(ps_s.t[:, sb, :], kwT.t[:, kt * 128:(kt + 1) * 128], qr.t[:, hh, :], True, False, [kwT.r0, qr.r0], [ps_s.r[sb]])
                            btab = wb_s.t[:, r + 4, :] if r < 0 else cb_s.t[:, r, :]
                            mm(ps_s.t[:, sb, :], identb.t[:, :], btab, False, True, [identb.r0, cb_s.r0, wb_s.r0], [ps_s.r[sb]])
                        pb = ptr[0] % 3
                        ptr[0] += 1
                        P.op("act", "activation", dict(out=pt.t[:, pb, :], in_=ps_s.t[:, sb, :], func=AF.Exp, scale=SCALE), [ps_s.r[sb]], [pt.r[pb]])
                        return (kt, pb)

                    def emit_pv(kt, pb):
                        r = kt - qc * 4
                        for qt in range(4):
                            if r > qt:
                                continue
                            if br == 2 and r < qt - 4:
                                continue
                            bank = aset[0] * 2 + qt // 2
                            off = (qt % 2) * 129
                            mm(ps_acc.t[:, bank, off:off + 129], pt.t[:, pb, qt * 128:(qt + 1) * 128], vsrc.t[:, kt, :],
                               bank not in started, False, [pt.r[pb], vsrc.r0], [ps_acc.r[bank]])
                            started[bank] = 1

                    pend = None
                    for kt in kts:
                        cur = emit_scores(kt)
                        if pend is not None:
                            emit_pv(*pend)
                        pend = cur
                    emit_pv(*pend)
                    for qt in range(4):
                        post(hh, qt, aset[0] * 2 + qt // 2, (qt % 2) * 129, br, False)
            if (g, qc) in ((0, 0), (1, 1)):
                DBG(tg + "_oacc_w", oacc, [128, 4, 4, 128])
            P.op("act", "copy", dict(out=obb.t[:], in_=oacc.t[:]), [oacc.r0], [obb.r0])
            for hh in range(4):
                for qt in range(4):
                    P.op("pe", "transpose", dict(out=ps_trb.t[:, qt * 128:(qt + 1) * 128], in_=obb.t[:, qt, hh, :], identity=identb.t[:, :]),
                         [obb.r0, identb.r0], [ps_trb.r0])
                cast(obT.t[:, hh, :], ps_trb.t[:, 0:512], [ps_trb.r0], [obT.r0])
            for hq in range(4):
                P.dma("pool", obt_s[4 * g + hq, :, q0:q0 + 512], obT.t[:, hq, :], reads=[obT.r0])
        P.emit_stage()
        P.pop()
    P.pop()
    if upto == "C":
        P.close()
        return nc

    def rmsnorm_tile(src, gain, dst_f32, dst_bf, sq, junk_):
        P.op("act", "activation", dict(out=junk_.t[:, :], in_=src.t[:, :], func=AF.Square, accum_out=sq.t[:, 0:1]), [src.r0], [junk_.r0, sq.r0])
        P.op("act", "activation", dict(out=sq.t[:, 1:2], in_=sq.t[:, 0:1], func=AF.Sqrt, scale=1.0 / D, bias=1e-6), [sq.r0], [sq.r0])
        P.op("dve", "reciprocal", dict(out=sq.t[:, 2:3], in_=sq.t[:, 1:2]), [sq.r0], [sq.r0])
        P.op("dve", "scalar_tensor_tensor", dict(out=dst_f32.t[:, :], in0=src.t[:, :], scalar=sq.t[:, 2:3], in1=gain.t[:, :], op0=ALU.mult, op1=ALU.mult),
             [src.r0, sq.r0, gain.r0], [dst_f32.r0])
        if dst_bf is not None:
            P.op("act", "copy", dict(out=dst_bf.t[:, :], in_=dst_f32.t[:, :]), [dst_f32.r0], [dst_bf.r0])

    P.push()
    w3 = [P.sbuf(f"w3s{i}", [128, 8, D], BF16) for i in range(3)]
    for i in range(3):
        P.dma("sp", w3[i].t[:], w5bf[i], writes=[w3[i].r0])
    wA, wB, wO = w3
    obTq = P.sbuf("obTq", [128, 8, 512], BF16, nres=8)
    maTq = P.sbuf("maTq", [128, 8, 512], BF16, nres=8)
    mgT = P.sbuf("mgT", [128, 8, 512], BF16)
    g01 = P.sbuf("g01", [128, 2, 16, 512], F32, nres=2)
    tA = P.sbuf("tA", [128, 2, 512], F32, nres=2)
    tB = P.sbuf("tB", [128, 2, 512], F32, nres=2)
    xtl = P.sbuf("xtl", [128, 2, D], F32, nres=2)
    h1t = P.sbuf("h1t", [128, 2, D], F32, nres=2)
    ps_y = P.psum("ps_y", [128, 4, 512], F32, nres=4)
    ps_h = P.psum("ps_h", [128, 2, 512], F32, nres=2)
    ti = 0
    for qc in range(NQC):
        q0 = qc * 512
        gb = qc % 2
        P.dma("sp", obTq.t[:], obt_s[:, :, q0:q0 + 512].rearrange("c p t -> p c t"), writes=list(obTq.r))
        P.dma("sp", maTq.t[:], ma_s[:, :, q0:q0 + 512].rearrange("c p t -> p c t"), writes=list(maTq.r))
        P.dma("pool", g01.t[:, gb], zfm[C_M:C_M + 16, :, q0:q0 + 512].rearrange("c p t -> p c t"), writes=[g01.r[gb]])
        for c in range(8):
            b = c % 2
            for k in range(8):
                mm(ps_y.t[:, 2 * b, :], wA.t[:, k, c * 128:(c + 1) * 128], maTq.t[:, k, :], k == 0, k == 7, [wA.r0, maTq.r[k]], [ps_y.r[2 * b]])
            for k in range(8):
                mm(ps_y.t[:, 2 * b + 1, :], wB.t[:, k, c * 128:(c + 1) * 128], obTq.t[:, k, :], k == 0, k == 7, [wB.r0, obTq.r[k]], [ps_y.r[2 * b + 1]])
            P.op("dve", "tensor_tensor", dict(out=tA.t[:, b, :], in0=ps_y.t[:, 2 * b, :], in1=g01.t[:, gb, c, :], op=ALU.mult), [ps_y.r[2 * b], g01.r[gb]], [tA.r[b]])
            P.op("dve", "tensor_tensor", dict(out=tB.t[:, b, :], in0=ps_y.t[:, 2 * b + 1, :], in1=g01.t[:, gb, 8 + c, :], op=ALU.mult), [ps_y.r[2 * b + 1], g01.r[gb]], [tB.r[b]])
            P.op("pool", "tensor_tensor", dict(out=mgT.t[:, c, :], in0=tA.t[:, b, :], in1=tB.t[:, b, :], op=ALU.add), [tA.r[b], tB.r[b]], [mgT.r0])
        for qt in range(4):
            tt = qc * 4 + qt
            b = ti % 2
            ti += 1
            P.dma("sp", xtl.t[:, b, :], x_d[tt * 128:(tt + 1) * 128, :], writes=[xtl.r[b]])
            for half in range(2):
                for c in range(8):
                    mm(ps_h.t[:, half, :], mgT.t[:, c, qt * 128:(qt + 1) * 128], wO.t[:, c, half * 512:(half + 1) * 512], c == 0, c == 7, [mgT.r0, wO.r0], [ps_h.r[half]])
                P.op("dve", "tensor_tensor", dict(out=h1t.t[:, b, half * 512:(half + 1) * 512], in0=ps_h.t[:, half, :], in1=xtl.t[:, b, half * 512:(half + 1) * 512], op=ALU.add),
                     [ps_h.r[half], xtl.r[b]], [h1t.r[b]])
            P.dma("pool", h1_s[tt * 128:(tt + 1) * 128, :], h1t.t[:, b, :], reads=[h1t.r[b]])
    P.emit_stage()
    P.pop()
    if upto == "D1":
        P.close()
        return nc

    P.push()
    wQ = P.sbuf("wQ", [128, 8, D], BF16)
    wG = P.sbuf("wG", [128, 8, D], BF16)
    wPs = P.sbuf("wPs", [128, 2, D], BF16)
    P.dma("sp", wQ.t[:], w5bf[3], writes=[wQ.r0])
    P.dma("sp", wG.t[:], w5bf[4], writes=[wG.r0])
    P.dma("sp", wPs.t[:], wpbf, writes=[wPs.r0])
    kbdf = P.sbuf("kbdf", [128, 256], F32)
    kbdb = P.sbuf("kbdb", [128, 256], BF16)
    P.dma("sp", kbdf.t[:, :], kbd_d, writes=[kbdf.r0])
    P.op("dve", "tensor_copy", dict(out=kbdb.t[:, :], in_=kbdf.t[:, :]), [kbdf.r0], [kbdb.r0])
    gains = []
    for nm, gd in (("gfb", gf_d), ("gpb", gp_d), ("glb", gl_d)):
        t_ = P.sbuf(nm, [128, D], F32)
        P.dma("sp", t_.t[:, :], gd.partition_broadcast(128), writes=[t_.r0])
        gains.append(t_)
    gfb, gpb, glb = gains
    io128 = P.sbuf("io128", [128, 128], I32)
    io256 = P.sbuf("io256", [128, 256], I32)
    io16 = P.sbuf("io16", [128, 16], F32)
    P.dma("sp", io128.t[:, :], io128_d[:, 0, :], writes=[io128.r0])
    P.dma("sp", io256.t[:, :], io256_d[:, 0, :], writes=[io256.r0])
    P.dma("sp", io16.t[:, :], io16_d[:, 0, 0, :], writes=[io16.r0])
    NG = int(_os.environ.get("K_NG", "15"))

    def dbl(name, shape, dt):
        return [P.sbuf(f"{name}{i}", shape, dt) for i in range(2)]

    hh2 = [P.sbuf(f"hh{i}", [128, D], F32) for i in range(3)]
    ptl2 = [P.sbuf(f"ptl{i}", [128, 256], F32) for i in range(3)]
    xn2 = dbl("xn", [128, D], F32)
    eidx2 = dbl("eidx", [128, 128], I32)
    gte2 = dbl("gte", [128, 128], F32)
    ptb = P.sbuf("ptb", [128, 256], BF16)
    pT = P.sbuf("pT", [128, 2, 128], BF16)
    xnb2 = dbl("xnb", [128, D], BF16)
    prodb = P.sbuf("prodb", [128, 4, D], BF16, nres=4)
    junkA = P.sbuf("junkA", [128, D], BF16)
    xnT = P.sbuf("xnT", [128, 8, 128], BF16)
    xne = P.sbuf("xne", [128, D], F32)
    xnbe = P.sbuf("xnbe", [128, D], BF16)
    xnTe = P.sbuf("xnTe", [128, 8, 128], BF16)
    qpT = P.sbuf("qpT", [128, 8, 128], BF16)
    junkF = P.sbuf("junkF", [128, D], BF16)
    junkE = P.sbuf("junkE", [128, D], BF16)
    sqF = P.sbuf("sqF", [128, 4], F32)
    sqE = P.sbuf("sqE", [128, 4], F32)
    s_all = P.sbuf("s_all", [128, 16, 128], F32)
    stmp = P.sbuf("stmp", [128, 256], F32)
    v12 = P.sbuf("v12", [128, 16, 16], F32)
    i12i = P.sbuf("i12i", [128, 16, 16], I32)
    i12f = P.sbuf("i12f", [128, 16, 16], F32)
    cand = P.sbuf("cand", [128, 8, 256], F32)
    oh = cand
    best = P.sbuf("best", [128, 8, 16], F32)
    posi2 = P.sbuf("posi2", [128, 8, 16], I32)
    abi = P.sbuf("abi", [128, 2, 8, 16], I32)
    abf = P.sbuf("abf", [128, 2, 8, 16], F32)
    isel = P.sbuf("isel", [128, 2, 8, 16], F32)
    ef = P.sbuf("ef", [128, 128], F32)
    bex = P.sbuf("bex", [128, 8, 16], F32)
    bsm = P.sbuf("bsm", [128, 2, 8], F32)
    dots = P.sbuf("dots", [128, 128], F32, nres=128)
    a1 = P.sbuf("a1", [128, 128], F32, nres=32)
    ug = P.sbuf("ug", [128, NG, 2 * D], BF16, nres=NG)
    gate = P.sbuf("gate", [128, D], F32)
    tmpg = P.sbuf("tmpg", [128, 512], F32)
    ot = P.sbuf("ot", [128, D], F32)
    ps_t = P.psum("ps_t2", [128, 1024], BF16)
    ps_q = P.psum("ps_q", [128, 2, 512], F32, nres=2)
    ps_c = P.psum("ps_c", [128, 2, 512], F32, nres=2)
    ps_o = P.psum("ps_o", [128, 2, 512], F32, nres=2)
    cnt = {"gi": 0, "tvi": 0}

    def transpose8(src_bf, dst, n):
        for c in range(n):
            P.op("pe", "transpose", dict(out=ps_t.t[:, c * 128:(c + 1) * 128], in_=src_bf.t[:, c * 128:(c + 1) * 128], identity=identb.t[:, :]),
                 [src_bf.r0, identb.r0], [ps_t.r0])
        cast(dst.t[:, 0:n, :], ps_t.t[:, 0:n * 128].rearrange("p (c t) -> p c t", c=n), [ps_t.r0], [dst.r0])

    v12v = v12.t[:].rearrange("p (h c) k -> p h c k", c=2)
    i12v = i12f.t[:].rearrange("p (h c) k -> p h c k", c=2)

    def front(tt):
        par = tt % 2
        hh_, ptl, xn, eidx, gte = hh2[tt % 3], ptl2[tt % 3], xn2[par], eidx2[par], gte2[par]
        xnb = xnb2[par]
        P.dma("sp", hh_.t[:, :], h1_s[tt * 128:(tt + 1) * 128, :], writes=[hh_.r0])
        P.dma("sp", ptl.t[:, :], p_d[tt * 128:(tt + 1) * 128, :], writes=[ptl.r0])
        rmsnorm_tile(hh_, gfb, xn, xnb, sqF, junkF)
        yield
        transpose8(xnb, xnT, 8)
        yield
        for hd in range(8):
            bk = hd // 4
            for k in range(8):
                mm(ps_q.t[:, bk, (hd % 4) * 128:(hd % 4 + 1) * 128], wQ.t[:, k, hd * 128:(hd + 1) * 128], xnT.t[:, k, :],
                   (k == 0 and hd % 4 == 0), False, [wQ.r0, xnT.r0], [ps_q.r[bk]])
        for bk in range(2):
            cast(qpT.t[:, bk * 4:(bk + 1) * 4, :], ps_q.t[:, bk, :].rearrange("p (a t) -> p a t", a=4), [ps_q.r[bk]], [qpT.r0])
        for rnd in range(2):
            for h4 in range(4):
                hd = rnd * 4 + h4
                bk = h4 // 2
                mm(ps_c.t[:, bk, (hd % 2) * 256:(hd % 2 + 1) * 256], qpT.t[:, hd, :], kbdb.t[:, :], hd % 2 == 0, False, [qpT.r0, kbdb.r0], [ps_c.r[bk]])
            for bk in range(2):
                cast(s_all.t[:, rnd * 8 + bk * 4:rnd * 8 + (bk + 1) * 4, :], ps_c.t[:, bk, :].rearrange("p (a n) -> p a n", a=4), [ps_c.r[bk]], [s_all.r0])
            yield
        sI = s_all.t[:].bitcast(I32)
        P.op("dve", "tensor_scalar", dict(out=sI, in0=sI, scalar1=-128, scalar2=None, op0=ALU.bitwise_and), [s_all.r0], [s_all.r0])
        P.op("dve", "tensor_tensor", dict(out=sI, in0=sI, in1=io128.t[:, :].unsqueeze(1).broadcast_to([128, 16, 128]), op=ALU.bitwise_or), [s_all.r0, io128.r0], [s_all.r0])
        yield
        for j in range(16):
            P.op("dve", "max", dict(out=v12.t[:, j, 0:8], in_=s_all.t[:, j, :]), [s_all.r0], [v12.r0])
            P.op("dve", "match_replace", dict(out=stmp.t[:, 0:128], in_to_replace=v12.t[:, j, 0:8], in_values=s_all.t[:, j, :], imm_value=-3.0e38),
                 [s_all.r0, v12.r0], [stmp.r0])
            P.op("dve", "max", dict(out=v12.t[:, j, 8:16], in_=stmp.t[:, 0:128]), [stmp.r0], [v12.r0])
            if j % 2 == 1:
                yield
        P.op("dve", "tensor_scalar", dict(out=i12i.t[:], in0=v12.t[:].bitcast(I32), scalar1=127, scalar2=None, op0=ALU.bitwise_and), [v12.r0], [i12i.r0])
        P.op("dve", "tensor_copy", dict(out=i12f.t[:], in_=i12i.t[:]), [i12i.r0], [i12f.r0])
        candv = cand.t[:].rearrange("p h (a b) -> p h a b", a=16)
        P.op("dve", "tensor_tensor", dict(out=candv, in0=v12v[:, :, 0, :].unsqueeze(3).broadcast_to([128, 8, 16, 16]),
                                          in1=v12v[:, :, 1, :].unsqueeze(2).broadcast_to([128, 8, 16, 16]), op=ALU.add), [v12.r0], [cand.r0])
        yield
        cI = cand.t[:].bitcast(I32)
        P.op("dve", "tensor_scalar", dict(out=cI, in0=cI, scalar1=-256, scalar2=None, op0=ALU.bitwise_and), [cand.r0], [cand.r0])
        P.op("dve", "tensor_tensor", dict(out=cI, in0=cI, in1=io256.t[:, :].unsqueeze(1).broadcast_to([128, 8, 256]), op=ALU.bitwise_or), [cand.r0, io256.r0], [cand.r0])
        yield
        for hd in range(8):
            P.op("dve", "max", dict(out=best.t[:, hd, 0:8], in_=cand.t[:, hd, :]), [cand.r0], [best.r0])
            P.op("dve", "match_replace", dict(out=stmp.t[:, :], in_to_replace=best.t[:, hd, 0:8], in_values=cand.t[:, hd, :], imm_value=-3.0e38),
                 [cand.r0, best.r0], [stmp.r0])
            P.op("dve", "max", dict(out=best.t[:, hd, 8:16], in_=stmp.t[:, :]), [stmp.r0], [best.r0])
            if hd % 2 == 1:
                yield
        P.op("dve", "tensor_scalar", dict(out=posi2.t[:], in0=best.t[:].bitcast(I32), scalar1=255, scalar2=None, op0=ALU.bitwise_and), [best.r0], [posi2.r0])
        P.op("dve", "tensor_scalar", dict(out=abi.t[:, 0], in0=posi2.t[:], scalar1=4, scalar2=None, op0=ALU.arith_shift_right), [posi2.r0], [abi.r0])
        P.op("dve", "tensor_scalar", dict(out=abi.t[:, 1], in0=posi2.t[:], scalar1=15, scalar2=None, op0=ALU.bitwise_and), [posi2.r0, abi.r0], [abi.r0])
        P.op("dve", "tensor_copy", dict(out=abf.t[:], in_=abi.t[:]), [abi.r0], [abf.r0])
        yield
        ohv = oh.t[:].rearrange("p h (a b) -> p h a b", a=16)
        for c in range(2):
            P.op("dve", "tensor_tensor", dict(out=ohv, in0=abf.t[:, c].unsqueeze(3).broadcast_to([128, 8, 16, 16]),
                                              in1=io16.t[:, :].unsqueeze(1).unsqueeze(1).broadcast_to([128, 8, 16, 16]), op=ALU.is_equal),
                 [abf.r0, io16.r0], [oh.r0])
            P.op("dve", "tensor_tensor", dict(out=ohv, in0=ohv, in1=i12v[:, :, c, :].unsqueeze(2).broadcast_to([128, 8, 16, 16]), op=ALU.mult),
                 [oh.r0, i12f.r0], [oh.r0])
            P.op("dve", "tensor_reduce", dict(out=isel.t[:, c], in_=ohv, axis=AX.X, op=ALU.add), [oh.r0], [isel.r0])
            yield
        P.op("dve", "scalar_tensor_tensor", dict(out=ef.t[:, :], in0=isel.t[:, 0].rearrange("p h k -> p (h k)"), scalar=128.0,
                                                 in1=isel.t[:, 1].rearrange("p h k -> p (h k)"), op0=ALU.mult, op1=ALU.add), [isel.r0], [ef.r0])
        P.op("dve", "tensor_copy", dict(out=eidx.t[:, :], in_=ef.t[:, :]), [ef.r0], [eidx.r0])
        P.op("dve", "tensor_tensor", dict(out=bex.t[:], in0=best.t[:], in1=best.t[:, :, 0:1].broadcast_to([128, 8, 16]), op=ALU.subtract), [best.r0], [bex.r0])
        P.op("act", "activation", dict(out=bex.t[:], in_=bex.t[:], func=AF.Exp), [bex.r0], [bex.r0])
        yield
        P.op("dve", "tensor_reduce", dict(out=bsm.t[:, 0], in_=bex.t[:], axis=AX.X, op=ALU.add), [bex.r0], [bsm.r0])
        P.op("dve", "reciprocal", dict(out=bsm.t[:, 1], in_=bsm.t[:, 0]), [bsm.r0], [bsm.r0])
        P.op("dve", "tensor_tensor", dict(out=gte.t[:, :].rearrange("p (h k) -> p h k", h=8), in0=bex.t[:],
                                          in1=bsm.t[:, 1].unsqueeze(2).broadcast_to([128, 8, 16]), op=ALU.mult), [bex.r0, bsm.r0], [gte.r0])
        yield

    diagb = P.sbuf("diagb", [128, 4, 128], BF16, nres=4)

    def gather_group(tt, grp):
        par = tt % 2
        xn, eidx, gte = xn2[par], eidx2[par], gte2[par]
        bufs = []
        for j in range(4):
            slot = grp * 4 + j
            b = cnt["gi"] % NG
            cnt["gi"] += 1
            bufs.append(b)
            P.dma("pool", ug.t[:, b, :], uv_s[:, :], reads=[eidx.r0], writes=[ug.r[b]],
                  indirect=dict(out_offset=None, in_offset=bass.IndirectOffsetOnAxis(ap=eidx.t[:, slot:slot + 1], axis=0)))
            pb_ = cnt.get("pbi", 0) % 4
            cnt["pbi"] = cnt.get("pbi", 0) + 1
            P.op("dve", "tensor_tensor", dict(out=prodb.t[:, pb_, :], in0=ug.t[:, b, 0:D], in1=xnb2[par].t[:, :], op=ALU.mult),
                 [ug.r[b], xnb2[par].r0], [prodb.r[pb_]])
            P.op("act", "activation", dict(out=junkA.t[:, :], in_=prodb.t[:, pb_, :], func=AF.Copy, accum_out=dots.t[:, slot:slot + 1]),
                 [prodb.r[pb_]], [dots.r[slot]])
        if cnt.get("pend") is not None:
            finish_group(*cnt["pend"])
        gs = slice(grp * 4, grp * 4 + 4)
        P.op("act", "activation", dict(out=a1.t[:, gs], in_=dots.t[:, gs], func=AF.Gelu_apprx_tanh), dots.r[grp * 4:grp * 4 + 4], [a1.r[grp]])
        cnt["pend"] = (tt, grp, bufs)
        if grp == 31:
            finish_group(*cnt["pend"])
            cnt["pend"] = None

    def finish_group(tt, grp, bufs):
        gte = gte2[tt % 2]
        for j in range(4):
            slot = grp * 4 + j
            b = bufs[j]
            db = cnt["tvi"] % 4
            cnt["tvi"] += 1
            P.op("dve", "tensor_scalar", dict(out=diagb.t[:, db, :], in0=identb.t[:, :], scalar1=a1.t[:, slot:slot + 1], scalar2=gte.t[:, slot:slot + 1],
                                              op0=ALU.mult, op1=ALU.mult), [identb.r0, a1.r[grp], gte.r0], [diagb.r[db]])
            for half in range(2):
                mm(ps_o.t[:, half, :], diagb.t[:, db, :], ug.t[:, b, D + half * 512:D + (half + 1) * 512], slot == 0, slot == 127,
                   [diagb.r[db], ug.r[b]], [ps_o.r[half]])

    def epilogue(tt):
        hh_, ptl = hh2[tt % 3], ptl2[tt % 3]
        for half in range(2):
            P.op("dve", "tensor_tensor", dict(out=hh_.t[:, half * 512:(half + 1) * 512], in0=ps_o.t[:, half, :], in1=hh_.t[:, half * 512:(half + 1) * 512], op=ALU.add),
                 [ps_o.r[half], hh_.r0], [hh_.r0])
        yield
        rmsnorm_tile(hh_, gpb, xne, xnbe, sqE, junkE)
        yield
        transpose8(xnbe, xnTe, 8)
        yield
        P.op("act", "copy", dict(out=ptb.t[:, :], in_=ptl.t[:, :]), [ptl.r0], [ptb.r0])
        transpose8(ptb, pT, 2)
        yield
        for half in range(2):
            for k in range(8):
                mm(ps_q.t[:, half, :], xnTe.t[:, k, :], wG.t[:, k, half * 512:(half + 1) * 512], k == 0, k == 7, [xnTe.r0, wG.r0], [ps_q.r[half]])
            P.op("act", "activation", dict(out=gate.t[:, half * 512:(half + 1) * 512], in_=ps_q.t[:, half, :], func=AF.Sigmoid), [ps_q.r[half]], [gate.r0])
            for k in range(2):
                mm(ps_c.t[:, half, :], pT.t[:, k, :], wPs.t[:, k, half * 512:(half + 1) * 512], k == 0, k == 1, [pT.r0, wPs.r0], [ps_c.r[half]])
            P.op("dve", "tensor_tensor", dict(out=tmpg.t[:, :], in0=ps_c.t[:, half, :], in1=gate.t[:, half * 512:(half + 1) * 512], op=ALU.mult),
                 [ps_c.r[half], gate.r0], [tmpg.r0])
            P.op("dve", "tensor_tensor", dict(out=hh_.t[:, half * 512:(half + 1) * 512], in0=hh_.t[:, half * 512:(half + 1) * 512], in1=tmpg.t[:, :], op=ALU.add),
                 [hh_.r0, tmpg.r0], [hh_.r0])
            yield
        rmsnorm_tile(hh_, glb, ot, None, sqE, junkE)
        yield
        P.dma("sp", out_d[tt * 128:(tt + 1) * 128, :], ot.t[:, :], reads=[ot.r0])

    for _ in front(0):
        pass
    ep = iter(())
    EPG = int(_os.environ.get("K_EPG", "8"))
    for tt in range(NTT):
        nxt = front(tt + 1) if tt + 1 < NTT else iter(())
        for grp in range(32):
            gather_group(tt, grp)
            if grp == EPG:
                for _ in ep:
                    pass
            if grp >= 1:
                next(nxt, None)
        for _ in nxt:
            pass
        ep = epilogue(tt)
        next(ep)
    for _ in ep:
        pass
    P.emit_stage()
    P.pop()

    P.close()
    return nc


def kernel(**inputs):
    consts = make_consts()
    shared = prep_shared(inputs)
    shared.update(consts)
    in_maps = []
    for b in range(8):
        m = dict(shared)
        m["x"] = np.ascontiguousarray(inputs["x"][b])
        m["p"] = np.ascontiguousarray(inputs["p"][0, b])
        m["positions"] = np.ascontiguousarray(inputs["positions"][b].reshape(1, S).astype(np.int32))
        in_maps.append(m)
    nc = build_nc()
    res = run_bass_kernel_spmd(nc, in_maps, core_ids=list(range(8)))
    return np.stack([np.asarray(r["out"]) for r in res.results], axis=0).astype(np.float32)
```
